# Optimizing a Trainium2 kernel written in Bass

```python
import jax, jax.numpy as jnp
from jax import lax
import numpy as np

D_MODEL = 1024
BATCH = 2
SEQ = 16384
DEPTH = 1

ATT_Q_HEADS = 8
ATT_KV_HEADS = 2
ATT_HEAD_DIM = 64
WINDOW = 128
ATT_BLOCK = 128
GLA_HEADS = 4
GLA_DK = 64
GLA_DV = 128
GLA_GATE_RANK = 16
GLA_GATE_NORMALIZER = 16.0
GLA_CHUNK = 64
N_EXPERTS = 32
TOP_K = 4
D_EXPERT = 1024
SWIGLU_LIMIT = 7.0
SWIGLU_ALPHA = 1.702
MOE_BLOCK = 128
NORM_EPS = 1e-6

ATT_WIDTH = ATT_Q_HEADS * ATT_HEAD_DIM
ATT_KV_WIDTH = ATT_KV_HEADS * ATT_HEAD_DIM
GLA_K_WIDTH = GLA_HEADS * GLA_DK
GLA_V_WIDTH = GLA_HEADS * GLA_DV
MIX_WIDTH = ATT_WIDTH + GLA_V_WIDTH
IN_SPLITS = (ATT_WIDTH, ATT_KV_WIDTH, ATT_KV_WIDTH, GLA_K_WIDTH, GLA_K_WIDTH, GLA_V_WIDTH, GLA_V_WIDTH, GLA_GATE_RANK)
IN_WIDTH = 2 * ATT_WIDTH + 2 * ATT_KV_WIDTH + 2 * GLA_K_WIDTH + 2 * GLA_V_WIDTH + GLA_GATE_RANK - ATT_WIDTH

kernel_name = "hymba_swa_sink_gla_moe_sandwich_adaln"


def rms_norm(t, g):
    tf = t.astype(jnp.float32)
    tf = tf * lax.rsqrt(jnp.mean(tf * tf, axis=-1, keepdims=True) + NORM_EPS)
    return (tf * g.astype(jnp.float32)).astype(t.dtype)


def split_cols(t, sizes):
    idx = np.cumsum(sizes)[:-1].tolist()
    return jnp.split(t, idx, axis=-1)


def sliding_window_sink_attention(q, k, v, sinks):
    B, S = q.shape[0], q.shape[1]
    nb = S // ATT_BLOCK
    G = ATT_Q_HEADS // ATT_KV_HEADS
    qb = q.reshape(B, nb, ATT_BLOCK, ATT_KV_HEADS, G, ATT_HEAD_DIM)

    def with_prev(t):
        tb = t.reshape(B, nb, ATT_BLOCK, ATT_KV_HEADS, ATT_HEAD_DIM)
        prev = jnp.pad(tb[:, :-1], ((0, 0), (1, 0), (0, 0), (0, 0), (0, 0)))
        return jnp.concatenate([prev, tb], axis=2)

    kb, vb = with_prev(k), with_prev(v)
    scale = ATT_HEAD_DIM ** -0.5
    s = jnp.einsum('bnqhgd,bnkhd->bnhgqk', qb, kb).astype(jnp.float32) * scale
    qi = jnp.arange(ATT_BLOCK)[:, None]
    kj = jnp.arange(2 * ATT_BLOCK)[None, :]
    rel = qi + ATT_BLOCK - kj
    blk = jnp.arange(nb)[:, None, None]
    valid = (rel >= 0) & (rel < WINDOW) & ((blk > 0) | (kj >= ATT_BLOCK))
    s = jnp.where(valid[None, :, None, None], s, -jnp.inf)
    sink = sinks.astype(jnp.float32).reshape(ATT_KV_HEADS, G)[None, None, :, :, None, None]
    m = jnp.maximum(jnp.max(s, axis=-1, keepdims=True), sink)
    p = jnp.exp(s - m)
    p = p / (jnp.sum(p, axis=-1, keepdims=True) + jnp.exp(sink - m))
    o = jnp.einsum('bnhgqk,bnkhd->bnqhgd', p.astype(v.dtype), vb)
    return o.reshape(B, S, ATT_WIDTH)


def gla_chunked(q, k, v, g):
    B, S, H, dk = q.shape
    dv = v.shape[-1]
    C = GLA_CHUNK
    n = S // C
    r = lambda t: t.astype(jnp.float32).reshape(B, n, C, H, t.shape[-1])
    q, k, v, g = r(q), r(k), r(v), r(g)
    b = jnp.cumsum(g, axis=2)
    b_last = b[:, :, -1:]
    b_mid = b[:, :, C // 2 - 1:C // 2]
    A = jnp.einsum('bnihd,bnjhd->bnhij', q * jnp.exp(b - b_mid), k * jnp.exp(b_mid - b))
    causal = jnp.tril(jnp.ones((C, C), dtype=bool))
    A = jnp.where(causal, A, 0.0)
    o = jnp.einsum('bnhij,bnjhv->bnihv', A, v)
    kv = jnp.einsum('bnjhd,bnjhv->bnhdv', k * jnp.exp(b_last - b), v)
    decay = jnp.exp(b_last[:, :, 0])

    def step(state, inp):
        d, u = inp
        return d[..., None] * state + u, state

    _, states = lax.scan(step, jnp.zeros((B, H, dk, dv), jnp.float32),
                         (jnp.moveaxis(decay, 1, 0), jnp.moveaxis(kv, 1, 0)))
    states = jnp.moveaxis(states, 0, 1)
    o = o + jnp.einsum('bnihd,bnhdv->bnihv', q * jnp.exp(b), states)
    return o.reshape(B, S, H, dv)


def hybrid_mixer(h, w_in, w_gla_gate_up, b_gla_gate, g_gla_norm, sinks, w_out):
    B, S, _ = h.shape
    proj = h @ w_in
    aq, ak, av, gq, gk, gv, gg, ga = split_cols(proj, IN_SPLITS)
    att = sliding_window_sink_attention(
        aq.reshape(B, S, ATT_Q_HEADS, ATT_HEAD_DIM),
        ak.reshape(B, S, ATT_KV_HEADS, ATT_HEAD_DIM),
        av.reshape(B, S, ATT_KV_HEADS, ATT_HEAD_DIM), sinks)
    glog = jax.nn.log_sigmoid((ga @ w_gla_gate_up + b_gla_gate).astype(jnp.float32)) / GLA_GATE_NORMALIZER
    o = gla_chunked(gq.reshape(B, S, GLA_HEADS, GLA_DK) * (GLA_DK ** -0.5),
                    gk.reshape(B, S, GLA_HEADS, GLA_DK),
                    gv.reshape(B, S, GLA_HEADS, GLA_DV),
                    glog.reshape(B, S, GLA_HEADS, GLA_DK))
    o = rms_norm(o, g_gla_norm).reshape(B, S, GLA_V_WIDTH).astype(h.dtype) * jax.nn.silu(gg)
    return jnp.concatenate([att.astype(h.dtype), o], axis=-1) @ w_out


def moe_ffn(h, w_router, b_router, w_mlp1, b_mlp1, w_mlp2, b_mlp2):
    T, D = h.shape
    logits = (h @ w_router).astype(jnp.float32) + b_router.astype(jnp.float32)
    top_v, top_i = lax.top_k(logits, TOP_K)
    gates = jax.nn.softmax(top_v, axis=-1)
    n_assign = T * TOP_K
    flat_e = top_i.reshape(-1)
    order = jnp.argsort(flat_e)
    sorted_e = flat_e[order]
    counts = jnp.bincount(flat_e, length=N_EXPERTS)
    padded = (counts + MOE_BLOCK - 1) // MOE_BLOCK * MOE_BLOCK
    start = jnp.cumsum(counts) - counts
    pstart = jnp.cumsum(padded) - padded
    dest = pstart[sorted_e] + jnp.arange(n_assign) - start[sorted_e]
    n_slots = n_assign + N_EXPERTS * MOE_BLOCK
    n_blocks = n_slots // MOE_BLOCK
    slot_assign = jnp.full((n_slots,), n_assign, jnp.int32).at[dest].set(order.astype(jnp.int32))
    slot_tok = slot_assign // TOP_K
    slot_gate = jnp.concatenate([gates.reshape(-1), jnp.zeros((1,), jnp.float32)])[slot_assign]
    block_e = jnp.minimum(jnp.searchsorted(jnp.cumsum(padded), jnp.arange(n_blocks) * MOE_BLOCK, side='right'),
                          N_EXPERTS - 1)
    h_pad = jnp.concatenate([h, jnp.zeros((1, D), h.dtype)], axis=0)
    xs = h_pad[slot_tok].reshape(n_blocks, MOE_BLOCK, D)

    def expert_block(args):
        xb, e = args
        u = xb @ w_mlp1[e] + b_mlp1[e]
        x_glu = jnp.minimum(u[..., ::2], SWIGLU_LIMIT)
        x_lin = jnp.clip(u[..., 1::2], -SWIGLU_LIMIT, SWIGLU_LIMIT)
        a = x_glu * jax.nn.sigmoid(SWIGLU_ALPHA * x_glu) * (x_lin + 1)
        return a @ w_mlp2[e] + b_mlp2[e]

    ys = lax.map(expert_block, (xs, block_e)).reshape(n_slots, D)
    out = jnp.zeros((T + 1, D), jnp.float32).at[slot_tok].add(ys.astype(jnp.float32) * slot_gate[:, None])
    return out[:T].astype(h.dtype)


def setup_inputs(seed: int = 0) -> dict:
    key = jax.random.key(seed)
    ks = jax.random.split(key, 24)
    nrm = lambda k, shape, s: jax.random.normal(k, shape, jnp.float32) * s
    L, D, E, F = DEPTH, D_MODEL, N_EXPERTS, D_EXPERT
    return {
        "x": nrm(ks[0], (BATCH, SEQ, D), 1.0),
        "c": nrm(ks[1], (BATCH, D), 1.0),
        "w_ada": nrm(ks[2], (L, D, 6 * D), 0.5 * D ** -0.5),
        "b_ada": nrm(ks[3], (L, 6 * D), 0.02),
        "g_pre_mix": 1.0 + nrm(ks[4], (L, D), 0.02),
        "g_post_mix": 1.0 + nrm(ks[5], (L, D), 0.02),
        "g_pre_ffn": 1.0 + nrm(ks[6], (L, D), 0.02),
        "g_post_ffn": 1.0 + nrm(ks[7], (L, D), 0.02),
        "w_in": nrm(ks[8], (L, D, IN_WIDTH), D ** -0.5),
        "w_gla_gate_up": nrm(ks[9], (L, GLA_GATE_RANK, GLA_K_WIDTH), GLA_GATE_RANK ** -0.5),
        "b_gla_gate": nrm(ks[10], (L, GLA_K_WIDTH), 0.1),
        "g_gla_norm": 1.0 + nrm(ks[11], (L, GLA_DV), 0.02),
        "sinks": nrm(ks[12], (L, ATT_Q_HEADS), 1.0),
        "w_out": nrm(ks[13], (L, MIX_WIDTH, D), MIX_WIDTH ** -0.5),
        "w_router": nrm(ks[14], (L, D, E), D ** -0.5),
        "b_router": nrm(ks[15], (L, E), 0.01),
        "w_mlp1": nrm(ks[16], (L, E, D, 2 * F), D ** -0.5),
        "b_mlp1": nrm(ks[17], (L, E, 2 * F), 0.02),
        "w_mlp2": nrm(ks[18], (L, E, F, D), F ** -0.5),
        "b_mlp2": nrm(ks[19], (L, E, D), 0.02),
    }


def reference(x, c, w_ada, b_ada, g_pre_mix, g_post_mix, g_pre_ffn, g_post_ffn, w_in, w_gla_gate_up,
              b_gla_gate, g_gla_norm, sinks, w_out, w_router, b_router, w_mlp1, b_mlp1, w_mlp2, b_mlp2):
    B, S, D = x.shape
    for l in range(DEPTH):
        mod = jax.nn.silu(c) @ w_ada[l] + b_ada[l]
        sh1, sc1, gt1, sh2, sc2, gt2 = [m[:, None, :] for m in jnp.split(mod, 6, axis=-1)]
        h = rms_norm(x, g_pre_mix[l]) * (1 + sc1) + sh1
        y = hybrid_mixer(h, w_in[l], w_gla_gate_up[l], b_gla_gate[l], g_gla_norm[l], sinks[l], w_out[l])
        x = x + gt1 * rms_norm(y, g_post_mix[l])
        h = rms_norm(x, g_pre_ffn[l]) * (1 + sc2) + sh2
        y = moe_ffn(h.reshape(B * S, D), w_router[l], b_router[l], w_mlp1[l], b_mlp1[l],
                    w_mlp2[l], b_mlp2[l]).reshape(B, S, D)
        x = x + gt2 * rms_norm(y, g_post_ffn[l])
    return x
```

```python
import os
import numpy as np
from contextlib import ExitStack
import concourse.bass as bass
import concourse.mybir as mybir
from concourse.bass_utils import run_bass_kernel_spmd

F32 = mybir.dt.float32
BF16 = mybir.dt.bfloat16
I32 = mybir.dt.int32
ALU = mybir.AluOpType
AF = mybir.ActivationFunctionType
AX = mybir.AxisListType

D = 1024
SEQ = 16384
NCORE = 8
SEG = 4096
INW = 2320
NEXP = 32
EPS = 1e-6
NEG = -30000.0

ENGS = ("sync", "scalar", "vector", "gpsimd", "tensor")
CENG = ("scalar", "vector", "gpsimd", "tensor")
DENG = ("sync", "gpsimd", "scalar")
NDSEM = 8

C_MASK2 = 0
C_MASKF = 512
C_TRI4 = 1024
C_UINCL = 1536
C_UREV = 1664
C_IDENT = 1792
C_GNORM = 1920
C_SINK = 2432
C_BROUT = 2440
C_PFLAG = 2472
C_ONES = 2568
C_LSTR = 2696
C_TH = 2824
C_IOTA = 3848
C_TOT = 3856


class Sched:
    def __init__(self, nc, stack):
        self.nc = nc
        self.q = {e: [] for e in ENGS}
        self.esem = {e: stack.enter_context(nc.semaphore("es_" + e)) for e in CENG}
        self.ecnt = {e: 0 for e in CENG}
        self.dsem = {e: [stack.enter_context(nc.semaphore("ds_%s%d" % (e, i))) for i in range(NDSEM)]
                     for e in DENG}
        self.dcnt = {e: 0 for e in DENG}
        self.waited = {e: {} for e in ENGS}
        self.lastw = {}
        self.readers = {}
        self.nins = 0
        self.enabled = True
        self.cur_guard = None
        self.gid = 0

    def _deps(self, reads, writes):
        toks = []
        for k in reads:
            if k in self.lastw:
                toks.append(self.lastw[k])
        for k in writes:
            if k in self.lastw:
                toks.append(self.lastw[k])
            toks.extend(self.readers.get(k, ()))
        return toks

    def _need(self, eng, toks):
        out = {}
        for (sid, sem, val, owner) in toks:
            if owner == eng and eng == "tensor":
                continue
            if self.waited[eng].get(sid, 0) >= val:
                continue
            if sid not in out or out[sid][1] < val:
                out[sid] = (sem, val)
        for sid, (sem, val) in out.items():
            self.waited[eng][sid] = val
        return list(out.values())

    def _commit(self, tok, reads, writes):
        for k in writes:
            self.lastw[k] = tok
            self.readers[k] = []
        for k in reads:
            if k not in writes:
                self.readers.setdefault(k, []).append(tok)

    def op(self, eng, fn, reads=(), writes=(), sig=True):
        if not self.enabled:
            return
        px = [k for k in reads if k.startswith("ps")]
        if px:
            reads = [k for k in reads if not k.startswith("ps")]
            writes = list(writes) + px
        waits = self._need(eng, self._deps(reads, writes))
        if sig:
            self.ecnt[eng] += 1
            val = self.ecnt[eng]
        else:
            val = self.ecnt[eng] + 1
        tok = ("e_" + eng, self.esem[eng], val, eng)
        self.q[eng].append((waits, fn, (self.esem[eng], 1) if sig else None, self.cur_guard))
        self._commit(tok, reads, writes)
        self.nins += 1 + len(waits)

    def dma(self, eng, fn, reads=(), writes=()):
        if not self.enabled:
            return
        i = self.dcnt[eng]
        self.dcnt[eng] += 1
        sem = self.dsem[eng][i % NDSEM]
        sid = "d_%s%d" % (eng, i % NDSEM)
        val = 16 * (i // NDSEM + 1)
        toks = self._deps(reads, writes)
        if val > 16:
            toks.append((sid, sem, val - 16, "dma"))
        waits = self._need(eng, toks)
        self.q[eng].append((waits, fn, (sem, 16), self.cur_guard))
        self._commit((sid, sem, val, "dma"), reads, writes)
        self.nins += 1 + len(waits)

    def barrier(self):
        if not self.enabled:
            return
        toks = []
        for e in CENG:
            if self.ecnt[e] > 0:
                toks.append(("e_" + e, self.esem[e], self.ecnt[e], "x"))
        for e in DENG:
            n = self.dcnt[e]
            for j in range(min(n, NDSEM)):
                cnt = (n - 1 - j) // NDSEM + 1
                toks.append(("d_%s%d" % (e, j), self.dsem[e][j], 16 * cnt, "dma"))
        for e in ENGS:
            waits = self._need(e, toks)
            if waits:
                self.q[e].append((waits, None, None, None))
        self.lastw = {}
        self.readers = {}

    def guard_begin(self, flag_ap):
        self.gid += 1
        self.cur_guard = (self.gid, flag_ap)
        self._wsnap = {e: dict(self.waited[e]) for e in ENGS}

    def guard_end(self):
        self.cur_guard = None
        self.waited = self._wsnap

    def emit(self, block):
        for e in ENGS:
            def body(engine, e=e):
                tot = {}

                def emit_entry(waits, fn, inc):
                    for sem, val in waits:
                        engine.wait_ge(sem, val)
                    if fn is not None:
                        ins = fn(engine)
                        if inc is not None:
                            ins.then_inc(inc[0], inc[1])

                entries = self.q[e]
                i = 0
                reg = None
                while i < len(entries):
                    g = entries[i][3]
                    if g is None:
                        w, fn, inc, _ = entries[i]
                        emit_entry(w, fn, inc)
                        if inc is not None and fn is not None:
                            tot[id(inc[0])] = tot.get(id(inc[0]), 0) + inc[1]
                        i += 1
                        continue
                    j = i
                    while j < len(entries) and entries[j][3] is not None and entries[j][3][0] == g[0]:
                        j += 1
                    group = entries[i:j]
                    if reg is None:
                        reg = engine.alloc_register("gflag_" + e)
                    engine.reg_load(reg, g[1])
                    adds = {}
                    for (w, fn, inc, _) in group:
                        if inc is not None and fn is not None:
                            k = id(inc[0])
                            adds[k] = (inc[0], adds.get(k, (None, 0))[1] + inc[1])
                    with engine.If_ne(reg, 0):
                        for (w, fn, inc, _) in group:
                            emit_entry(w, fn, inc)
                    if adds:
                        with engine.Else():
                            for k, (sem, n) in adds.items():
                                b = tot.get(k, 0)
                                if b > 0:
                                    engine.wait_ge(sem, b)
                                if e == "gpsimd" and any(sem is d for d in self.dsem["gpsimd"]):
                                    engine.inc_swdge_sem([sem], [n], mode="add")
                                else:
                                    engine.sem_inc(sem, n)
                    for k, (sem, n) in adds.items():
                        tot[k] = tot.get(k, 0) + n
                    i = j
            getattr(block, e)(body)


class Ring:
    def __init__(self, name, tiles):
        self.t = tiles
        self.name = name
        self.i = 0

    def next(self):
        k = self.i % len(self.t)
        self.i += 1
        return self.t[k], "%s%d" % (self.name, k)


class _Stop(Exception):
    pass


def build_nc(NT=32, NP=96, NE=32, debug=False, stage=9, sub=99):
    nc = bass.Bass("TRN2", target_bir_lowering=False)
    TOK = NT * 128
    TQ = min(8, NT)
    NQ = NT // TQ
    GT = min(4, TQ)
    NG = TQ // GT

    def din(name, shape, dt=F32):
        return nc.dram_tensor(name, list(shape), dt, kind="ExternalInput")

    x_d = din("x", [TOK, D])
    xp_d = din("xpre", [max(NP, 1) * 128, D])
    cT_d = din("cT", [128, 8])
    wada_d = din("w_ada", [D, 6 * D])
    bada_d = din("b_ada_bc", [128, 6 * D])
    gvec_d = din("gvec_bc", [128, 4 * D])
    consts_d = din("consts", [128, C_TOT])
    win_d = din("w_in", [D, INW])
    wup_d = din("wup_aug", [17, 256])
    wout_d = din("w_out", [D, D])
    wr_d = din("w_router", [D, NEXP])
    w1_d = din("w_mlp1", [NEXP, D, 2 * D])
    w2_d = din("w_mlp2", [NEXP, D, D])
    b1r_d = din("b1r", [NEXP, 2 * D])
    b2_d = din("b_mlp2", [NEXP, D])
    out_d = nc.dram_tensor("out", [TOK, D], F32, kind="ExternalOutput")
    x1_d = nc.dram_tensor("x1_d", [TOK, D], F32, kind="ExternalOutput" if debug else "Internal")
    JM = min(32, NT)
    NSLOT = TOK * 4 + NEXP * 128
    NBLK = NSLOT // 128
    h2_d = nc.dram_tensor("h2_d", [TOK, D], BF16)
    xs_d = nc.dram_tensor("xs_d", [NSLOT, D], BF16)
    ys_d = nc.dram_tensor("ys_d", [NSLOT, D], F32)
    G_d = nc.dram_tensor("G_d", [128, NT * NEXP], F32, kind="ExternalOutput") if debug else None

    stack = ExitStack()
    with stack:
        S = Sched(nc, stack)

        def sb(st, name, shape, dt=F32):
            return st.enter_context(nc.sbuf_tensor("s_" + name, list(shape), dt))

        def mkring(st, name, n, shape, dt=F32):
            return Ring(name, [sb(st, "%s_%d" % (name, i), shape, dt) for i in range(n)])

        def mm(out, lhsT, rhs, start, stop, r, w, sig=True):
            S.op("tensor", lambda e: e.matmul(out, lhsT, rhs, start=start, stop=stop), r, w, sig=sig)

        def tr(out, in_, ident, r, w, sig=True):
            S.op("tensor", lambda e: e.transpose(out, in_, ident), r, w, sig=sig)

        def act(out, in_, func, r, w, bias=None, scale=None, accum=None):
            kw = {}
            if bias is not None:
                kw["bias"] = bias
            if scale is not None:
                kw["scale"] = scale
            if accum is not None:
                kw["accum_out"] = accum
            S.op("scalar", lambda e: e.activation(out, in_, func, **kw), r, w)

        def ts(eng, out, in0, s1, s2, op0, op1, r, w, accum=None):
            if accum is not None:
                S.op(eng, lambda e: e.tensor_scalar(out, in0, s1, s2, op0, op1, accum_out=accum), r, w)
            elif op1 is None:
                S.op(eng, lambda e: e.tensor_scalar(out, in0, s1, None, op0), r, w)
            else:
                S.op(eng, lambda e: e.tensor_scalar(out, in0, s1, s2, op0, op1), r, w)

        def tt(eng, out, in0, in1, op, r, w):
            S.op(eng, lambda e: e.tensor_tensor(out, in0, in1, op), r, w)

        def stt(out, in0, scalar, in1, op0, op1, r, w, accum=None):
            if accum is not None:
                S.op("vector", lambda e: e.scalar_tensor_tensor(out, in0, scalar, in1, op0, op1, accum_out=accum), r, w)
            else:
                S.op("vector", lambda e: e.scalar_tensor_tensor(out, in0, scalar, in1, op0, op1), r, w)

        def cp(eng, out, in_, r, w):
            if eng == "scalar":
                S.op(eng, lambda e: e.copy(out, in_), r, w)
            else:
                S.op(eng, lambda e: e.tensor_copy(out, in_), r, w)

        def dma(eng, out, in_, r, w):
            S.dma(eng, lambda e: e.dma_start(out=out, in_=in_), r, w)

        P = stack
        consts = sb(P, "consts", [128, C_TOT])
        identb = sb(P, "identb", [128, 128], BF16)
        mods = sb(P, "mods", [128, 6, D])
        G_all = sb(P, "G_all", [128, NT, NEXP])
        Qm_all = sb(P, "Qm_all", [128, NT, NEXP])
        Orun = sb(P, "Orun", [128, NEXP])
        onesb = sb(P, "onesb", [128, 128], BF16)
        lstrb = sb(P, "lstrb", [128, 128], BF16)
        GM1, SH1, GP1, GM2, SH2, GP2 = range(6)

        ps = Ring("ps", [stack.enter_context(nc.psum_tensor("ps%d" % i, [128, 512], F32)) for i in range(8)])

        identf = consts[:, C_IDENT:C_IDENT + 128]
        ones_f = consts[:, C_ONES:C_ONES + 128]

        dma("sync", consts[:], consts_d.ap(), [], ["consts"])
        cp("vector", identb[:], identf, ["consts"], ["identb"])
        cp("vector", onesb[:], ones_f, ["consts"], ["onesb"])
        cp("vector", lstrb[:], consts[:, C_LSTR:C_LSTR + 128], ["consts"], ["lstrb"])
        S.op("vector", lambda e: e.memset(Orun[:], 0.0), [], ["Orun"])
        breg = nc.gpsimd.alloc_register("bndreg")
        S.op("gpsimd", lambda e: e.reg_mov(breg, NSLOT - 1), [], [], sig=False)

        try:
            with ExitStack() as st:
                cT = sb(st, "cT", [128, 8])
                sg = sb(st, "sgc", [128, 8])
                rep = sb(st, "rep", [128, 8, 128])
                gvec = sb(st, "gvec", [128, 4 * D])
                bada = sb(st, "bada", [128, 6 * D])
                modbc = sb(st, "modbc", [128, 6 * D])
                wring = mkring(st, "wada", 2, [128, 8, 512])
                zt = sb(st, "zt", [128, D], BF16)
                S.op("gpsimd", lambda e: e.memset(zt[:], 0.0), [], ["zt"])
                xsz_v = xs_d.ap().rearrange("(b p) n -> b p n", p=128)
                ztf = sb(st, "ztf", [128, D])
                S.op("gpsimd", lambda e: e.memset(ztf[:], 0.0), [], ["ztf"])
                ysz_v = ys_d.ap().rearrange("(b p) n -> b p n", p=128)
                for b_ in range(NBLK):
                    dma("scalar", xsz_v[b_], zt[:, :], ["zt"], ["xsz%d" % b_])
                    dma("scalar", ysz_v[b_], ztf[:, :], ["ztf"], ["ysz%d" % b_])
                dma("sync", cT[:], cT_d.ap(), [], ["cT"])
                dma("sync", gvec[:], gvec_d.ap(), [], ["gvec"])
                dma("sync", bada[:], bada_d.ap(), [], ["bada"])
                act(sg[:], cT[:], AF.Sigmoid, ["cT"], ["sgc"])
                tt("vector", sg[:], sg[:], cT[:], ALU.mult, ["sgc", "cT"], ["sgc"])
                for kc in range(8):
                    ts("vector", rep[:, kc, :], ones_f, sg[:, kc:kc + 1], None, ALU.mult, None,
                       ["consts", "sgc"], ["rep%d" % kc])
                wada_v = wada_d.ap().rearrange("(k p) n -> p k n", p=128)
                for ng in range(12):
                    wt, wk = wring.next()
                    dma("sync", wt[:], wada_v[:, :, ng * 512:(ng + 1) * 512], [], [wk])
                    bank, bk = ps.next()
                    for kc in range(8):
                        mm(bank[:, :], rep[:, kc, :], wt[:, kc, :], kc == 0, kc == 7,
                           [wk, "rep%d" % kc], [bk], sig=(kc == 7))
                    tt("vector", modbc[:, ng * 512:(ng + 1) * 512], bank[:, :], bada[:, ng * 512:(ng + 1) * 512],
                       ALU.add, [bk, "bada"], ["modbc"])
                m = lambda i: modbc[:, i * D:(i + 1) * D]
                g = lambda i: gvec[:, i * D:(i + 1) * D]
                stt(mods[:, GM1, :], m(1), 1.0, g(0), ALU.add, ALU.mult, ["modbc", "gvec"], ["mods"])
                cp("vector", mods[:, SH1, :], m(0), ["modbc"], ["mods"])
                tt("vector", mods[:, GP1, :], m(2), g(1), ALU.mult, ["modbc", "gvec"], ["mods"])
                stt(mods[:, GM2, :], m(4), 1.0, g(2), ALU.add, ALU.mult, ["modbc", "gvec"], ["mods"])
                cp("vector", mods[:, SH2, :], m(3), ["modbc"], ["mods"])
                tt("vector", mods[:, GP2, :], m(5), g(3), ALU.mult, ["modbc", "gvec"], ["mods"])
                S.barrier()
                if stage <= 0:
                    S.enabled = False

            with ExitStack() as st:
                w_in = sb(st, "w_in_sb", [128, 8, INW], BF16)
                w_out = sb(st, "w_out_sb", [128, 8, D], BF16)
                w_r = sb(st, "w_r_sb", [128, 8, NEXP])
                wup = sb(st, "wup_sb", [32, 256])
                kTs = sb(st, "kTs", [64, 2, 2, 128], BF16)
                vs = sb(st, "vs", [128, 2, 128], BF16)
                gaT = sb(st, "gaT", [32, 128])
                Sf = [sb(st, "Sf%d" % i, [64, 4, 128]) for i in range(2)]
                Sb = [sb(st, "Sb%d" % i, [64, 4, 128], BF16) for i in range(2)]
                Qc0 = sb(st, "Qc0", [64, 4, 128], BF16)
                Qc1 = sb(st, "Qc1", [64, 4, 128], BF16)

                stg = mkring(st, "stg", 2, [128, 1160])
                xr = mkring(st, "xr", 2, [128, D])
                junk = mkring(st, "junk", 1, [128, D], BF16)
                tmpr = mkring(st, "tmpr", 1, [128, D])
                hbr = mkring(st, "hbr", 1, [128, D], BF16)
                hTr = mkring(st, "hTr", 1, [128, 8, 128], BF16)
                smr = mkring(st, "smr", 4, [128, 16])
                qTa = mkring(st, "qTa", 1, [64, 8, 128], BF16)
                gqr = mkring(st, "gqr", 1, [64, 4, 128])
                gkr = mkring(st, "gkr", 1, [64, 4, 128])
                ktokr = mkring(st, "ktokr", 1, [128, 256])
                vtokr = mkring(st, "vtokr", 1, [128, 512], BF16)
                sgg = mkring(st, "sgg", 1, [128, 512])
                enr = mkring(st, "enr", 1, [128, 256])
                ltok = mkring(st, "ltok", 1, [128, 256])
                bTr = mkring(st, "bTr", 1, [64, 4, 128])
                nbm = mkring(st, "nbm", 1, [64, 4, 2])
                decr = mkring(st, "decr", 1, [64, 4, 2])
                E1r = mkring(st, "E1r", 1, [64, 4, 128])
                E2r = mkring(st, "E2r", 1, [64, 4, 128])
                E3r = mkring(st, "E3r", 1, [64, 4, 128])
                QpT = mkring(st, "QpT", 1, [64, 4, 128], BF16)
                KpT = mkring(st, "KpT", 1, [64, 4, 128], BF16)
                E4r = mkring(st, "E4r", 1, [128, 256])
                Kpp = mkring(st, "Kpp", 1, [128, 256], BF16)
                ATr = mkring(st, "ATr", 1, [128, 4, 128], BF16)
                scr = mkring(st, "scr", 1, [128, 8, 256])
                pbr = mkring(st, "pbr", 1, [128, 8, 256], BF16)
                pTr = mkring(st, "pTr", 1, [128, 16, 128], BF16)
                mixr = mkring(st, "mixr", 1, [128, D], BF16)
                mixTr = mkring(st, "mixTr", 1, [128, 8, 128], BF16)
                osq = mkring(st, "osq", 1, [128, 512])
                x1r = mkring(st, "x1r", 1, [128, D])
                h2r = mkring(st, "h2r", 1, [128, D])
                h2Tf = mkring(st, "h2Tf", 1, [128, 8, 128])
                h2br = mkring(st, "h2br", 1, [128, D], BF16)
                mkbr = mkring(st, "mkbr", 1, [128, NEXP], BF16)
                lgr = mkring(st, "lgr", 1, [128, 4, NEXP])

                win_v = win_d.ap().rearrange("(k p) n -> k p n", p=128)
                wout_v = wout_d.ap().rearrange("(k p) n -> k p n", p=128)
                ceng = ["vector", "gpsimd"]
                for kc in range(8):
                    for hf_ in range(2):
                        s_, sk = stg.next()
                        dma("sync", s_[:, :], win_v[kc][:, hf_ * 1160:(hf_ + 1) * 1160], [], [sk])
                        cp(ceng[hf_], w_in[:, kc, hf_ * 1160:(hf_ + 1) * 1160], s_[:, :], [sk], ["w_in"])
                for kc in range(8):
                    s_, sk = stg.next()
                    dma("sync", s_[:, 0:D], wout_v[kc], [], [sk])
                    cp(ceng[kc % 2], w_out[:, kc, :], s_[:, 0:D], [sk], ["w_out"])
                dma("sync", w_r[:], wr_d.ap().rearrange("(k p) n -> p k n", p=128), [], ["w_r"])
                dma("sync", wup[0:17, :], wup_d.ap(), [], ["wup"])
                S.op("vector", lambda e: e.memset(gaT[:], 1.0), [], ["gaT"])
                for i in range(2):
                    S.op("gpsimd", lambda e, i=i: e.memset(Sf[i][:], 0.0), [], ["Sf%d" % i])
                    S.op("gpsimd", lambda e, i=i: e.memset(Sb[i][:], 0.0), [], ["Sb%d" % i])
                S.op("gpsimd", lambda e: e.memset(Qc0[:], 0.0), [], ["Qc0"])
                S.op("gpsimd", lambda e: e.memset(Qc1[:], 0.0), [], ["Qc1"])
                S.barrier()
                if stage <= 1:
                    S.enabled = False

                mask2 = consts[:, C_MASK2:C_MASK2 + 512]
                maskF = consts[:, C_MASKF:C_MASKF + 512]
                tri4 = consts[:, C_TRI4:C_TRI4 + 512]
                Uincl = consts[:, C_UINCL:C_UINCL + 128]
                Urev = consts[:, C_UREV:C_UREV + 128]
                gnorm = consts[:, C_GNORM:C_GNORM + 512]
                sinks = consts[:, C_SINK:C_SINK + 8]
                brout = consts[:, C_BROUT:C_BROUT + NEXP]

                x_v = x_d.ap().rearrange("(t p) n -> t p n", p=128)
                xp_v = xp_d.ap().rearrange("(t p) n -> t p n", p=128)
                x1_v = x1_d.ap().rearrange("(t p) n -> t p n", p=128)
                out_v = out_d.ap().rearrange("(t p) n -> t p n", p=128)
                h2_v = h2_d.ap().rearrange("(t p) n -> t p n", p=128)

                state = {"cur": 0}

                def rstd_from_ss(sm, smk, col_in, col_out, n, cnt=1):
                    a = sm[:, col_in:col_in + cnt]
                    o = sm[:, col_out:col_out + cnt]
                    ts("vector", o, a, 1.0 / n, EPS, ALU.mult, ALU.add, [smk], [smk])
                    act(o, o, AF.Sqrt, [smk], [smk])
                    S.op("vector", lambda e: e.reciprocal(o, o), [smk], [smk])

                def norm_mod(x_t, xk, gi, si, out_ap, outk, sm, smk, c0):
                    jk_t, jk = junk.next()
                    act(jk_t[:, :], x_t[:, :], AF.Square, [xk], [jk, smk], accum=sm[:, c0:c0 + 1])
                    rstd_from_ss(sm, smk, c0, c0 + 1, float(D))
                    tm, tk = tmpr.next()
                    stt(tm[:, :], x_t[:, :], sm[:, c0 + 1:c0 + 2], mods[:, gi, :], ALU.mult, ALU.mult, [xk, smk], [tk])
                    tt("gpsimd", out_ap, tm[:, :], mods[:, si, :], ALU.add, [tk], [outk])

                def transpose8(src, srck, dst, dstk, evac_eng):
                    for half in range(2):
                        bank, bk = ps.next()
                        for j in range(4):
                            kc = half * 4 + j
                            mm(bank[:, j * 128:(j + 1) * 128], src[:, kc * 128:(kc + 1) * 128], identb[:], True, True,
                               [srck], [bk], sig=(j == 3))
                        cp("scalar" if half == 0 else "vector",
                           dst[:, half * 4:half * 4 + 4, :].rearrange("p k n -> p (k n)"), bank[:, :], [bk], [dstk])

                def gla_gates(hT, hTk, slot_rows):
                    bank, bk = ps.next()
                    for kc in range(8):
                        mm(bank[0:16, 0:128], w_in[:, kc, 2304:2320], hT[:, kc, :], kc == 0, kc == 7,
                           [hTk], [bk], sig=(kc == 7))
                    cp("vector", gaT[0:16, :], bank[0:16, 0:128], [bk], ["gaT"])
                    zb, zk = ps.next()
                    mm(zb[:, 0:256], gaT[0:17, :], wup[0:17, :], True, True, ["gaT"], [zk])
                    en, ek = enr.next()
                    act(en[:, :], zb[:, 0:256], AF.Exp, [zk], [ek], scale=-1.0)
                    l, lk = ltok.next()
                    act(l[:, :], en[:, :], AF.Ln, [ek], [lk], bias=1.0)
                    return l, lk

                def tile_body(t, pre):
                    main = not pre
                    slot = t + 1 if main else 0
                    last_pre = pre and (t == NP - 1)
                    xt, xk = xr.next()
                    dma("sync", xt[:, :], (x_v if main else xp_v)[t], [], [xk])
                    sm, smk = smr.next()
                    hb, hbk = hbr.next()
                    norm_mod(xt, xk, GM1, SH1, hb[:, :], hbk, sm, smk, 0)
                    hT, hTk = hTr.next()
                    if sub <= 0:
                        return
                    transpose8(hb, hbk, hT, hTk, "scalar")
                    if debug and main and t == 0:
                        dh = nc.dram_tensor("dbg_h", [128, D], BF16, kind="ExternalOutput")
                        dma("sync", dh.ap(), hb[:, :], [hbk], ["dbg_h"])
                        dhT = nc.dram_tensor("dbg_hT", [128, D], BF16, kind="ExternalOutput")
                        dma("sync", dhT.ap(), hT[:].rearrange("p k n -> p (k n)"), [hTk], ["dbg_hT"])
                    if sub <= 1:
                        return

                    def fm_group(cols_list, bank, bk):
                        for j, c0 in enumerate(cols_list):
                            for kc in range(8):
                                mm(bank[0:64, j * 128:(j + 1) * 128], w_in[:, kc, c0:c0 + 64], hT[:, kc, :],
                                   kc == 0, kc == 7, [hTk], [bk], sig=(kc == 7 and j == len(cols_list) - 1))

                    def tm_group(c0, n, bank, bk, off=0, last=True):
                        for kc in range(8):
                            mm(bank[:, off:off + n], hT[:, kc, :], w_in[:, kc, c0:c0 + n], kc == 0, kc == 7,
                               [hTk], [bk], sig=(kc == 7 and last))

                    if main:
                        qa, qak = qTa.next()
                        for half in range(2):
                            bank, bk = ps.next()
                            fm_group([h * 64 for h in range(half * 4, half * 4 + 4)], bank, bk)
                            S.op("scalar", lambda e, bank=bank, half=half: e.mul(
                                qa[:, half * 4:half * 4 + 4, :].rearrange("p h n -> p (h n)"), bank[0:64, :], 0.125),
                                [bk], [qak])
                        gq, gqk = gqr.next()
                        bank, bk = ps.next()
                        fm_group([768 + h * 64 for h in range(4)], bank, bk)
                        S.op("scalar", lambda e, bank=bank: e.mul(gq[:].rearrange("p h n -> p (h n)"), bank[0:64, :], 0.125),
                             [bk], [gqk])
                        gk, gkk = gkr.next()
                        bank, bk = ps.next()
                        fm_group([1024 + h * 64 for h in range(4)], bank, bk)
                        cp("vector", gk[:].rearrange("p h n -> p (h n)"), bank[0:64, :], [bk], [gkk])
                    KD = os.environ.get("KDBG", "")
                    if KD == "tm":
                        pass
                    elif main or last_pre:
                        bank, bk = ps.next()
                        fm_group([512, 576], bank, bk)
                        cp("vector", kTs[:, slot % 2, :, :],
                           bank[0:64, 0:256].rearrange("p (h n) -> p h n", h=2), [bk], ["kT%d" % (slot % 2)])
                    if KD == "fm":
                        return
                    bank, bk = ps.next()
                    if (main or last_pre) and KD != "tm1b":
                        tm_group(640, 128, bank, bk, off=0, last=False)
                    tm_group(1024, 256, bank, bk, off=128)
                    if (main or last_pre) and KD != "tm1b":
                        cp("vector" if KD == "tm1c" else "scalar", vs[:, slot % 2, :], bank[:, 0:128], [bk], ["v%d" % (slot % 2)])
                    ktok, ktk = ktokr.next()
                    cp("vector", ktok[:, :], bank[:, 128:384], [bk], [ktk])
                    if KD in ("tm1", "tm1b", "tm1c"):
                        return
                    bank, bk = ps.next()
                    tm_group(1280, 512, bank, bk)
                    vtok, vtk = vtokr.next()
                    if pre:
                        ts("vector", vtok[:, :], bank[:, :], consts[:, C_PFLAG + t:C_PFLAG + t + 1], None, ALU.mult, None,
                           [bk], [vtk])
                    else:
                        cp("scalar", vtok[:, :], bank[:, :], [bk], [vtk])
                    if main:
                        bank, bk = ps.next()
                        tm_group(1792, 512, bank, bk)
                        sg_t, sgk = sgg.next()
                        act(sg_t[:, :], bank[:, :], AF.Silu, [bk], [sgk])
                        tt("gpsimd", sg_t[:, :], sg_t[:, :], gnorm, ALU.mult, [sgk], [sgk])

                    if sub <= 2:
                        return
                    l, lk = gla_gates(hT, hTk, None)
                    if sub <= 3:
                        return
                    rvb, rvk = ps.next()
                    mm(rvb[:, 0:256], Urev, l[:, :], True, True, [lk], [rvk])
                    E4, E4k = E4r.next()
                    act(E4[:, :], rvb[:, 0:256], AF.Exp, [rvk], [E4k])
                    kpp, kppk = Kpp.next()
                    tt("vector", kpp[:, :], ktok[:, :], E4[:, :], ALU.mult, [ktk, E4k], [kppk])
                    bTb, bTbk = ps.next()
                    if main:
                        for hd in range(4):
                            mm(bTb[0:64, hd * 128:(hd + 1) * 128], l[:, hd * 64:(hd + 1) * 64], Uincl, True, True,
                               [lk], [bTbk], sig=(hd == 3))
                        bT, bTk = bTr.next()
                        cp("vector", bT[:].rearrange("p h n -> p (h n)"), bTb[0:64, :], [bTbk], [bTk])
                        nb, nbk = nbm.next()
                        ts("vector", nb[:], bT[:, :, 31:128:64], -1.0, None, ALU.mult, None, [bTk], [nbk])
                        E1, E1k = E1r.next()
                        for hd in range(4):
                            for c in range(2):
                                act(E1[:, hd, c * 64:(c + 1) * 64], bT[:, hd, c * 64:(c + 1) * 64], AF.Exp,
                                    [bTk, nbk], [E1k], bias=nb[:, hd, c:c + 1])
                        E2, E2k = E2r.next()
                        S.op("vector", lambda e, E2=E2, E1=E1: e.reciprocal(E2[:], E1[:]), [E1k], [E2k])
                        E3, E3k = E3r.next()
                        act(E3[:], bT[:], AF.Exp, [bTk], [E3k])
                        qp, qpk = QpT.next()
                        tt("vector", qp[:], gq[:], E1[:], ALU.mult, [gqk, E1k], [qpk])
                        kp, kpk = KpT.next()
                        tt("gpsimd", kp[:], gk[:], E2[:], ALU.mult, [gkk, E2k], [kpk])
                        tt("gpsimd", Qc0[:, :, 0:64], gq[:, :, 0:64], E3[:, :, 0:64], ALU.mult, [gqk, E3k], ["Qc0"])
                        tt("gpsimd", Qc1[:, :, 64:128], gq[:, :, 64:128], E3[:, :, 64:128], ALU.mult, [gqk, E3k], ["Qc1"])
                        dec = lambda hd, c: E3[:, hd, c * 64 + 63:c * 64 + 64]
                        deck = E3k
                    else:
                        for hd in range(4):
                            mm(bTb[0:64, hd * 2:hd * 2 + 2], l[:, hd * 64:(hd + 1) * 64],
                               consts[:, C_UINCL + 63:C_UINCL + 128:64], True, True, [lk], [bTbk], sig=(hd == 3))
                        dc, dck = decr.next()
                        act(dc[:].rearrange("p h c -> p (h c)"), bTb[0:64, 0:8], AF.Exp, [bTbk], [dck])
                        dec = lambda hd, c: dc[:, hd, c:c + 1]
                        deck = dck

                    if sub <= 4:
                        return
                    cur = state["cur"]
                    S0f, S0b, S1f, S1b = Sf[cur], Sb[cur], Sf[1 - cur], Sb[1 - cur]
                    k0, k1 = "S%d" % cur, "S%d" % (1 - cur)
                    if main:
                        atb, atk = ps.next()
                        for hd in range(4):
                            mm(atb[:, hd * 128:(hd + 1) * 128], kp[:, hd, :], qp[:, hd, :], True, True,
                               [kpk, qpk], [atk], sig=(hd == 3))
                        AT, ATk = ATr.next()
                        tt("vector", AT[:].rearrange("p h n -> p (h n)"), atb[:, :], tri4, ALU.mult, [atk], [ATk])
                    kvb, kvk = ps.next()
                    for hd in range(4):
                        mm(kvb[0:64, hd * 128:(hd + 1) * 128], kpp[0:64, hd * 64:(hd + 1) * 64],
                           vtok[0:64, hd * 128:(hd + 1) * 128], True, True, [kppk, vtk], [kvk], sig=(hd == 3))
                    for hd in range(4):
                        stt(S1f[:, hd, :], S0f[:, hd, :], dec(hd, 0), kvb[0:64, hd * 128:(hd + 1) * 128],
                            ALU.mult, ALU.add, [k0 + "f", deck, kvk], [k1 + "f"])
                    cp("gpsimd", S1b[:], S1f[:], [k1 + "f"], [k1 + "b"])
                    kvb2, kvk2 = ps.next()
                    for hd in range(4):
                        mm(kvb2[0:64, hd * 128:(hd + 1) * 128], kpp[64:128, hd * 64:(hd + 1) * 64],
                           vtok[64:128, hd * 128:(hd + 1) * 128], True, True, [kppk, vtk], [kvk2], sig=(hd == 3))
                    if main:
                        ob, obk = ps.next()
                        for hd in range(4):
                            o_ap = ob[:, hd * 128:(hd + 1) * 128]
                            mm(o_ap, AT[:, hd, :], vtok[:, hd * 128:(hd + 1) * 128], True, False, [ATk, vtk], [obk], sig=False)
                            mm(o_ap, Qc0[:, hd, :], S0b[:, hd, :], False, False, ["Qc0", k0 + "b"], [obk], sig=False)
                            mm(o_ap, Qc1[:, hd, :], S1b[:, hd, :], False, True, ["Qc1", k1 + "b"], [obk], sig=(hd == 3))
                    for hd in range(4):
                        stt(S0f[:, hd, :], S1f[:, hd, :], dec(hd, 1), kvb2[0:64, hd * 128:(hd + 1) * 128],
                            ALU.mult, ALU.add, [k1 + "f", deck, kvk2], [k0 + "f"])
                    cp("gpsimd", S0b[:], S0f[:], [k0 + "f"], [k0 + "b"])
                    if pre:
                        return

                    mix, mixk = mixr.next()
                    sq, sqk = osq.next()
                    sm4, sm4k = smr.next()
                    for hd in range(4):
                        act(sq[:, hd * 128:(hd + 1) * 128], ob[:, hd * 128:(hd + 1) * 128], AF.Square, [obk], [sqk, sm4k],
                            accum=sm4[:, hd:hd + 1])
                    rstd_from_ss(sm4, sm4k, 0, 4, 128.0, cnt=4)
                    for hd in range(4):
                        stt(mix[:, 512 + hd * 128:512 + (hd + 1) * 128], ob[:, hd * 128:(hd + 1) * 128], sm4[:, 4 + hd:5 + hd],
                            sg_t[:, hd * 128:(hd + 1) * 128], ALU.mult, ALU.mult, [obk, sm4k, sgk], [mixk])

                    sc, sck = scr.next()
                    for pair in range(4):
                        bank, bk = ps.next()
                        for j in range(2):
                            h = pair * 2 + j
                            for c in range(2):
                                mm(bank[:, j * 256 + c * 128:j * 256 + (c + 1) * 128], qa[:, h, :],
                                   kTs[:, (t + c) % 2, h // 4, :], True, True, [qak, "kT%d" % ((t + c) % 2)], [bk],
                                   sig=(j == 1 and c == 1))
                        tt("vector", sc[:, pair * 2:pair * 2 + 2, :].rearrange("p h n -> p (h n)"), bank[:, :],
                           maskF if t == 0 else mask2, ALU.add, [bk], [sck])
                    sm2, sm2k = smr.next()
                    S.op("vector", lambda e: e.tensor_reduce(sm2[:, 0:8], sc[:], AX.X, ALU.max), [sck], [sm2k])
                    tt("vector", sm2[:, 0:8], sm2[:, 0:8], sinks, ALU.max, [sm2k], [sm2k])
                    ts("vector", sm2[:, 0:8], sm2[:, 0:8], -1.0, None, ALU.mult, None, [sm2k], [sm2k])
                    sm3, sm3k = smr.next()
                    pb, pbk = pbr.next()
                    for h in range(8):
                        act(pb[:, h, :], sc[:, h, :], AF.Exp, [sck, sm2k], [pbk, sm3k], bias=sm2[:, h:h + 1],
                            accum=sm3[:, h:h + 1])
                    tt("vector", sm2[:, 8:16], sinks, sm2[:, 0:8], ALU.add, [sm2k], [sm2k])
                    act(sm2[:, 8:16], sm2[:, 8:16], AF.Exp, [sm2k], [sm2k])
                    tt("vector", sm3[:, 0:8], sm3[:, 0:8], sm2[:, 8:16], ALU.add, [sm2k, sm3k], [sm3k])
                    S.op("vector", lambda e: e.reciprocal(sm3[:, 8:16], sm3[:, 0:8]), [sm3k], [sm3k])
                    pT, pTk = pTr.next()
                    for q4 in range(4):
                        bank, bk = ps.next()
                        for j in range(2):
                            h = q4 * 2 + j
                            for c in range(2):
                                mm(bank[:, (j * 2 + c) * 128:(j * 2 + c + 1) * 128], pb[:, h, c * 128:(c + 1) * 128],
                                   identb[:], True, True, [pbk], [bk], sig=(j == 1 and c == 1))
                        cp("scalar" if q4 % 2 == 0 else "vector",
                           pT[:, q4 * 4:q4 * 4 + 4, :].rearrange("p a n -> p (a n)"), bank[:, :], [bk], [pTk])
                    ab, abk = ps.next()
                    for h in range(8):
                        kv = h // 4
                        mm(ab[:, h * 64:(h + 1) * 64], pT[:, h * 2, :], vs[:, t % 2, kv * 64:(kv + 1) * 64], True, False,
                           [pTk, "v%d" % (t % 2)], [abk], sig=False)
                        mm(ab[:, h * 64:(h + 1) * 64], pT[:, h * 2 + 1, :], vs[:, (t + 1) % 2, kv * 64:(kv + 1) * 64], False, True,
                           [pTk, "v%d" % ((t + 1) % 2)], [abk], sig=(h == 7))
                    for h in range(8):
                        ts("vector", mix[:, h * 64:(h + 1) * 64], ab[:, h * 64:(h + 1) * 64],
                           sm3[:, 8 + h:9 + h], None, ALU.mult, None, [abk, sm3k], [mixk])

                    mixT, mixTk = mixTr.next()
                    transpose8(mix, mixk, mixT, mixTk, "scalar")
                    ybanks = []
                    for n in range(2):
                        bank, bk = ps.next()
                        for kc in range(8):
                            mm(bank[:, :], mixT[:, kc, :], w_out[:, kc, n * 512:(n + 1) * 512], kc == 0, kc == 7,
                               [mixTk], [bk], sig=(kc == 7))
                        ybanks.append((bank, bk))
                    sm5, sm5k = smr.next()
                    jk_t, jk = junk.next()
                    for n in range(2):
                        act(jk_t[:, n * 512:(n + 1) * 512], ybanks[n][0][:, :], AF.Square, [ybanks[n][1]], [jk, sm5k],
                            accum=sm5[:, n:n + 1])
                    tt("vector", sm5[:, 2:3], sm5[:, 0:1], sm5[:, 1:2], ALU.add, [sm5k], [sm5k])
                    rstd_from_ss(sm5, sm5k, 2, 3, float(D))
                    tm, tk = tmpr.next()
                    for n in range(2):
                        stt(tm[:, n * 512:(n + 1) * 512], ybanks[n][0][:, :], sm5[:, 3:4], mods[:, GP1, n * 512:(n + 1) * 512],
                            ALU.mult, ALU.mult, [ybanks[n][1], sm5k], [tk])
                    x1, x1k = x1r.next()
                    tt("gpsimd", x1[:, :], tm[:, :], xt[:, :], ALU.add, [tk, xk], [x1k])
                    dma("sync", x1_v[t], x1[:, :], [x1k], ["x1d%d" % t])

                    h2, h2k = h2r.next()
                    norm_mod(x1, x1k, GM2, SH2, h2[:, :], h2k, sm5, sm5k, 4)
                    hf, hfk = h2Tf.next()
                    for half in range(2):
                        bank, bk = ps.next()
                        for j in range(4):
                            kc = half * 4 + j
                            mm(bank[:, j * 128:(j + 1) * 128], h2[:, kc * 128:(kc + 1) * 128], identf, True, True,
                               [h2k], [bk], sig=(j == 3))
                        cp("vector" if half == 0 else "scalar", hf[:, half * 4:half * 4 + 4, :].rearrange("p k n -> p (k n)"),
                           bank[:, :], [bk], [hfk])
                    hbf, hbfk = h2br.next()
                    cp("gpsimd", hbf[:, :], h2[:, :], [h2k], [hbfk])
                    dma("sync", h2_v[t], hbf[:, :], [hbfk], ["h2d%d" % t])
                    lb, lbk = ps.next()
                    for kc in range(8):
                        mm(lb[:, 0:NEXP], hf[:, kc, :], w_r[:, kc, :], kc == 0, kc == 7, [hfk], [lbk], sig=(kc == 7))
                    lg, lgk = lgr.next()
                    LG, MK, EX, M8 = 0, 1, 2, 3
                    tt("vector", lg[:, LG, :], lb[:, 0:NEXP], brout, ALU.add, [lbk], [lgk])
                    S.op("vector", lambda e: e.max(lg[:, M8, 0:8], lg[:, LG, :]), [lgk], [lgk])
                    ts("vector", lg[:, MK, :], lg[:, LG, :], lg[:, M8, 3:4], None, ALU.is_ge, None, [lgk], [lgk])
                    ts("vector", lg[:, M8, 8:9], lg[:, M8, 0:1], -1.0, None, ALU.mult, None, [lgk], [lgk])
                    act(lg[:, EX, :], lg[:, LG, :], AF.Exp, [lgk], [lgk], bias=lg[:, M8, 8:9])
                    tt("vector", lg[:, EX, :], lg[:, EX, :], lg[:, MK, :], ALU.mult, [lgk], [lgk])
                    S.op("vector", lambda e: e.tensor_reduce(lg[:, M8, 9:10], lg[:, EX, :], AX.X, ALU.add), [lgk], [lgk])
                    S.op("vector", lambda e: e.reciprocal(lg[:, M8, 10:11], lg[:, M8, 9:10]), [lgk], [lgk])
                    ts("vector", G_all[:, t, :], lg[:, EX, :], lg[:, M8, 10:11], None, ALU.mult, None, [lgk], ["G%d" % t])
                    mkb, mkbk = mkbr.next()
                    cp("vector", mkb[:, :], lg[:, MK, :], [lgk], [mkbk])
                    rb, rbk = ps.next()
                    mm(rb[:, 0:NEXP], lstrb[:], mkb[:, :], True, True, [mkbk], [rbk], sig=False)
                    mm(rb[:, NEXP:2 * NEXP], onesb[:], mkb[:, :], True, True, [mkbk], [rbk])
                    stt(Qm_all[:, t, :], rb[:, 0:NEXP], 1.0, Orun[:, :], ALU.add, ALU.add, [rbk, "Orun"], ["Qm%d" % t])
                    tt("vector", Qm_all[:, t, :], Qm_all[:, t, :], lg[:, MK, :], ALU.mult, ["Qm%d" % t, lgk], ["Qm%d" % t])
                    tt("vector", Orun[:, :], Orun[:, :], rb[:, NEXP:2 * NEXP], ALU.add, ["Orun", rbk], ["Orun"])

                for p in range(NP):
                    tile_body(p, True)
                if stage <= 2:
                    S.enabled = False
                for t in range(NT):
                    tile_body(t, False)
                if debug:
                    dma("sync", G_d.ap(), G_all[:].rearrange("p t e -> p (t e)"), ["G%d" % t for t in range(NT)], ["Gd"])
                S.barrier()
                if stage <= 3:
                    S.enabled = False

            h2_v = h2_d.ap().rearrange("(t p) n -> t p n", p=128)
            x1_v = x1_d.ap().rearrange("(t p) n -> t p n", p=128)
            out_v = out_d.ap().rearrange("(t p) n -> t p n", p=128)
            gk_all = sb(P, "gk_all", [128, NT, 4])
            idx4_all = sb(P, "idx4_all", [128, NT, 4], I32)
            flags_i = sb(P, "flags_i", [128, NEXP * JM], I32)
            idxb_i = sb(P, "idxb_i", [128, NEXP * JM], I32)
            with ExitStack() as st:
                flf = sb(st, "flf", [128, NEXP, JM])
                nbt = sb(st, "nbt", [128, NEXP])
                cA = sb(st, "cA", [128, NEXP])
                cB = sb(st, "cB", [128, NEXP])
                pst = sb(st, "pst", [128, NEXP])
                idf = sb(st, "idf", [128, NEXP, JM])
                vr = mkring(st, "vr", 2, [128, 3, NEXP])
                m8r = mkring(st, "m8r", 2, [128, 16])
                h2l = mkring(st, "h2l", 2, [128, D], BF16)
                TH3 = consts[:, C_TH:C_TH + NEXP * 32].rearrange("p (e j) -> p e j", e=NEXP)[:, :, 0:JM]
                S.op("vector", lambda e: e.tensor_tensor(flf[:], Orun[:].unsqueeze(2).to_broadcast([128, NEXP, JM]), TH3,
                                                         ALU.is_gt), ["Orun"], ["flf"])
                S.op("vector", lambda e: e.tensor_reduce(nbt[:], flf[:], AX.X, ALU.add), ["flf"], ["nbt"])
                ts("vector", cA[:], nbt[:], 128.0, None, ALU.mult, None, ["nbt"], ["cA"])
                cp("vector", pst[:], cA[:], ["cA"], ["pst"])
                ca, cb, cak, cbk = cA, cB, "cA", "cB"
                for sft in (1, 2, 4, 8, 16):
                    cp("vector", cb[:, 0:sft], ca[:, 0:sft], [cak], [cbk])
                    tt("vector", cb[:, sft:NEXP], ca[:, sft:NEXP], ca[:, 0:NEXP - sft], ALU.add, [cak], [cbk])
                    ca, cb, cak, cbk = cb, ca, cbk, cak
                tt("vector", pst[:], ca[:], pst[:], ALU.subtract, [cak, "pst"], ["pst"])
                cp("vector", flags_i[:], flf[:].rearrange("p e j -> p (e j)"), ["flf"], ["flags"])
                S.op("vector", lambda e: e.tensor_tensor(idf[:], TH3, pst[:].unsqueeze(2).to_broadcast([128, NEXP, JM]),
                                                         ALU.add), ["pst"], ["idf"])
                ts("vector", idf[:], idf[:], consts[:, C_IOTA:C_IOTA + 1], -65536.0, ALU.add, ALU.add, ["idf"], ["idf"])
                tt("vector", idf[:], idf[:], flf[:], ALU.mult, ["idf", "flf"], ["idf"])
                ts("vector", idf[:], idf[:], 65536.0, None, ALU.add, None, ["idf"], ["idf"])
                cp("vector", idxb_i[:], idf[:].rearrange("p e j -> p (e j)"), ["idf"], ["idxb"])
                for t in range(NT):
                    v, vk = vr.next()
                    ts("vector", v[:, 0, :], Qm_all[:, t, :], 0.0, None, ALU.is_gt, None, [], [vk])
                    tt("vector", v[:, 1, :], Qm_all[:, t, :], pst[:], ALU.add, ["pst"], [vk])
                    tt("vector", v[:, 1, :], v[:, 1, :], v[:, 0, :], ALU.mult, [vk], [vk])
                    m8, m8k = m8r.next()
                    S.op("vector", lambda e, m8=m8, v=v: e.max(m8[:, 0:8], v[:, 1, :]), [vk], [m8k])
                    ts("vector", m8[:, 8:12], m8[:, 0:4], -1.0, None, ALU.add, None, [m8k], [m8k])
                    cp("vector", idx4_all[:, t, :], m8[:, 8:12], [m8k], ["idx4_%d" % t])
                    for k in range(4):
                        ts("vector", v[:, 2, :], v[:, 1, :], m8[:, k:k + 1], None, ALU.is_equal, None, [vk, m8k], [vk])
                        stt(v[:, 0, :], v[:, 2, :], 1.0, G_all[:, t, :], ALU.mult, ALU.mult, [vk], [vk, "gk%d" % t],
                            accum=gk_all[:, t, k:k + 1])
                    h2t, h2tk = h2l.next()
                    dma("sync", h2t[:, :], h2_v[t], [], [h2tk])
                    for k in range(4):
                        S.dma("gpsimd", lambda en, t=t, k=k, h2t=h2t: en.indirect_dma_start(
                            out=xs_d.ap(), out_offset=bass.IndirectOffsetOnAxis(ap=idx4_all[:, t, k:k + 1], axis=0),
                            in_=h2t[:, :], in_offset=None), [h2tk, "idx4_%d" % t], ["xsd"])
                S.barrier()

            with ExitStack() as st:
                w1b = mkring(st, "w1b", 2, [128, 8, 2, D], BF16)
                w2b = mkring(st, "w2b", 2, [128, 8, D], BF16)
                w1s = mkring(st, "w1s", 2, [128, 2, 256])
                w2s = mkring(st, "w2s", 2, [128, 512])
                b1s = mkring(st, "b1s", 1, [1, D])
                b1b = mkring(st, "b1b", 2, [1, 2 * D], BF16)
                xsr = mkring(st, "xsr", 2, [128, D], BF16)
                xsTr = mkring(st, "xsTr", 2, [128, 8, 128], BF16)
                aTr = mkring(st, "aTr", 2, [128, 8, 128], BF16)
                xgr = mkring(st, "xgr", 2, [128, 512])
                sgr = mkring(st, "sgr", 2, [128, 512])
                xlr = mkring(st, "xlr", 2, [128, 512])
                ysr = mkring(st, "ysr", 2, [128, D])
                for i_ in range(2):
                    S.op("gpsimd", lambda e, i_=i_: e.memset(ysr.t[i_][:], 0.0), [], ["ysr%d" % i_])
                w1_v = w1_d.ap().rearrange("e (k p) n -> e p k n", p=128)
                w2_v = w2_d.ap().rearrange("e (k p) n -> e k p n", p=128)

                def block(e, j, w1t, w1k, w2t, w2k, bb, bbk):
                    col = e * JM + j
                    xs, xsk = xsr.next()
                    S.dma("gpsimd", lambda en, xs=xs, col=col: en.indirect_dma_start(
                        out=xs[:, :], out_offset=None, in_=xs_d.ap(),
                        in_offset=bass.IndirectOffsetOnAxis(ap=idxb_i[:, col:col + 1], axis=0),
                        bounds_check=breg, oob_is_err=False), [], [xsk])
                    S.guard_begin(flags_i[0:1, col:col + 1])
                    xT, xTk = xsTr.next()
                    transpose8(xs, xsk, xT, xTk, None)
                    aT, aTk = aTr.next()
                    for half in range(2):
                        banks = []
                        for two in range(2):
                            bank, bk = ps.next()
                            for q in range(4):
                                fc = half * 4 + q
                                o_ap = bank[:, q * 128:(q + 1) * 128]
                                for kc in range(8):
                                    mm(o_ap, w1t[:, kc, two, fc * 128:(fc + 1) * 128], xT[:, kc, :], kc == 0, False,
                                       [w1k, xTk], [bk], sig=False)
                                mm(o_ap, bb[0:1, two * D + fc * 128:two * D + (fc + 1) * 128], onesb[0:1, :], False, True,
                                   [bbk], [bk], sig=(q == 3))
                            banks.append((bank, bk))
                        (bg, bgk), (bl, blk) = banks
                        xg, xgk = xgr.next()
                        ts("vector", xg[:, :], bg[:, :], 7.0, None, ALU.min, None, [bgk], [xgk])
                        sgt, sgk2 = sgr.next()
                        act(sgt[:, :], xg[:, :], AF.Sigmoid, [xgk], [sgk2], scale=1.702)
                        xl, xlk = xlr.next()
                        ts("vector", xl[:, :], bl[:, :], 7.0, -7.0, ALU.min, ALU.max, [blk], [xlk])
                        tt("gpsimd", xg[:, :], xg[:, :], sgt[:, :], ALU.mult, [xgk, sgk2], [xgk])
                        stt(aT[:, half * 4:half * 4 + 4, :].rearrange("p k n -> p (k n)"), xl[:, :], 1.0, xg[:, :],
                            ALU.add, ALU.mult, [xlk, xgk], [aTk])
                    ysb, ysk = ysr.next()
                    for n in range(2):
                        yb, ybk = ps.next()
                        for fc in range(8):
                            mm(yb[:, :], aT[:, fc, :], w2t[:, fc, n * 512:(n + 1) * 512], fc == 0, fc == 7,
                               [aTk, w2k], [ybk], sig=(fc == 7))
                        cp("scalar" if n == 0 else "vector", ysb[:, n * 512:(n + 1) * 512], yb[:, :], [ybk], [ysk])
                    S.guard_end()
                    S.dma("gpsimd", lambda en, ysb=ysb, col=col: en.indirect_dma_start(
                        out=ys_d.ap(), out_offset=bass.IndirectOffsetOnAxis(ap=idxb_i[:, col:col + 1], axis=0),
                        in_=ysb[:, :], in_offset=None, bounds_check=breg, oob_is_err=False), [ysk], ["ysd"])

                for e in range(NE):
                    w1t, w1k = w1b.next()
                    w2t, w2k = w2b.next()
                    bb, bbk = b1b.next()
                    for j in range(8):
                        for kh in range(4):
                            ws, wsk = w1s.next()
                            dma("sync", ws[:], w1_v[e][:, kh * 2:(kh + 1) * 2, j * 256:(j + 1) * 256], [], [wsk])
                            cp("gpsimd", w1t[:, kh * 2:(kh + 1) * 2, :, j * 128:(j + 1) * 128],
                               ws[:].rearrange("p k (m two) -> p k two m", two=2), [wsk], [w1k])
                        for nh in range(2):
                            s2, s2k = w2s.next()
                            dma("sync", s2[:], w2_v[e][j][:, nh * 512:(nh + 1) * 512], [], [s2k])
                            cp("gpsimd", w2t[:, j, nh * 512:(nh + 1) * 512], s2[:], [s2k], [w2k])
                    for bh in range(2):
                        bs, bsk = b1s.next()
                        dma("sync", bs[:], b1r_d.ap()[e:e + 1, bh * D:(bh + 1) * D], [], [bsk])
                        cp("vector", bb[:, bh * D:(bh + 1) * D], bs[:], [bsk], [bbk])
                    for j in range(JM):
                        block(e, j, w1t, w1k, w2t, w2k, bb, bbk)
                S.barrier()

            with ExitStack() as st:
                b2_sb = sb(st, "b2_sb", [NEXP, D])
                dma("sync", b2_sb[:], b2_d.ap(), [], ["b2"])
                yr = mkring(st, "yr", 4, [128, D])
                accr = mkring(st, "accr", 2, [128, D])
                GTr = mkring(st, "GTr", 2, [NEXP, 128])
                x1l = mkring(st, "x1l", 2, [128, D])
                outr = mkring(st, "outr", 2, [128, D])
                junk2 = mkring(st, "junk2", 1, [128, D], BF16)
                smq = mkring(st, "smq", 2, [128, 8])
                for t in range(NT):
                    ys4 = []
                    for k in range(4):
                        y_, yk_ = yr.next()
                        S.dma("gpsimd", lambda en, y_=y_, t=t, k=k: en.indirect_dma_start(
                            out=y_[:, :], out_offset=None, in_=ys_d.ap(),
                            in_offset=bass.IndirectOffsetOnAxis(ap=idx4_all[:, t, k:k + 1], axis=0)), [], [yk_])
                        ys4.append((y_, yk_))
                    ac, ack = accr.next()
                    ts("vector", ac[:, :], ys4[0][0][:, :], gk_all[:, t, 0:1], None, ALU.mult, None, [ys4[0][1]], [ack])
                    for k in range(1, 4):
                        stt(ac[:, :], ys4[k][0][:, :], gk_all[:, t, k:k + 1], ac[:, :], ALU.mult, ALU.add,
                            [ys4[k][1], ack], [ack])
                    gb, gbk = ps.next()
                    mm(gb[0:NEXP, 0:128], G_all[:, t, :], identf, True, True, [], [gbk])
                    gt_, gtk = GTr.next()
                    cp("vector", gt_[:, :], gb[0:NEXP, 0:128], [gbk], [gtk])
                    x1t, x1tk = x1l.next()
                    dma("sync", x1t[:, :], x1_v[t], [], [x1tk])
                    sm, smk = smq.next()
                    jk_t, jk = junk2.next()
                    for n in range(2):
                        yb, ybk = ps.next()
                        mm(yb[:, :], gt_[:, :], b2_sb[:, n * 512:(n + 1) * 512], True, True, [gtk, "b2"], [ybk])
                        a_ap = ac[:, n * 512:(n + 1) * 512]
                        tt("vector", a_ap, a_ap, yb[:, :], ALU.add, [ack, ybk], [ack])
                        act(jk_t[:, n * 512:(n + 1) * 512], a_ap, AF.Square, [ack], [jk, smk], accum=sm[:, n:n + 1])
                    tt("vector", sm[:, 2:3], sm[:, 0:1], sm[:, 1:2], ALU.add, [smk], [smk])
                    o_ = sm[:, 3:4]
                    ts("vector", o_, sm[:, 2:3], 1.0 / D, EPS, ALU.mult, ALU.add, [smk], [smk])
                    act(o_, o_, AF.Sqrt, [smk], [smk])
                    S.op("vector", lambda e, o_=o_: e.reciprocal(o_, o_), [smk], [smk])
                    ot, otk = outr.next()
                    stt(ot[:, :], ac[:, :], o_, mods[:, GP2, :], ALU.mult, ALU.mult, [ack, smk], [otk])
                    tt("gpsimd", ot[:, :], ot[:, :], x1t[:, :], ALU.add, [otk, x1tk], [otk])
                    dma("sync", out_v[t], ot[:, :], [otk], ["outd%d" % t])
                S.barrier()
        except _Stop:
            S.barrier()

        with nc.Block() as block:
            S.emit(block)
    return nc, S


def host_inputs(inp, NT=32, NP=96, segs=None):
    x = np.asarray(inp["x"], np.float32)
    TOK = NT * 128
    f = lambda k: np.ascontiguousarray(np.asarray(inp[k], np.float32)[0])
    w_ada, b_ada = f("w_ada"), f("b_ada")
    gvec = np.concatenate([f("g_pre_mix"), f("g_post_mix"), f("g_pre_ffn"), f("g_post_ffn")])
    gvec_bc = np.ascontiguousarray(np.broadcast_to(gvec[None, :], (128, 4 * D)))
    bada_bc = np.ascontiguousarray(np.broadcast_to(b_ada[None, :], (128, 6 * D)))
    wup_aug = np.concatenate([f("w_gla_gate_up"), f("b_gla_gate")[None, :]], axis=0)
    b1 = f("b_mlp1")
    b1r = np.ascontiguousarray(b1.reshape(NEXP, D, 2).transpose(0, 2, 1).reshape(NEXP, 2 * D))
    qi = np.arange(128)[:, None]
    kj = np.arange(256)[None, :]
    valid = ((kj < 128) & (kj > qi)) | ((kj >= 128) & (kj - 128 <= qi))
    m1 = np.where(valid, 0.0, NEG).astype(np.float32)
    validF = (kj >= 128) & (kj - 128 <= qi)
    mF = np.where(validF, 0.0, NEG).astype(np.float32)
    j = np.arange(128)[:, None]
    i = np.arange(128)[None, :]
    same = (j // 64) == (i // 64)
    tri = (same & (j <= i)).astype(np.float32)
    uincl = tri * (-1.0 / 16.0)
    urev = (same & (j > i)).astype(np.float32) * (-1.0 / 16.0)
    cbase = np.zeros((128, C_TOT), np.float32)
    cbase[:, C_MASK2:C_MASK2 + 512] = np.tile(m1, (1, 2))
    cbase[:, C_TRI4:C_TRI4 + 512] = np.tile(tri, (1, 4))
    cbase[:, C_UINCL:C_UINCL + 128] = uincl
    cbase[:, C_UREV:C_UREV + 128] = urev
    cbase[:, C_IDENT:C_IDENT + 128] = np.eye(128, dtype=np.float32)
    cbase[:, C_GNORM:C_GNORM + 512] = np.tile(f("g_gla_norm")[None, :], (128, 4))
    cbase[:, C_SINK:C_SINK + 8] = f("sinks")[None, :]
    cbase[:, C_BROUT:C_BROUT + NEXP] = f("b_router")[None, :]
    cbase[:, C_ONES:C_ONES + 128] = 1.0
    cbase[:, C_LSTR:C_LSTR + 128] = (j < i).astype(np.float32)
    cbase[:, C_TH:C_TH + NEXP * 32] = np.tile(128.0 * np.arange(32, dtype=np.float32), NEXP)[None, :]
    cbase[:, C_IOTA] = np.arange(128, dtype=np.float32)
    shared = {"w_ada": w_ada, "b_ada_bc": bada_bc, "gvec_bc": gvec_bc, "w_in": f("w_in"), "wup_aug": wup_aug,
              "w_out": f("w_out"), "w_router": f("w_router"), "w_mlp1": f("w_mlp1"), "w_mlp2": f("w_mlp2"),
              "b1r": b1r, "b_mlp2": f("b_mlp2")}
    maps = []
    nseg = SEQ // SEG
    if segs is None:
        segs = [(core // nseg, (core % nseg) * SEG) for core in range(NCORE)]
    for (b, s0) in segs:
        cm = cbase.copy()
        cm[:, C_MASKF:C_MASKF + 512] = np.tile(mF if s0 == 0 else m1, (1, 2))
        xpre = np.zeros((max(NP, 1) * 128, D), np.float32)
        p0 = s0 - NP * 128
        for p in range(NP):
            a = p0 + p * 128
            if a >= 0:
                xpre[p * 128:(p + 1) * 128] = x[b, a:a + 128]
                cm[:, C_PFLAG + p] = 1.0
        mp = dict(shared)
        mp["x"] = np.ascontiguousarray(x[b, s0:s0 + TOK])
        mp["xpre"] = xpre
        mp["cT"] = np.ascontiguousarray(np.asarray(inp["c"], np.float32)[b].reshape(8, 128).T)
        mp["consts"] = cm
        maps.append(mp)
    return maps


_CACHE = {}


def kernel(**inputs):
    if "nc" not in _CACHE:
        _CACHE["nc"] = build_nc()[0]
    nc = _CACHE["nc"]
    maps = host_inputs(inputs)
    res = run_bass_kernel_spmd(nc, maps, core_ids=list(range(NCORE)))
    out = np.empty((2, SEQ, D), np.float32)
    nseg = SEQ // SEG
    for core in range(NCORE):
        b, seg = core // nseg, core % nseg
        out[b, seg * SEG:(seg + 1) * SEG] = np.asarray(res.results[core]["out"], np.float32)
    return out
```

```python
import os
import numpy as np
from contextlib import ExitStack
import concourse.bass as bass
import concourse.mybir as mybir
from concourse.bass_utils import run_bass_kernel_spmd

F32 = mybir.dt.float32
BF16 = mybir.dt.bfloat16
I32 = mybir.dt.int32
ALU = mybir.AluOpType
AF = mybir.ActivationFunctionType
AX = mybir.AxisListType

D = 1024
SEQ = 16384
NCORE = 8
SEG = 4096
INW = 2320
NEXP = 32
EPS = 1e-6
NEG = -30000.0

ENGS = ("sync", "scalar", "vector", "gpsimd", "tensor")
CENG = ("scalar", "vector", "gpsimd", "tensor")
DENG = ("sync", "gpsimd", "scalar")
NDSEM = 8

C_MASK2 = 0
C_MASKF = 512
C_TRI4 = 1024
C_UINCL = 1536
C_UREV = 1664
C_IDENT = 1792
C_GNORM = 1920
C_SINK = 2432
C_BROUT = 2440
C_PFLAG = 2472
C_ONES = 2568
C_LSTR = 2696
C_TH = 2824
C_IOTA = 3848
C_TOT = 3856


class Sched:
    def __init__(self, nc, stack):
        self.nc = nc
        self.q = {e: [] for e in ENGS}
        self.esem = {e: stack.enter_context(nc.semaphore("es_" + e)) for e in CENG}
        self.ecnt = {e: 0 for e in CENG}
        self.dsem = {e: [stack.enter_context(nc.semaphore("ds_%s%d" % (e, i))) for i in range(NDSEM)]
                     for e in DENG}
        self.dcnt = {e: 0 for e in DENG}
        self.waited = {e: {} for e in ENGS}
        self.lastw = {}
        self.readers = {}
        self.nins = 0
        self.enabled = True
        self.cur_guard = None
        self.gid = 0
        self.chain_flags = {}

    def _deps(self, reads, writes):
        toks = []
        for k in reads:
            if k in self.lastw:
                toks.append(self.lastw[k])
        for k in writes:
            if k in self.lastw:
                toks.append(self.lastw[k])
            toks.extend(self.readers.get(k, ()))
        return toks

    def _need(self, eng, toks):
        out = {}
        for (sid, sem, val, owner) in toks:
            if owner == eng and eng == "tensor":
                continue
            if self.waited[eng].get(sid, 0) >= val:
                continue
            if sid not in out or out[sid][1] < val:
                out[sid] = (sem, val)
        for sid, (sem, val) in out.items():
            self.waited[eng][sid] = val
        return list(out.values())

    def _commit(self, tok, reads, writes):
        for k in writes:
            self.lastw[k] = tok
            self.readers[k] = []
        for k in reads:
            if k not in writes:
                self.readers.setdefault(k, []).append(tok)

    def op(self, eng, fn, reads=(), writes=(), sig=True):
        if not self.enabled:
            return
        px = [k for k in reads if k.startswith("ps")]
        if px:
            reads = [k for k in reads if not k.startswith("ps")]
            writes = list(writes) + px
        waits = self._need(eng, self._deps(reads, writes))
        if sig:
            self.ecnt[eng] += 1
            val = self.ecnt[eng]
        else:
            val = self.ecnt[eng] + 1
        tok = ("e_" + eng, self.esem[eng], val, eng)
        self.q[eng].append((waits, fn, (self.esem[eng], 1) if sig else None, self.cur_guard))
        self._commit(tok, reads, writes)
        self.nins += 1 + len(waits)

    def dma(self, eng, fn, reads=(), writes=()):
        if not self.enabled:
            return
        i = self.dcnt[eng]
        self.dcnt[eng] += 1
        sem = self.dsem[eng][i % NDSEM]
        sid = "d_%s%d" % (eng, i % NDSEM)
        val = 16 * (i // NDSEM + 1)
        toks = self._deps(reads, writes)
        if val > 16:
            toks.append((sid, sem, val - 16, "dma"))
        waits = self._need(eng, toks)
        self.q[eng].append((waits, fn, (sem, 16), self.cur_guard))
        self._commit((sid, sem, val, "dma"), reads, writes)
        self.nins += 1 + len(waits)

    def barrier(self):
        if not self.enabled:
            return
        toks = []
        for e in CENG:
            if self.ecnt[e] > 0:
                toks.append(("e_" + e, self.esem[e], self.ecnt[e], "x"))
        for e in DENG:
            n = self.dcnt[e]
            for j in range(min(n, NDSEM)):
                cnt = (n - 1 - j) // NDSEM + 1
                toks.append(("d_%s%d" % (e, j), self.dsem[e][j], 16 * cnt, "dma"))
        for e in ENGS:
            waits = self._need(e, toks)
            if waits:
                self.q[e].append((waits, None, None, None))
        self.lastw = {}
        self.readers = {}

    def chain_begin(self):
        self.gid += 1
        self.cur_guard = (self.gid, 0)
        self.chain_flags[self.gid] = [None]
        self._wsnap = {e: dict(self.waited[e]) for e in ENGS}

    def level_push(self, flag_ap):
        cid, d = self.cur_guard
        self.chain_flags[cid].append(flag_ap)
        self.cur_guard = (cid, d + 1)

    def chain_end(self):
        self.cur_guard = None
        self.waited = self._wsnap

    def emit(self, block, scratch):
        for e in ENGS:
            def body(engine, e=e):
                tot = {}
                entries = self.q[e]
                state = {"reg": None}

                def emit_entry(ent):
                    waits, fn, inc, _ = ent
                    for sem, val in waits:
                        engine.wait_ge(sem, val)
                    if fn is not None:
                        ins = fn(engine)
                        if inc is not None:
                            ins.then_inc(inc[0], inc[1])
                            tot[id(inc[0])] = tot.get(id(inc[0]), 0) + inc[1]

                def depth_of(ent):
                    return 0 if ent[3] is None else ent[3][1]

                def emit_level(i, end, cid, depth):
                    while i < end:
                        d = depth_of(entries[i])
                        if d == depth:
                            emit_entry(entries[i])
                            i += 1
                            continue
                        flag = self.chain_flags[cid][depth + 1]
                        if state["reg"] is None:
                            state["reg"] = engine.alloc_register("gflag_" + e)
                        reg = state["reg"]
                        adds = {}
                        for ent in entries[i:end]:
                            if ent[2] is not None and ent[1] is not None:
                                k = id(ent[2][0])
                                adds[k] = (ent[2][0], adds.get(k, (None, 0))[1] + ent[2][1])
                        before = dict(tot)
                        engine.reg_load(reg, flag)
                        with engine.If_ne(reg, 0):
                            emit_level(i, end, cid, depth + 1)
                        if adds:
                            with engine.Else():
                                for k, (sem, n) in adds.items():
                                    b = before.get(k, 0)
                                    if b > 0:
                                        engine.wait_ge(sem, b)
                                    if e == "gpsimd" and any(sem is d_ for d_ in self.dsem["gpsimd"]):
                                        engine.dma_start(out=scratch[0:1, 8:9], in_=scratch[0:1, 0:1]).then_inc(sem, n)
                                    else:
                                        engine.sem_inc(sem, n)
                        for k, (sem, n) in adds.items():
                            tot[k] = before.get(k, 0) + n
                        i = end

                i = 0
                while i < len(entries):
                    g = entries[i][3]
                    if g is None:
                        emit_entry(entries[i])
                        i += 1
                        continue
                    cid = g[0]
                    end = i
                    while end < len(entries) and entries[end][3] is not None and entries[end][3][0] == cid:
                        end += 1
                    emit_level(i, end, cid, 0)
                    i = end
            getattr(block, e)(body)


class Ring:
    def __init__(self, name, tiles):
        self.t = tiles
        self.name = name
        self.i = 0

    def next(self):
        k = self.i % len(self.t)
        self.i += 1
        return self.t[k], "%s%d" % (self.name, k)


class _Stop(Exception):
    pass


def build_nc(NT=32, NP=96, NE=32, debug=False, stage=9, sub=99):
    nc = bass.Bass("TRN2", target_bir_lowering=False)
    TOK = NT * 128
    TQ = min(8, NT)
    NQ = NT // TQ
    GT = min(4, TQ)
    NG = TQ // GT

    def din(name, shape, dt=F32):
        return nc.dram_tensor(name, list(shape), dt, kind="ExternalInput")

    x_d = din("x", [TOK, D])
    xp_d = din("xpre", [max(NP, 1) * 128, D])
    cT_d = din("cT", [128, 8])
    wada_d = din("w_ada", [D, 6 * D])
    bada_d = din("b_ada_bc", [128, 6 * D])
    gvec_d = din("gvec_bc", [128, 4 * D])
    consts_d = din("consts", [128, C_TOT])
    win_d = din("w_in", [D, INW])
    wup_d = din("wup_aug", [17, 256])
    wout_d = din("w_out", [D, D])
    wr_d = din("w_router", [D, NEXP])
    w1_d = din("w_mlp1", [NEXP, D, 2 * D])
    w2_d = din("w_mlp2", [NEXP, D, D])
    b1r_d = din("b1r", [NEXP, 2 * D])
    b2_d = din("b_mlp2", [NEXP, D])
    out_d = nc.dram_tensor("out", [TOK, D], F32, kind="ExternalOutput")
    x1_d = nc.dram_tensor("x1_d", [TOK, D], F32, kind="ExternalOutput" if debug else "Internal")
    JM = min(32, NT)
    NSLOT = TOK * 4 + NEXP * 128
    NBLK = NSLOT // 128
    h2_d = nc.dram_tensor("h2_d", [TOK, D], BF16)
    xs_d = nc.dram_tensor("xs_d", [NSLOT, D], BF16)
    ys_d = nc.dram_tensor("ys_d", [NSLOT, D], F32)
    G_d = nc.dram_tensor("G_d", [128, NT * NEXP], F32, kind="ExternalOutput") if debug else None

    stack = ExitStack()
    with stack:
        S = Sched(nc, stack)

        def sb(st, name, shape, dt=F32):
            return st.enter_context(nc.sbuf_tensor("s_" + name, list(shape), dt))

        def mkring(st, name, n, shape, dt=F32):
            return Ring(name, [sb(st, "%s_%d" % (name, i), shape, dt) for i in range(n)])

        def mm(out, lhsT, rhs, start, stop, r, w, sig=True):
            S.op("tensor", lambda e: e.matmul(out, lhsT, rhs, start=start, stop=stop), r, w, sig=sig)

        def tr(out, in_, ident, r, w, sig=True):
            S.op("tensor", lambda e: e.transpose(out, in_, ident), r, w, sig=sig)

        def act(out, in_, func, r, w, bias=None, scale=None, accum=None):
            kw = {}
            if bias is not None:
                kw["bias"] = bias
            if scale is not None:
                kw["scale"] = scale
            if accum is not None:
                kw["accum_out"] = accum
            S.op("scalar", lambda e: e.activation(out, in_, func, **kw), r, w)

        def ts(eng, out, in0, s1, s2, op0, op1, r, w, accum=None):
            if accum is not None:
                S.op(eng, lambda e: e.tensor_scalar(out, in0, s1, s2, op0, op1, accum_out=accum), r, w)
            elif op1 is None:
                S.op(eng, lambda e: e.tensor_scalar(out, in0, s1, None, op0), r, w)
            else:
                S.op(eng, lambda e: e.tensor_scalar(out, in0, s1, s2, op0, op1), r, w)

        def tt(eng, out, in0, in1, op, r, w):
            S.op(eng, lambda e: e.tensor_tensor(out, in0, in1, op), r, w)

        def stt(out, in0, scalar, in1, op0, op1, r, w, accum=None):
            if accum is not None:
                S.op("vector", lambda e: e.scalar_tensor_tensor(out, in0, scalar, in1, op0, op1, accum_out=accum), r, w)
            else:
                S.op("vector", lambda e: e.scalar_tensor_tensor(out, in0, scalar, in1, op0, op1), r, w)

        def cp(eng, out, in_, r, w):
            if eng == "scalar":
                S.op(eng, lambda e: e.copy(out, in_), r, w)
            else:
                S.op(eng, lambda e: e.tensor_copy(out, in_), r, w)

        def dma(eng, out, in_, r, w):
            S.dma(eng, lambda e: e.dma_start(out=out, in_=in_), r, w)

        P = stack
        consts = sb(P, "consts", [128, C_TOT])
        identb = sb(P, "identb", [128, 128], BF16)
        mods = sb(P, "mods", [128, 6, D])
        G_all = sb(P, "G_all", [128, NT, NEXP])
        Qm_all = sb(P, "Qm_all", [128, NT, NEXP])
        Orun = sb(P, "Orun", [128, NEXP])
        onesb = sb(P, "onesb", [128, 128], BF16)
        lstrb = sb(P, "lstrb", [128, 128], BF16)
        GM1, SH1, GP1, GM2, SH2, GP2 = range(6)

        ps = Ring("ps", [stack.enter_context(nc.psum_tensor("ps%d" % i, [128, 512], F32)) for i in range(8)])

        identf = consts[:, C_IDENT:C_IDENT + 128]
        ones_f = consts[:, C_ONES:C_ONES + 128]

        dma("sync", consts[:], consts_d.ap(), [], ["consts"])
        cp("vector", identb[:], identf, ["consts"], ["identb"])
        cp("vector", onesb[:], ones_f, ["consts"], ["onesb"])
        cp("vector", lstrb[:], consts[:, C_LSTR:C_LSTR + 128], ["consts"], ["lstrb"])
        S.op("vector", lambda e: e.memset(Orun[:], 0.0), [], ["Orun"])
        gscr = sb(P, "gscr", [1, 16])
        S.op("gpsimd", lambda e: e.memset(gscr[:], 0.0), [], ["gscr"])
        breg = nc.gpsimd.alloc_register("bndreg")
        S.op("gpsimd", lambda e: e.reg_mov(breg, NSLOT - 1), [], [], sig=False)

        try:
            with ExitStack() as st:
                cT = sb(st, "cT", [128, 8])
                sg = sb(st, "sgc", [128, 8])
                rep = sb(st, "rep", [128, 8, 128])
                gvec = sb(st, "gvec", [128, 4 * D])
                bada = sb(st, "bada", [128, 6 * D])
                modbc = sb(st, "modbc", [128, 6 * D])
                wring = mkring(st, "wada", 2, [128, 8, 512])
                zt = sb(st, "zt", [128, D], BF16)
                S.op("gpsimd", lambda e: e.memset(zt[:], 0.0), [], ["zt"])
                xsz_v = xs_d.ap().rearrange("(b p) n -> b p n", p=128)
                ztf = sb(st, "ztf", [128, D])
                S.op("gpsimd", lambda e: e.memset(ztf[:], 0.0), [], ["ztf"])
                ysz_v = ys_d.ap().rearrange("(b p) n -> b p n", p=128)
                for b_ in range(NBLK):
                    dma("scalar", xsz_v[b_], zt[:, :], ["zt"], ["xsz%d" % b_])
                    dma("scalar", ysz_v[b_], ztf[:, :], ["ztf"], ["ysz%d" % b_])
                dma("sync", cT[:], cT_d.ap(), [], ["cT"])
                dma("sync", gvec[:], gvec_d.ap(), [], ["gvec"])
                dma("sync", bada[:], bada_d.ap(), [], ["bada"])
                act(sg[:], cT[:], AF.Sigmoid, ["cT"], ["sgc"])
                tt("vector", sg[:], sg[:], cT[:], ALU.mult, ["sgc", "cT"], ["sgc"])
                for kc in range(8):
                    ts("vector", rep[:, kc, :], ones_f, sg[:, kc:kc + 1], None, ALU.mult, None,
                       ["consts", "sgc"], ["rep%d" % kc])
                wada_v = wada_d.ap().rearrange("(k p) n -> p k n", p=128)
                for ng in range(12):
                    wt, wk = wring.next()
                    dma("sync", wt[:], wada_v[:, :, ng * 512:(ng + 1) * 512], [], [wk])
                    bank, bk = ps.next()
                    for kc in range(8):
                        mm(bank[:, :], rep[:, kc, :], wt[:, kc, :], kc == 0, kc == 7,
                           [wk, "rep%d" % kc], [bk], sig=(kc == 7))
                    tt("vector", modbc[:, ng * 512:(ng + 1) * 512], bank[:, :], bada[:, ng * 512:(ng + 1) * 512],
                       ALU.add, [bk, "bada"], ["modbc"])
                m = lambda i: modbc[:, i * D:(i + 1) * D]
                g = lambda i: gvec[:, i * D:(i + 1) * D]
                stt(mods[:, GM1, :], m(1), 1.0, g(0), ALU.add, ALU.mult, ["modbc", "gvec"], ["mods"])
                cp("vector", mods[:, SH1, :], m(0), ["modbc"], ["mods"])
                tt("vector", mods[:, GP1, :], m(2), g(1), ALU.mult, ["modbc", "gvec"], ["mods"])
                stt(mods[:, GM2, :], m(4), 1.0, g(2), ALU.add, ALU.mult, ["modbc", "gvec"], ["mods"])
                cp("vector", mods[:, SH2, :], m(3), ["modbc"], ["mods"])
                tt("vector", mods[:, GP2, :], m(5), g(3), ALU.mult, ["modbc", "gvec"], ["mods"])
                S.barrier()
                if stage <= 0:
                    S.enabled = False

            with ExitStack() as st:
                w_in = sb(st, "w_in_sb", [128, 8, INW], BF16)
                w_out = sb(st, "w_out_sb", [128, 8, D], BF16)
                w_r = sb(st, "w_r_sb", [128, 8, NEXP])
                wup = sb(st, "wup_sb", [32, 256])
                kTs = sb(st, "kTs", [64, 2, 2, 128], BF16)
                vs = sb(st, "vs", [128, 2, 128], BF16)
                gaT = sb(st, "gaT", [32, 128])
                Sf = [sb(st, "Sf%d" % i, [64, 4, 128]) for i in range(2)]
                Sb = [sb(st, "Sb%d" % i, [64, 4, 128], BF16) for i in range(2)]
                Qc0 = sb(st, "Qc0", [64, 4, 128], BF16)
                Qc1 = sb(st, "Qc1", [64, 4, 128], BF16)

                with ExitStack() as stw:
                    stg = mkring(stw, "stg", 2, [128, 1160])
                    win_v = win_d.ap().rearrange("(k p) n -> k p n", p=128)
                    wout_v = wout_d.ap().rearrange("(k p) n -> k p n", p=128)
                    ceng = ["vector", "gpsimd"]
                    for kc in range(8):
                        for hf_ in range(2):
                            s_, sk = stg.next()
                            dma("sync", s_[:, :], win_v[kc][:, hf_ * 1160:(hf_ + 1) * 1160], [], [sk])
                            cp(ceng[hf_], w_in[:, kc, hf_ * 1160:(hf_ + 1) * 1160], s_[:, :], [sk], ["w_in"])
                    for kc in range(8):
                        s_, sk = stg.next()
                        dma("sync", s_[:, 0:D], wout_v[kc], [], [sk])
                        cp(ceng[kc % 2], w_out[:, kc, :], s_[:, 0:D], [sk], ["w_out"])
                    dma("sync", w_r[:], wr_d.ap().rearrange("(k p) n -> p k n", p=128), [], ["w_r"])
                    dma("sync", wup[0:17, :], wup_d.ap(), [], ["wup"])
                    S.op("vector", lambda e: e.memset(gaT[:], 1.0), [], ["gaT"])
                    for i in range(2):
                        S.op("gpsimd", lambda e, i=i: e.memset(Sf[i][:], 0.0), [], ["Sf%d" % i])
                        S.op("gpsimd", lambda e, i=i: e.memset(Sb[i][:], 0.0), [], ["Sb%d" % i])
                    S.op("gpsimd", lambda e: e.memset(Qc0[:], 0.0), [], ["Qc0"])
                    S.op("gpsimd", lambda e: e.memset(Qc1[:], 0.0), [], ["Qc1"])
                    S.barrier()

                xr = mkring(st, "xr", 2, [128, D])
                junk = mkring(st, "junk", 2, [128, D], BF16)
                tmpr = mkring(st, "tmpr", 1, [128, D])
                hbr = mkring(st, "hbr", 2, [128, D], BF16)
                hTr = mkring(st, "hTr", 2, [128, 8, 128], BF16)
                smr = mkring(st, "smr", 4, [128, 16])
                qTa = mkring(st, "qTa", 1, [64, 8, 128], BF16)
                gqr = mkring(st, "gqr", 1, [64, 4, 128])
                gkr = mkring(st, "gkr", 1, [64, 4, 128])
                ktokr = mkring(st, "ktokr", 2, [128, 256])
                vtokr = mkring(st, "vtokr", 2, [128, 512], BF16)
                sgg = mkring(st, "sgg", 1, [128, 512])
                enr = mkring(st, "enr", 2, [128, 256])
                ltok = mkring(st, "ltok", 2, [128, 256])
                bTr = mkring(st, "bTr", 1, [64, 4, 128])
                nbm = mkring(st, "nbm", 1, [64, 4, 2])
                decr = mkring(st, "decr", 2, [64, 4, 2])
                E1r = mkring(st, "E1r", 1, [64, 4, 128])
                E2r = mkring(st, "E2r", 1, [64, 4, 128])
                E3r = mkring(st, "E3r", 1, [64, 4, 128])
                QpT = mkring(st, "QpT", 1, [64, 4, 128], BF16)
                KpT = mkring(st, "KpT", 1, [64, 4, 128], BF16)
                E4r = mkring(st, "E4r", 2, [128, 256])
                Kpp = mkring(st, "Kpp", 2, [128, 256], BF16)
                ATr = mkring(st, "ATr", 1, [128, 4, 128], BF16)
                scr = mkring(st, "scr", 1, [128, 8, 256])
                pbr = mkring(st, "pbr", 1, [128, 8, 256], BF16)
                pTr = mkring(st, "pTr", 1, [128, 16, 128], BF16)
                mixr = mkring(st, "mixr", 1, [128, D], BF16)
                mixTr = mkring(st, "mixTr", 1, [128, 8, 128], BF16)
                osq = mkring(st, "osq", 1, [128, 512])
                x1r = mkring(st, "x1r", 1, [128, D])
                h2r = mkring(st, "h2r", 1, [128, D])
                h2Tf = mkring(st, "h2Tf", 1, [128, 8, 128])
                h2br = mkring(st, "h2br", 1, [128, D], BF16)
                mkbr = mkring(st, "mkbr", 1, [128, NEXP], BF16)
                lgr = mkring(st, "lgr", 1, [128, 4, NEXP])

                if stage <= 1:
                    S.enabled = False

                mask2 = consts[:, C_MASK2:C_MASK2 + 512]
                maskF = consts[:, C_MASKF:C_MASKF + 512]
                tri4 = consts[:, C_TRI4:C_TRI4 + 512]
                Uincl = consts[:, C_UINCL:C_UINCL + 128]
                Urev = consts[:, C_UREV:C_UREV + 128]
                gnorm = consts[:, C_GNORM:C_GNORM + 512]
                sinks = consts[:, C_SINK:C_SINK + 8]
                brout = consts[:, C_BROUT:C_BROUT + NEXP]

                x_v = x_d.ap().rearrange("(t p) n -> t p n", p=128)
                xp_v = xp_d.ap().rearrange("(t p) n -> t p n", p=128)
                x1_v = x1_d.ap().rearrange("(t p) n -> t p n", p=128)
                out_v = out_d.ap().rearrange("(t p) n -> t p n", p=128)
                h2_v = h2_d.ap().rearrange("(t p) n -> t p n", p=128)

                state = {"cur": 0}

                def rstd_from_ss(sm, smk, col_in, col_out, n, cnt=1):
                    a = sm[:, col_in:col_in + cnt]
                    o = sm[:, col_out:col_out + cnt]
                    ts("vector", o, a, 1.0 / n, EPS, ALU.mult, ALU.add, [smk], [smk])
                    act(o, o, AF.Sqrt, [smk], [smk])
                    S.op("vector", lambda e: e.reciprocal(o, o), [smk], [smk])

                def norm_mod(x_t, xk, gi, si, out_ap, outk, sm, smk, c0):
                    jk_t, jk = junk.next()
                    act(jk_t[:, :], x_t[:, :], AF.Square, [xk], [jk, smk], accum=sm[:, c0:c0 + 1])
                    rstd_from_ss(sm, smk, c0, c0 + 1, float(D))
                    tm, tk = tmpr.next()
                    stt(tm[:, :], x_t[:, :], sm[:, c0 + 1:c0 + 2], mods[:, gi, :], ALU.mult, ALU.mult, [xk, smk], [tk])
                    tt("gpsimd", out_ap, tm[:, :], mods[:, si, :], ALU.add, [tk], [outk])

                def transpose8(src, srck, dst, dstk, evac_eng):
                    for half in range(2):
                        bank, bk = ps.next()
                        for j in range(4):
                            kc = half * 4 + j
                            mm(bank[:, j * 128:(j + 1) * 128], src[:, kc * 128:(kc + 1) * 128], identb[:], True, True,
                               [srck], [bk], sig=(j == 3))
                        cp("scalar" if half == 0 else "vector",
                           dst[:, half * 4:half * 4 + 4, :].rearrange("p k n -> p (k n)"), bank[:, :], [bk], [dstk])

                def gla_gates(hT, hTk, slot_rows):
                    bank, bk = ps.next()
                    for kc in range(8):
                        mm(bank[0:16, 0:128], w_in[:, kc, 2304:2320], hT[:, kc, :], kc == 0, kc == 7,
                           [hTk], [bk], sig=(kc == 7))
                    cp("vector", gaT[0:16, :], bank[0:16, 0:128], [bk], ["gaT"])
                    zb, zk = ps.next()
                    mm(zb[:, 0:256], gaT[0:17, :], wup[0:17, :], True, True, ["gaT"], [zk])
                    en, ek = enr.next()
                    act(en[:, :], zb[:, 0:256], AF.Exp, [zk], [ek], scale=-1.0)
                    l, lk = ltok.next()
                    act(l[:, :], en[:, :], AF.Ln, [ek], [lk], bias=1.0)
                    return l, lk

                def tile_body(t, pre):
                    main = not pre
                    slot = t + 1 if main else 0
                    last_pre = pre and (t == NP - 1)
                    xt, xk = xr.next()
                    dma("sync", xt[:, :], (x_v if main else xp_v)[t], [], [xk])
                    sm, smk = smr.next()
                    hb, hbk = hbr.next()
                    norm_mod(xt, xk, GM1, SH1, hb[:, :], hbk, sm, smk, 0)
                    hT, hTk = hTr.next()
                    if sub <= 0:
                        return
                    transpose8(hb, hbk, hT, hTk, "scalar")
                    if debug and main and t == 0:
                        dh = nc.dram_tensor("dbg_h", [128, D], BF16, kind="ExternalOutput")
                        dma("sync", dh.ap(), hb[:, :], [hbk], ["dbg_h"])
                        dhT = nc.dram_tensor("dbg_hT", [128, D], BF16, kind="ExternalOutput")
                        dma("sync", dhT.ap(), hT[:].rearrange("p k n -> p (k n)"), [hTk], ["dbg_hT"])
                    if sub <= 1:
                        return

                    def fm_group(cols_list, bank, bk):
                        for j, c0 in enumerate(cols_list):
                            for kc in range(8):
                                mm(bank[0:64, j * 128:(j + 1) * 128], w_in[:, kc, c0:c0 + 64], hT[:, kc, :],
                                   kc == 0, kc == 7, [hTk], [bk], sig=(kc == 7 and j == len(cols_list) - 1))

                    def tm_group(c0, n, bank, bk, off=0, last=True):
                        for kc in range(8):
                            mm(bank[:, off:off + n], hT[:, kc, :], w_in[:, kc, c0:c0 + n], kc == 0, kc == 7,
                               [hTk], [bk], sig=(kc == 7 and last))

                    if main:
                        qa, qak = qTa.next()
                        for half in range(2):
                            bank, bk = ps.next()
                            fm_group([h * 64 for h in range(half * 4, half * 4 + 4)], bank, bk)
                            S.op("scalar", lambda e, bank=bank, half=half: e.mul(
                                qa[:, half * 4:half * 4 + 4, :].rearrange("p h n -> p (h n)"), bank[0:64, :], 0.125),
                                [bk], [qak])
                        gq, gqk = gqr.next()
                        bank, bk = ps.next()
                        fm_group([768 + h * 64 for h in range(4)], bank, bk)
                        S.op("scalar", lambda e, bank=bank: e.mul(gq[:].rearrange("p h n -> p (h n)"), bank[0:64, :], 0.125),
                             [bk], [gqk])
                        gk, gkk = gkr.next()
                        bank, bk = ps.next()
                        fm_group([1024 + h * 64 for h in range(4)], bank, bk)
                        cp("vector", gk[:].rearrange("p h n -> p (h n)"), bank[0:64, :], [bk], [gkk])
                    KD = os.environ.get("KDBG", "")
                    if KD == "tm":
                        pass
                    elif main or last_pre:
                        bank, bk = ps.next()
                        fm_group([512, 576], bank, bk)
                        cp("vector", kTs[:, slot % 2, :, :],
                           bank[0:64, 0:256].rearrange("p (h n) -> p h n", h=2), [bk], ["kT%d" % (slot % 2)])
                    if KD == "fm":
                        return
                    bank, bk = ps.next()
                    if (main or last_pre) and KD != "tm1b":
                        tm_group(640, 128, bank, bk, off=0, last=False)
                    tm_group(1024, 256, bank, bk, off=128)
                    if (main or last_pre) and KD != "tm1b":
                        cp("vector" if KD == "tm1c" else "scalar", vs[:, slot % 2, :], bank[:, 0:128], [bk], ["v%d" % (slot % 2)])
                    ktok, ktk = ktokr.next()
                    cp("vector", ktok[:, :], bank[:, 128:384], [bk], [ktk])
                    if KD in ("tm1", "tm1b", "tm1c"):
                        return
                    bank, bk = ps.next()
                    tm_group(1280, 512, bank, bk)
                    vtok, vtk = vtokr.next()
                    if pre:
                        ts("vector", vtok[:, :], bank[:, :], consts[:, C_PFLAG + t:C_PFLAG + t + 1], None, ALU.mult, None,
                           [bk], [vtk])
                    else:
                        cp("scalar", vtok[:, :], bank[:, :], [bk], [vtk])
                    if main:
                        bank, bk = ps.next()
                        tm_group(1792, 512, bank, bk)
                        sg_t, sgk = sgg.next()
                        act(sg_t[:, :], bank[:, :], AF.Silu, [bk], [sgk])
                        tt("gpsimd", sg_t[:, :], sg_t[:, :], gnorm, ALU.mult, [sgk], [sgk])

                    if sub <= 2:
                        return
                    l, lk = gla_gates(hT, hTk, None)
                    if sub <= 3:
                        return
                    rvb, rvk = ps.next()
                    mm(rvb[:, 0:256], Urev, l[:, :], True, True, [lk], [rvk])
                    E4, E4k = E4r.next()
                    act(E4[:, :], rvb[:, 0:256], AF.Exp, [rvk], [E4k])
                    kpp, kppk = Kpp.next()
                    tt("vector", kpp[:, :], ktok[:, :], E4[:, :], ALU.mult, [ktk, E4k], [kppk])
                    bTb, bTbk = ps.next()
                    if main:
                        for hd in range(4):
                            mm(bTb[0:64, hd * 128:(hd + 1) * 128], l[:, hd * 64:(hd + 1) * 64], Uincl, True, True,
                               [lk], [bTbk], sig=(hd == 3))
                        bT, bTk = bTr.next()
                        cp("vector", bT[:].rearrange("p h n -> p (h n)"), bTb[0:64, :], [bTbk], [bTk])
                        nb, nbk = nbm.next()
                        ts("vector", nb[:], bT[:, :, 31:128:64], -1.0, None, ALU.mult, None, [bTk], [nbk])
                        E1, E1k = E1r.next()
                        for hd in range(4):
                            for c in range(2):
                                act(E1[:, hd, c * 64:(c + 1) * 64], bT[:, hd, c * 64:(c + 1) * 64], AF.Exp,
                                    [bTk, nbk], [E1k], bias=nb[:, hd, c:c + 1])
                        E2, E2k = E2r.next()
                        S.op("vector", lambda e, E2=E2, E1=E1: e.reciprocal(E2[:], E1[:]), [E1k], [E2k])
                        E3, E3k = E3r.next()
                        act(E3[:], bT[:], AF.Exp, [bTk], [E3k])
                        qp, qpk = QpT.next()
                        tt("vector", qp[:], gq[:], E1[:], ALU.mult, [gqk, E1k], [qpk])
                        kp, kpk = KpT.next()
                        tt("gpsimd", kp[:], gk[:], E2[:], ALU.mult, [gkk, E2k], [kpk])
                        tt("gpsimd", Qc0[:, :, 0:64], gq[:, :, 0:64], E3[:, :, 0:64], ALU.mult, [gqk, E3k], ["Qc0"])
                        tt("gpsimd", Qc1[:, :, 64:128], gq[:, :, 64:128], E3[:, :, 64:128], ALU.mult, [gqk, E3k], ["Qc1"])
                        dec = lambda hd, c: E3[:, hd, c * 64 + 63:c * 64 + 64]
                        deck = E3k
                    else:
                        for hd in range(4):
                            mm(bTb[0:64, hd * 2:hd * 2 + 2], l[:, hd * 64:(hd + 1) * 64],
                               consts[:, C_UINCL + 63:C_UINCL + 128:64], True, True, [lk], [bTbk], sig=(hd == 3))
                        dc, dck = decr.next()
                        act(dc[:].rearrange("p h c -> p (h c)"), bTb[0:64, 0:8], AF.Exp, [bTbk], [dck])
                        dec = lambda hd, c: dc[:, hd, c:c + 1]
                        deck = dck

                    if sub <= 4:
                        return
                    cur = state["cur"]
                    S0f, S0b, S1f, S1b = Sf[cur], Sb[cur], Sf[1 - cur], Sb[1 - cur]
                    k0, k1 = "S%d" % cur, "S%d" % (1 - cur)
                    if main:
                        atb, atk = ps.next()
                        for hd in range(4):
                            mm(atb[:, hd * 128:(hd + 1) * 128], kp[:, hd, :], qp[:, hd, :], True, True,
                               [kpk, qpk], [atk], sig=(hd == 3))
                        AT, ATk = ATr.next()
                        tt("vector", AT[:].rearrange("p h n -> p (h n)"), atb[:, :], tri4, ALU.mult, [atk], [ATk])
                    kvb, kvk = ps.next()
                    for hd in range(4):
                        mm(kvb[0:64, hd * 128:(hd + 1) * 128], kpp[0:64, hd * 64:(hd + 1) * 64],
                           vtok[0:64, hd * 128:(hd + 1) * 128], True, True, [kppk, vtk], [kvk], sig=(hd == 3))
                    for hd in range(4):
                        stt(S1f[:, hd, :], S0f[:, hd, :], dec(hd, 0), kvb[0:64, hd * 128:(hd + 1) * 128],
                            ALU.mult, ALU.add, [k0 + "f", deck, kvk], [k1 + "f"])
                    cp("gpsimd", S1b[:], S1f[:], [k1 + "f"], [k1 + "b"])
                    kvb2, kvk2 = ps.next()
                    for hd in range(4):
                        mm(kvb2[0:64, hd * 128:(hd + 1) * 128], kpp[64:128, hd * 64:(hd + 1) * 64],
                           vtok[64:128, hd * 128:(hd + 1) * 128], True, True, [kppk, vtk], [kvk2], sig=(hd == 3))
                    if main:
                        ob, obk = ps.next()
                        for hd in range(4):
                            o_ap = ob[:, hd * 128:(hd + 1) * 128]
                            mm(o_ap, AT[:, hd, :], vtok[:, hd * 128:(hd + 1) * 128], True, False, [ATk, vtk], [obk], sig=False)
                            mm(o_ap, Qc0[:, hd, :], S0b[:, hd, :], False, False, ["Qc0", k0 + "b"], [obk], sig=False)
                            mm(o_ap, Qc1[:, hd, :], S1b[:, hd, :], False, True, ["Qc1", k1 + "b"], [obk], sig=(hd == 3))
                    for hd in range(4):
                        stt(S0f[:, hd, :], S1f[:, hd, :], dec(hd, 1), kvb2[0:64, hd * 128:(hd + 1) * 128],
                            ALU.mult, ALU.add, [k1 + "f", deck, kvk2], [k0 + "f"])
                    cp("gpsimd", S0b[:], S0f[:], [k0 + "f"], [k0 + "b"])
                    if pre:
                        return

                    mix, mixk = mixr.next()
                    sq, sqk = osq.next()
                    sm4, sm4k = smr.next()
                    for hd in range(4):
                        act(sq[:, hd * 128:(hd + 1) * 128], ob[:, hd * 128:(hd + 1) * 128], AF.Square, [obk], [sqk, sm4k],
                            accum=sm4[:, hd:hd + 1])
                    rstd_from_ss(sm4, sm4k, 0, 4, 128.0, cnt=4)
                    for hd in range(4):
                        stt(mix[:, 512 + hd * 128:512 + (hd + 1) * 128], ob[:, hd * 128:(hd + 1) * 128], sm4[:, 4 + hd:5 + hd],
                            sg_t[:, hd * 128:(hd + 1) * 128], ALU.mult, ALU.mult, [obk, sm4k, sgk], [mixk])

                    sc, sck = scr.next()
                    for pair in range(4):
                        bank, bk = ps.next()
                        for j in range(2):
                            h = pair * 2 + j
                            for c in range(2):
                                mm(bank[:, j * 256 + c * 128:j * 256 + (c + 1) * 128], qa[:, h, :],
                                   kTs[:, (t + c) % 2, h // 4, :], True, True, [qak, "kT%d" % ((t + c) % 2)], [bk],
                                   sig=(j == 1 and c == 1))
                        tt("vector", sc[:, pair * 2:pair * 2 + 2, :].rearrange("p h n -> p (h n)"), bank[:, :],
                           maskF if t == 0 else mask2, ALU.add, [bk], [sck])
                    sm2, sm2k = smr.next()
                    S.op("vector", lambda e: e.tensor_reduce(sm2[:, 0:8], sc[:], AX.X, ALU.max), [sck], [sm2k])
                    tt("vector", sm2[:, 0:8], sm2[:, 0:8], sinks, ALU.max, [sm2k], [sm2k])
                    ts("vector", sm2[:, 0:8], sm2[:, 0:8], -1.0, None, ALU.mult, None, [sm2k], [sm2k])
                    sm3, sm3k = smr.next()
                    pb, pbk = pbr.next()
                    for h in range(8):
                        act(pb[:, h, :], sc[:, h, :], AF.Exp, [sck, sm2k], [pbk, sm3k], bias=sm2[:, h:h + 1],
                            accum=sm3[:, h:h + 1])
                    tt("vector", sm2[:, 8:16], sinks, sm2[:, 0:8], ALU.add, [sm2k], [sm2k])
                    act(sm2[:, 8:16], sm2[:, 8:16], AF.Exp, [sm2k], [sm2k])
                    tt("vector", sm3[:, 0:8], sm3[:, 0:8], sm2[:, 8:16], ALU.add, [sm2k, sm3k], [sm3k])
                    S.op("vector", lambda e: e.reciprocal(sm3[:, 8:16], sm3[:, 0:8]), [sm3k], [sm3k])
                    pT, pTk = pTr.next()
                    for q4 in range(4):
                        bank, bk = ps.next()
                        for j in range(2):
                            h = q4 * 2 + j
                            for c in range(2):
                                mm(bank[:, (j * 2 + c) * 128:(j * 2 + c + 1) * 128], pb[:, h, c * 128:(c + 1) * 128],
                                   identb[:], True, True, [pbk], [bk], sig=(j == 1 and c == 1))
                        cp("scalar" if q4 % 2 == 0 else "vector",
                           pT[:, q4 * 4:q4 * 4 + 4, :].rearrange("p a n -> p (a n)"), bank[:, :], [bk], [pTk])
                    ab, abk = ps.next()
                    for h in range(8):
                        kv = h // 4
                        mm(ab[:, h * 64:(h + 1) * 64], pT[:, h * 2, :], vs[:, t % 2, kv * 64:(kv + 1) * 64], True, False,
                           [pTk, "v%d" % (t % 2)], [abk], sig=False)
                        mm(ab[:, h * 64:(h + 1) * 64], pT[:, h * 2 + 1, :], vs[:, (t + 1) % 2, kv * 64:(kv + 1) * 64], False, True,
                           [pTk, "v%d" % ((t + 1) % 2)], [abk], sig=(h == 7))
                    for h in range(8):
                        ts("vector", mix[:, h * 64:(h + 1) * 64], ab[:, h * 64:(h + 1) * 64],
                           sm3[:, 8 + h:9 + h], None, ALU.mult, None, [abk, sm3k], [mixk])

                    mixT, mixTk = mixTr.next()
                    transpose8(mix, mixk, mixT, mixTk, "scalar")
                    ybanks = []
                    for n in range(2):
                        bank, bk = ps.next()
                        for kc in range(8):
                            mm(bank[:, :], mixT[:, kc, :], w_out[:, kc, n * 512:(n + 1) * 512], kc == 0, kc == 7,
                               [mixTk], [bk], sig=(kc == 7))
                        ybanks.append((bank, bk))
                    sm5, sm5k = smr.next()
                    jk_t, jk = junk.next()
                    for n in range(2):
                        act(jk_t[:, n * 512:(n + 1) * 512], ybanks[n][0][:, :], AF.Square, [ybanks[n][1]], [jk, sm5k],
                            accum=sm5[:, n:n + 1])
                    tt("vector", sm5[:, 2:3], sm5[:, 0:1], sm5[:, 1:2], ALU.add, [sm5k], [sm5k])
                    rstd_from_ss(sm5, sm5k, 2, 3, float(D))
                    tm, tk = tmpr.next()
                    for n in range(2):
                        stt(tm[:, n * 512:(n + 1) * 512], ybanks[n][0][:, :], sm5[:, 3:4], mods[:, GP1, n * 512:(n + 1) * 512],
                            ALU.mult, ALU.mult, [ybanks[n][1], sm5k], [tk])
                    x1, x1k = x1r.next()
                    tt("gpsimd", x1[:, :], tm[:, :], xt[:, :], ALU.add, [tk, xk], [x1k])
                    dma("sync", x1_v[t], x1[:, :], [x1k], ["x1d%d" % t])

                    h2, h2k = h2r.next()
                    norm_mod(x1, x1k, GM2, SH2, h2[:, :], h2k, sm5, sm5k, 4)
                    hf, hfk = h2Tf.next()
                    for half in range(2):
                        bank, bk = ps.next()
                        for j in range(4):
                            kc = half * 4 + j
                            mm(bank[:, j * 128:(j + 1) * 128], h2[:, kc * 128:(kc + 1) * 128], identf, True, True,
                               [h2k], [bk], sig=(j == 3))
                        cp("vector" if half == 0 else "scalar", hf[:, half * 4:half * 4 + 4, :].rearrange("p k n -> p (k n)"),
                           bank[:, :], [bk], [hfk])
                    hbf, hbfk = h2br.next()
                    cp("gpsimd", hbf[:, :], h2[:, :], [h2k], [hbfk])
                    dma("sync", h2_v[t], hbf[:, :], [hbfk], ["h2d%d" % t])
                    lb, lbk = ps.next()
                    for kc in range(8):
                        mm(lb[:, 0:NEXP], hf[:, kc, :], w_r[:, kc, :], kc == 0, kc == 7, [hfk], [lbk], sig=(kc == 7))
                    lg, lgk = lgr.next()
                    LG, MK, EX, M8 = 0, 1, 2, 3
                    tt("vector", lg[:, LG, :], lb[:, 0:NEXP], brout, ALU.add, [lbk], [lgk])
                    S.op("vector", lambda e: e.max(lg[:, M8, 0:8], lg[:, LG, :]), [lgk], [lgk])
                    ts("vector", lg[:, MK, :], lg[:, LG, :], lg[:, M8, 3:4], None, ALU.is_ge, None, [lgk], [lgk])
                    ts("vector", lg[:, M8, 8:9], lg[:, M8, 0:1], -1.0, None, ALU.mult, None, [lgk], [lgk])
                    act(lg[:, EX, :], lg[:, LG, :], AF.Exp, [lgk], [lgk], bias=lg[:, M8, 8:9])
                    tt("vector", lg[:, EX, :], lg[:, EX, :], lg[:, MK, :], ALU.mult, [lgk], [lgk])
                    S.op("vector", lambda e: e.tensor_reduce(lg[:, M8, 9:10], lg[:, EX, :], AX.X, ALU.add), [lgk], [lgk])
                    S.op("vector", lambda e: e.reciprocal(lg[:, M8, 10:11], lg[:, M8, 9:10]), [lgk], [lgk])
                    ts("vector", G_all[:, t, :], lg[:, EX, :], lg[:, M8, 10:11], None, ALU.mult, None, [lgk], ["G%d" % t])
                    mkb, mkbk = mkbr.next()
                    cp("vector", mkb[:, :], lg[:, MK, :], [lgk], [mkbk])
                    rb, rbk = ps.next()
                    mm(rb[:, 0:NEXP], lstrb[:], mkb[:, :], True, True, [mkbk], [rbk], sig=False)
                    mm(rb[:, NEXP:2 * NEXP], onesb[:], mkb[:, :], True, True, [mkbk], [rbk])
                    stt(Qm_all[:, t, :], rb[:, 0:NEXP], 1.0, Orun[:, :], ALU.add, ALU.add, [rbk, "Orun"], ["Qm%d" % t])
                    tt("vector", Qm_all[:, t, :], Qm_all[:, t, :], lg[:, MK, :], ALU.mult, ["Qm%d" % t, lgk], ["Qm%d" % t])
                    tt("vector", Orun[:, :], Orun[:, :], rb[:, NEXP:2 * NEXP], ALU.add, ["Orun", rbk], ["Orun"])

                for p in range(NP):
                    tile_body(p, True)
                if stage <= 2:
                    S.enabled = False
                for t in range(NT):
                    tile_body(t, False)
                if debug:
                    dma("sync", G_d.ap(), G_all[:].rearrange("p t e -> p (t e)"), ["G%d" % t for t in range(NT)], ["Gd"])
                S.barrier()
                if stage <= 3:
                    S.enabled = False

            h2_v = h2_d.ap().rearrange("(t p) n -> t p n", p=128)
            x1_v = x1_d.ap().rearrange("(t p) n -> t p n", p=128)
            out_v = out_d.ap().rearrange("(t p) n -> t p n", p=128)
            gk_all = sb(P, "gk_all", [128, NT, 4])
            idx4_all = sb(P, "idx4_all", [128, NT, 4], I32)
            flags_i = sb(P, "flags_i", [128, NEXP * JM], I32)
            idxb_i = sb(P, "idxb_i", [128, NEXP * JM], I32)
            with ExitStack() as st:
                flf = sb(st, "flf", [128, NEXP, JM])
                nbt = sb(st, "nbt", [128, NEXP])
                cA = sb(st, "cA", [128, NEXP])
                cB = sb(st, "cB", [128, NEXP])
                pst = sb(st, "pst", [128, NEXP])
                idf = sb(st, "idf", [128, NEXP, JM])
                vr = mkring(st, "vr", 2, [128, 3, NEXP])
                m8r = mkring(st, "m8r", 2, [128, 16])
                h2l = mkring(st, "h2l", 2, [128, D], BF16)
                TH3 = consts[:, C_TH:C_TH + NEXP * 32].rearrange("p (e j) -> p e j", e=NEXP)[:, :, 0:JM]
                S.op("vector", lambda e: e.tensor_tensor(flf[:], Orun[:].unsqueeze(2).to_broadcast([128, NEXP, JM]), TH3,
                                                         ALU.is_gt), ["Orun"], ["flf"])
                S.op("vector", lambda e: e.tensor_reduce(nbt[:], flf[:], AX.X, ALU.add), ["flf"], ["nbt"])
                ts("vector", cA[:], nbt[:], 128.0, None, ALU.mult, None, ["nbt"], ["cA"])
                cp("vector", pst[:], cA[:], ["cA"], ["pst"])
                ca, cb, cak, cbk = cA, cB, "cA", "cB"
                for sft in (1, 2, 4, 8, 16):
                    cp("vector", cb[:, 0:sft], ca[:, 0:sft], [cak], [cbk])
                    tt("vector", cb[:, sft:NEXP], ca[:, sft:NEXP], ca[:, 0:NEXP - sft], ALU.add, [cak], [cbk])
                    ca, cb, cak, cbk = cb, ca, cbk, cak
                tt("vector", pst[:], ca[:], pst[:], ALU.subtract, [cak, "pst"], ["pst"])
                cp("vector", flags_i[:], flf[:].rearrange("p e j -> p (e j)"), ["flf"], ["flags"])
                S.op("vector", lambda e: e.tensor_tensor(idf[:], TH3, pst[:].unsqueeze(2).to_broadcast([128, NEXP, JM]),
                                                         ALU.add), ["pst"], ["idf"])
                ts("vector", idf[:], idf[:], consts[:, C_IOTA:C_IOTA + 1], -65536.0, ALU.add, ALU.add, ["idf"], ["idf"])
                tt("vector", idf[:], idf[:], flf[:], ALU.mult, ["idf", "flf"], ["idf"])
                ts("vector", idf[:], idf[:], 65536.0, None, ALU.add, None, ["idf"], ["idf"])
                cp("vector", idxb_i[:], idf[:].rearrange("p e j -> p (e j)"), ["idf"], ["idxb"])
                for t in range(NT):
                    v, vk = vr.next()
                    ts("vector", v[:, 0, :], Qm_all[:, t, :], 0.0, None, ALU.is_gt, None, [], [vk])
                    tt("vector", v[:, 1, :], Qm_all[:, t, :], pst[:], ALU.add, ["pst"], [vk])
                    tt("vector", v[:, 1, :], v[:, 1, :], v[:, 0, :], ALU.mult, [vk], [vk])
                    m8, m8k = m8r.next()
                    S.op("vector", lambda e, m8=m8, v=v: e.max(m8[:, 0:8], v[:, 1, :]), [vk], [m8k])
                    ts("vector", m8[:, 8:12], m8[:, 0:4], -1.0, None, ALU.add, None, [m8k], [m8k])
                    cp("vector", idx4_all[:, t, :], m8[:, 8:12], [m8k], ["idx4_%d" % t])
                    for k in range(4):
                        ts("vector", v[:, 2, :], v[:, 1, :], m8[:, k:k + 1], None, ALU.is_equal, None, [vk, m8k], [vk])
                        stt(v[:, 0, :], v[:, 2, :], 1.0, G_all[:, t, :], ALU.mult, ALU.mult, [vk], [vk, "gk%d" % t],
                            accum=gk_all[:, t, k:k + 1])
                    h2t, h2tk = h2l.next()
                    dma("sync", h2t[:, :], h2_v[t], [], [h2tk])
                    for k in range(4):
                        S.dma("gpsimd", lambda en, t=t, k=k, h2t=h2t: en.indirect_dma_start(
                            out=xs_d.ap(), out_offset=bass.IndirectOffsetOnAxis(ap=idx4_all[:, t, k:k + 1], axis=0),
                            in_=h2t[:, :], in_offset=None), [h2tk, "idx4_%d" % t], ["xsd"])
                S.barrier()

            with ExitStack() as st:
                w1b = mkring(st, "w1b", 2, [128, 8, 2, D], BF16)
                w2b = mkring(st, "w2b", 2, [128, 8, D], BF16)
                w1s = mkring(st, "w1s", 2, [128, 2, 256])
                w2s = mkring(st, "w2s", 2, [128, 512])
                b1s = mkring(st, "b1s", 1, [1, D])
                b1b = mkring(st, "b1b", 2, [1, 2 * D], BF16)
                xsr = mkring(st, "xsr", 2, [128, D], BF16)
                xsTr = mkring(st, "xsTr", 2, [128, 8, 128], BF16)
                aTr = mkring(st, "aTr", 2, [128, 8, 128], BF16)
                xgr = mkring(st, "xgr", 2, [128, 512])
                sgr = mkring(st, "sgr", 2, [128, 512])
                xlr = mkring(st, "xlr", 2, [128, 512])
                ysr = mkring(st, "ysr", 2, [128, D])
                for i_ in range(2):
                    S.op("gpsimd", lambda e, i_=i_: e.memset(ysr.t[i_][:], 0.0), [], ["ysr%d" % i_])
                w1_v = w1_d.ap().rearrange("e (k p) n -> e p k n", p=128)
                w2_v = w2_d.ap().rearrange("e (k p) n -> e k p n", p=128)

                def gather(e, j):
                    col = e * JM + j
                    xs, xsk = xsr.next()
                    S.dma("gpsimd", lambda en, xs=xs, col=col: en.indirect_dma_start(
                        out=xs[:, :], out_offset=None, in_=xs_d.ap(),
                        in_offset=bass.IndirectOffsetOnAxis(ap=idxb_i[:, col:col + 1], axis=0),
                        bounds_check=breg, oob_is_err=False), [], [xsk])
                    return xs, xsk

                def block(e, j, w1t, w1k, w2t, w2k, bb, bbk, xs, xsk):
                    col = e * JM + j
                    xT, xTk = xsTr.next()
                    transpose8(xs, xsk, xT, xTk, None)
                    aT, aTk = aTr.next()
                    for half in range(2):
                        banks = []
                        for two in range(2):
                            bank, bk = ps.next()
                            for q in range(4):
                                fc = half * 4 + q
                                o_ap = bank[:, q * 128:(q + 1) * 128]
                                for kc in range(8):
                                    mm(o_ap, w1t[:, kc, two, fc * 128:(fc + 1) * 128], xT[:, kc, :], kc == 0, False,
                                       [w1k, xTk], [bk], sig=False)
                                mm(o_ap, bb[0:1, two * D + fc * 128:two * D + (fc + 1) * 128], onesb[0:1, :], False, True,
                                   [bbk], [bk], sig=(q == 3))
                            banks.append((bank, bk))
                        (bg, bgk), (bl, blk) = banks
                        xg, xgk = xgr.next()
                        ts("vector", xg[:, :], bg[:, :], 7.0, None, ALU.min, None, [bgk], [xgk])
                        sgt, sgk2 = sgr.next()
                        act(sgt[:, :], xg[:, :], AF.Sigmoid, [xgk], [sgk2], scale=1.702)
                        xl, xlk = xlr.next()
                        ts("vector", xl[:, :], bl[:, :], 7.0, -7.0, ALU.min, ALU.max, [blk], [xlk])
                        tt("gpsimd", xg[:, :], xg[:, :], sgt[:, :], ALU.mult, [xgk, sgk2], [xgk])
                        stt(aT[:, half * 4:half * 4 + 4, :].rearrange("p k n -> p (k n)"), xl[:, :], 1.0, xg[:, :],
                            ALU.add, ALU.mult, [xlk, xgk], [aTk + "h%d" % half])
                    ysb, ysk = ysr.next()
                    ybs = [ps.next(), ps.next()]
                    for h2_ in range(2):
                        for n in range(2):
                            yb, ybk = ybs[n]
                            for q in range(4):
                                fc = h2_ * 4 + q
                                mm(yb[:, :], aT[:, fc, :], w2t[:, fc, n * 512:(n + 1) * 512], fc == 0, fc == 7,
                                   [aTk + "h%d" % h2_, w2k], [ybk], sig=(fc == 7))
                    for n in range(2):
                        yb, ybk = ybs[n]
                        cp("scalar" if n == 0 else "vector", ysb[:, n * 512:(n + 1) * 512], yb[:, :], [ybk], [ysk])
                    S.dma("gpsimd", lambda en, ysb=ysb, col=col: en.indirect_dma_start(
                        out=ys_d.ap(), out_offset=bass.IndirectOffsetOnAxis(ap=idxb_i[:, col:col + 1], axis=0),
                        in_=ysb[:, :], in_offset=None, bounds_check=breg, oob_is_err=False), [ysk], ["ysd"])

                def load_weights(e):
                    w1t, w1k = w1b.next()
                    w2t, w2k = w2b.next()
                    bb, bbk = b1b.next()
                    ci = 0
                    for j in range(8):
                        for kh in range(4):
                            ws, wsk = w1s.next()
                            dma("sync", ws[:], w1_v[e][:, kh * 2:(kh + 1) * 2, j * 256:(j + 1) * 256], [], [wsk])
                            cp("scalar" if ci % 2 == 0 else "vector", w1t[:, kh * 2:(kh + 1) * 2, :, j * 128:(j + 1) * 128],
                               ws[:].rearrange("p k (m two) -> p k two m", two=2), [wsk], [w1k])
                            ci += 1
                        for nh in range(2):
                            s2, s2k = w2s.next()
                            dma("sync", s2[:], w2_v[e][j][:, nh * 512:(nh + 1) * 512], [], [s2k])
                            cp("scalar" if ci % 2 == 0 else "vector", w2t[:, j, nh * 512:(nh + 1) * 512], s2[:], [s2k], [w2k])
                            ci += 1
                    for bh in range(2):
                        bs, bsk = b1s.next()
                        dma("sync", bs[:], b1r_d.ap()[e:e + 1, bh * D:(bh + 1) * D], [], [bsk])
                        cp("vector", bb[:, bh * D:(bh + 1) * D], bs[:], [bsk], [bbk])
                    return w1t, w1k, w2t, w2k, bb, bbk

                wnext = load_weights(0)
                for e in range(NE):
                    w1t, w1k, w2t, w2k, bb, bbk = wnext
                    if e + 1 < NE:
                        wnext = load_weights(e + 1)
                    S.chain_begin()
                    nxt = gather(e, 0)
                    for j in range(JM):
                        S.level_push(flags_i[0:1, e * JM + j:e * JM + j + 1])
                        cur = nxt
                        if j + 1 < JM:
                            nxt = gather(e, j + 1)
                        block(e, j, w1t, w1k, w2t, w2k, bb, bbk, cur[0], cur[1])
                    S.chain_end()
                S.barrier()

            with ExitStack() as st:
                b2_sb = sb(st, "b2_sb", [NEXP, D])
                dma("sync", b2_sb[:], b2_d.ap(), [], ["b2"])
                yr = mkring(st, "yr", 4, [128, D])
                accr = mkring(st, "accr", 2, [128, D])
                GTr = mkring(st, "GTr", 2, [NEXP, 128])
                x1l = mkring(st, "x1l", 2, [128, D])
                outr = mkring(st, "outr", 2, [128, D])
                junk2 = mkring(st, "junk2", 1, [128, D], BF16)
                smq = mkring(st, "smq", 2, [128, 8])
                for t in range(NT):
                    ys4 = []
                    for k in range(4):
                        y_, yk_ = yr.next()
                        S.dma("gpsimd", lambda en, y_=y_, t=t, k=k: en.indirect_dma_start(
                            out=y_[:, :], out_offset=None, in_=ys_d.ap(),
                            in_offset=bass.IndirectOffsetOnAxis(ap=idx4_all[:, t, k:k + 1], axis=0)), [], [yk_])
                        ys4.append((y_, yk_))
                    ac, ack = accr.next()
                    ts("vector", ac[:, :], ys4[0][0][:, :], gk_all[:, t, 0:1], None, ALU.mult, None, [ys4[0][1]], [ack])
                    for k in range(1, 4):
                        stt(ac[:, :], ys4[k][0][:, :], gk_all[:, t, k:k + 1], ac[:, :], ALU.mult, ALU.add,
                            [ys4[k][1], ack], [ack])
                    gb, gbk = ps.next()
                    mm(gb[0:NEXP, 0:128], G_all[:, t, :], identf, True, True, [], [gbk])
                    gt_, gtk = GTr.next()
                    cp("vector", gt_[:, :], gb[0:NEXP, 0:128], [gbk], [gtk])
                    x1t, x1tk = x1l.next()
                    dma("sync", x1t[:, :], x1_v[t], [], [x1tk])
                    sm, smk = smq.next()
                    jk_t, jk = junk2.next()
                    for n in range(2):
                        yb, ybk = ps.next()
                        mm(yb[:, :], gt_[:, :], b2_sb[:, n * 512:(n + 1) * 512], True, True, [gtk, "b2"], [ybk])
                        a_ap = ac[:, n * 512:(n + 1) * 512]
                        tt("vector", a_ap, a_ap, yb[:, :], ALU.add, [ack, ybk], [ack])
                        act(jk_t[:, n * 512:(n + 1) * 512], a_ap, AF.Square, [ack], [jk, smk], accum=sm[:, n:n + 1])
                    tt("vector", sm[:, 2:3], sm[:, 0:1], sm[:, 1:2], ALU.add, [smk], [smk])
                    o_ = sm[:, 3:4]
                    ts("vector", o_, sm[:, 2:3], 1.0 / D, EPS, ALU.mult, ALU.add, [smk], [smk])
                    act(o_, o_, AF.Sqrt, [smk], [smk])
                    S.op("vector", lambda e, o_=o_: e.reciprocal(o_, o_), [smk], [smk])
                    ot, otk = outr.next()
                    stt(ot[:, :], ac[:, :], o_, mods[:, GP2, :], ALU.mult, ALU.mult, [ack, smk], [otk])
                    tt("gpsimd", ot[:, :], ot[:, :], x1t[:, :], ALU.add, [otk, x1tk], [otk])
                    dma("sync", out_v[t], ot[:, :], [otk], ["outd%d" % t])
                S.barrier()
        except _Stop:
            S.barrier()

        with nc.Block() as block:
            S.emit(block, gscr)
    return nc, S


def host_inputs(inp, NT=32, NP=96, segs=None):
    x = np.asarray(inp["x"], np.float32)
    TOK = NT * 128
    f = lambda k: np.ascontiguousarray(np.asarray(inp[k], np.float32)[0])
    w_ada, b_ada = f("w_ada"), f("b_ada")
    gvec = np.concatenate([f("g_pre_mix"), f("g_post_mix"), f("g_pre_ffn"), f("g_post_ffn")])
    gvec_bc = np.ascontiguousarray(np.broadcast_to(gvec[None, :], (128, 4 * D)))
    bada_bc = np.ascontiguousarray(np.broadcast_to(b_ada[None, :], (128, 6 * D)))
    wup_aug = np.concatenate([f("w_gla_gate_up"), f("b_gla_gate")[None, :]], axis=0)
    b1 = f("b_mlp1")
    b1r = np.ascontiguousarray(b1.reshape(NEXP, D, 2).transpose(0, 2, 1).reshape(NEXP, 2 * D))
    qi = np.arange(128)[:, None]
    kj = np.arange(256)[None, :]
    valid = ((kj < 128) & (kj > qi)) | ((kj >= 128) & (kj - 128 <= qi))
    m1 = np.where(valid, 0.0, NEG).astype(np.float32)
    validF = (kj >= 128) & (kj - 128 <= qi)
    mF = np.where(validF, 0.0, NEG).astype(np.float32)
    j = np.arange(128)[:, None]
    i = np.arange(128)[None, :]
    same = (j // 64) == (i // 64)
    tri = (same & (j <= i)).astype(np.float32)
    uincl = tri * (-1.0 / 16.0)
    urev = (same & (j > i)).astype(np.float32) * (-1.0 / 16.0)
    cbase = np.zeros((128, C_TOT), np.float32)
    cbase[:, C_MASK2:C_MASK2 + 512] = np.tile(m1, (1, 2))
    cbase[:, C_TRI4:C_TRI4 + 512] = np.tile(tri, (1, 4))
    cbase[:, C_UINCL:C_UINCL + 128] = uincl
    cbase[:, C_UREV:C_UREV + 128] = urev
    cbase[:, C_IDENT:C_IDENT + 128] = np.eye(128, dtype=np.float32)
    cbase[:, C_GNORM:C_GNORM + 512] = np.tile(f("g_gla_norm")[None, :], (128, 4))
    cbase[:, C_SINK:C_SINK + 8] = f("sinks")[None, :]
    cbase[:, C_BROUT:C_BROUT + NEXP] = f("b_router")[None, :]
    cbase[:, C_ONES:C_ONES + 128] = 1.0
    cbase[:, C_LSTR:C_LSTR + 128] = (j < i).astype(np.float32)
    cbase[:, C_TH:C_TH + NEXP * 32] = np.tile(128.0 * np.arange(32, dtype=np.float32), NEXP)[None, :]
    cbase[:, C_IOTA] = np.arange(128, dtype=np.float32)
    shared = {"w_ada": w_ada, "b_ada_bc": bada_bc, "gvec_bc": gvec_bc, "w_in": f("w_in"), "wup_aug": wup_aug,
              "w_out": f("w_out"), "w_router": f("w_router"), "w_mlp1": f("w_mlp1"), "w_mlp2": f("w_mlp2"),
              "b1r": b1r, "b_mlp2": f("b_mlp2")}
    maps = []
    nseg = SEQ // SEG
    if segs is None:
        segs = [(core // nseg, (core % nseg) * SEG) for core in range(NCORE)]
    for (b, s0) in segs:
        cm = cbase.copy()
        cm[:, C_MASKF:C_MASKF + 512] = np.tile(mF if s0 == 0 else m1, (1, 2))
        xpre = np.zeros((max(NP, 1) * 128, D), np.float32)
        p0 = s0 - NP * 128
        for p in range(NP):
            a = p0 + p * 128
            if a >= 0:
                xpre[p * 128:(p + 1) * 128] = x[b, a:a + 128]
                cm[:, C_PFLAG + p] = 1.0
        mp = dict(shared)
        mp["x"] = np.ascontiguousarray(x[b, s0:s0 + TOK])
        mp["xpre"] = xpre
        mp["cT"] = np.ascontiguousarray(np.asarray(inp["c"], np.float32)[b].reshape(8, 128).T)
        mp["consts"] = cm
        maps.append(mp)
    return maps


_CACHE = {}


def kernel(**inputs):
    if "nc" not in _CACHE:
        _CACHE["nc"] = build_nc()[0]
    nc = _CACHE["nc"]
    maps = host_inputs(inputs)
    res = run_bass_kernel_spmd(nc, maps, core_ids=list(range(NCORE)))
    out = np.empty((2, SEQ, D), np.float32)
    nseg = SEQ // SEG
    for core in range(NCORE):
        b, seg = core // nseg, core % nseg
        out[b, seg * SEG:(seg + 1) * SEG] = np.asarray(res.results[core]["out"], np.float32)
    return out
```

```python
import os
import numpy as np
from contextlib import ExitStack
import concourse.bass as bass
import concourse.mybir as mybir
from concourse.bass_utils import run_bass_kernel_spmd

F32 = mybir.dt.float32
BF16 = mybir.dt.bfloat16
I32 = mybir.dt.int32
ALU = mybir.AluOpType
AF = mybir.ActivationFunctionType
AX = mybir.AxisListType

D = 1024
SEQ = 16384
NCORE = 8
SEG = 4096
INW = 2320
NEXP = 32
EPS = 1e-6
NEG = -30000.0

ENGS = ("sync", "scalar", "vector", "gpsimd", "tensor")
CENG = ("scalar", "vector", "gpsimd", "tensor")
DENG = ("sync", "gpsimd", "scalar")
NDSEM = 8

C_MASK2 = 0
C_MASKF = 512
C_TRI4 = 1024
C_UINCL = 1536
C_UREV = 1664
C_IDENT = 1792
C_GNORM = 1920
C_SINK = 2432
C_BROUT = 2440
C_PFLAG = 2472
C_ONES = 2568
C_LSTR = 2696
C_TH = 2824
C_IOTA = 3848
C_TOT = 3856


class Sched:
    def __init__(self, nc, stack):
        self.nc = nc
        self.q = {e: [] for e in ENGS}
        self.esem = {e: stack.enter_context(nc.semaphore("es_" + e)) for e in CENG}
        self.ecnt = {e: 0 for e in CENG}
        self.dsem = {e: [stack.enter_context(nc.semaphore("ds_%s%d" % (e, i))) for i in range(NDSEM)]
                     for e in DENG}
        self.dcnt = {e: 0 for e in DENG}
        self.waited = {e: {} for e in ENGS}
        self.lastw = {}
        self.readers = {}
        self.nins = 0
        self.enabled = True
        self.cur_guard = None
        self.gid = 0
        self.chain_flags = {}

    def _deps(self, reads, writes):
        toks = []
        for k in reads:
            if k in self.lastw:
                toks.append(self.lastw[k])
        for k in writes:
            if k in self.lastw:
                toks.append(self.lastw[k])
            toks.extend(self.readers.get(k, ()))
        return toks

    def _need(self, eng, toks):
        out = {}
        for (sid, sem, val, owner) in toks:
            if owner == eng and eng == "tensor":
                continue
            if self.waited[eng].get(sid, 0) >= val:
                continue
            if sid not in out or out[sid][1] < val:
                out[sid] = (sem, val)
        for sid, (sem, val) in out.items():
            self.waited[eng][sid] = val
        return list(out.values())

    def _commit(self, tok, reads, writes):
        for k in writes:
            self.lastw[k] = tok
            self.readers[k] = []
        for k in reads:
            if k not in writes:
                self.readers.setdefault(k, []).append(tok)

    def op(self, eng, fn, reads=(), writes=(), sig=True):
        if not self.enabled:
            return
        px = [k for k in reads if k.startswith("ps")]
        if px:
            reads = [k for k in reads if not k.startswith("ps")]
            writes = list(writes) + px
        waits = self._need(eng, self._deps(reads, writes))
        if sig:
            self.ecnt[eng] += 1
            val = self.ecnt[eng]
        else:
            val = self.ecnt[eng] + 1
        tok = ("e_" + eng, self.esem[eng], val, eng)
        self.q[eng].append((waits, fn, (self.esem[eng], 1) if sig else None, self.cur_guard))
        self._commit(tok, reads, writes)
        self.nins += 1 + len(waits)

    def dma(self, eng, fn, reads=(), writes=()):
        if not self.enabled:
            return
        i = self.dcnt[eng]
        self.dcnt[eng] += 1
        sem = self.dsem[eng][i % NDSEM]
        sid = "d_%s%d" % (eng, i % NDSEM)
        val = 16 * (i // NDSEM + 1)
        toks = self._deps(reads, writes)
        if val > 16:
            toks.append((sid, sem, val - 16, "dma"))
        waits = self._need(eng, toks)
        self.q[eng].append((waits, fn, (sem, 16), self.cur_guard))
        self._commit((sid, sem, val, "dma"), reads, writes)
        self.nins += 1 + len(waits)

    def barrier(self):
        if not self.enabled:
            return
        toks = []
        for e in CENG:
            if self.ecnt[e] > 0:
                toks.append(("e_" + e, self.esem[e], self.ecnt[e], "x"))
        for e in DENG:
            n = self.dcnt[e]
            for j in range(min(n, NDSEM)):
                cnt = (n - 1 - j) // NDSEM + 1
                toks.append(("d_%s%d" % (e, j), self.dsem[e][j], 16 * cnt, "dma"))
        for e in ENGS:
            waits = self._need(e, toks)
            if waits:
                self.q[e].append((waits, None, None, None))
        self.lastw = {}
        self.readers = {}

    def chain_begin(self):
        self.gid += 1
        self.cur_guard = (self.gid, 0)
        self.chain_flags[self.gid] = [None]
        self._wsnap = {e: dict(self.waited[e]) for e in ENGS}

    def level_push(self, flag_ap):
        cid, d = self.cur_guard
        self.chain_flags[cid].append(flag_ap)
        self.cur_guard = (cid, d + 1)

    def chain_end(self):
        self.cur_guard = None
        self.waited = self._wsnap

    def emit(self, block, scratch):
        for e in ENGS:
            def body(engine, e=e):
                tot = {}
                entries = self.q[e]
                state = {"reg": None}

                def emit_entry(ent):
                    waits, fn, inc, _ = ent
                    for sem, val in waits:
                        engine.wait_ge(sem, val)
                    if fn is not None:
                        ins = fn(engine)
                        if inc is not None:
                            ins.then_inc(inc[0], inc[1])
                            tot[id(inc[0])] = tot.get(id(inc[0]), 0) + inc[1]

                def depth_of(ent):
                    return 0 if ent[3] is None else ent[3][1]

                def emit_level(i, end, cid, depth):
                    while i < end:
                        d = depth_of(entries[i])
                        if d == depth:
                            emit_entry(entries[i])
                            i += 1
                            continue
                        flag = self.chain_flags[cid][depth + 1]
                        if state["reg"] is None:
                            state["reg"] = engine.alloc_register("gflag_" + e)
                        reg = state["reg"]
                        adds = {}
                        for ent in entries[i:end]:
                            if ent[2] is not None and ent[1] is not None:
                                k = id(ent[2][0])
                                adds[k] = (ent[2][0], adds.get(k, (None, 0))[1] + ent[2][1])
                        before = dict(tot)
                        engine.reg_load(reg, flag)
                        with engine.If_ne(reg, 0):
                            emit_level(i, end, cid, depth + 1)
                        if adds:
                            with engine.Else():
                                for k, (sem, n) in adds.items():
                                    b = before.get(k, 0)
                                    if b > 0:
                                        engine.wait_ge(sem, b)
                                    if e == "gpsimd" and any(sem is d_ for d_ in self.dsem["gpsimd"]):
                                        engine.dma_start(out=scratch[0:1, 8:9], in_=scratch[0:1, 0:1]).then_inc(sem, n)
                                    else:
                                        engine.sem_inc(sem, n)
                        for k, (sem, n) in adds.items():
                            tot[k] = before.get(k, 0) + n
                        i = end

                i = 0
                while i < len(entries):
                    g = entries[i][3]
                    if g is None:
                        emit_entry(entries[i])
                        i += 1
                        continue
                    cid = g[0]
                    end = i
                    while end < len(entries) and entries[end][3] is not None and entries[end][3][0] == cid:
                        end += 1
                    emit_level(i, end, cid, 0)
                    i = end
            getattr(block, e)(body)


class Ring:
    def __init__(self, name, tiles):
        self.t = tiles
        self.name = name
        self.i = 0

    def next(self):
        k = self.i % len(self.t)
        self.i += 1
        return self.t[k], "%s%d" % (self.name, k)


class _Stop(Exception):
    pass


def build_nc(NT=32, NP=96, NE=32, debug=False, stage=9, sub=99):
    nc = bass.Bass("TRN2", target_bir_lowering=False)
    TOK = NT * 128
    TQ = min(8, NT)
    NQ = NT // TQ
    GT = min(4, TQ)
    NG = TQ // GT

    def din(name, shape, dt=F32):
        return nc.dram_tensor(name, list(shape), dt, kind="ExternalInput")

    x_d = din("x", [TOK, D])
    xp_d = din("xpre", [max(NP, 1) * 128, D])
    cT_d = din("cT", [128, 8])
    wada_d = din("w_ada", [D, 6 * D])
    bada_d = din("b_ada_bc", [128, 6 * D])
    gvec_d = din("gvec_bc", [128, 4 * D])
    consts_d = din("consts", [128, C_TOT])
    win_d = din("w_in", [D, INW])
    wup_d = din("wup_aug", [17, 256])
    wout_d = din("w_out", [D, D])
    wr_d = din("w_router", [D, NEXP])
    w1_d = din("w_mlp1", [NEXP, D, 2 * D])
    w2_d = din("w_mlp2", [NEXP, D, D])
    b1r_d = din("b1r", [NEXP, 2 * D])
    b2_d = din("b_mlp2", [NEXP, D])
    out_d = nc.dram_tensor("out", [TOK, D], F32, kind="ExternalOutput")
    x1_d = nc.dram_tensor("x1_d", [TOK, D], F32, kind="ExternalOutput" if debug else "Internal")
    JM = min(32, NT)
    NSLOT = TOK * 4 + NEXP * 128
    NBLK = NSLOT // 128
    h2_d = nc.dram_tensor("h2_d", [TOK, D], BF16)
    xs_d = nc.dram_tensor("xs_d", [NSLOT, D], BF16)
    ys_d = nc.dram_tensor("ys_d", [NSLOT, D], F32)
    G_d = nc.dram_tensor("G_d", [128, NT * NEXP], F32, kind="ExternalOutput") if debug else None

    stack = ExitStack()
    with stack:
        S = Sched(nc, stack)

        def sb(st, name, shape, dt=F32):
            return st.enter_context(nc.sbuf_tensor("s_" + name, list(shape), dt))

        def mkring(st, name, n, shape, dt=F32):
            return Ring(name, [sb(st, "%s_%d" % (name, i), shape, dt) for i in range(n)])

        def mm(out, lhsT, rhs, start, stop, r, w, sig=True):
            S.op("tensor", lambda e: e.matmul(out, lhsT, rhs, start=start, stop=stop), r, w, sig=sig)

        def tr(out, in_, ident, r, w, sig=True):
            S.op("tensor", lambda e: e.transpose(out, in_, ident), r, w, sig=sig)

        def act(out, in_, func, r, w, bias=None, scale=None, accum=None):
            kw = {}
            if bias is not None:
                kw["bias"] = bias
            if scale is not None:
                kw["scale"] = scale
            if accum is not None:
                kw["accum_out"] = accum
            S.op("scalar", lambda e: e.activation(out, in_, func, **kw), r, w)

        def ts(eng, out, in0, s1, s2, op0, op1, r, w, accum=None):
            if accum is not None:
                S.op(eng, lambda e: e.tensor_scalar(out, in0, s1, s2, op0, op1, accum_out=accum), r, w)
            elif op1 is None:
                S.op(eng, lambda e: e.tensor_scalar(out, in0, s1, None, op0), r, w)
            else:
                S.op(eng, lambda e: e.tensor_scalar(out, in0, s1, s2, op0, op1), r, w)

        def tt(eng, out, in0, in1, op, r, w):
            S.op(eng, lambda e: e.tensor_tensor(out, in0, in1, op), r, w)

        def stt(out, in0, scalar, in1, op0, op1, r, w, accum=None):
            if accum is not None:
                S.op("vector", lambda e: e.scalar_tensor_tensor(out, in0, scalar, in1, op0, op1, accum_out=accum), r, w)
            else:
                S.op("vector", lambda e: e.scalar_tensor_tensor(out, in0, scalar, in1, op0, op1), r, w)

        def cp(eng, out, in_, r, w):
            if eng == "scalar":
                S.op(eng, lambda e: e.copy(out, in_), r, w)
            else:
                S.op(eng, lambda e: e.tensor_copy(out, in_), r, w)

        def dma(eng, out, in_, r, w):
            S.dma(eng, lambda e: e.dma_start(out=out, in_=in_), r, w)

        P = stack
        consts = sb(P, "consts", [128, C_TOT])
        identb = sb(P, "identb", [128, 128], BF16)
        mods = sb(P, "mods", [128, 6, D])
        G_all = sb(P, "G_all", [128, NT, NEXP])
        Qm_all = sb(P, "Qm_all", [128, NT, NEXP])
        Orun = sb(P, "Orun", [128, NEXP])
        onesb = sb(P, "onesb", [128, 128], BF16)
        lstrb = sb(P, "lstrb", [128, 128], BF16)
        GM1, SH1, GP1, GM2, SH2, GP2 = range(6)

        ps = Ring("ps", [stack.enter_context(nc.psum_tensor("ps%d" % i, [128, 512], F32)) for i in range(8)])

        identf = consts[:, C_IDENT:C_IDENT + 128]
        ones_f = consts[:, C_ONES:C_ONES + 128]

        dma("sync", consts[:], consts_d.ap(), [], ["consts"])
        cp("vector", identb[:], identf, ["consts"], ["identb"])
        cp("vector", onesb[:], ones_f, ["consts"], ["onesb"])
        cp("vector", lstrb[:], consts[:, C_LSTR:C_LSTR + 128], ["consts"], ["lstrb"])
        S.op("vector", lambda e: e.memset(Orun[:], 0.0), [], ["Orun"])
        gscr = sb(P, "gscr", [1, 16])
        S.op("gpsimd", lambda e: e.memset(gscr[:], 0.0), [], ["gscr"])
        breg = nc.gpsimd.alloc_register("bndreg")
        S.op("gpsimd", lambda e: e.reg_mov(breg, NSLOT - 1), [], [], sig=False)

        try:
            with ExitStack() as st:
                cT = sb(st, "cT", [128, 8])
                sg = sb(st, "sgc", [128, 8])
                rep = sb(st, "rep", [128, 8, 128])
                gvec = sb(st, "gvec", [128, 4 * D])
                bada = sb(st, "bada", [128, 6 * D])
                modbc = sb(st, "modbc", [128, 6 * D])
                wring = mkring(st, "wada", 2, [128, 8, 512])
                zt = sb(st, "zt", [128, D], BF16)
                S.op("gpsimd", lambda e: e.memset(zt[:], 0.0), [], ["zt"])
                xsz_v = xs_d.ap().rearrange("(b p) n -> b p n", p=128)
                ztf = sb(st, "ztf", [128, D])
                S.op("gpsimd", lambda e: e.memset(ztf[:], 0.0), [], ["ztf"])
                ysz_v = ys_d.ap().rearrange("(b p) n -> b p n", p=128)
                for b_ in range(NBLK):
                    dma("scalar", xsz_v[b_], zt[:, :], ["zt"], ["xsz%d" % b_])
                    dma("scalar", ysz_v[b_], ztf[:, :], ["ztf"], ["ysz%d" % b_])
                dma("sync", cT[:], cT_d.ap(), [], ["cT"])
                dma("sync", gvec[:], gvec_d.ap(), [], ["gvec"])
                dma("sync", bada[:], bada_d.ap(), [], ["bada"])
                act(sg[:], cT[:], AF.Sigmoid, ["cT"], ["sgc"])
                tt("vector", sg[:], sg[:], cT[:], ALU.mult, ["sgc", "cT"], ["sgc"])
                for kc in range(8):
                    ts("vector", rep[:, kc, :], ones_f, sg[:, kc:kc + 1], None, ALU.mult, None,
                       ["consts", "sgc"], ["rep%d" % kc])
                wada_v = wada_d.ap().rearrange("(k p) n -> p k n", p=128)
                for ng in range(12):
                    wt, wk = wring.next()
                    dma("sync", wt[:], wada_v[:, :, ng * 512:(ng + 1) * 512], [], [wk])
                    bank, bk = ps.next()
                    for kc in range(8):
                        mm(bank[:, :], rep[:, kc, :], wt[:, kc, :], kc == 0, kc == 7,
                           [wk, "rep%d" % kc], [bk], sig=(kc == 7))
                    tt("vector", modbc[:, ng * 512:(ng + 1) * 512], bank[:, :], bada[:, ng * 512:(ng + 1) * 512],
                       ALU.add, [bk, "bada"], ["modbc"])
                m = lambda i: modbc[:, i * D:(i + 1) * D]
                g = lambda i: gvec[:, i * D:(i + 1) * D]
                stt(mods[:, GM1, :], m(1), 1.0, g(0), ALU.add, ALU.mult, ["modbc", "gvec"], ["mods"])
                cp("vector", mods[:, SH1, :], m(0), ["modbc"], ["mods"])
                tt("vector", mods[:, GP1, :], m(2), g(1), ALU.mult, ["modbc", "gvec"], ["mods"])
                stt(mods[:, GM2, :], m(4), 1.0, g(2), ALU.add, ALU.mult, ["modbc", "gvec"], ["mods"])
                cp("vector", mods[:, SH2, :], m(3), ["modbc"], ["mods"])
                tt("vector", mods[:, GP2, :], m(5), g(3), ALU.mult, ["modbc", "gvec"], ["mods"])
                S.barrier()
                if stage <= 0:
                    S.enabled = False

            with ExitStack() as st:
                w_in = sb(st, "w_in_sb", [128, 8, INW], BF16)
                w_out = sb(st, "w_out_sb", [128, 8, D], BF16)
                w_r = sb(st, "w_r_sb", [128, 8, NEXP])
                wup = sb(st, "wup_sb", [32, 256])
                kTs = sb(st, "kTs", [64, 3, 2, 128], BF16)
                vs = sb(st, "vs", [128, 3, 128], BF16)
                gaT = sb(st, "gaT", [32, 128])
                Sf = [sb(st, "Sf%d" % i, [64, 4, 128]) for i in range(2)]
                Sb = [sb(st, "Sb%d" % i, [64, 4, 128], BF16) for i in range(2)]
                Qc0 = sb(st, "Qc0", [64, 4, 128], BF16)
                Qc1 = sb(st, "Qc1", [64, 4, 128], BF16)

                with ExitStack() as stw:
                    stg = mkring(stw, "stg", 2, [128, 1160])
                    win_v = win_d.ap().rearrange("(k p) n -> k p n", p=128)
                    wout_v = wout_d.ap().rearrange("(k p) n -> k p n", p=128)
                    ceng = ["vector", "gpsimd"]
                    for kc in range(8):
                        for hf_ in range(2):
                            s_, sk = stg.next()
                            dma("sync", s_[:, :], win_v[kc][:, hf_ * 1160:(hf_ + 1) * 1160], [], [sk])
                            cp(ceng[hf_], w_in[:, kc, hf_ * 1160:(hf_ + 1) * 1160], s_[:, :], [sk], ["w_in"])
                    for kc in range(8):
                        s_, sk = stg.next()
                        dma("sync", s_[:, 0:D], wout_v[kc], [], [sk])
                        cp(ceng[kc % 2], w_out[:, kc, :], s_[:, 0:D], [sk], ["w_out"])
                    dma("sync", w_r[:], wr_d.ap().rearrange("(k p) n -> p k n", p=128), [], ["w_r"])
                    dma("sync", wup[0:17, :], wup_d.ap(), [], ["wup"])
                    S.op("vector", lambda e: e.memset(gaT[:], 1.0), [], ["gaT"])
                    for i in range(2):
                        S.op("gpsimd", lambda e, i=i: e.memset(Sf[i][:], 0.0), [], ["Sf%d" % i])
                        S.op("gpsimd", lambda e, i=i: e.memset(Sb[i][:], 0.0), [], ["Sb%d" % i])
                    S.op("gpsimd", lambda e: e.memset(Qc0[:], 0.0), [], ["Qc0"])
                    S.op("gpsimd", lambda e: e.memset(Qc1[:], 0.0), [], ["Qc1"])
                    S.barrier()

                xr = mkring(st, "xr", 2, [128, D])
                junk = mkring(st, "junk", 1, [128, D], BF16)
                tmpr = mkring(st, "tmpr", 1, [128, D])
                hbr = mkring(st, "hbr", 1, [128, D], BF16)
                hTr = mkring(st, "hTr", 2, [128, 8, 128], BF16)
                smr = mkring(st, "smr", 4, [128, 16])
                qTa = mkring(st, "qTa", 2, [64, 8, 128], BF16)
                gqr = mkring(st, "gqr", 2, [64, 4, 128])
                gkr = mkring(st, "gkr", 2, [64, 4, 128])
                ktokr = mkring(st, "ktokr", 2, [128, 256])
                vtokr = mkring(st, "vtokr", 2, [128, 512], BF16)
                sgg = mkring(st, "sgg", 2, [128, 512])
                enr = mkring(st, "enr", 1, [128, 256])
                ltok = mkring(st, "ltok", 1, [128, 256])
                bTr = mkring(st, "bTr", 1, [64, 4, 128])
                nbm = mkring(st, "nbm", 1, [64, 4, 2])
                decr = mkring(st, "decr", 1, [64, 4, 2])
                E1r = mkring(st, "E1r", 1, [64, 4, 128])
                E2r = mkring(st, "E2r", 1, [64, 4, 128])
                E3r = mkring(st, "E3r", 1, [64, 4, 128])
                QpT = mkring(st, "QpT", 1, [64, 4, 128], BF16)
                KpT = mkring(st, "KpT", 1, [64, 4, 128], BF16)
                E4r = mkring(st, "E4r", 1, [128, 256])
                Kpp = mkring(st, "Kpp", 1, [128, 256], BF16)
                ATr = mkring(st, "ATr", 1, [128, 4, 128], BF16)
                scr = mkring(st, "scr", 1, [128, 8, 256])
                pbr = mkring(st, "pbr", 1, [128, 8, 256], BF16)
                pTr = mkring(st, "pTr", 1, [128, 16, 128], BF16)
                mixr = mkring(st, "mixr", 1, [128, D], BF16)
                mixTr = mkring(st, "mixTr", 1, [128, 8, 128], BF16)
                osq = mkring(st, "osq", 1, [128, 512])
                x1r = mkring(st, "x1r", 1, [128, D])
                h2r = mkring(st, "h2r", 1, [128, D])
                h2Tf = mkring(st, "h2Tf", 1, [128, 8, 128])
                h2br = mkring(st, "h2br", 1, [128, D], BF16)
                mkbr = mkring(st, "mkbr", 1, [128, NEXP], BF16)
                lgr = mkring(st, "lgr", 1, [128, 4, NEXP])

                if stage <= 1:
                    S.enabled = False

                mask2 = consts[:, C_MASK2:C_MASK2 + 512]
                maskF = consts[:, C_MASKF:C_MASKF + 512]
                tri4 = consts[:, C_TRI4:C_TRI4 + 512]
                Uincl = consts[:, C_UINCL:C_UINCL + 128]
                Urev = consts[:, C_UREV:C_UREV + 128]
                gnorm = consts[:, C_GNORM:C_GNORM + 512]
                sinks = consts[:, C_SINK:C_SINK + 8]
                brout = consts[:, C_BROUT:C_BROUT + NEXP]

                x_v = x_d.ap().rearrange("(t p) n -> t p n", p=128)
                xp_v = xp_d.ap().rearrange("(t p) n -> t p n", p=128)
                x1_v = x1_d.ap().rearrange("(t p) n -> t p n", p=128)
                out_v = out_d.ap().rearrange("(t p) n -> t p n", p=128)
                h2_v = h2_d.ap().rearrange("(t p) n -> t p n", p=128)

                state = {"cur": 0}

                def rstd_from_ss(sm, smk, col_in, col_out, n, cnt=1):
                    a = sm[:, col_in:col_in + cnt]
                    o = sm[:, col_out:col_out + cnt]
                    ts("vector", o, a, 1.0 / n, EPS, ALU.mult, ALU.add, [smk], [smk])
                    act(o, o, AF.Ln, [smk], [smk])
                    act(o, o, AF.Exp, [smk], [smk], scale=-0.5)

                def norm_mod(x_t, xk, gi, si, out_ap, outk, sm, smk, c0):
                    jk_t, jk = junk.next()
                    act(jk_t[:, :], x_t[:, :], AF.Square, [xk], [jk, smk], accum=sm[:, c0:c0 + 1])
                    rstd_from_ss(sm, smk, c0, c0 + 1, float(D))
                    tm, tk = tmpr.next()
                    stt(tm[:, :], x_t[:, :], sm[:, c0 + 1:c0 + 2], mods[:, gi, :], ALU.mult, ALU.mult, [xk, smk], [tk])
                    tt("gpsimd", out_ap[:, 0:384], tm[:, 0:384], mods[:, si, 0:384], ALU.add, [tk], [outk])
                    tt("vector", out_ap[:, 384:D], tm[:, 384:D], mods[:, si, 384:D], ALU.add, [tk], [outk])

                def transpose8(src, srck, dst, dstk, evac_eng):
                    for half in range(2):
                        bank, bk = ps.next()
                        for j in range(4):
                            kc = half * 4 + j
                            mm(bank[:, j * 128:(j + 1) * 128], src[:, kc * 128:(kc + 1) * 128], identb[:], True, True,
                               [srck], [bk], sig=(j == 3))
                        cp("scalar" if half == 0 else "vector",
                           dst[:, half * 4:half * 4 + 4, :].rearrange("p k n -> p (k n)"), bank[:, :], [bk], [dstk])

                def gla_gates(hT, hTk, slot_rows):
                    bank, bk = ps.next()
                    for kc in range(8):
                        mm(bank[0:16, 0:128], w_in[:, kc, 2304:2320], hT[:, kc, :], kc == 0, kc == 7,
                           [hTk], [bk], sig=(kc == 7))
                    cp("vector", gaT[0:16, :], bank[0:16, 0:128], [bk], ["gaT"])
                    zb, zk = ps.next()
                    mm(zb[:, 0:256], gaT[0:17, :], wup[0:17, :], True, True, ["gaT"], [zk])
                    en, ek = enr.next()
                    act(en[:, :], zb[:, 0:256], AF.Exp, [zk], [ek], scale=-1.0)
                    l, lk = ltok.next()
                    act(l[:, :], en[:, :], AF.Ln, [ek], [lk], bias=1.0)
                    return l, lk

                def tile_body(t, pre):
                    main = not pre
                    slot = t + 1 if main else 0
                    last_pre = pre and (t == NP - 1)
                    xt, xk = xr.next()
                    dma("sync", xt[:, :], (x_v if main else xp_v)[t], [], [xk])
                    sm, smk = smr.next()
                    hb, hbk = hbr.next()
                    norm_mod(xt, xk, GM1, SH1, hb[:, :], hbk, sm, smk, 0)
                    hT, hTk = hTr.next()
                    if sub <= 0:
                        return
                    transpose8(hb, hbk, hT, hTk, "scalar")
                    if debug and main and t == 0:
                        dh = nc.dram_tensor("dbg_h", [128, D], BF16, kind="ExternalOutput")
                        dma("sync", dh.ap(), hb[:, :], [hbk], ["dbg_h"])
                        dhT = nc.dram_tensor("dbg_hT", [128, D], BF16, kind="ExternalOutput")
                        dma("sync", dhT.ap(), hT[:].rearrange("p k n -> p (k n)"), [hTk], ["dbg_hT"])
                    if sub <= 1:
                        return

                    def fm_group(cols_list, bank, bk):
                        for j, c0 in enumerate(cols_list):
                            for kc in range(8):
                                mm(bank[0:64, j * 128:(j + 1) * 128], w_in[:, kc, c0:c0 + 64], hT[:, kc, :],
                                   kc == 0, kc == 7, [hTk], [bk], sig=(kc == 7 and j == len(cols_list) - 1))

                    def tm_group(c0, n, bank, bk, off=0, last=True):
                        for kc in range(8):
                            mm(bank[:, off:off + n], hT[:, kc, :], w_in[:, kc, c0:c0 + n], kc == 0, kc == 7,
                               [hTk], [bk], sig=(kc == 7 and last))

                    if main:
                        qa, qak = qTa.next()
                        for half in range(2):
                            bank, bk = ps.next()
                            fm_group([h * 64 for h in range(half * 4, half * 4 + 4)], bank, bk)
                            S.op("scalar", lambda e, bank=bank, half=half: e.mul(
                                qa[:, half * 4:half * 4 + 4, :].rearrange("p h n -> p (h n)"), bank[0:64, :], 0.125),
                                [bk], [qak])
                        gq, gqk = gqr.next()
                        bank, bk = ps.next()
                        fm_group([768 + h * 64 for h in range(4)], bank, bk)
                        S.op("scalar", lambda e, bank=bank: e.mul(gq[:].rearrange("p h n -> p (h n)"), bank[0:64, :], 0.125),
                             [bk], [gqk])
                        gk, gkk = gkr.next()
                        bank, bk = ps.next()
                        fm_group([1024 + h * 64 for h in range(4)], bank, bk)
                        cp("vector", gk[:].rearrange("p h n -> p (h n)"), bank[0:64, :], [bk], [gkk])
                    KD = os.environ.get("KDBG", "")
                    if KD == "tm":
                        pass
                    elif main or last_pre:
                        bank, bk = ps.next()
                        fm_group([512, 576], bank, bk)
                        cp("vector", kTs[:, slot % 3, :, :],
                           bank[0:64, 0:256].rearrange("p (h n) -> p h n", h=2), [bk], ["kT%d" % (slot % 3)])
                    if KD == "fm":
                        return
                    bank, bk = ps.next()
                    if (main or last_pre) and KD != "tm1b":
                        tm_group(640, 128, bank, bk, off=0, last=False)
                    tm_group(1024, 256, bank, bk, off=128)
                    if (main or last_pre) and KD != "tm1b":
                        cp("vector" if KD == "tm1c" else "scalar", vs[:, slot % 3, :], bank[:, 0:128], [bk], ["v%d" % (slot % 3)])
                    ktok, ktk = ktokr.next()
                    cp("vector", ktok[:, :], bank[:, 128:384], [bk], [ktk])
                    if KD in ("tm1", "tm1b", "tm1c"):
                        return
                    bank, bk = ps.next()
                    tm_group(1280, 512, bank, bk)
                    vtok, vtk = vtokr.next()
                    if pre:
                        ts("vector", vtok[:, :], bank[:, :], consts[:, C_PFLAG + t:C_PFLAG + t + 1], None, ALU.mult, None,
                           [bk], [vtk])
                    else:
                        cp("scalar", vtok[:, :], bank[:, :], [bk], [vtk])
                    if main:
                        bank, bk = ps.next()
                        tm_group(1792, 512, bank, bk)
                        sg_t, sgk = sgg.next()
                        act(sg_t[:, :], bank[:, :], AF.Silu, [bk], [sgk])
                        tt("gpsimd", sg_t[:, :], sg_t[:, :], gnorm, ALU.mult, [sgk], [sgk])

                    if sub <= 2:
                        return
                    yield
                    l, lk = gla_gates(hT, hTk, None)
                    if sub <= 3:
                        return
                    rvb, rvk = ps.next()
                    mm(rvb[:, 0:256], Urev, l[:, :], True, True, [lk], [rvk])
                    E4, E4k = E4r.next()
                    act(E4[:, :], rvb[:, 0:256], AF.Exp, [rvk], [E4k])
                    kpp, kppk = Kpp.next()
                    tt("vector", kpp[:, :], ktok[:, :], E4[:, :], ALU.mult, [ktk, E4k], [kppk])
                    bTb, bTbk = ps.next()
                    if main:
                        for hd in range(4):
                            mm(bTb[0:64, hd * 128:(hd + 1) * 128], l[:, hd * 64:(hd + 1) * 64], Uincl, True, True,
                               [lk], [bTbk], sig=(hd == 3))
                        bT, bTk = bTr.next()
                        cp("vector", bT[:].rearrange("p h n -> p (h n)"), bTb[0:64, :], [bTbk], [bTk])
                        nb, nbk = nbm.next()
                        ts("vector", nb[:], bT[:, :, 31:128:64], -1.0, None, ALU.mult, None, [bTk], [nbk])
                        E1, E1k = E1r.next()
                        for hd in range(4):
                            for c in range(2):
                                act(E1[:, hd, c * 64:(c + 1) * 64], bT[:, hd, c * 64:(c + 1) * 64], AF.Exp,
                                    [bTk, nbk], [E1k], bias=nb[:, hd, c:c + 1])
                        E2, E2k = E2r.next()
                        S.op("vector", lambda e, E2=E2, E1=E1: e.reciprocal(E2[:], E1[:]), [E1k], [E2k])
                        E3, E3k = E3r.next()
                        act(E3[:], bT[:], AF.Exp, [bTk], [E3k])
                        qp, qpk = QpT.next()
                        tt("vector", qp[:], gq[:], E1[:], ALU.mult, [gqk, E1k], [qpk])
                        kp, kpk = KpT.next()
                        tt("gpsimd", kp[:], gk[:], E2[:], ALU.mult, [gkk, E2k], [kpk])
                        tt("gpsimd", Qc0[:, :, 0:64], gq[:, :, 0:64], E3[:, :, 0:64], ALU.mult, [gqk, E3k], ["Qc0"])
                        tt("gpsimd", Qc1[:, :, 64:128], gq[:, :, 64:128], E3[:, :, 64:128], ALU.mult, [gqk, E3k], ["Qc1"])
                        dec = lambda hd, c: E3[:, hd, c * 64 + 63:c * 64 + 64]
                        deck = E3k
                    else:
                        for hd in range(4):
                            mm(bTb[0:64, hd * 2:hd * 2 + 2], l[:, hd * 64:(hd + 1) * 64],
                               consts[:, C_UINCL + 63:C_UINCL + 128:64], True, True, [lk], [bTbk], sig=(hd == 3))
                        dc, dck = decr.next()
                        act(dc[:].rearrange("p h c -> p (h c)"), bTb[0:64, 0:8], AF.Exp, [bTbk], [dck])
                        dec = lambda hd, c: dc[:, hd, c:c + 1]
                        deck = dck

                    if sub <= 4:
                        return
                    cur = state["cur"]
                    S0f, S0b, S1f, S1b = Sf[cur], Sb[cur], Sf[1 - cur], Sb[1 - cur]
                    k0, k1 = "S%d" % cur, "S%d" % (1 - cur)
                    if main:
                        atb, atk = ps.next()
                        for hd in range(4):
                            mm(atb[:, hd * 128:(hd + 1) * 128], kp[:, hd, :], qp[:, hd, :], True, True,
                               [kpk, qpk], [atk], sig=(hd == 3))
                        AT, ATk = ATr.next()
                        tt("vector", AT[:].rearrange("p h n -> p (h n)"), atb[:, :], tri4, ALU.mult, [atk], [ATk])
                    kvb, kvk = ps.next()
                    for hd in range(4):
                        mm(kvb[0:64, hd * 128:(hd + 1) * 128], kpp[0:64, hd * 64:(hd + 1) * 64],
                           vtok[0:64, hd * 128:(hd + 1) * 128], True, True, [kppk, vtk], [kvk], sig=(hd == 3))
                    for hd in range(4):
                        stt(S1f[:, hd, :], S0f[:, hd, :], dec(hd, 0), kvb[0:64, hd * 128:(hd + 1) * 128],
                            ALU.mult, ALU.add, [k0 + "f", deck, kvk], [k1 + "f"])
                    cp("scalar", S1b[:], S1f[:], [k1 + "f"], [k1 + "b"])
                    kvb2, kvk2 = ps.next()
                    for hd in range(4):
                        mm(kvb2[0:64, hd * 128:(hd + 1) * 128], kpp[64:128, hd * 64:(hd + 1) * 64],
                           vtok[64:128, hd * 128:(hd + 1) * 128], True, True, [kppk, vtk], [kvk2], sig=(hd == 3))
                    if main:
                        ob, obk = ps.next()
                        for hd in range(4):
                            o_ap = ob[:, hd * 128:(hd + 1) * 128]
                            mm(o_ap, AT[:, hd, :], vtok[:, hd * 128:(hd + 1) * 128], True, False, [ATk, vtk], [obk], sig=False)
                            mm(o_ap, Qc0[:, hd, :], S0b[:, hd, :], False, False, ["Qc0", k0 + "b"], [obk], sig=False)
                            mm(o_ap, Qc1[:, hd, :], S1b[:, hd, :], False, True, ["Qc1", k1 + "b"], [obk], sig=(hd == 3))
                    for hd in range(4):
                        stt(S0f[:, hd, :], S1f[:, hd, :], dec(hd, 1), kvb2[0:64, hd * 128:(hd + 1) * 128],
                            ALU.mult, ALU.add, [k1 + "f", deck, kvk2], [k0 + "f"])
                    cp("scalar", S0b[:], S0f[:], [k0 + "f"], [k0 + "b"])
                    if pre:
                        return

                    mix, mixk = mixr.next()
                    sq, sqk = osq.next()
                    sm4, sm4k = smr.next()
                    for hd in range(4):
                        act(sq[:, hd * 128:(hd + 1) * 128], ob[:, hd * 128:(hd + 1) * 128], AF.Square, [obk], [sqk, sm4k],
                            accum=sm4[:, hd:hd + 1])
                    rstd_from_ss(sm4, sm4k, 0, 4, 128.0, cnt=4)
                    for hd in range(4):
                        stt(mix[:, 512 + hd * 128:512 + (hd + 1) * 128], ob[:, hd * 128:(hd + 1) * 128], sm4[:, 4 + hd:5 + hd],
                            sg_t[:, hd * 128:(hd + 1) * 128], ALU.mult, ALU.mult, [obk, sm4k, sgk], [mixk])

                    sc, sck = scr.next()
                    for pair in range(4):
                        bank, bk = ps.next()
                        for j in range(2):
                            h = pair * 2 + j
                            for c in range(2):
                                mm(bank[:, j * 256 + c * 128:j * 256 + (c + 1) * 128], qa[:, h, :],
                                   kTs[:, (t + c) % 3, h // 4, :], True, True, [qak, "kT%d" % ((t + c) % 3)], [bk],
                                   sig=(j == 1 and c == 1))
                        tt("vector", sc[:, pair * 2:pair * 2 + 2, :].rearrange("p h n -> p (h n)"), bank[:, :],
                           maskF if t == 0 else mask2, ALU.add, [bk], [sck])
                    sm2, sm2k = smr.next()
                    S.op("vector", lambda e: e.tensor_reduce(sm2[:, 0:8], sc[:], AX.X, ALU.max), [sck], [sm2k])
                    tt("vector", sm2[:, 0:8], sm2[:, 0:8], sinks, ALU.max, [sm2k], [sm2k])
                    ts("vector", sm2[:, 0:8], sm2[:, 0:8], -1.0, None, ALU.mult, None, [sm2k], [sm2k])
                    sm3, sm3k = smr.next()
                    pb, pbk = pbr.next()
                    for h in range(8):
                        act(pb[:, h, :], sc[:, h, :], AF.Exp, [sck, sm2k], [pbk, sm3k], bias=sm2[:, h:h + 1],
                            accum=sm3[:, h:h + 1])
                    tt("vector", sm2[:, 8:16], sinks, sm2[:, 0:8], ALU.add, [sm2k], [sm2k])
                    act(sm2[:, 8:16], sm2[:, 8:16], AF.Exp, [sm2k], [sm2k])
                    tt("vector", sm3[:, 0:8], sm3[:, 0:8], sm2[:, 8:16], ALU.add, [sm2k, sm3k], [sm3k])
                    S.op("vector", lambda e: e.reciprocal(sm3[:, 8:16], sm3[:, 0:8]), [sm3k], [sm3k])
                    pT, pTk = pTr.next()
                    for q4 in range(4):
                        bank, bk = ps.next()
                        for j in range(2):
                            h = q4 * 2 + j
                            for c in range(2):
                                mm(bank[:, (j * 2 + c) * 128:(j * 2 + c + 1) * 128], pb[:, h, c * 128:(c + 1) * 128],
                                   identb[:], True, True, [pbk], [bk], sig=(j == 1 and c == 1))
                        cp("scalar" if q4 % 2 == 0 else "vector",
                           pT[:, q4 * 4:q4 * 4 + 4, :].rearrange("p a n -> p (a n)"), bank[:, :], [bk], [pTk])
                    ab, abk = ps.next()
                    for h in range(8):
                        kv = h // 4
                        mm(ab[:, h * 64:(h + 1) * 64], pT[:, h * 2, :], vs[:, t % 3, kv * 64:(kv + 1) * 64], True, False,
                           [pTk, "v%d" % (t % 3)], [abk], sig=False)
                        mm(ab[:, h * 64:(h + 1) * 64], pT[:, h * 2 + 1, :], vs[:, (t + 1) % 3, kv * 64:(kv + 1) * 64], False, True,
                           [pTk, "v%d" % ((t + 1) % 3)], [abk], sig=(h == 7))
                    for h in range(8):
                        ts("vector", mix[:, h * 64:(h + 1) * 64], ab[:, h * 64:(h + 1) * 64],
                           sm3[:, 8 + h:9 + h], None, ALU.mult, None, [abk, sm3k], [mixk])

                    mixT, mixTk = mixTr.next()
                    transpose8(mix, mixk, mixT, mixTk, "scalar")
                    ybanks = []
                    for n in range(2):
                        bank, bk = ps.next()
                        for kc in range(8):
                            mm(bank[:, :], mixT[:, kc, :], w_out[:, kc, n * 512:(n + 1) * 512], kc == 0, kc == 7,
                               [mixTk], [bk], sig=(kc == 7))
                        ybanks.append((bank, bk))
                    sm5, sm5k = smr.next()
                    jk_t, jk = junk.next()
                    for n in range(2):
                        act(jk_t[:, n * 512:(n + 1) * 512], ybanks[n][0][:, :], AF.Square, [ybanks[n][1]], [jk, sm5k],
                            accum=sm5[:, n:n + 1])
                    tt("vector", sm5[:, 2:3], sm5[:, 0:1], sm5[:, 1:2], ALU.add, [sm5k], [sm5k])
                    rstd_from_ss(sm5, sm5k, 2, 3, float(D))
                    tm, tk = tmpr.next()
                    for n in range(2):
                        stt(tm[:, n * 512:(n + 1) * 512], ybanks[n][0][:, :], sm5[:, 3:4], mods[:, GP1, n * 512:(n + 1) * 512],
                            ALU.mult, ALU.mult, [ybanks[n][1], sm5k], [tk])
                    x1, x1k = x1r.next()
                    tt("gpsimd", x1[:, :], tm[:, :], xt[:, :], ALU.add, [tk, xk], [x1k])
                    dma("sync", x1_v[t], x1[:, :], [x1k], ["x1d%d" % t])

                    h2, h2k = h2r.next()
                    norm_mod(x1, x1k, GM2, SH2, h2[:, :], h2k, sm5, sm5k, 4)
                    hf, hfk = h2Tf.next()
                    for half in range(2):
                        bank, bk = ps.next()
                        for j in range(4):
                            kc = half * 4 + j
                            mm(bank[:, j * 128:(j + 1) * 128], h2[:, kc * 128:(kc + 1) * 128], identf, True, True,
                               [h2k], [bk], sig=(j == 3))
                        cp("vector" if half == 0 else "scalar", hf[:, half * 4:half * 4 + 4, :].rearrange("p k n -> p (k n)"),
                           bank[:, :], [bk], [hfk])
                    hbf, hbfk = h2br.next()
                    cp("gpsimd", hbf[:, :], h2[:, :], [h2k], [hbfk])
                    dma("sync", h2_v[t], hbf[:, :], [hbfk], ["h2d%d" % t])
                    lb, lbk = ps.next()
                    for kc in range(8):
                        mm(lb[:, 0:NEXP], hf[:, kc, :], w_r[:, kc, :], kc == 0, kc == 7, [hfk], [lbk], sig=(kc == 7))
                    lg, lgk = lgr.next()
                    LG, MK, EX, M8 = 0, 1, 2, 3
                    tt("vector", lg[:, LG, :], lb[:, 0:NEXP], brout, ALU.add, [lbk], [lgk])
                    S.op("vector", lambda e: e.max(lg[:, M8, 0:8], lg[:, LG, :]), [lgk], [lgk])
                    ts("vector", lg[:, MK, :], lg[:, LG, :], lg[:, M8, 3:4], None, ALU.is_ge, None, [lgk], [lgk])
                    ts("vector", lg[:, M8, 8:9], lg[:, M8, 0:1], -1.0, None, ALU.mult, None, [lgk], [lgk])
                    act(lg[:, EX, :], lg[:, LG, :], AF.Exp, [lgk], [lgk], bias=lg[:, M8, 8:9])
                    tt("vector", lg[:, EX, :], lg[:, EX, :], lg[:, MK, :], ALU.mult, [lgk], [lgk])
                    S.op("vector", lambda e: e.tensor_reduce(lg[:, M8, 9:10], lg[:, EX, :], AX.X, ALU.add), [lgk], [lgk])
                    S.op("vector", lambda e: e.reciprocal(lg[:, M8, 10:11], lg[:, M8, 9:10]), [lgk], [lgk])
                    ts("vector", G_all[:, t, :], lg[:, EX, :], lg[:, M8, 10:11], None, ALU.mult, None, [lgk], ["G%d" % t])
                    mkb, mkbk = mkbr.next()
                    cp("vector", mkb[:, :], lg[:, MK, :], [lgk], [mkbk])
                    rb, rbk = ps.next()
                    mm(rb[:, 0:NEXP], lstrb[:], mkb[:, :], True, True, [mkbk], [rbk], sig=False)
                    mm(rb[:, NEXP:2 * NEXP], onesb[:], mkb[:, :], True, True, [mkbk], [rbk])
                    stt(Qm_all[:, t, :], rb[:, 0:NEXP], 1.0, Orun[:, :], ALU.add, ALU.add, [rbk, "Orun"], ["Qm%d" % t])
                    tt("vector", Qm_all[:, t, :], Qm_all[:, t, :], lg[:, MK, :], ALU.mult, ["Qm%d" % t, lgk], ["Qm%d" % t])
                    tt("vector", Orun[:, :], Orun[:, :], rb[:, NEXP:2 * NEXP], ALU.add, ["Orun", rbk], ["Orun"])

                def drain(g):
                    for _ in g:
                        pass

                pend = None
                for (ti, pre_) in [(p, True) for p in range(NP)] + [(t, False) for t in range(NT)]:
                    if (not pre_) and ti == 0 and stage <= 2:
                        break
                    g = tile_body(ti, pre_)
                    try:
                        next(g)
                    except StopIteration:
                        g = None
                    if pend is not None:
                        drain(pend)
                    pend = g
                if pend is not None:
                    drain(pend)
                if stage <= 2:
                    S.enabled = False
                if debug:
                    dma("sync", G_d.ap(), G_all[:].rearrange("p t e -> p (t e)"), ["G%d" % t for t in range(NT)], ["Gd"])
                S.barrier()
                if stage <= 3:
                    S.enabled = False

            h2_v = h2_d.ap().rearrange("(t p) n -> t p n", p=128)
            x1_v = x1_d.ap().rearrange("(t p) n -> t p n", p=128)
            out_v = out_d.ap().rearrange("(t p) n -> t p n", p=128)
            gk_all = sb(P, "gk_all", [128, NT, 4])
            idx4_all = sb(P, "idx4_all", [128, NT, 4], I32)
            flags_i = sb(P, "flags_i", [128, NEXP * JM], I32)
            idxb_i = sb(P, "idxb_i", [128, NEXP * JM], I32)
            with ExitStack() as st:
                flf = sb(st, "flf", [128, NEXP, JM])
                nbt = sb(st, "nbt", [128, NEXP])
                cA = sb(st, "cA", [128, NEXP])
                cB = sb(st, "cB", [128, NEXP])
                pst = sb(st, "pst", [128, NEXP])
                idf = sb(st, "idf", [128, NEXP, JM])
                vr = mkring(st, "vr", 2, [128, 3, NEXP])
                m8r = mkring(st, "m8r", 2, [128, 16])
                h2l = mkring(st, "h2l", 2, [128, D], BF16)
                TH3 = consts[:, C_TH:C_TH + NEXP * 32].rearrange("p (e j) -> p e j", e=NEXP)[:, :, 0:JM]
                S.op("vector", lambda e: e.tensor_tensor(flf[:], Orun[:].unsqueeze(2).to_broadcast([128, NEXP, JM]), TH3,
                                                         ALU.is_gt), ["Orun"], ["flf"])
                S.op("vector", lambda e: e.tensor_reduce(nbt[:], flf[:], AX.X, ALU.add), ["flf"], ["nbt"])
                ts("vector", cA[:], nbt[:], 128.0, None, ALU.mult, None, ["nbt"], ["cA"])
                cp("vector", pst[:], cA[:], ["cA"], ["pst"])
                ca, cb, cak, cbk = cA, cB, "cA", "cB"
                for sft in (1, 2, 4, 8, 16):
                    cp("vector", cb[:, 0:sft], ca[:, 0:sft], [cak], [cbk])
                    tt("vector", cb[:, sft:NEXP], ca[:, sft:NEXP], ca[:, 0:NEXP - sft], ALU.add, [cak], [cbk])
                    ca, cb, cak, cbk = cb, ca, cbk, cak
                tt("vector", pst[:], ca[:], pst[:], ALU.subtract, [cak, "pst"], ["pst"])
                cp("vector", flags_i[:], flf[:].rearrange("p e j -> p (e j)"), ["flf"], ["flags"])
                S.op("vector", lambda e: e.tensor_tensor(idf[:], TH3, pst[:].unsqueeze(2).to_broadcast([128, NEXP, JM]),
                                                         ALU.add), ["pst"], ["idf"])
                ts("vector", idf[:], idf[:], consts[:, C_IOTA:C_IOTA + 1], -65536.0, ALU.add, ALU.add, ["idf"], ["idf"])
                tt("vector", idf[:], idf[:], flf[:], ALU.mult, ["idf", "flf"], ["idf"])
                ts("vector", idf[:], idf[:], 65536.0, None, ALU.add, None, ["idf"], ["idf"])
                cp("vector", idxb_i[:], idf[:].rearrange("p e j -> p (e j)"), ["idf"], ["idxb"])
                for t in range(NT):
                    v, vk = vr.next()
                    ts("vector", v[:, 0, :], Qm_all[:, t, :], 0.0, None, ALU.is_gt, None, [], [vk])
                    tt("vector", v[:, 1, :], Qm_all[:, t, :], pst[:], ALU.add, ["pst"], [vk])
                    tt("vector", v[:, 1, :], v[:, 1, :], v[:, 0, :], ALU.mult, [vk], [vk])
                    m8, m8k = m8r.next()
                    S.op("vector", lambda e, m8=m8, v=v: e.max(m8[:, 0:8], v[:, 1, :]), [vk], [m8k])
                    ts("vector", m8[:, 8:12], m8[:, 0:4], -1.0, None, ALU.add, None, [m8k], [m8k])
                    cp("vector", idx4_all[:, t, :], m8[:, 8:12], [m8k], ["idx4_%d" % t])
                    for k in range(4):
                        ts("vector", v[:, 2, :], v[:, 1, :], m8[:, k:k + 1], None, ALU.is_equal, None, [vk, m8k], [vk])
                        stt(v[:, 0, :], v[:, 2, :], 1.0, G_all[:, t, :], ALU.mult, ALU.mult, [vk], [vk, "gk%d" % t],
                            accum=gk_all[:, t, k:k + 1])
                    h2t, h2tk = h2l.next()
                    dma("sync", h2t[:, :], h2_v[t], [], [h2tk])
                    for k in range(4):
                        S.dma("gpsimd", lambda en, t=t, k=k, h2t=h2t: en.indirect_dma_start(
                            out=xs_d.ap(), out_offset=bass.IndirectOffsetOnAxis(ap=idx4_all[:, t, k:k + 1], axis=0),
                            in_=h2t[:, :], in_offset=None), [h2tk, "idx4_%d" % t], ["xsd"])
                S.barrier()

            with ExitStack() as st:
                w1b = mkring(st, "w1b", 2, [128, 8, 2, D], BF16)
                w2b = mkring(st, "w2b", 2, [128, 8, D], BF16)
                w1s = mkring(st, "w1s", 2, [128, 2, 256])
                w2s = mkring(st, "w2s", 2, [128, 512])
                b1s = mkring(st, "b1s", 1, [1, D])
                b1b = mkring(st, "b1b", 2, [1, 2 * D], BF16)
                xsr = mkring(st, "xsr", 2, [128, D], BF16)
                xsTr = mkring(st, "xsTr", 2, [128, 8, 128], BF16)
                aTr = mkring(st, "aTr", 2, [128, 8, 128], BF16)
                atokr = mkring(st, "atokr", 1, [128, D], BF16)
                xgr = mkring(st, "xgr", 2, [128, 512])
                sgr = mkring(st, "sgr", 2, [128, 512])
                xlr = mkring(st, "xlr", 2, [128, 512])
                ysr = mkring(st, "ysr", 2, [128, D])
                for i_ in range(2):
                    S.op("gpsimd", lambda e, i_=i_: e.memset(ysr.t[i_][:], 0.0), [], ["ysr%d" % i_])
                w1_v = w1_d.ap().rearrange("e (k p) n -> e p k n", p=128)
                w2_v = w2_d.ap().rearrange("e (k p) n -> e k p n", p=128)

                def gather(e, j):
                    col = e * JM + j
                    xs, xsk = xsr.next()
                    S.dma("gpsimd", lambda en, xs=xs, col=col: en.indirect_dma_start(
                        out=xs[:, :], out_offset=None, in_=xs_d.ap(),
                        in_offset=bass.IndirectOffsetOnAxis(ap=idxb_i[:, col:col + 1], axis=0),
                        bounds_check=breg, oob_is_err=False), [], [xsk])
                    return xs, xsk

                def block(e, j, w1t, w1k, w2t, w2k, bb, bbk, xs, xsk):
                    col = e * JM + j
                    xT, xTk = xsTr.next()
                    transpose8(xs, xsk, xT, xTk, None)
                    aT, aTk = aTr.next()
                    atok, atokk = atokr.next()
                    for fh in range(2):
                        banks = []
                        for two in range(2):
                            bank, bk = ps.next()
                            for kc in range(8):
                                mm(bank[:, :], xT[:, kc, :], w1t[:, kc, two, fh * 512:(fh + 1) * 512], kc == 0, False,
                                   [w1k, xTk], [bk], sig=False)
                            mm(bank[:, :], onesb[0:1, :], bb[0:1, two * D + fh * 512:two * D + (fh + 1) * 512], False, True,
                               [bbk], [bk])
                            banks.append((bank, bk))
                        (bg, bgk), (bl, blk) = banks
                        xg, xgk = xgr.next()
                        ts("vector", xg[:, :], bg[:, :], 7.0, None, ALU.min, None, [bgk], [xgk])
                        sgt, sgk2 = sgr.next()
                        act(sgt[:, :], xg[:, :], AF.Sigmoid, [xgk], [sgk2], scale=1.702)
                        xl, xlk = xlr.next()
                        ts("vector", xl[:, :], bl[:, :], 7.0, -7.0, ALU.min, ALU.max, [blk], [xlk])
                        tt("gpsimd", xg[:, :], xg[:, :], sgt[:, :], ALU.mult, [xgk, sgk2], [xgk])
                        stt(atok[:, fh * 512:(fh + 1) * 512], xl[:, :], 1.0, xg[:, :], ALU.add, ALU.mult, [xlk, xgk], [atokk])
                    transpose8(atok, atokk, aT, aTk, None)
                    ysb, ysk = ysr.next()
                    ybs = [ps.next(), ps.next()]
                    for n in range(2):
                        yb, ybk = ybs[n]
                        for fc in range(8):
                            mm(yb[:, :], aT[:, fc, :], w2t[:, fc, n * 512:(n + 1) * 512], fc == 0, fc == 7,
                               [aTk, w2k], [ybk], sig=(fc == 7))
                    for n in range(2):
                        yb, ybk = ybs[n]
                        cp("scalar" if n == 0 else "vector", ysb[:, n * 512:(n + 1) * 512], yb[:, :], [ybk], [ysk])
                    S.dma("gpsimd", lambda en, ysb=ysb, col=col: en.indirect_dma_start(
                        out=ys_d.ap(), out_offset=bass.IndirectOffsetOnAxis(ap=idxb_i[:, col:col + 1], axis=0),
                        in_=ysb[:, :], in_offset=None, bounds_check=breg, oob_is_err=False), [ysk], ["ysd"])

                def load_weights(e):
                    w1t, w1k = w1b.next()
                    w2t, w2k = w2b.next()
                    bb, bbk = b1b.next()
                    ci = 0
                    for j in range(8):
                        for kh in range(4):
                            ws, wsk = w1s.next()
                            dma("sync", ws[:], w1_v[e][:, kh * 2:(kh + 1) * 2, j * 256:(j + 1) * 256], [], [wsk])
                            cp("scalar" if ci % 2 == 0 else "vector", w1t[:, kh * 2:(kh + 1) * 2, :, j * 128:(j + 1) * 128],
                               ws[:].rearrange("p k (m two) -> p k two m", two=2), [wsk], [w1k])
                            ci += 1
                        for nh in range(2):
                            s2, s2k = w2s.next()
                            dma("sync", s2[:], w2_v[e][j][:, nh * 512:(nh + 1) * 512], [], [s2k])
                            cp("scalar" if ci % 2 == 0 else "vector", w2t[:, j, nh * 512:(nh + 1) * 512], s2[:], [s2k], [w2k])
                            ci += 1
                    for bh in range(2):
                        bs, bsk = b1s.next()
                        dma("sync", bs[:], b1r_d.ap()[e:e + 1, bh * D:(bh + 1) * D], [], [bsk])
                        cp("vector", bb[:, bh * D:(bh + 1) * D], bs[:], [bsk], [bbk])
                    return w1t, w1k, w2t, w2k, bb, bbk

                wnext = load_weights(0)
                for e in range(NE):
                    w1t, w1k, w2t, w2k, bb, bbk = wnext
                    if e + 1 < NE:
                        wnext = load_weights(e + 1)
                    S.chain_begin()
                    nxt = gather(e, 0)
                    for j in range(JM):
                        S.level_push(flags_i[0:1, e * JM + j:e * JM + j + 1])
                        cur = nxt
                        if j + 1 < JM:
                            nxt = gather(e, j + 1)
                        block(e, j, w1t, w1k, w2t, w2k, bb, bbk, cur[0], cur[1])
                    S.chain_end()
                S.barrier()

            with ExitStack() as st:
                b2_sb = sb(st, "b2_sb", [NEXP, D])
                dma("sync", b2_sb[:], b2_d.ap(), [], ["b2"])
                yr = mkring(st, "yr", 4, [128, D])
                accr = mkring(st, "accr", 2, [128, D])
                GTr = mkring(st, "GTr", 2, [NEXP, 128])
                x1l = mkring(st, "x1l", 2, [128, D])
                outr = mkring(st, "outr", 2, [128, D])
                junk2 = mkring(st, "junk2", 1, [128, D], BF16)
                smq = mkring(st, "smq", 2, [128, 8])
                for t in range(NT):
                    ys4 = []
                    for k in range(4):
                        y_, yk_ = yr.next()
                        S.dma("gpsimd", lambda en, y_=y_, t=t, k=k: en.indirect_dma_start(
                            out=y_[:, :], out_offset=None, in_=ys_d.ap(),
                            in_offset=bass.IndirectOffsetOnAxis(ap=idx4_all[:, t, k:k + 1], axis=0)), [], [yk_])
                        ys4.append((y_, yk_))
                    ac, ack = accr.next()
                    ts("vector", ac[:, :], ys4[0][0][:, :], gk_all[:, t, 0:1], None, ALU.mult, None, [ys4[0][1]], [ack])
                    for k in range(1, 4):
                        stt(ac[:, :], ys4[k][0][:, :], gk_all[:, t, k:k + 1], ac[:, :], ALU.mult, ALU.add,
                            [ys4[k][1], ack], [ack])
                    gb, gbk = ps.next()
                    mm(gb[0:NEXP, 0:128], G_all[:, t, :], identf, True, True, [], [gbk])
                    gt_, gtk = GTr.next()
                    cp("vector", gt_[:, :], gb[0:NEXP, 0:128], [gbk], [gtk])
                    x1t, x1tk = x1l.next()
                    dma("sync", x1t[:, :], x1_v[t], [], [x1tk])
                    sm, smk = smq.next()
                    jk_t, jk = junk2.next()
                    for n in range(2):
                        yb, ybk = ps.next()
                        mm(yb[:, :], gt_[:, :], b2_sb[:, n * 512:(n + 1) * 512], True, True, [gtk, "b2"], [ybk])
                        a_ap = ac[:, n * 512:(n + 1) * 512]
                        tt("vector", a_ap, a_ap, yb[:, :], ALU.add, [ack, ybk], [ack])
                        act(jk_t[:, n * 512:(n + 1) * 512], a_ap, AF.Square, [ack], [jk, smk], accum=sm[:, n:n + 1])
                    tt("vector", sm[:, 2:3], sm[:, 0:1], sm[:, 1:2], ALU.add, [smk], [smk])
                    o_ = sm[:, 3:4]
                    ts("vector", o_, sm[:, 2:3], 1.0 / D, EPS, ALU.mult, ALU.add, [smk], [smk])
                    act(o_, o_, AF.Sqrt, [smk], [smk])
                    S.op("vector", lambda e, o_=o_: e.reciprocal(o_, o_), [smk], [smk])
                    ot, otk = outr.next()
                    stt(ot[:, :], ac[:, :], o_, mods[:, GP2, :], ALU.mult, ALU.mult, [ack, smk], [otk])
                    tt("gpsimd", ot[:, :], ot[:, :], x1t[:, :], ALU.add, [otk, x1tk], [otk])
                    dma("sync", out_v[t], ot[:, :], [otk], ["outd%d" % t])
                S.barrier()
        except _Stop:
            S.barrier()

        with nc.Block() as block:
            S.emit(block, gscr)
    return nc, S


def host_inputs(inp, NT=32, NP=96, segs=None):
    x = np.asarray(inp["x"], np.float32)
    TOK = NT * 128
    f = lambda k: np.ascontiguousarray(np.asarray(inp[k], np.float32)[0])
    w_ada, b_ada = f("w_ada"), f("b_ada")
    gvec = np.concatenate([f("g_pre_mix"), f("g_post_mix"), f("g_pre_ffn"), f("g_post_ffn")])
    gvec_bc = np.ascontiguousarray(np.broadcast_to(gvec[None, :], (128, 4 * D)))
    bada_bc = np.ascontiguousarray(np.broadcast_to(b_ada[None, :], (128, 6 * D)))
    wup_aug = np.concatenate([f("w_gla_gate_up"), f("b_gla_gate")[None, :]], axis=0)
    b1 = f("b_mlp1")
    b1r = np.ascontiguousarray(b1.reshape(NEXP, D, 2).transpose(0, 2, 1).reshape(NEXP, 2 * D))
    qi = np.arange(128)[:, None]
    kj = np.arange(256)[None, :]
    valid = ((kj < 128) & (kj > qi)) | ((kj >= 128) & (kj - 128 <= qi))
    m1 = np.where(valid, 0.0, NEG).astype(np.float32)
    validF = (kj >= 128) & (kj - 128 <= qi)
    mF = np.where(validF, 0.0, NEG).astype(np.float32)
    j = np.arange(128)[:, None]
    i = np.arange(128)[None, :]
    same = (j // 64) == (i // 64)
    tri = (same & (j <= i)).astype(np.float32)
    uincl = tri * (-1.0 / 16.0)
    urev = (same & (j > i)).astype(np.float32) * (-1.0 / 16.0)
    cbase = np.zeros((128, C_TOT), np.float32)
    cbase[:, C_MASK2:C_MASK2 + 512] = np.tile(m1, (1, 2))
    cbase[:, C_TRI4:C_TRI4 + 512] = np.tile(tri, (1, 4))
    cbase[:, C_UINCL:C_UINCL + 128] = uincl
    cbase[:, C_UREV:C_UREV + 128] = urev
    cbase[:, C_IDENT:C_IDENT + 128] = np.eye(128, dtype=np.float32)
    cbase[:, C_GNORM:C_GNORM + 512] = np.tile(f("g_gla_norm")[None, :], (128, 4))
    cbase[:, C_SINK:C_SINK + 8] = f("sinks")[None, :]
    cbase[:, C_BROUT:C_BROUT + NEXP] = f("b_router")[None, :]
    cbase[:, C_ONES:C_ONES + 128] = 1.0
    cbase[:, C_LSTR:C_LSTR + 128] = (j < i).astype(np.float32)
    cbase[:, C_TH:C_TH + NEXP * 32] = np.tile(128.0 * np.arange(32, dtype=np.float32), NEXP)[None, :]
    cbase[:, C_IOTA] = np.arange(128, dtype=np.float32)
    shared = {"w_ada": w_ada, "b_ada_bc": bada_bc, "gvec_bc": gvec_bc, "w_in": f("w_in"), "wup_aug": wup_aug,
              "w_out": f("w_out"), "w_router": f("w_router"), "w_mlp1": f("w_mlp1"), "w_mlp2": f("w_mlp2"),
              "b1r": b1r, "b_mlp2": f("b_mlp2")}
    maps = []
    nseg = SEQ // SEG
    if segs is None:
        segs = [(core // nseg, (core % nseg) * SEG) for core in range(NCORE)]
    for (b, s0) in segs:
        cm = cbase.copy()
        cm[:, C_MASKF:C_MASKF + 512] = np.tile(mF if s0 == 0 else m1, (1, 2))
        xpre = np.zeros((max(NP, 1) * 128, D), np.float32)
        p0 = s0 - NP * 128
        for p in range(NP):
            a = p0 + p * 128
            if a >= 0:
                xpre[p * 128:(p + 1) * 128] = x[b, a:a + 128]
                cm[:, C_PFLAG + p] = 1.0
        mp = dict(shared)
        mp["x"] = np.ascontiguousarray(x[b, s0:s0 + TOK])
        mp["xpre"] = xpre
        mp["cT"] = np.ascontiguousarray(np.asarray(inp["c"], np.float32)[b].reshape(8, 128).T)
        mp["consts"] = cm
        maps.append(mp)
    return maps


_CACHE = {}


def kernel(**inputs):
    if "nc" not in _CACHE:
        _CACHE["nc"] = build_nc()[0]
    nc = _CACHE["nc"]
    maps = host_inputs(inputs)
    res = run_bass_kernel_spmd(nc, maps, core_ids=list(range(NCORE)))
    out = np.empty((2, SEQ, D), np.float32)
    nseg = SEQ // SEG
    for core in range(NCORE):
        b, seg = core // nseg, core % nseg
        out[b, seg * SEG:(seg + 1) * SEG] = np.asarray(res.results[core]["out"], np.float32)
    return out
```

```python
import os
import numpy as np
from contextlib import ExitStack
import concourse.bass as bass
import concourse.mybir as mybir
from concourse.bass_utils import run_bass_kernel_spmd

F32 = mybir.dt.float32
BF16 = mybir.dt.bfloat16
I32 = mybir.dt.int32
ALU = mybir.AluOpType
AF = mybir.ActivationFunctionType
AX = mybir.AxisListType

D = 1024
SEQ = 16384
NCORE = 8
SEG = 4096
INW = 2320
NEXP = 32
EPS = 1e-6
NEG = -30000.0

ENGS = ("sync", "scalar", "vector", "gpsimd", "tensor")
CENG = ("scalar", "vector", "gpsimd", "tensor")
DENG = ("sync", "gpsimd", "scalar")
NDSEM = 8

C_MASK2 = 0
C_MASKF = 512
C_TRI4 = 1024
C_UINCL = 1536
C_UREV = 1664
C_IDENT = 1792
C_GNORM = 1920
C_SINK = 2432
C_BROUT = 2440
C_PFLAG = 2472
C_ONES = 2568
C_LSTR = 2696
C_TH = 2824
C_IOTA = 3848
C_TOT = 3856


class Sched:
    def __init__(self, nc, stack):
        self.nc = nc
        self.q = {e: [] for e in ENGS}
        self.esem = {e: stack.enter_context(nc.semaphore("es_" + e)) for e in CENG}
        self.ecnt = {e: 0 for e in CENG}
        self.dsem = {e: [stack.enter_context(nc.semaphore("ds_%s%d" % (e, i))) for i in range(NDSEM)]
                     for e in DENG}
        self.dcnt = {e: 0 for e in DENG}
        self.waited = {e: {} for e in ENGS}
        self.lastw = {}
        self.readers = {}
        self.nins = 0
        self.enabled = True
        self.cur_guard = None
        self.gid = 0
        self.chain_flags = {}

    def _deps(self, reads, writes):
        toks = []
        for k in reads:
            if k in self.lastw:
                toks.append(self.lastw[k])
        for k in writes:
            if k in self.lastw:
                toks.append(self.lastw[k])
            toks.extend(self.readers.get(k, ()))
        return toks

    def _need(self, eng, toks):
        out = {}
        for (sid, sem, val, owner) in toks:
            if owner == eng and eng == "tensor":
                continue
            if self.waited[eng].get(sid, 0) >= val:
                continue
            if sid not in out or out[sid][1] < val:
                out[sid] = (sem, val)
        for sid, (sem, val) in out.items():
            self.waited[eng][sid] = val
        return list(out.values())

    def _commit(self, tok, reads, writes):
        for k in writes:
            self.lastw[k] = tok
            self.readers[k] = []
        for k in reads:
            if k not in writes:
                self.readers.setdefault(k, []).append(tok)

    def op(self, eng, fn, reads=(), writes=(), sig=True):
        if not self.enabled:
            return
        px = [k for k in reads if k.startswith("ps")]
        if px:
            reads = [k for k in reads if not k.startswith("ps")]
            writes = list(writes) + px
        waits = self._need(eng, self._deps(reads, writes))
        if sig:
            self.ecnt[eng] += 1
            val = self.ecnt[eng]
        else:
            val = self.ecnt[eng] + 1
        tok = ("e_" + eng, self.esem[eng], val, eng)
        self.q[eng].append((waits, fn, (self.esem[eng], 1) if sig else None, self.cur_guard))
        self._commit(tok, reads, writes)
        self.nins += 1 + len(waits)

    def dma(self, eng, fn, reads=(), writes=()):
        if not self.enabled:
            return
        i = self.dcnt[eng]
        self.dcnt[eng] += 1
        sem = self.dsem[eng][i % NDSEM]
        sid = "d_%s%d" % (eng, i % NDSEM)
        val = 16 * (i // NDSEM + 1)
        toks = self._deps(reads, writes)
        if val > 16:
            toks.append((sid, sem, val - 16, "dma"))
        waits = self._need(eng, toks)
        self.q[eng].append((waits, fn, (sem, 16), self.cur_guard))
        self._commit((sid, sem, val, "dma"), reads, writes)
        self.nins += 1 + len(waits)

    def barrier(self):
        if not self.enabled:
            return
        toks = []
        for e in CENG:
            if self.ecnt[e] > 0:
                toks.append(("e_" + e, self.esem[e], self.ecnt[e], "x"))
        for e in DENG:
            n = self.dcnt[e]
            for j in range(min(n, NDSEM)):
                cnt = (n - 1 - j) // NDSEM + 1
                toks.append(("d_%s%d" % (e, j), self.dsem[e][j], 16 * cnt, "dma"))
        for e in ENGS:
            waits = self._need(e, toks)
            if waits:
                self.q[e].append((waits, None, None, None))
        self.lastw = {}
        self.readers = {}

    def chain_begin(self):
        self.gid += 1
        self.cur_guard = (self.gid, 0)
        self.chain_flags[self.gid] = [None]
        self._wsnap = {e: dict(self.waited[e]) for e in ENGS}

    def level_push(self, flag_ap):
        cid, d = self.cur_guard
        self.chain_flags[cid].append(flag_ap)
        self.cur_guard = (cid, d + 1)

    def chain_end(self):
        self.cur_guard = None
        self.waited = self._wsnap

    def emit(self, block, scratch):
        for e in ENGS:
            def body(engine, e=e):
                tot = {}
                entries = self.q[e]
                state = {"reg": None}

                def emit_entry(ent):
                    waits, fn, inc, _ = ent
                    for sem, val in waits:
                        engine.wait_ge(sem, val)
                    if fn is not None:
                        ins = fn(engine)
                        if inc is not None:
                            ins.then_inc(inc[0], inc[1])
                            tot[id(inc[0])] = tot.get(id(inc[0]), 0) + inc[1]

                def depth_of(ent):
                    return 0 if ent[3] is None else ent[3][1]

                def emit_level(i, end, cid, depth):
                    while i < end:
                        d = depth_of(entries[i])
                        if d == depth:
                            emit_entry(entries[i])
                            i += 1
                            continue
                        flag = self.chain_flags[cid][depth + 1]
                        if state["reg"] is None:
                            state["reg"] = engine.alloc_register("gflag_" + e)
                        reg = state["reg"]
                        adds = {}
                        for ent in entries[i:end]:
                            if ent[2] is not None and ent[1] is not None:
                                k = id(ent[2][0])
                                adds[k] = (ent[2][0], adds.get(k, (None, 0))[1] + ent[2][1])
                        before = dict(tot)
                        engine.reg_load(reg, flag)
                        with engine.If_ne(reg, 0):
                            emit_level(i, end, cid, depth + 1)
                        if adds:
                            with engine.Else():
                                for k, (sem, n) in adds.items():
                                    b = before.get(k, 0)
                                    if b > 0:
                                        engine.wait_ge(sem, b)
                                    if e == "gpsimd" and any(sem is d_ for d_ in self.dsem["gpsimd"]):
                                        engine.dma_start(out=scratch[0:1, 8:9], in_=scratch[0:1, 0:1]).then_inc(sem, n)
                                    else:
                                        engine.sem_inc(sem, n)
                        for k, (sem, n) in adds.items():
                            tot[k] = before.get(k, 0) + n
                        i = end

                i = 0
                while i < len(entries):
                    g = entries[i][3]
                    if g is None:
                        emit_entry(entries[i])
                        i += 1
                        continue
                    cid = g[0]
                    end = i
                    while end < len(entries) and entries[end][3] is not None and entries[end][3][0] == cid:
                        end += 1
                    emit_level(i, end, cid, 0)
                    i = end
            getattr(block, e)(body)


class Ring:
    def __init__(self, name, tiles):
        self.t = tiles
        self.name = name
        self.i = 0

    def next(self):
        k = self.i % len(self.t)
        self.i += 1
        return self.t[k], "%s%d" % (self.name, k)


class _Stop(Exception):
    pass


def build_nc(NT=32, NP=96, NE=32, debug=False, stage=9, sub=99):
    nc = bass.Bass("TRN2", target_bir_lowering=False)
    TOK = NT * 128
    TQ = min(8, NT)
    NQ = NT // TQ
    GT = min(4, TQ)
    NG = TQ // GT

    def din(name, shape, dt=F32):
        return nc.dram_tensor(name, list(shape), dt, kind="ExternalInput")

    x_d = din("x", [TOK, D])
    xp_d = din("xpre", [max(NP, 1) * 128, D])
    cT_d = din("cT", [128, 8])
    wada_d = din("w_ada", [D, 6 * D])
    bada_d = din("b_ada_bc", [128, 6 * D])
    gvec_d = din("gvec_bc", [128, 4 * D])
    consts_d = din("consts", [128, C_TOT])
    win_d = din("w_in", [D, INW])
    wup_d = din("wup_aug", [17, 256])
    wout_d = din("w_out", [D, D])
    wr_d = din("w_router", [D, NEXP])
    w1_d = din("w_mlp1", [NEXP, D, 2 * D])
    w2_d = din("w_mlp2", [NEXP, D, D])
    b1r_d = din("b1r", [NEXP, 2 * D])
    b2_d = din("b_mlp2", [NEXP, D])
    out_d = nc.dram_tensor("out", [TOK, D], F32, kind="ExternalOutput")
    x1_d = nc.dram_tensor("x1_d", [TOK, D], F32, kind="ExternalOutput" if debug else "Internal")
    JM = min(32, NT)
    NSLOT = TOK * 4 + NEXP * 128
    NBLK = NSLOT // 128
    h2_d = nc.dram_tensor("h2_d", [TOK, D], BF16)
    xs_d = nc.dram_tensor("xs_d", [NSLOT, D], BF16)
    ys_d = nc.dram_tensor("ys_d", [NSLOT, D], F32)
    G_d = nc.dram_tensor("G_d", [128, NT * NEXP], F32, kind="ExternalOutput") if debug else None

    stack = ExitStack()
    with stack:
        S = Sched(nc, stack)

        def sb(st, name, shape, dt=F32):
            return st.enter_context(nc.sbuf_tensor("s_" + name, list(shape), dt))

        def mkring(st, name, n, shape, dt=F32):
            return Ring(name, [sb(st, "%s_%d" % (name, i), shape, dt) for i in range(n)])

        def mm(out, lhsT, rhs, start, stop, r, w, sig=True):
            S.op("tensor", lambda e: e.matmul(out, lhsT, rhs, start=start, stop=stop), r, w, sig=sig)

        def tr(out, in_, ident, r, w, sig=True):
            S.op("tensor", lambda e: e.transpose(out, in_, ident), r, w, sig=sig)

        def act(out, in_, func, r, w, bias=None, scale=None, accum=None):
            kw = {}
            if bias is not None:
                kw["bias"] = bias
            if scale is not None:
                kw["scale"] = scale
            if accum is not None:
                kw["accum_out"] = accum
            S.op("scalar", lambda e: e.activation(out, in_, func, **kw), r, w)

        def ts(eng, out, in0, s1, s2, op0, op1, r, w, accum=None):
            if accum is not None:
                S.op(eng, lambda e: e.tensor_scalar(out, in0, s1, s2, op0, op1, accum_out=accum), r, w)
            elif op1 is None:
                S.op(eng, lambda e: e.tensor_scalar(out, in0, s1, None, op0), r, w)
            else:
                S.op(eng, lambda e: e.tensor_scalar(out, in0, s1, s2, op0, op1), r, w)

        def tt(eng, out, in0, in1, op, r, w):
            S.op(eng, lambda e: e.tensor_tensor(out, in0, in1, op), r, w)

        def stt(out, in0, scalar, in1, op0, op1, r, w, accum=None):
            if accum is not None:
                S.op("vector", lambda e: e.scalar_tensor_tensor(out, in0, scalar, in1, op0, op1, accum_out=accum), r, w)
            else:
                S.op("vector", lambda e: e.scalar_tensor_tensor(out, in0, scalar, in1, op0, op1), r, w)

        def cp(eng, out, in_, r, w):
            if eng == "scalar":
                S.op(eng, lambda e: e.copy(out, in_), r, w)
            else:
                S.op(eng, lambda e: e.tensor_copy(out, in_), r, w)

        def dma(eng, out, in_, r, w):
            S.dma(eng, lambda e: e.dma_start(out=out, in_=in_), r, w)

        P = stack
        consts = sb(P, "consts", [128, C_TOT])
        identb = sb(P, "identb", [128, 128], BF16)
        mods = sb(P, "mods", [128, 6, D])
        G_all = sb(P, "G_all", [128, NT, NEXP])
        Qm_all = sb(P, "Qm_all", [128, NT, NEXP])
        Orun = sb(P, "Orun", [128, NEXP])
        onesb = sb(P, "onesb", [128, 128], BF16)
        lstrb = sb(P, "lstrb", [128, 128], BF16)
        GM1, SH1, GP1, GM2, SH2, GP2 = range(6)

        ps = Ring("ps", [stack.enter_context(nc.psum_tensor("ps%d" % i, [128, 512], F32)) for i in range(8)])

        identf = consts[:, C_IDENT:C_IDENT + 128]
        ones_f = consts[:, C_ONES:C_ONES + 128]

        dma("sync", consts[:], consts_d.ap(), [], ["consts"])
        cp("vector", identb[:], identf, ["consts"], ["identb"])
        cp("vector", onesb[:], ones_f, ["consts"], ["onesb"])
        cp("vector", lstrb[:], consts[:, C_LSTR:C_LSTR + 128], ["consts"], ["lstrb"])
        S.op("vector", lambda e: e.memset(Orun[:], 0.0), [], ["Orun"])
        gscr = sb(P, "gscr", [1, 16])
        S.op("gpsimd", lambda e: e.memset(gscr[:], 0.0), [], ["gscr"])
        breg = nc.gpsimd.alloc_register("bndreg")
        S.op("gpsimd", lambda e: e.reg_mov(breg, NSLOT - 1), [], [], sig=False)

        try:
            with ExitStack() as st:
                cT = sb(st, "cT", [128, 8])
                sg = sb(st, "sgc", [128, 8])
                rep = sb(st, "rep", [128, 8, 128])
                gvec = sb(st, "gvec", [128, 4 * D])
                bada = sb(st, "bada", [128, 6 * D])
                modbc = sb(st, "modbc", [128, 6 * D])
                wring = mkring(st, "wada", 2, [128, 8, 512])
                zt = sb(st, "zt", [128, D], BF16)
                S.op("gpsimd", lambda e: e.memset(zt[:], 0.0), [], ["zt"])
                xsz_v = xs_d.ap().rearrange("(b p) n -> b p n", p=128)
                ztf = sb(st, "ztf", [128, D])
                S.op("gpsimd", lambda e: e.memset(ztf[:], 0.0), [], ["ztf"])
                ysz_v = ys_d.ap().rearrange("(b p) n -> b p n", p=128)
                for b_ in range(NBLK):
                    dma("scalar", xsz_v[b_], zt[:, :], ["zt"], ["xsz%d" % b_])
                    dma("scalar", ysz_v[b_], ztf[:, :], ["ztf"], ["ysz%d" % b_])
                dma("sync", cT[:], cT_d.ap(), [], ["cT"])
                dma("sync", gvec[:], gvec_d.ap(), [], ["gvec"])
                dma("sync", bada[:], bada_d.ap(), [], ["bada"])
                act(sg[:], cT[:], AF.Sigmoid, ["cT"], ["sgc"])
                tt("vector", sg[:], sg[:], cT[:], ALU.mult, ["sgc", "cT"], ["sgc"])
                for kc in range(8):
                    ts("vector", rep[:, kc, :], ones_f, sg[:, kc:kc + 1], None, ALU.mult, None,
                       ["consts", "sgc"], ["rep%d" % kc])
                wada_v = wada_d.ap().rearrange("(k p) n -> p k n", p=128)
                for ng in range(12):
                    wt, wk = wring.next()
                    dma("sync", wt[:], wada_v[:, :, ng * 512:(ng + 1) * 512], [], [wk])
                    bank, bk = ps.next()
                    for kc in range(8):
                        mm(bank[:, :], rep[:, kc, :], wt[:, kc, :], kc == 0, kc == 7,
                           [wk, "rep%d" % kc], [bk], sig=(kc == 7))
                    tt("vector", modbc[:, ng * 512:(ng + 1) * 512], bank[:, :], bada[:, ng * 512:(ng + 1) * 512],
                       ALU.add, [bk, "bada"], ["modbc"])
                m = lambda i: modbc[:, i * D:(i + 1) * D]
                g = lambda i: gvec[:, i * D:(i + 1) * D]
                stt(mods[:, GM1, :], m(1), 1.0, g(0), ALU.add, ALU.mult, ["modbc", "gvec"], ["mods"])
                cp("vector", mods[:, SH1, :], m(0), ["modbc"], ["mods"])
                tt("vector", mods[:, GP1, :], m(2), g(1), ALU.mult, ["modbc", "gvec"], ["mods"])
                stt(mods[:, GM2, :], m(4), 1.0, g(2), ALU.add, ALU.mult, ["modbc", "gvec"], ["mods"])
                cp("vector", mods[:, SH2, :], m(3), ["modbc"], ["mods"])
                tt("vector", mods[:, GP2, :], m(5), g(3), ALU.mult, ["modbc", "gvec"], ["mods"])
                S.barrier()
                if stage <= 0:
                    S.enabled = False

            with ExitStack() as st:
                w_in = sb(st, "w_in_sb", [128, 8, INW], BF16)
                w_out = sb(st, "w_out_sb", [128, 8, D], BF16)
                w_r = sb(st, "w_r_sb", [128, 8, NEXP])
                wup = sb(st, "wup_sb", [32, 256])
                kTs = sb(st, "kTs", [64, 3, 2, 128], BF16)
                vs = sb(st, "vs", [128, 3, 128], BF16)
                gaT = sb(st, "gaT", [32, 128])
                Sf = [sb(st, "Sf%d" % i, [64, 4, 128]) for i in range(2)]
                Sb = [sb(st, "Sb%d" % i, [64, 4, 128], BF16) for i in range(2)]
                Qc0 = sb(st, "Qc0", [64, 4, 128], BF16)
                Qc1 = sb(st, "Qc1", [64, 4, 128], BF16)

                with ExitStack() as stw:
                    stg = mkring(stw, "stg", 2, [128, 1160])
                    win_v = win_d.ap().rearrange("(k p) n -> k p n", p=128)
                    wout_v = wout_d.ap().rearrange("(k p) n -> k p n", p=128)
                    ceng = ["vector", "gpsimd"]
                    for kc in range(8):
                        for hf_ in range(2):
                            s_, sk = stg.next()
                            dma("sync", s_[:, :], win_v[kc][:, hf_ * 1160:(hf_ + 1) * 1160], [], [sk])
                            cp(ceng[hf_], w_in[:, kc, hf_ * 1160:(hf_ + 1) * 1160], s_[:, :], [sk], ["w_in"])
                    for kc in range(8):
                        s_, sk = stg.next()
                        dma("sync", s_[:, 0:D], wout_v[kc], [], [sk])
                        cp(ceng[kc % 2], w_out[:, kc, :], s_[:, 0:D], [sk], ["w_out"])
                    dma("sync", w_r[:], wr_d.ap().rearrange("(k p) n -> p k n", p=128), [], ["w_r"])
                    dma("sync", wup[0:17, :], wup_d.ap(), [], ["wup"])
                    S.op("vector", lambda e: e.memset(gaT[:], 1.0), [], ["gaT"])
                    for i in range(2):
                        S.op("gpsimd", lambda e, i=i: e.memset(Sf[i][:], 0.0), [], ["Sf%d" % i])
                        S.op("gpsimd", lambda e, i=i: e.memset(Sb[i][:], 0.0), [], ["Sb%d" % i])
                    S.op("gpsimd", lambda e: e.memset(Qc0[:], 0.0), [], ["Qc0"])
                    S.op("gpsimd", lambda e: e.memset(Qc1[:], 0.0), [], ["Qc1"])
                    S.barrier()

                xr = mkring(st, "xr", 2, [128, D])
                junk = mkring(st, "junk", 1, [128, D], BF16)
                tmpr = mkring(st, "tmpr", 1, [128, D])
                hbr = mkring(st, "hbr", 1, [128, D], BF16)
                hTr = mkring(st, "hTr", 2, [128, 8, 128], BF16)
                smr = mkring(st, "smr", 4, [128, 16])
                qTa = mkring(st, "qTa", 2, [64, 8, 128], BF16)
                gqr = mkring(st, "gqr", 2, [64, 4, 128])
                gkr = mkring(st, "gkr", 2, [64, 4, 128])
                ktokr = mkring(st, "ktokr", 2, [128, 256])
                vtokr = mkring(st, "vtokr", 2, [128, 512], BF16)
                sgg = mkring(st, "sgg", 2, [128, 512])
                enr = mkring(st, "enr", 1, [128, 256])
                ltok = mkring(st, "ltok", 1, [128, 256])
                bTr = mkring(st, "bTr", 1, [64, 4, 128])
                nbm = mkring(st, "nbm", 1, [64, 4, 2])
                decr = mkring(st, "decr", 1, [64, 4, 2])
                E1r = mkring(st, "E1r", 1, [64, 4, 128])
                E2r = mkring(st, "E2r", 1, [64, 4, 128])
                E3r = mkring(st, "E3r", 1, [64, 4, 128])
                QpT = mkring(st, "QpT", 1, [64, 4, 128], BF16)
                KpT = mkring(st, "KpT", 1, [64, 4, 128], BF16)
                E4r = mkring(st, "E4r", 1, [128, 256])
                Kpp = mkring(st, "Kpp", 1, [128, 256], BF16)
                ATr = mkring(st, "ATr", 1, [128, 4, 128], BF16)
                scr = mkring(st, "scr", 1, [128, 8, 256])
                pbr = mkring(st, "pbr", 1, [128, 8, 256], BF16)
                pTr = mkring(st, "pTr", 1, [128, 16, 128], BF16)
                mixr = mkring(st, "mixr", 1, [128, D], BF16)
                mixTr = mkring(st, "mixTr", 1, [128, 8, 128], BF16)
                osq = mkring(st, "osq", 1, [128, 512])
                x1r = mkring(st, "x1r", 1, [128, D])
                h2r = mkring(st, "h2r", 1, [128, D])
                h2Tf = mkring(st, "h2Tf", 1, [128, 8, 128])
                h2br = mkring(st, "h2br", 1, [128, D], BF16)
                mkbr = mkring(st, "mkbr", 1, [128, NEXP], BF16)
                lgr = mkring(st, "lgr", 1, [128, 4, NEXP])

                if stage <= 1:
                    S.enabled = False

                mask2 = consts[:, C_MASK2:C_MASK2 + 512]
                maskF = consts[:, C_MASKF:C_MASKF + 512]
                tri4 = consts[:, C_TRI4:C_TRI4 + 512]
                Uincl = consts[:, C_UINCL:C_UINCL + 128]
                Urev = consts[:, C_UREV:C_UREV + 128]
                gnorm = consts[:, C_GNORM:C_GNORM + 512]
                sinks = consts[:, C_SINK:C_SINK + 8]
                brout = consts[:, C_BROUT:C_BROUT + NEXP]

                x_v = x_d.ap().rearrange("(t p) n -> t p n", p=128)
                xp_v = xp_d.ap().rearrange("(t p) n -> t p n", p=128)
                x1_v = x1_d.ap().rearrange("(t p) n -> t p n", p=128)
                out_v = out_d.ap().rearrange("(t p) n -> t p n", p=128)
                h2_v = h2_d.ap().rearrange("(t p) n -> t p n", p=128)

                state = {"cur": 0}

                def rstd_from_ss(sm, smk, col_in, col_out, n, cnt=1):
                    a = sm[:, col_in:col_in + cnt]
                    o = sm[:, col_out:col_out + cnt]
                    ts("vector", o, a, 1.0 / n, EPS, ALU.mult, ALU.add, [smk], [smk])
                    act(o, o, AF.Ln, [smk], [smk])
                    act(o, o, AF.Exp, [smk], [smk], scale=-0.5)

                def norm_mod(x_t, xk, gi, si, out_ap, outk, sm, smk, c0):
                    jk_t, jk = junk.next()
                    act(jk_t[:, :], x_t[:, :], AF.Square, [xk], [jk, smk], accum=sm[:, c0:c0 + 1])
                    rstd_from_ss(sm, smk, c0, c0 + 1, float(D))
                    tm, tk = tmpr.next()
                    stt(tm[:, :], x_t[:, :], sm[:, c0 + 1:c0 + 2], mods[:, gi, :], ALU.mult, ALU.mult, [xk, smk], [tk])
                    tt("gpsimd", out_ap[:, 0:384], tm[:, 0:384], mods[:, si, 0:384], ALU.add, [tk], [outk])
                    tt("vector", out_ap[:, 384:D], tm[:, 384:D], mods[:, si, 384:D], ALU.add, [tk], [outk])

                def transpose8(src, srck, dst, dstk, evac_eng):
                    for half in range(2):
                        bank, bk = ps.next()
                        for j in range(4):
                            kc = half * 4 + j
                            mm(bank[:, j * 128:(j + 1) * 128], src[:, kc * 128:(kc + 1) * 128], identb[:], True, True,
                               [srck], [bk], sig=(j == 3))
                        cp("scalar" if half == 0 else "vector",
                           dst[:, half * 4:half * 4 + 4, :].rearrange("p k n -> p (k n)"), bank[:, :], [bk], [dstk])

                def gla_gates(hT, hTk, slot_rows):
                    bank, bk = ps.next()
                    for kc in range(8):
                        mm(bank[0:16, 0:128], w_in[:, kc, 2304:2320], hT[:, kc, :], kc == 0, kc == 7,
                           [hTk], [bk], sig=(kc == 7))
                    cp("vector", gaT[0:16, :], bank[0:16, 0:128], [bk], ["gaT"])
                    zb, zk = ps.next()
                    mm(zb[:, 0:256], gaT[0:17, :], wup[0:17, :], True, True, ["gaT"], [zk])
                    en, ek = enr.next()
                    act(en[:, :], zb[:, 0:256], AF.Exp, [zk], [ek], scale=-1.0)
                    l, lk = ltok.next()
                    act(l[:, :], en[:, :], AF.Ln, [ek], [lk], bias=1.0)
                    return l, lk

                def tile_body(t, pre):
                    main = not pre
                    slot = t + 1 if main else 0
                    last_pre = pre and (t == NP - 1)
                    xt, xk = xr.next()
                    dma("sync", xt[:, :], (x_v if main else xp_v)[t], [], [xk])
                    sm, smk = smr.next()
                    hb, hbk = hbr.next()
                    norm_mod(xt, xk, GM1, SH1, hb[:, :], hbk, sm, smk, 0)
                    hT, hTk = hTr.next()
                    if sub <= 0:
                        return
                    transpose8(hb, hbk, hT, hTk, "scalar")
                    if debug and main and t == 0:
                        dh = nc.dram_tensor("dbg_h", [128, D], BF16, kind="ExternalOutput")
                        dma("sync", dh.ap(), hb[:, :], [hbk], ["dbg_h"])
                        dhT = nc.dram_tensor("dbg_hT", [128, D], BF16, kind="ExternalOutput")
                        dma("sync", dhT.ap(), hT[:].rearrange("p k n -> p (k n)"), [hTk], ["dbg_hT"])
                    if sub <= 1:
                        return

                    def fm_group(cols_list, bank, bk):
                        for j, c0 in enumerate(cols_list):
                            for kc in range(8):
                                mm(bank[0:64, j * 128:(j + 1) * 128], w_in[:, kc, c0:c0 + 64], hT[:, kc, :],
                                   kc == 0, kc == 7, [hTk], [bk], sig=(kc == 7 and j == len(cols_list) - 1))

                    def tm_group(c0, n, bank, bk, off=0, last=True):
                        for kc in range(8):
                            mm(bank[:, off:off + n], hT[:, kc, :], w_in[:, kc, c0:c0 + n], kc == 0, kc == 7,
                               [hTk], [bk], sig=(kc == 7 and last))

                    if main:
                        qa, qak = qTa.next()
                        for half in range(2):
                            bank, bk = ps.next()
                            fm_group([h * 64 for h in range(half * 4, half * 4 + 4)], bank, bk)
                            S.op("scalar", lambda e, bank=bank, half=half: e.mul(
                                qa[:, half * 4:half * 4 + 4, :].rearrange("p h n -> p (h n)"), bank[0:64, :], 0.125),
                                [bk], [qak])
                        gq, gqk = gqr.next()
                        bank, bk = ps.next()
                        fm_group([768 + h * 64 for h in range(4)], bank, bk)
                        S.op("scalar", lambda e, bank=bank: e.mul(gq[:].rearrange("p h n -> p (h n)"), bank[0:64, :], 0.125),
                             [bk], [gqk])
                        gk, gkk = gkr.next()
                        bank, bk = ps.next()
                        fm_group([1024 + h * 64 for h in range(4)], bank, bk)
                        cp("vector", gk[:].rearrange("p h n -> p (h n)"), bank[0:64, :], [bk], [gkk])
                    KD = os.environ.get("KDBG", "")
                    if KD == "tm":
                        pass
                    elif main or last_pre:
                        bank, bk = ps.next()
                        fm_group([512, 576], bank, bk)
                        cp("vector", kTs[:, slot % 3, :, :],
                           bank[0:64, 0:256].rearrange("p (h n) -> p h n", h=2), [bk], ["kT%d" % (slot % 3)])
                    if KD == "fm":
                        return
                    bank, bk = ps.next()
                    if (main or last_pre) and KD != "tm1b":
                        tm_group(640, 128, bank, bk, off=0, last=False)
                    tm_group(1024, 256, bank, bk, off=128)
                    if (main or last_pre) and KD != "tm1b":
                        cp("vector" if KD == "tm1c" else "scalar", vs[:, slot % 3, :], bank[:, 0:128], [bk], ["v%d" % (slot % 3)])
                    ktok, ktk = ktokr.next()
                    cp("vector", ktok[:, :], bank[:, 128:384], [bk], [ktk])
                    if KD in ("tm1", "tm1b", "tm1c"):
                        return
                    bank, bk = ps.next()
                    tm_group(1280, 512, bank, bk)
                    vtok, vtk = vtokr.next()
                    if pre:
                        ts("vector", vtok[:, :], bank[:, :], consts[:, C_PFLAG + t:C_PFLAG + t + 1], None, ALU.mult, None,
                           [bk], [vtk])
                    else:
                        cp("scalar", vtok[:, :], bank[:, :], [bk], [vtk])
                    if main:
                        bank, bk = ps.next()
                        tm_group(1792, 512, bank, bk)
                        sg_t, sgk = sgg.next()
                        act(sg_t[:, :], bank[:, :], AF.Silu, [bk], [sgk])
                        tt("gpsimd", sg_t[:, :], sg_t[:, :], gnorm, ALU.mult, [sgk], [sgk])

                    if sub <= 2:
                        return
                    yield
                    l, lk = gla_gates(hT, hTk, None)
                    if sub <= 3:
                        return
                    rvb, rvk = ps.next()
                    mm(rvb[:, 0:256], Urev, l[:, :], True, True, [lk], [rvk])
                    E4, E4k = E4r.next()
                    act(E4[:, :], rvb[:, 0:256], AF.Exp, [rvk], [E4k])
                    kpp, kppk = Kpp.next()
                    tt("vector", kpp[:, :], ktok[:, :], E4[:, :], ALU.mult, [ktk, E4k], [kppk])
                    bTb, bTbk = ps.next()
                    if main:
                        for hd in range(4):
                            mm(bTb[0:64, hd * 128:(hd + 1) * 128], l[:, hd * 64:(hd + 1) * 64], Uincl, True, True,
                               [lk], [bTbk], sig=(hd == 3))
                        bT, bTk = bTr.next()
                        cp("vector", bT[:].rearrange("p h n -> p (h n)"), bTb[0:64, :], [bTbk], [bTk])
                        nb, nbk = nbm.next()
                        ts("vector", nb[:], bT[:, :, 31:128:64], -1.0, None, ALU.mult, None, [bTk], [nbk])
                        E1, E1k = E1r.next()
                        for hd in range(4):
                            for c in range(2):
                                act(E1[:, hd, c * 64:(c + 1) * 64], bT[:, hd, c * 64:(c + 1) * 64], AF.Exp,
                                    [bTk, nbk], [E1k], bias=nb[:, hd, c:c + 1])
                        E2, E2k = E2r.next()
                        S.op("vector", lambda e, E2=E2, E1=E1: e.reciprocal(E2[:], E1[:]), [E1k], [E2k])
                        E3, E3k = E3r.next()
                        act(E3[:], bT[:], AF.Exp, [bTk], [E3k])
                        qp, qpk = QpT.next()
                        tt("vector", qp[:], gq[:], E1[:], ALU.mult, [gqk, E1k], [qpk])
                        kp, kpk = KpT.next()
                        tt("gpsimd", kp[:], gk[:], E2[:], ALU.mult, [gkk, E2k], [kpk])
                        tt("gpsimd", Qc0[:, :, 0:64], gq[:, :, 0:64], E3[:, :, 0:64], ALU.mult, [gqk, E3k], ["Qc0"])
                        tt("gpsimd", Qc1[:, :, 64:128], gq[:, :, 64:128], E3[:, :, 64:128], ALU.mult, [gqk, E3k], ["Qc1"])
                        dec = lambda hd, c: E3[:, hd, c * 64 + 63:c * 64 + 64]
                        deck = E3k
                    else:
                        for hd in range(4):
                            mm(bTb[0:64, hd * 2:hd * 2 + 2], l[:, hd * 64:(hd + 1) * 64],
                               consts[:, C_UINCL + 63:C_UINCL + 128:64], True, True, [lk], [bTbk], sig=(hd == 3))
                        dc, dck = decr.next()
                        act(dc[:].rearrange("p h c -> p (h c)"), bTb[0:64, 0:8], AF.Exp, [bTbk], [dck])
                        dec = lambda hd, c: dc[:, hd, c:c + 1]
                        deck = dck

                    if sub <= 4:
                        return
                    cur = state["cur"]
                    S0f, S0b, S1f, S1b = Sf[cur], Sb[cur], Sf[1 - cur], Sb[1 - cur]
                    k0, k1 = "S%d" % cur, "S%d" % (1 - cur)
                    if main:
                        atb, atk = ps.next()
                        for hd in range(4):
                            mm(atb[:, hd * 128:(hd + 1) * 128], kp[:, hd, :], qp[:, hd, :], True, True,
                               [kpk, qpk], [atk], sig=(hd == 3))
                        AT, ATk = ATr.next()
                        tt("vector", AT[:].rearrange("p h n -> p (h n)"), atb[:, :], tri4, ALU.mult, [atk], [ATk])
                    kvb, kvk = ps.next()
                    for hd in range(4):
                        mm(kvb[0:64, hd * 128:(hd + 1) * 128], kpp[0:64, hd * 64:(hd + 1) * 64],
                           vtok[0:64, hd * 128:(hd + 1) * 128], True, True, [kppk, vtk], [kvk], sig=(hd == 3))
                    for hd in range(4):
                        stt(S1f[:, hd, :], S0f[:, hd, :], dec(hd, 0), kvb[0:64, hd * 128:(hd + 1) * 128],
                            ALU.mult, ALU.add, [k0 + "f", deck, kvk], [k1 + "f"])
                    cp("scalar", S1b[:], S1f[:], [k1 + "f"], [k1 + "b"])
                    kvb2, kvk2 = ps.next()
                    for hd in range(4):
                        mm(kvb2[0:64, hd * 128:(hd + 1) * 128], kpp[64:128, hd * 64:(hd + 1) * 64],
                           vtok[64:128, hd * 128:(hd + 1) * 128], True, True, [kppk, vtk], [kvk2], sig=(hd == 3))
                    if main:
                        ob, obk = ps.next()
                        for hd in range(4):
                            o_ap = ob[:, hd * 128:(hd + 1) * 128]
                            mm(o_ap, AT[:, hd, :], vtok[:, hd * 128:(hd + 1) * 128], True, False, [ATk, vtk], [obk], sig=False)
                            mm(o_ap, Qc0[:, hd, :], S0b[:, hd, :], False, False, ["Qc0", k0 + "b"], [obk], sig=False)
                            mm(o_ap, Qc1[:, hd, :], S1b[:, hd, :], False, True, ["Qc1", k1 + "b"], [obk], sig=(hd == 3))
                    for hd in range(4):
                        stt(S0f[:, hd, :], S1f[:, hd, :], dec(hd, 1), kvb2[0:64, hd * 128:(hd + 1) * 128],
                            ALU.mult, ALU.add, [k1 + "f", deck, kvk2], [k0 + "f"])
                    cp("scalar", S0b[:], S0f[:], [k0 + "f"], [k0 + "b"])
                    if pre:
                        return

                    mix, mixk = mixr.next()
                    sq, sqk = osq.next()
                    sm4, sm4k = smr.next()
                    for hd in range(4):
                        act(sq[:, hd * 128:(hd + 1) * 128], ob[:, hd * 128:(hd + 1) * 128], AF.Square, [obk], [sqk, sm4k],
                            accum=sm4[:, hd:hd + 1])
                    rstd_from_ss(sm4, sm4k, 0, 4, 128.0, cnt=4)
                    for hd in range(4):
                        stt(mix[:, 512 + hd * 128:512 + (hd + 1) * 128], ob[:, hd * 128:(hd + 1) * 128], sm4[:, 4 + hd:5 + hd],
                            sg_t[:, hd * 128:(hd + 1) * 128], ALU.mult, ALU.mult, [obk, sm4k, sgk], [mixk])

                    sc, sck = scr.next()
                    for pair in range(4):
                        bank, bk = ps.next()
                        for j in range(2):
                            h = pair * 2 + j
                            for c in range(2):
                                mm(bank[:, j * 256 + c * 128:j * 256 + (c + 1) * 128], qa[:, h, :],
                                   kTs[:, (t + c) % 3, h // 4, :], True, True, [qak, "kT%d" % ((t + c) % 3)], [bk],
                                   sig=(j == 1 and c == 1))
                        tt("vector", sc[:, pair * 2:pair * 2 + 2, :].rearrange("p h n -> p (h n)"), bank[:, :],
                           maskF if t == 0 else mask2, ALU.add, [bk], [sck])
                    sm2, sm2k = smr.next()
                    S.op("vector", lambda e: e.tensor_reduce(sm2[:, 0:8], sc[:], AX.X, ALU.max), [sck], [sm2k])
                    tt("vector", sm2[:, 0:8], sm2[:, 0:8], sinks, ALU.max, [sm2k], [sm2k])
                    ts("vector", sm2[:, 0:8], sm2[:, 0:8], -1.0, None, ALU.mult, None, [sm2k], [sm2k])
                    sm3, sm3k = smr.next()
                    pb, pbk = pbr.next()
                    for h in range(8):
                        act(pb[:, h, :], sc[:, h, :], AF.Exp, [sck, sm2k], [pbk, sm3k], bias=sm2[:, h:h + 1],
                            accum=sm3[:, h:h + 1])
                    tt("vector", sm2[:, 8:16], sinks, sm2[:, 0:8], ALU.add, [sm2k], [sm2k])
                    act(sm2[:, 8:16], sm2[:, 8:16], AF.Exp, [sm2k], [sm2k])
                    tt("vector", sm3[:, 0:8], sm3[:, 0:8], sm2[:, 8:16], ALU.add, [sm2k, sm3k], [sm3k])
                    S.op("vector", lambda e: e.reciprocal(sm3[:, 8:16], sm3[:, 0:8]), [sm3k], [sm3k])
                    pT, pTk = pTr.next()
                    for q4 in range(4):
                        bank, bk = ps.next()
                        for j in range(2):
                            h = q4 * 2 + j
                            for c in range(2):
                                mm(bank[:, (j * 2 + c) * 128:(j * 2 + c + 1) * 128], pb[:, h, c * 128:(c + 1) * 128],
                                   identb[:], True, True, [pbk], [bk], sig=(j == 1 and c == 1))
                        cp("scalar" if q4 % 2 == 0 else "vector",
                           pT[:, q4 * 4:q4 * 4 + 4, :].rearrange("p a n -> p (a n)"), bank[:, :], [bk], [pTk])
                    ab, abk = ps.next()
                    for h in range(8):
                        kv = h // 4
                        mm(ab[:, h * 64:(h + 1) * 64], pT[:, h * 2, :], vs[:, t % 3, kv * 64:(kv + 1) * 64], True, False,
                           [pTk, "v%d" % (t % 3)], [abk], sig=False)
                        mm(ab[:, h * 64:(h + 1) * 64], pT[:, h * 2 + 1, :], vs[:, (t + 1) % 3, kv * 64:(kv + 1) * 64], False, True,
                           [pTk, "v%d" % ((t + 1) % 3)], [abk], sig=(h == 7))
                    for h in range(8):
                        ts("vector", mix[:, h * 64:(h + 1) * 64], ab[:, h * 64:(h + 1) * 64],
                           sm3[:, 8 + h:9 + h], None, ALU.mult, None, [abk, sm3k], [mixk])

                    mixT, mixTk = mixTr.next()
                    transpose8(mix, mixk, mixT, mixTk, "scalar")
                    ybanks = []
                    for n in range(2):
                        bank, bk = ps.next()
                        for kc in range(8):
                            mm(bank[:, :], mixT[:, kc, :], w_out[:, kc, n * 512:(n + 1) * 512], kc == 0, kc == 7,
                               [mixTk], [bk], sig=(kc == 7))
                        ybanks.append((bank, bk))
                    sm5, sm5k = smr.next()
                    jk_t, jk = junk.next()
                    for n in range(2):
                        act(jk_t[:, n * 512:(n + 1) * 512], ybanks[n][0][:, :], AF.Square, [ybanks[n][1]], [jk, sm5k],
                            accum=sm5[:, n:n + 1])
                    tt("vector", sm5[:, 2:3], sm5[:, 0:1], sm5[:, 1:2], ALU.add, [sm5k], [sm5k])
                    rstd_from_ss(sm5, sm5k, 2, 3, float(D))
                    tm, tk = tmpr.next()
                    for n in range(2):
                        stt(tm[:, n * 512:(n + 1) * 512], ybanks[n][0][:, :], sm5[:, 3:4], mods[:, GP1, n * 512:(n + 1) * 512],
                            ALU.mult, ALU.mult, [ybanks[n][1], sm5k], [tk])
                    x1, x1k = x1r.next()
                    tt("gpsimd", x1[:, :], tm[:, :], xt[:, :], ALU.add, [tk, xk], [x1k])
                    dma("sync", x1_v[t], x1[:, :], [x1k], ["x1d%d" % t])

                    h2, h2k = h2r.next()
                    norm_mod(x1, x1k, GM2, SH2, h2[:, :], h2k, sm5, sm5k, 4)
                    hf, hfk = h2Tf.next()
                    for half in range(2):
                        bank, bk = ps.next()
                        for j in range(4):
                            kc = half * 4 + j
                            mm(bank[:, j * 128:(j + 1) * 128], h2[:, kc * 128:(kc + 1) * 128], identf, True, True,
                               [h2k], [bk], sig=(j == 3))
                        cp("vector" if half == 0 else "scalar", hf[:, half * 4:half * 4 + 4, :].rearrange("p k n -> p (k n)"),
                           bank[:, :], [bk], [hfk])
                    hbf, hbfk = h2br.next()
                    cp("gpsimd", hbf[:, :], h2[:, :], [h2k], [hbfk])
                    dma("sync", h2_v[t], hbf[:, :], [hbfk], ["h2d%d" % t])
                    lb, lbk = ps.next()
                    for kc in range(8):
                        mm(lb[:, 0:NEXP], hf[:, kc, :], w_r[:, kc, :], kc == 0, kc == 7, [hfk], [lbk], sig=(kc == 7))
                    lg, lgk = lgr.next()
                    LG, MK, EX, M8 = 0, 1, 2, 3
                    tt("vector", lg[:, LG, :], lb[:, 0:NEXP], brout, ALU.add, [lbk], [lgk])
                    S.op("vector", lambda e: e.max(lg[:, M8, 0:8], lg[:, LG, :]), [lgk], [lgk])
                    ts("vector", lg[:, MK, :], lg[:, LG, :], lg[:, M8, 3:4], None, ALU.is_ge, None, [lgk], [lgk])
                    ts("vector", lg[:, M8, 8:9], lg[:, M8, 0:1], -1.0, None, ALU.mult, None, [lgk], [lgk])
                    act(lg[:, EX, :], lg[:, LG, :], AF.Exp, [lgk], [lgk], bias=lg[:, M8, 8:9])
                    tt("vector", lg[:, EX, :], lg[:, EX, :], lg[:, MK, :], ALU.mult, [lgk], [lgk])
                    S.op("vector", lambda e: e.tensor_reduce(lg[:, M8, 9:10], lg[:, EX, :], AX.X, ALU.add), [lgk], [lgk])
                    S.op("vector", lambda e: e.reciprocal(lg[:, M8, 10:11], lg[:, M8, 9:10]), [lgk], [lgk])
                    ts("vector", G_all[:, t, :], lg[:, EX, :], lg[:, M8, 10:11], None, ALU.mult, None, [lgk], ["G%d" % t])
                    mkb, mkbk = mkbr.next()
                    cp("vector", mkb[:, :], lg[:, MK, :], [lgk], [mkbk])
                    rb, rbk = ps.next()
                    mm(rb[:, 0:NEXP], lstrb[:], mkb[:, :], True, True, [mkbk], [rbk], sig=False)
                    mm(rb[:, NEXP:2 * NEXP], onesb[:], mkb[:, :], True, True, [mkbk], [rbk])
                    stt(Qm_all[:, t, :], rb[:, 0:NEXP], 1.0, Orun[:, :], ALU.add, ALU.add, [rbk, "Orun"], ["Qm%d" % t])
                    tt("vector", Qm_all[:, t, :], Qm_all[:, t, :], lg[:, MK, :], ALU.mult, ["Qm%d" % t, lgk], ["Qm%d" % t])
                    tt("vector", Orun[:, :], Orun[:, :], rb[:, NEXP:2 * NEXP], ALU.add, ["Orun", rbk], ["Orun"])

                def drain(g):
                    for _ in g:
                        pass

                pend = None
                for (ti, pre_) in [(p, True) for p in range(NP)] + [(t, False) for t in range(NT)]:
                    if (not pre_) and ti == 0 and stage <= 2:
                        break
                    g = tile_body(ti, pre_)
                    try:
                        next(g)
                    except StopIteration:
                        g = None
                    if pend is not None:
                        drain(pend)
                    pend = g
                if pend is not None:
                    drain(pend)
                if stage <= 2:
                    S.enabled = False
                if debug:
                    dma("sync", G_d.ap(), G_all[:].rearrange("p t e -> p (t e)"), ["G%d" % t for t in range(NT)], ["Gd"])
                S.barrier()
                if stage <= 3:
                    S.enabled = False

            h2_v = h2_d.ap().rearrange("(t p) n -> t p n", p=128)
            x1_v = x1_d.ap().rearrange("(t p) n -> t p n", p=128)
            out_v = out_d.ap().rearrange("(t p) n -> t p n", p=128)
            gk_all = sb(P, "gk_all", [128, NT, 4])
            idx4_all = sb(P, "idx4_all", [128, NT, 4], I32)
            flags_i = sb(P, "flags_i", [128, NEXP * JM], I32)
            idxb_i = sb(P, "idxb_i", [128, NEXP * JM], I32)
            with ExitStack() as st:
                flf = sb(st, "flf", [128, NEXP, JM])
                nbt = sb(st, "nbt", [128, NEXP])
                cA = sb(st, "cA", [128, NEXP])
                cB = sb(st, "cB", [128, NEXP])
                pst = sb(st, "pst", [128, NEXP])
                idf = sb(st, "idf", [128, NEXP, JM])
                vr = mkring(st, "vr", 2, [128, 3, NEXP])
                m8r = mkring(st, "m8r", 2, [128, 16])
                h2l = mkring(st, "h2l", 2, [128, D], BF16)
                TH3 = consts[:, C_TH:C_TH + NEXP * 32].rearrange("p (e j) -> p e j", e=NEXP)[:, :, 0:JM]
                S.op("vector", lambda e: e.tensor_tensor(flf[:], Orun[:].unsqueeze(2).to_broadcast([128, NEXP, JM]), TH3,
                                                         ALU.is_gt), ["Orun"], ["flf"])
                S.op("vector", lambda e: e.tensor_reduce(nbt[:], flf[:], AX.X, ALU.add), ["flf"], ["nbt"])
                ts("vector", cA[:], nbt[:], 128.0, None, ALU.mult, None, ["nbt"], ["cA"])
                cp("vector", pst[:], cA[:], ["cA"], ["pst"])
                ca, cb, cak, cbk = cA, cB, "cA", "cB"
                for sft in (1, 2, 4, 8, 16):
                    cp("vector", cb[:, 0:sft], ca[:, 0:sft], [cak], [cbk])
                    tt("vector", cb[:, sft:NEXP], ca[:, sft:NEXP], ca[:, 0:NEXP - sft], ALU.add, [cak], [cbk])
                    ca, cb, cak, cbk = cb, ca, cbk, cak
                tt("vector", pst[:], ca[:], pst[:], ALU.subtract, [cak, "pst"], ["pst"])
                cp("vector", flags_i[:], flf[:].rearrange("p e j -> p (e j)"), ["flf"], ["flags"])
                S.op("vector", lambda e: e.tensor_tensor(idf[:], TH3, pst[:].unsqueeze(2).to_broadcast([128, NEXP, JM]),
                                                         ALU.add), ["pst"], ["idf"])
                ts("vector", idf[:], idf[:], consts[:, C_IOTA:C_IOTA + 1], -65536.0, ALU.add, ALU.add, ["idf"], ["idf"])
                tt("vector", idf[:], idf[:], flf[:], ALU.mult, ["idf", "flf"], ["idf"])
                ts("vector", idf[:], idf[:], 65536.0, None, ALU.add, None, ["idf"], ["idf"])
                cp("vector", idxb_i[:], idf[:].rearrange("p e j -> p (e j)"), ["idf"], ["idxb"])
                for t in range(NT):
                    v, vk = vr.next()
                    ts("vector", v[:, 0, :], Qm_all[:, t, :], 0.0, None, ALU.is_gt, None, [], [vk])
                    tt("vector", v[:, 1, :], Qm_all[:, t, :], pst[:], ALU.add, ["pst"], [vk])
                    tt("vector", v[:, 1, :], v[:, 1, :], v[:, 0, :], ALU.mult, [vk], [vk])
                    m8, m8k = m8r.next()
                    S.op("vector", lambda e, m8=m8, v=v: e.max(m8[:, 0:8], v[:, 1, :]), [vk], [m8k])
                    ts("vector", m8[:, 8:12], m8[:, 0:4], -1.0, None, ALU.add, None, [m8k], [m8k])
                    cp("vector", idx4_all[:, t, :], m8[:, 8:12], [m8k], ["idx4_%d" % t])
                    for k in range(4):
                        ts("vector", v[:, 2, :], v[:, 1, :], m8[:, k:k + 1], None, ALU.is_equal, None, [vk, m8k], [vk])
                        stt(v[:, 0, :], v[:, 2, :], 1.0, G_all[:, t, :], ALU.mult, ALU.mult, [vk], [vk, "gk%d" % t],
                            accum=gk_all[:, t, k:k + 1])
                    h2t, h2tk = h2l.next()
                    dma("sync", h2t[:, :], h2_v[t], [], [h2tk])
                    for k in range(4):
                        S.dma("gpsimd", lambda en, t=t, k=k, h2t=h2t: en.indirect_dma_start(
                            out=xs_d.ap(), out_offset=bass.IndirectOffsetOnAxis(ap=idx4_all[:, t, k:k + 1], axis=0),
                            in_=h2t[:, :], in_offset=None), [h2tk, "idx4_%d" % t], ["xsd"])
                S.barrier()

            with ExitStack() as st:
                w1b = mkring(st, "w1b", 2, [128, 8, 2, D], BF16)
                w2b = mkring(st, "w2b", 2, [128, 8, D], BF16)
                w1s = mkring(st, "w1s", 5, [128, 2, 256])
                w2s = mkring(st, "w2s", 4, [128, 512])
                b1s = mkring(st, "b1s", 1, [1, D])
                b1b = mkring(st, "b1b", 2, [1, 2 * D], BF16)
                xsr = mkring(st, "xsr", 2, [128, D], BF16)
                xsTr = mkring(st, "xsTr", 2, [128, 8, 128], BF16)
                aTr = mkring(st, "aTr", 2, [128, 8, 128], BF16)
                atokr = mkring(st, "atokr", 1, [128, D], BF16)
                xgr = mkring(st, "xgr", 1, [128, 512])
                sgr = mkring(st, "sgr", 1, [128, 512])
                xlr = mkring(st, "xlr", 1, [128, 512])
                ysr = mkring(st, "ysr", 1, [128, D])
                for i_ in range(len(ysr.t)):
                    S.op("gpsimd", lambda e, i_=i_: e.memset(ysr.t[i_][:], 0.0), [], ["ysr%d" % i_])
                w1_v = w1_d.ap().rearrange("e (k p) n -> e p k n", p=128)
                w2_v = w2_d.ap().rearrange("e (k p) n -> e k p n", p=128)

                def gather(e, j):
                    col = e * JM + j
                    xs, xsk = xsr.next()
                    S.dma("gpsimd", lambda en, xs=xs, col=col: en.indirect_dma_start(
                        out=xs[:, :], out_offset=None, in_=xs_d.ap(),
                        in_offset=bass.IndirectOffsetOnAxis(ap=idxb_i[:, col:col + 1], axis=0),
                        bounds_check=breg, oob_is_err=False), [], [xsk])
                    return xs, xsk

                def block(e, j, w1t, w1k, w2t, w2k, bb, bbk, xs, xsk):
                    col = e * JM + j
                    xT, xTk = xsTr.next()
                    transpose8(xs, xsk, xT, xTk, None)
                    aT, aTk = aTr.next()
                    atok, atokk = atokr.next()
                    for fh in range(2):
                        banks = []
                        for two in range(2):
                            bank, bk = ps.next()
                            for kc in range(8):
                                mm(bank[:, :], xT[:, kc, :], w1t[:, kc, two, fh * 512:(fh + 1) * 512], kc == 0, False,
                                   [w1k, xTk], [bk], sig=False)
                            mm(bank[:, :], onesb[0:1, :], bb[0:1, two * D + fh * 512:two * D + (fh + 1) * 512], False, True,
                               [bbk], [bk])
                            banks.append((bank, bk))
                        (bg, bgk), (bl, blk) = banks
                        xg, xgk = xgr.next()
                        ts("vector", xg[:, :], bg[:, :], 7.0, None, ALU.min, None, [bgk], [xgk])
                        sgt, sgk2 = sgr.next()
                        act(sgt[:, :], xg[:, :], AF.Sigmoid, [xgk], [sgk2], scale=1.702)
                        xl, xlk = xlr.next()
                        ts("vector", xl[:, :], bl[:, :], 7.0, -7.0, ALU.min, ALU.max, [blk], [xlk])
                        tt("gpsimd", xg[:, :], xg[:, :], sgt[:, :], ALU.mult, [xgk, sgk2], [xgk])
                        stt(atok[:, fh * 512:(fh + 1) * 512], xl[:, :], 1.0, xg[:, :], ALU.add, ALU.mult, [xlk, xgk], [atokk])
                    transpose8(atok, atokk, aT, aTk, None)
                    ysb, ysk = ysr.next()
                    ybs = [ps.next(), ps.next()]
                    for n in range(2):
                        yb, ybk = ybs[n]
                        for fc in range(8):
                            mm(yb[:, :], aT[:, fc, :], w2t[:, fc, n * 512:(n + 1) * 512], fc == 0, fc == 7,
                               [aTk, w2k], [ybk], sig=(fc == 7))
                    for n in range(2):
                        yb, ybk = ybs[n]
                        cp("scalar" if n == 0 else "vector", ysb[:, n * 512:(n + 1) * 512], yb[:, :], [ybk], [ysk])
                    S.dma("gpsimd", lambda en, ysb=ysb, col=col: en.indirect_dma_start(
                        out=ys_d.ap(), out_offset=bass.IndirectOffsetOnAxis(ap=idxb_i[:, col:col + 1], axis=0),
                        in_=ysb[:, :], in_offset=None, bounds_check=breg, oob_is_err=False), [ysk], ["ysd"])

                def load_weights(e):
                    w1t, w1k = w1b.next()
                    w2t, w2k = w2b.next()
                    bb, bbk = b1b.next()
                    ci = 0
                    for j in range(8):
                        for kh in range(4):
                            ws, wsk = w1s.next()
                            dma("sync", ws[:], w1_v[e][:, kh * 2:(kh + 1) * 2, j * 256:(j + 1) * 256], [], [wsk])
                            cp("scalar" if ci % 2 == 0 else "vector", w1t[:, kh * 2:(kh + 1) * 2, :, j * 128:(j + 1) * 128],
                               ws[:].rearrange("p k (m two) -> p k two m", two=2), [wsk], [w1k])
                            ci += 1
                        for nh in range(2):
                            s2, s2k = w2s.next()
                            dma("sync", s2[:], w2_v[e][j][:, nh * 512:(nh + 1) * 512], [], [s2k])
                            cp("scalar" if ci % 2 == 0 else "vector", w2t[:, j, nh * 512:(nh + 1) * 512], s2[:], [s2k], [w2k])
                            ci += 1
                    for bh in range(2):
                        bs, bsk = b1s.next()
                        dma("sync", bs[:], b1r_d.ap()[e:e + 1, bh * D:(bh + 1) * D], [], [bsk])
                        cp("vector", bb[:, bh * D:(bh + 1) * D], bs[:], [bsk], [bbk])
                    return w1t, w1k, w2t, w2k, bb, bbk

                wnext = load_weights(0)
                for e in range(NE):
                    w1t, w1k, w2t, w2k, bb, bbk = wnext
                    if e + 1 < NE:
                        wnext = load_weights(e + 1)
                    S.chain_begin()
                    nxt = gather(e, 0)
                    for j in range(JM):
                        S.level_push(flags_i[0:1, e * JM + j:e * JM + j + 1])
                        cur = nxt
                        if j + 1 < JM:
                            nxt = gather(e, j + 1)
                        block(e, j, w1t, w1k, w2t, w2k, bb, bbk, cur[0], cur[1])
                    S.chain_end()
                S.barrier()

            with ExitStack() as st:
                b2_sb = sb(st, "b2_sb", [NEXP, D])
                dma("sync", b2_sb[:], b2_d.ap(), [], ["b2"])
                yr = mkring(st, "yr", 4, [128, D])
                accr = mkring(st, "accr", 2, [128, D])
                GTr = mkring(st, "GTr", 2, [NEXP, 128])
                x1l = mkring(st, "x1l", 2, [128, D])
                outr = mkring(st, "outr", 2, [128, D])
                junk2 = mkring(st, "junk2", 1, [128, D], BF16)
                smq = mkring(st, "smq", 2, [128, 8])
                for t in range(NT):
                    ys4 = []
                    for k in range(4):
                        y_, yk_ = yr.next()
                        S.dma("gpsimd", lambda en, y_=y_, t=t, k=k: en.indirect_dma_start(
                            out=y_[:, :], out_offset=None, in_=ys_d.ap(),
                            in_offset=bass.IndirectOffsetOnAxis(ap=idx4_all[:, t, k:k + 1], axis=0)), [], [yk_])
                        ys4.append((y_, yk_))
                    ac, ack = accr.next()
                    ts("vector", ac[:, :], ys4[0][0][:, :], gk_all[:, t, 0:1], None, ALU.mult, None, [ys4[0][1]], [ack])
                    for k in range(1, 4):
                        stt(ac[:, :], ys4[k][0][:, :], gk_all[:, t, k:k + 1], ac[:, :], ALU.mult, ALU.add,
                            [ys4[k][1], ack], [ack])
                    gb, gbk = ps.next()
                    mm(gb[0:NEXP, 0:128], G_all[:, t, :], identf, True, True, [], [gbk])
                    gt_, gtk = GTr.next()
                    cp("vector", gt_[:, :], gb[0:NEXP, 0:128], [gbk], [gtk])
                    x1t, x1tk = x1l.next()
                    dma("sync", x1t[:, :], x1_v[t], [], [x1tk])
                    sm, smk = smq.next()
                    jk_t, jk = junk2.next()
                    for n in range(2):
                        yb, ybk = ps.next()
                        mm(yb[:, :], gt_[:, :], b2_sb[:, n * 512:(n + 1) * 512], True, True, [gtk, "b2"], [ybk])
                        a_ap = ac[:, n * 512:(n + 1) * 512]
                        tt("vector", a_ap, a_ap, yb[:, :], ALU.add, [ack, ybk], [ack])
                        act(jk_t[:, n * 512:(n + 1) * 512], a_ap, AF.Square, [ack], [jk, smk], accum=sm[:, n:n + 1])
                    tt("vector", sm[:, 2:3], sm[:, 0:1], sm[:, 1:2], ALU.add, [smk], [smk])
                    o_ = sm[:, 3:4]
                    ts("vector", o_, sm[:, 2:3], 1.0 / D, EPS, ALU.mult, ALU.add, [smk], [smk])
                    act(o_, o_, AF.Sqrt, [smk], [smk])
                    S.op("vector", lambda e, o_=o_: e.reciprocal(o_, o_), [smk], [smk])
                    ot, otk = outr.next()
                    stt(ot[:, :], ac[:, :], o_, mods[:, GP2, :], ALU.mult, ALU.mult, [ack, smk], [otk])
                    tt("gpsimd", ot[:, :], ot[:, :], x1t[:, :], ALU.add, [otk, x1tk], [otk])
                    dma("sync", out_v[t], ot[:, :], [otk], ["outd%d" % t])
                S.barrier()
        except _Stop:
            S.barrier()

        with nc.Block() as block:
            S.emit(block, gscr)
    return nc, S


def host_inputs(inp, NT=32, NP=96, segs=None):
    x = np.asarray(inp["x"], np.float32)
    TOK = NT * 128
    f = lambda k: np.ascontiguousarray(np.asarray(inp[k], np.float32)[0])
    w_ada, b_ada = f("w_ada"), f("b_ada")
    gvec = np.concatenate([f("g_pre_mix"), f("g_post_mix"), f("g_pre_ffn"), f("g_post_ffn")])
    gvec_bc = np.ascontiguousarray(np.broadcast_to(gvec[None, :], (128, 4 * D)))
    bada_bc = np.ascontiguousarray(np.broadcast_to(b_ada[None, :], (128, 6 * D)))
    wup_aug = np.concatenate([f("w_gla_gate_up"), f("b_gla_gate")[None, :]], axis=0)
    b1 = f("b_mlp1")
    b1r = np.ascontiguousarray(b1.reshape(NEXP, D, 2).transpose(0, 2, 1).reshape(NEXP, 2 * D))
    qi = np.arange(128)[:, None]
    kj = np.arange(256)[None, :]
    valid = ((kj < 128) & (kj > qi)) | ((kj >= 128) & (kj - 128 <= qi))
    m1 = np.where(valid, 0.0, NEG).astype(np.float32)
    validF = (kj >= 128) & (kj - 128 <= qi)
    mF = np.where(validF, 0.0, NEG).astype(np.float32)
    j = np.arange(128)[:, None]
    i = np.arange(128)[None, :]
    same = (j // 64) == (i // 64)
    tri = (same & (j <= i)).astype(np.float32)
    uincl = tri * (-1.0 / 16.0)
    urev = (same & (j > i)).astype(np.float32) * (-1.0 / 16.0)
    cbase = np.zeros((128, C_TOT), np.float32)
    cbase[:, C_MASK2:C_MASK2 + 512] = np.tile(m1, (1, 2))
    cbase[:, C_TRI4:C_TRI4 + 512] = np.tile(tri, (1, 4))
    cbase[:, C_UINCL:C_UINCL + 128] = uincl
    cbase[:, C_UREV:C_UREV + 128] = urev
    cbase[:, C_IDENT:C_IDENT + 128] = np.eye(128, dtype=np.float32)
    cbase[:, C_GNORM:C_GNORM + 512] = np.tile(f("g_gla_norm")[None, :], (128, 4))
    cbase[:, C_SINK:C_SINK + 8] = f("sinks")[None, :]
    cbase[:, C_BROUT:C_BROUT + NEXP] = f("b_router")[None, :]
    cbase[:, C_ONES:C_ONES + 128] = 1.0
    cbase[:, C_LSTR:C_LSTR + 128] = (j < i).astype(np.float32)
    cbase[:, C_TH:C_TH + NEXP * 32] = np.tile(128.0 * np.arange(32, dtype=np.float32), NEXP)[None, :]
    cbase[:, C_IOTA] = np.arange(128, dtype=np.float32)
    shared = {"w_ada": w_ada, "b_ada_bc": bada_bc, "gvec_bc": gvec_bc, "w_in": f("w_in"), "wup_aug": wup_aug,
              "w_out": f("w_out"), "w_router": f("w_router"), "w_mlp1": f("w_mlp1"), "w_mlp2": f("w_mlp2"),
              "b1r": b1r, "b_mlp2": f("b_mlp2")}
    maps = []
    nseg = SEQ // SEG
    if segs is None:
        segs = [(core // nseg, (core % nseg) * SEG) for core in range(NCORE)]
    for (b, s0) in segs:
        cm = cbase.copy()
        cm[:, C_MASKF:C_MASKF + 512] = np.tile(mF if s0 == 0 else m1, (1, 2))
        xpre = np.zeros((max(NP, 1) * 128, D), np.float32)
        p0 = s0 - NP * 128
        for p in range(NP):
            a = p0 + p * 128
            if a >= 0:
                xpre[p * 128:(p + 1) * 128] = x[b, a:a + 128]
                cm[:, C_PFLAG + p] = 1.0
        mp = dict(shared)
        mp["x"] = np.ascontiguousarray(x[b, s0:s0 + TOK])
        mp["xpre"] = xpre
        mp["cT"] = np.ascontiguousarray(np.asarray(inp["c"], np.float32)[b].reshape(8, 128).T)
        mp["consts"] = cm
        maps.append(mp)
    return maps


_CACHE = {}


def kernel(**inputs):
    if "nc" not in _CACHE:
        _CACHE["nc"] = build_nc()[0]
    nc = _CACHE["nc"]
    maps = host_inputs(inputs)
    res = run_bass_kernel_spmd(nc, maps, core_ids=list(range(NCORE)))
    out = np.empty((2, SEQ, D), np.float32)
    nseg = SEQ // SEG
    for core in range(NCORE):
        b, seg = core // nseg, core % nseg
        out[b, seg * SEG:(seg + 1) * SEG] = np.asarray(res.results[core]["out"], np.float32)
    return out
```

```python
import os
import numpy as np
from contextlib import ExitStack
import concourse.bass as bass
import concourse.mybir as mybir
from concourse.bass_utils import run_bass_kernel_spmd

F32 = mybir.dt.float32
BF16 = mybir.dt.bfloat16
I32 = mybir.dt.int32
ALU = mybir.AluOpType
AF = mybir.ActivationFunctionType
AX = mybir.AxisListType

D = 1024
SEQ = 16384
NCORE = 8
SEG = 4096
INW = 2320
NEXP = 32
EPS = 1e-6
NEG = -30000.0

ENGS = ("sync", "scalar", "vector", "gpsimd", "tensor")
CENG = ("scalar", "vector", "gpsimd", "tensor")
DENG = ("sync", "gpsimd", "scalar")
NDSEM = 8

C_MASK2 = 0
C_MASKF = 512
C_TRI4 = 1024
C_UINCL = 1536
C_UREV = 1664
C_IDENT = 1792
C_GNORM = 1920
C_SINK = 2432
C_BROUT = 2440
C_PFLAG = 2472
C_ONES = 2568
C_LSTR = 2696
C_TH = 2824
C_IOTA = 3848
C_TOT = 3856


class Sched:
    def __init__(self, nc, stack):
        self.nc = nc
        self.q = {e: [] for e in ENGS}
        self.esem = {e: stack.enter_context(nc.semaphore("es_" + e)) for e in CENG}
        self.ecnt = {e: 0 for e in CENG}
        self.dsem = {e: [stack.enter_context(nc.semaphore("ds_%s%d" % (e, i))) for i in range(NDSEM)]
                     for e in DENG}
        self.dcnt = {e: 0 for e in DENG}
        self.waited = {e: {} for e in ENGS}
        self.lastw = {}
        self.readers = {}
        self.nins = 0
        self.enabled = True
        self.cur_guard = None
        self.gid = 0
        self.chain_flags = {}
        self.chain_base = {}
        self.allwaits = {}

    def _deps(self, reads, writes):
        toks = []
        for k in reads:
            if k in self.lastw:
                toks.append(self.lastw[k])
        for k in writes:
            if k in self.lastw:
                toks.append(self.lastw[k])
            toks.extend(self.readers.get(k, ()))
        return toks

    def _need(self, eng, toks):
        out = {}
        for (sid, sem, val, owner) in toks:
            if owner == eng and eng == "tensor":
                continue
            if self.waited[eng].get(sid, 0) >= val:
                continue
            if sid not in out or out[sid][1] < val:
                out[sid] = (sem, val)
        for sid, (sem, val) in out.items():
            self.waited[eng][sid] = val
        if self.cur_guard is not None and self.cur_guard[1] > 0:
            key = self.cur_guard
            d = self.allwaits.setdefault(key, {})
            for sid, (sem, val) in out.items():
                if sid not in d or d[sid][1] < val:
                    d[sid] = (sem, val)
        return list(out.values())

    def _commit(self, tok, reads, writes):
        for k in writes:
            self.lastw[k] = tok
            self.readers[k] = []
        for k in reads:
            if k not in writes:
                self.readers.setdefault(k, []).append(tok)

    def op(self, eng, fn, reads=(), writes=(), sig=True):
        if not self.enabled:
            return
        px = [k for k in reads if k.startswith("ps")]
        if px:
            reads = [k for k in reads if not k.startswith("ps")]
            writes = list(writes) + px
        waits = self._need(eng, self._deps(reads, writes))
        if sig:
            self.ecnt[eng] += 1
            val = self.ecnt[eng]
        else:
            val = self.ecnt[eng] + 1
        tok = ("e_" + eng, self.esem[eng], val, eng)
        self.q[eng].append((waits, fn, (self.esem[eng], 1) if sig else None, self.cur_guard))
        self._commit(tok, reads, writes)
        self.nins += 1 + len(waits)

    def dma(self, eng, fn, reads=(), writes=()):
        if not self.enabled:
            return
        i = self.dcnt[eng]
        self.dcnt[eng] += 1
        sem = self.dsem[eng][i % NDSEM]
        sid = "d_%s%d" % (eng, i % NDSEM)
        val = 16 * (i // NDSEM + 1)
        toks = self._deps(reads, writes)
        if val > 16:
            toks.append((sid, sem, val - 16, "dma"))
        waits = self._need(eng, toks)
        self.q[eng].append((waits, fn, (sem, 16), self.cur_guard))
        self._commit((sid, sem, val, "dma"), reads, writes)
        self.nins += 1 + len(waits)

    def barrier(self):
        if not self.enabled:
            return
        toks = []
        for e in CENG:
            if self.ecnt[e] > 0:
                toks.append(("e_" + e, self.esem[e], self.ecnt[e], "x"))
        for e in DENG:
            n = self.dcnt[e]
            for j in range(min(n, NDSEM)):
                cnt = (n - 1 - j) // NDSEM + 1
                toks.append(("d_%s%d" % (e, j), self.dsem[e][j], 16 * cnt, "dma"))
        for e in ENGS:
            waits = self._need(e, toks)
            if waits:
                self.q[e].append((waits, None, None, None))
        self.lastw = {}
        self.readers = {}

    def chain_begin(self):
        self.gid += 1
        self.cur_guard = (self.gid, 0)
        self.chain_flags[self.gid] = [None]
        self._wsnap = {e: dict(self.waited[e]) for e in ENGS}

    def _counters(self):
        c = {}
        for e in CENG:
            c["e_" + e] = self.ecnt[e]
        for e in DENG:
            n = self.dcnt[e]
            for j in range(min(n, NDSEM)):
                c["d_%s%d" % (e, j)] = 16 * ((n - 1 - j) // NDSEM + 1)
        return c

    def level_push(self, flag_ap):
        cid, d = self.cur_guard
        self.chain_flags[cid].append(flag_ap)
        self.chain_base.setdefault(cid, [None]).append(self._counters())
        self.cur_guard = (cid, d + 1)

    def chain_end(self):
        self.cur_guard = None
        self.waited = self._wsnap

    def emit(self, block, scratch):
        for e in ENGS:
            def body(engine, e=e):
                tot = {}
                entries = self.q[e]
                state = {"reg": None}

                def emit_entry(ent):
                    waits, fn, inc, _ = ent
                    for sem, val in waits:
                        engine.wait_ge(sem, val)
                    if fn is not None:
                        ins = fn(engine)
                        if inc is not None:
                            ins.then_inc(inc[0], inc[1])
                            tot[id(inc[0])] = tot.get(id(inc[0]), 0) + inc[1]

                def depth_of(ent):
                    return 0 if ent[3] is None else ent[3][1]

                def emit_level(i, end, cid, depth):
                    while i < end:
                        d = depth_of(entries[i])
                        if d == depth:
                            emit_entry(entries[i])
                            i += 1
                            continue
                        flag = self.chain_flags[cid][depth + 1]
                        if state["reg"] is None:
                            state["reg"] = engine.alloc_register("gflag_" + e)
                        reg = state["reg"]
                        adds = {}
                        for ent in entries[i:end]:
                            if ent[2] is not None and ent[1] is not None:
                                k = id(ent[2][0])
                                adds[k] = (ent[2][0], adds.get(k, (None, 0))[1] + ent[2][1])
                        before = dict(tot)
                        engine.reg_load(reg, flag)
                        with engine.If_ne(reg, 0):
                            emit_level(i, end, cid, depth + 1)
                        if adds:
                            with engine.Else():
                                base = self.chain_base[cid][depth + 1]
                                maxd = len(self.chain_flags[cid]) - 1
                                ext = {}
                                for dd in range(depth + 1, maxd + 1):
                                    for sid, (sem, val) in self.allwaits.get((cid, dd), {}).items():
                                        v = min(val, base.get(sid, 0))
                                        if v > 0 and (sid not in ext or ext[sid][1] < v):
                                            ext[sid] = (sem, v)
                                for sid, (sem, v) in ext.items():
                                    engine.wait_ge(sem, v)
                                for k, (sem, n) in adds.items():
                                    b = before.get(k, 0)
                                    if b > 0:
                                        engine.wait_ge(sem, b)
                                    if e == "gpsimd" and any(sem is d_ for d_ in self.dsem["gpsimd"]):
                                        engine.dma_start(out=scratch[0:1, 8:9], in_=scratch[0:1, 0:1]).then_inc(sem, n)
                                    else:
                                        engine.sem_inc(sem, n)
                        for k, (sem, n) in adds.items():
                            tot[k] = before.get(k, 0) + n
                        i = end

                i = 0
                while i < len(entries):
                    g = entries[i][3]
                    if g is None:
                        emit_entry(entries[i])
                        i += 1
                        continue
                    cid = g[0]
                    end = i
                    while end < len(entries) and entries[end][3] is not None and entries[end][3][0] == cid:
                        end += 1
                    emit_level(i, end, cid, 0)
                    i = end
            getattr(block, e)(body)


class Ring:
    def __init__(self, name, tiles):
        self.t = tiles
        self.name = name
        self.i = 0

    def next(self):
        k = self.i % len(self.t)
        self.i += 1
        return self.t[k], "%s%d" % (self.name, k)


class _Stop(Exception):
    pass


def build_nc(NT=32, NP=96, NE=32, debug=False, stage=9, sub=99):
    nc = bass.Bass("TRN2", target_bir_lowering=False)
    TOK = NT * 128
    TQ = min(8, NT)
    NQ = NT // TQ
    GT = min(4, TQ)
    NG = TQ // GT

    def din(name, shape, dt=F32):
        return nc.dram_tensor(name, list(shape), dt, kind="ExternalInput")

    x_d = din("x", [TOK, D])
    xp_d = din("xpre", [max(NP, 1) * 128, D])
    cT_d = din("cT", [128, 8])
    wada_d = din("w_ada", [D, 6 * D])
    bada_d = din("b_ada_bc", [128, 6 * D])
    gvec_d = din("gvec_bc", [128, 4 * D])
    consts_d = din("consts", [128, C_TOT])
    win_d = din("w_in", [D, INW])
    wup_d = din("wup_aug", [17, 256])
    wout_d = din("w_out", [D, D])
    wr_d = din("w_router", [D, NEXP])
    w1_d = din("w_mlp1", [NEXP, D, 2 * D])
    w2_d = din("w_mlp2", [NEXP, D, D])
    b1r_d = din("b1r", [NEXP, 2 * D])
    b2_d = din("b_mlp2", [NEXP, D])
    out_d = nc.dram_tensor("out", [TOK, D], F32, kind="ExternalOutput")
    x1_d = nc.dram_tensor("x1_d", [TOK, D], F32, kind="ExternalOutput" if debug else "Internal")
    JM = min(32, NT)
    NSLOT = TOK * 4 + NEXP * 128
    NBLK = NSLOT // 128
    h2_d = nc.dram_tensor("h2_d", [TOK, D], BF16)
    xs_d = nc.dram_tensor("xs_d", [NSLOT, D], BF16)
    ys_d = nc.dram_tensor("ys_d", [NSLOT, D], F32)
    G_d = nc.dram_tensor("G_d", [128, NT * NEXP], F32, kind="ExternalOutput") if debug else None

    stack = ExitStack()
    with stack:
        S = Sched(nc, stack)

        def sb(st, name, shape, dt=F32):
            return st.enter_context(nc.sbuf_tensor("s_" + name, list(shape), dt))

        def mkring(st, name, n, shape, dt=F32):
            return Ring(name, [sb(st, "%s_%d" % (name, i), shape, dt) for i in range(n)])

        def mm(out, lhsT, rhs, start, stop, r, w, sig=True):
            S.op("tensor", lambda e: e.matmul(out, lhsT, rhs, start=start, stop=stop), r, w, sig=sig)

        def tr(out, in_, ident, r, w, sig=True):
            S.op("tensor", lambda e: e.transpose(out, in_, ident), r, w, sig=sig)

        def act(out, in_, func, r, w, bias=None, scale=None, accum=None):
            kw = {}
            if bias is not None:
                kw["bias"] = bias
            if scale is not None:
                kw["scale"] = scale
            if accum is not None:
                kw["accum_out"] = accum
            S.op("scalar", lambda e: e.activation(out, in_, func, **kw), r, w)

        def ts(eng, out, in0, s1, s2, op0, op1, r, w, accum=None):
            if accum is not None:
                S.op(eng, lambda e: e.tensor_scalar(out, in0, s1, s2, op0, op1, accum_out=accum), r, w)
            elif op1 is None:
                S.op(eng, lambda e: e.tensor_scalar(out, in0, s1, None, op0), r, w)
            else:
                S.op(eng, lambda e: e.tensor_scalar(out, in0, s1, s2, op0, op1), r, w)

        def tt(eng, out, in0, in1, op, r, w):
            S.op(eng, lambda e: e.tensor_tensor(out, in0, in1, op), r, w)

        def stt(out, in0, scalar, in1, op0, op1, r, w, accum=None):
            if accum is not None:
                S.op("vector", lambda e: e.scalar_tensor_tensor(out, in0, scalar, in1, op0, op1, accum_out=accum), r, w)
            else:
                S.op("vector", lambda e: e.scalar_tensor_tensor(out, in0, scalar, in1, op0, op1), r, w)

        def cp(eng, out, in_, r, w):
            if eng == "scalar":
                S.op(eng, lambda e: e.copy(out, in_), r, w)
            else:
                S.op(eng, lambda e: e.tensor_copy(out, in_), r, w)

        def dma(eng, out, in_, r, w):
            S.dma(eng, lambda e: e.dma_start(out=out, in_=in_), r, w)

        P = stack
        consts = sb(P, "consts", [128, C_TOT])
        identb = sb(P, "identb", [128, 128], BF16)
        mods = sb(P, "mods", [128, 6, D])
        G_all = sb(P, "G_all", [128, NT, NEXP])
        Qm_all = sb(P, "Qm_all", [128, NT, NEXP])
        Orun = sb(P, "Orun", [128, NEXP])
        onesb = sb(P, "onesb", [128, 128], BF16)
        lstrb = sb(P, "lstrb", [128, 128], BF16)
        GM1, SH1, GP1, GM2, SH2, GP2 = range(6)

        ps = Ring("ps", [stack.enter_context(nc.psum_tensor("ps%d" % i, [128, 512], F32)) for i in range(8)])

        identf = consts[:, C_IDENT:C_IDENT + 128]
        ones_f = consts[:, C_ONES:C_ONES + 128]

        dma("sync", consts[:], consts_d.ap(), [], ["consts"])
        cp("vector", identb[:], identf, ["consts"], ["identb"])
        cp("vector", onesb[:], ones_f, ["consts"], ["onesb"])
        cp("vector", lstrb[:], consts[:, C_LSTR:C_LSTR + 128], ["consts"], ["lstrb"])
        S.op("vector", lambda e: e.memset(Orun[:], 0.0), [], ["Orun"])
        gscr = sb(P, "gscr", [1, 16])
        S.op("gpsimd", lambda e: e.memset(gscr[:], 0.0), [], ["gscr"])
        breg = nc.gpsimd.alloc_register("bndreg")
        S.op("gpsimd", lambda e: e.reg_mov(breg, NSLOT - 1), [], [], sig=False)

        try:
            with ExitStack() as st:
                cT = sb(st, "cT", [128, 8])
                sg = sb(st, "sgc", [128, 8])
                rep = sb(st, "rep", [128, 8, 128])
                gvec = sb(st, "gvec", [128, 4 * D])
                bada = sb(st, "bada", [128, 6 * D])
                modbc = sb(st, "modbc", [128, 6 * D])
                wring = mkring(st, "wada", 2, [128, 8, 512])
                zt = sb(st, "zt", [128, D], BF16)
                S.op("gpsimd", lambda e: e.memset(zt[:], 0.0), [], ["zt"])
                xsz_v = xs_d.ap().rearrange("(b p) n -> b p n", p=128)
                ztf = sb(st, "ztf", [128, D])
                S.op("gpsimd", lambda e: e.memset(ztf[:], 0.0), [], ["ztf"])
                ysz_v = ys_d.ap().rearrange("(b p) n -> b p n", p=128)
                for b_ in range(NBLK):
                    dma("scalar", xsz_v[b_], zt[:, :], ["zt"], ["xsz%d" % b_])
                    dma("scalar", ysz_v[b_], ztf[:, :], ["ztf"], ["ysz%d" % b_])
                dma("sync", cT[:], cT_d.ap(), [], ["cT"])
                dma("sync", gvec[:], gvec_d.ap(), [], ["gvec"])
                dma("sync", bada[:], bada_d.ap(), [], ["bada"])
                act(sg[:], cT[:], AF.Sigmoid, ["cT"], ["sgc"])
                tt("vector", sg[:], sg[:], cT[:], ALU.mult, ["sgc", "cT"], ["sgc"])
                for kc in range(8):
                    ts("vector", rep[:, kc, :], ones_f, sg[:, kc:kc + 1], None, ALU.mult, None,
                       ["consts", "sgc"], ["rep%d" % kc])
                wada_v = wada_d.ap().rearrange("(k p) n -> p k n", p=128)
                for ng in range(12):
                    wt, wk = wring.next()
                    dma("sync", wt[:], wada_v[:, :, ng * 512:(ng + 1) * 512], [], [wk])
                    bank, bk = ps.next()
                    for kc in range(8):
                        mm(bank[:, :], rep[:, kc, :], wt[:, kc, :], kc == 0, kc == 7,
                           [wk, "rep%d" % kc], [bk], sig=(kc == 7))
                    tt("vector", modbc[:, ng * 512:(ng + 1) * 512], bank[:, :], bada[:, ng * 512:(ng + 1) * 512],
                       ALU.add, [bk, "bada"], ["modbc"])
                m = lambda i: modbc[:, i * D:(i + 1) * D]
                g = lambda i: gvec[:, i * D:(i + 1) * D]
                stt(mods[:, GM1, :], m(1), 1.0, g(0), ALU.add, ALU.mult, ["modbc", "gvec"], ["mods"])
                cp("vector", mods[:, SH1, :], m(0), ["modbc"], ["mods"])
                tt("vector", mods[:, GP1, :], m(2), g(1), ALU.mult, ["modbc", "gvec"], ["mods"])
                stt(mods[:, GM2, :], m(4), 1.0, g(2), ALU.add, ALU.mult, ["modbc", "gvec"], ["mods"])
                cp("vector", mods[:, SH2, :], m(3), ["modbc"], ["mods"])
                tt("vector", mods[:, GP2, :], m(5), g(3), ALU.mult, ["modbc", "gvec"], ["mods"])
                S.barrier()
                if stage <= 0:
                    S.enabled = False

            with ExitStack() as st:
                w_in = sb(st, "w_in_sb", [128, 8, INW], BF16)
                w_out = sb(st, "w_out_sb", [128, 8, D], BF16)
                w_r = sb(st, "w_r_sb", [128, 8, NEXP])
                wup = sb(st, "wup_sb", [32, 256])
                kTs = sb(st, "kTs", [64, 3, 2, 128], BF16)
                vs = sb(st, "vs", [128, 3, 128], BF16)
                gaT = sb(st, "gaT", [32, 128])
                Sf = [sb(st, "Sf%d" % i, [64, 4, 128]) for i in range(2)]
                Sb = [sb(st, "Sb%d" % i, [64, 4, 128], BF16) for i in range(2)]
                Qc0 = sb(st, "Qc0", [64, 4, 128], BF16)
                Qc1 = sb(st, "Qc1", [64, 4, 128], BF16)

                with ExitStack() as stw:
                    stg = mkring(stw, "stg", 2, [128, 1160])
                    win_v = win_d.ap().rearrange("(k p) n -> k p n", p=128)
                    wout_v = wout_d.ap().rearrange("(k p) n -> k p n", p=128)
                    ceng = ["vector", "gpsimd"]
                    for kc in range(8):
                        for hf_ in range(2):
                            s_, sk = stg.next()
                            dma("sync", s_[:, :], win_v[kc][:, hf_ * 1160:(hf_ + 1) * 1160], [], [sk])
                            cp(ceng[hf_], w_in[:, kc, hf_ * 1160:(hf_ + 1) * 1160], s_[:, :], [sk], ["w_in"])
                    for kc in range(8):
                        s_, sk = stg.next()
                        dma("sync", s_[:, 0:D], wout_v[kc], [], [sk])
                        cp(ceng[kc % 2], w_out[:, kc, :], s_[:, 0:D], [sk], ["w_out"])
                    dma("sync", w_r[:], wr_d.ap().rearrange("(k p) n -> p k n", p=128), [], ["w_r"])
                    dma("sync", wup[0:17, :], wup_d.ap(), [], ["wup"])
                    S.op("vector", lambda e: e.memset(gaT[:], 1.0), [], ["gaT"])
                    for i in range(2):
                        S.op("gpsimd", lambda e, i=i: e.memset(Sf[i][:], 0.0), [], ["Sf%d" % i])
                        S.op("gpsimd", lambda e, i=i: e.memset(Sb[i][:], 0.0), [], ["Sb%d" % i])
                    S.op("gpsimd", lambda e: e.memset(Qc0[:], 0.0), [], ["Qc0"])
                    S.op("gpsimd", lambda e: e.memset(Qc1[:], 0.0), [], ["Qc1"])
                    S.barrier()

                xr = mkring(st, "xr", 2, [128, D])
                tmpr = mkring(st, "tmpr", 1, [128, D])
                hbr = mkring(st, "hbr", 1, [128, D], BF16)
                hTr = mkring(st, "hTr", 3, [128, 8, 128], BF16)
                smr = mkring(st, "smr", 4, [128, 16])
                qTa = mkring(st, "qTa", 2, [64, 8, 128], BF16)
                gqr = mkring(st, "gqr", 2, [64, 4, 128])
                gkr = mkring(st, "gkr", 2, [64, 4, 128])
                ktokr = mkring(st, "ktokr", 3, [128, 256])
                vtokr = mkring(st, "vtokr", 3, [128, 512], BF16)
                sgg = mkring(st, "sgg", 2, [128, 512])
                enr = mkring(st, "enr", 1, [128, 256])
                ltok = mkring(st, "ltok", 1, [128, 256])
                bTr = mkring(st, "bTr", 1, [64, 4, 128])
                nbm = mkring(st, "nbm", 1, [64, 4, 2])
                decr = mkring(st, "decr", 1, [64, 4, 2])
                E1r = mkring(st, "E1r", 1, [64, 4, 128])
                E2r = mkring(st, "E2r", 1, [64, 4, 128])
                E3r = mkring(st, "E3r", 1, [64, 4, 128])
                QpT = mkring(st, "QpT", 1, [64, 4, 128], BF16)
                KpT = mkring(st, "KpT", 1, [64, 4, 128], BF16)
                E4r = mkring(st, "E4r", 1, [128, 256])
                Kpp = mkring(st, "Kpp", 1, [128, 256], BF16)
                ATr = mkring(st, "ATr", 1, [128, 4, 128], BF16)
                scr = mkring(st, "scr", 1, [128, 8, 256])
                pbr = mkring(st, "pbr", 1, [128, 8, 256], BF16)
                pTr = mkring(st, "pTr", 1, [128, 16, 128], BF16)
                mixr = mkring(st, "mixr", 1, [128, D], BF16)
                mixTr = mkring(st, "mixTr", 1, [128, 8, 128], BF16)
                osq = mkring(st, "osq", 1, [128, 512])
                x1r = mkring(st, "x1r", 1, [128, D])
                h2r = mkring(st, "h2r", 1, [128, D])
                h2Tf = mkring(st, "h2Tf", 1, [128, 8, 128])
                h2br = mkring(st, "h2br", 1, [128, D], BF16)
                mkbr = mkring(st, "mkbr", 1, [128, NEXP], BF16)
                lgr = mkring(st, "lgr", 1, [128, 4, NEXP])

                if stage <= 1:
                    S.enabled = False

                mask2 = consts[:, C_MASK2:C_MASK2 + 512]
                maskF = consts[:, C_MASKF:C_MASKF + 512]
                tri4 = consts[:, C_TRI4:C_TRI4 + 512]
                Uincl = consts[:, C_UINCL:C_UINCL + 128]
                Urev = consts[:, C_UREV:C_UREV + 128]
                gnorm = consts[:, C_GNORM:C_GNORM + 512]
                sinks = consts[:, C_SINK:C_SINK + 8]
                brout = consts[:, C_BROUT:C_BROUT + NEXP]

                x_v = x_d.ap().rearrange("(t p) n -> t p n", p=128)
                xp_v = xp_d.ap().rearrange("(t p) n -> t p n", p=128)
                x1_v = x1_d.ap().rearrange("(t p) n -> t p n", p=128)
                out_v = out_d.ap().rearrange("(t p) n -> t p n", p=128)
                h2_v = h2_d.ap().rearrange("(t p) n -> t p n", p=128)

                state = {"cur": 0}

                def rstd_from_ss(sm, smk, col_in, col_out, n, cnt=1):
                    a = sm[:, col_in:col_in + cnt]
                    o = sm[:, col_out:col_out + cnt]
                    ts("vector", o, a, 1.0 / n, EPS, ALU.mult, ALU.add, [smk], [smk])
                    act(o, o, AF.Ln, [smk], [smk])
                    act(o, o, AF.Exp, [smk], [smk], scale=-0.5)

                def norm_mod(x_t, xk, gi, si, out_ap, outk, sm, smk, c0):
                    tm, tk = tmpr.next()
                    act(tm[:, :], x_t[:, :], AF.Square, [xk], [tk, smk], accum=sm[:, c0:c0 + 1])
                    rstd_from_ss(sm, smk, c0, c0 + 1, float(D))
                    stt(tm[:, :], x_t[:, :], sm[:, c0 + 1:c0 + 2], mods[:, gi, :], ALU.mult, ALU.mult, [xk, smk], [tk])
                    tt("gpsimd", out_ap[:, 0:384], tm[:, 0:384], mods[:, si, 0:384], ALU.add, [tk], [outk])
                    tt("vector", out_ap[:, 384:D], tm[:, 384:D], mods[:, si, 384:D], ALU.add, [tk], [outk])

                def transpose8(src, srck, dst, dstk, evac_eng):
                    for half in range(2):
                        bank, bk = ps.next()
                        for j in range(4):
                            kc = half * 4 + j
                            mm(bank[:, j * 128:(j + 1) * 128], src[:, kc * 128:(kc + 1) * 128], identb[:], True, True,
                               [srck], [bk], sig=(j == 3))
                        cp("scalar" if half == 0 else "vector",
                           dst[:, half * 4:half * 4 + 4, :].rearrange("p k n -> p (k n)"), bank[:, :], [bk], [dstk])

                def gla_gates(hT, hTk, slot_rows):
                    bank, bk = ps.next()
                    for kc in range(8):
                        mm(bank[0:16, 0:128], w_in[:, kc, 2304:2320], hT[:, kc, :], kc == 0, kc == 7,
                           [hTk], [bk], sig=(kc == 7))
                    cp("vector", gaT[0:16, :], bank[0:16, 0:128], [bk], ["gaT"])
                    zb, zk = ps.next()
                    mm(zb[:, 0:256], gaT[0:17, :], wup[0:17, :], True, True, ["gaT"], [zk])
                    en, ek = enr.next()
                    act(en[:, :], zb[:, 0:256], AF.Exp, [zk], [ek], scale=-1.0)
                    l, lk = ltok.next()
                    act(l[:, :], en[:, :], AF.Ln, [ek], [lk], bias=1.0)
                    return l, lk

                def tile_body(t, pre):
                    main = not pre
                    slot = t + 1 if main else 0
                    last_pre = pre and (t == NP - 1)
                    xt, xk = xr.next()
                    dma("sync", xt[:, :], (x_v if main else xp_v)[t], [], [xk])
                    sm, smk = smr.next()
                    hb, hbk = hbr.next()
                    norm_mod(xt, xk, GM1, SH1, hb[:, :], hbk, sm, smk, 0)
                    hT, hTk = hTr.next()
                    if sub <= 0:
                        return
                    transpose8(hb, hbk, hT, hTk, "scalar")
                    if debug and main and t == 0:
                        dh = nc.dram_tensor("dbg_h", [128, D], BF16, kind="ExternalOutput")
                        dma("sync", dh.ap(), hb[:, :], [hbk], ["dbg_h"])
                        dhT = nc.dram_tensor("dbg_hT", [128, D], BF16, kind="ExternalOutput")
                        dma("sync", dhT.ap(), hT[:].rearrange("p k n -> p (k n)"), [hTk], ["dbg_hT"])
                    if sub <= 1:
                        return

                    def fm_group(cols_list, bank, bk):
                        for j, c0 in enumerate(cols_list):
                            for kc in range(8):
                                mm(bank[0:64, j * 128:(j + 1) * 128], w_in[:, kc, c0:c0 + 64], hT[:, kc, :],
                                   kc == 0, kc == 7, [hTk], [bk], sig=(kc == 7 and j == len(cols_list) - 1))

                    def tm_group(c0, n, bank, bk, off=0, last=True):
                        for kc in range(8):
                            mm(bank[:, off:off + n], hT[:, kc, :], w_in[:, kc, c0:c0 + n], kc == 0, kc == 7,
                               [hTk], [bk], sig=(kc == 7 and last))

                    if main:
                        qa, qak = qTa.next()
                        for half in range(2):
                            bank, bk = ps.next()
                            fm_group([h * 64 for h in range(half * 4, half * 4 + 4)], bank, bk)
                            S.op("scalar", lambda e, bank=bank, half=half: e.mul(
                                qa[:, half * 4:half * 4 + 4, :].rearrange("p h n -> p (h n)"), bank[0:64, :], 0.125),
                                [bk], [qak])
                        gq, gqk = gqr.next()
                        bank, bk = ps.next()
                        fm_group([768 + h * 64 for h in range(4)], bank, bk)
                        S.op("scalar", lambda e, bank=bank: e.mul(gq[:].rearrange("p h n -> p (h n)"), bank[0:64, :], 0.125),
                             [bk], [gqk])
                        gk, gkk = gkr.next()
                        bank, bk = ps.next()
                        fm_group([1024 + h * 64 for h in range(4)], bank, bk)
                        cp("vector", gk[:].rearrange("p h n -> p (h n)"), bank[0:64, :], [bk], [gkk])
                    KD = os.environ.get("KDBG", "")
                    if KD == "tm":
                        pass
                    elif main or last_pre:
                        bank, bk = ps.next()
                        fm_group([512, 576], bank, bk)
                        cp("vector", kTs[:, slot % 3, :, :],
                           bank[0:64, 0:256].rearrange("p (h n) -> p h n", h=2), [bk], ["kT%d" % (slot % 3)])
                    if KD == "fm":
                        return
                    bank, bk = ps.next()
                    if (main or last_pre) and KD != "tm1b":
                        tm_group(640, 128, bank, bk, off=0, last=False)
                    tm_group(1024, 256, bank, bk, off=128)
                    if (main or last_pre) and KD != "tm1b":
                        cp("vector" if KD == "tm1c" else "scalar", vs[:, slot % 3, :], bank[:, 0:128], [bk], ["v%d" % (slot % 3)])
                    ktok, ktk = ktokr.next()
                    cp("vector", ktok[:, :], bank[:, 128:384], [bk], [ktk])
                    if KD in ("tm1", "tm1b", "tm1c"):
                        return
                    bank, bk = ps.next()
                    tm_group(1280, 512, bank, bk)
                    vtok, vtk = vtokr.next()
                    if pre:
                        ts("vector", vtok[:, :], bank[:, :], consts[:, C_PFLAG + t:C_PFLAG + t + 1], None, ALU.mult, None,
                           [bk], [vtk])
                    else:
                        cp("scalar", vtok[:, :], bank[:, :], [bk], [vtk])
                    if main:
                        bank, bk = ps.next()
                        tm_group(1792, 512, bank, bk)
                        sg_t, sgk = sgg.next()
                        act(sg_t[:, :], bank[:, :], AF.Silu, [bk], [sgk])
                        tt("gpsimd", sg_t[:, :], sg_t[:, :], gnorm, ALU.mult, [sgk], [sgk])

                    if sub <= 2:
                        return
                    yield
                    l, lk = gla_gates(hT, hTk, None)
                    if sub <= 3:
                        return
                    rvb, rvk = ps.next()
                    mm(rvb[:, 0:256], Urev, l[:, :], True, True, [lk], [rvk])
                    E4, E4k = E4r.next()
                    act(E4[:, :], rvb[:, 0:256], AF.Exp, [rvk], [E4k])
                    kpp, kppk = Kpp.next()
                    tt("vector", kpp[:, :], ktok[:, :], E4[:, :], ALU.mult, [ktk, E4k], [kppk])
                    bTb, bTbk = ps.next()
                    if main:
                        for hd in range(4):
                            mm(bTb[0:64, hd * 128:(hd + 1) * 128], l[:, hd * 64:(hd + 1) * 64], Uincl, True, True,
                               [lk], [bTbk], sig=(hd == 3))
                        bT, bTk = bTr.next()
                        cp("vector", bT[:].rearrange("p h n -> p (h n)"), bTb[0:64, :], [bTbk], [bTk])
                        nb, nbk = nbm.next()
                        ts("vector", nb[:], bT[:, :, 31:128:64], -1.0, None, ALU.mult, None, [bTk], [nbk])
                        E1, E1k = E1r.next()
                        for hd in range(4):
                            for c in range(2):
                                act(E1[:, hd, c * 64:(c + 1) * 64], bT[:, hd, c * 64:(c + 1) * 64], AF.Exp,
                                    [bTk, nbk], [E1k], bias=nb[:, hd, c:c + 1])
                        E2, E2k = E2r.next()
                        S.op("vector", lambda e, E2=E2, E1=E1: e.reciprocal(E2[:], E1[:]), [E1k], [E2k])
                        E3, E3k = E3r.next()
                        act(E3[:], bT[:], AF.Exp, [bTk], [E3k])
                        qp, qpk = QpT.next()
                        tt("vector", qp[:], gq[:], E1[:], ALU.mult, [gqk, E1k], [qpk])
                        kp, kpk = KpT.next()
                        tt("gpsimd", kp[:], gk[:], E2[:], ALU.mult, [gkk, E2k], [kpk])
                        tt("gpsimd", Qc0[:, :, 0:64], gq[:, :, 0:64], E3[:, :, 0:64], ALU.mult, [gqk, E3k], ["Qc0"])
                        tt("gpsimd", Qc1[:, :, 64:128], gq[:, :, 64:128], E3[:, :, 64:128], ALU.mult, [gqk, E3k], ["Qc1"])
                        dec = lambda hd, c: E3[:, hd, c * 64 + 63:c * 64 + 64]
                        decb = lambda c, E3=E3: E3[:, :, c * 64 + 63:c * 64 + 64].to_broadcast([64, 4, 128])
                        deck = E3k
                    else:
                        for hd in range(4):
                            mm(bTb[0:64, hd * 2:hd * 2 + 2], l[:, hd * 64:(hd + 1) * 64],
                               consts[:, C_UINCL + 63:C_UINCL + 128:64], True, True, [lk], [bTbk], sig=(hd == 3))
                        dc, dck = decr.next()
                        act(dc[:].rearrange("p h c -> p (h c)"), bTb[0:64, 0:8], AF.Exp, [bTbk], [dck])
                        dec = lambda hd, c: dc[:, hd, c:c + 1]
                        decb = lambda c, dc=dc: dc[:, :, c:c + 1].to_broadcast([64, 4, 128])
                        deck = dck

                    if sub <= 4:
                        return
                    cur = state["cur"]
                    S0f, S0b, S1f, S1b = Sf[cur], Sb[cur], Sf[1 - cur], Sb[1 - cur]
                    k0, k1 = "S%d" % cur, "S%d" % (1 - cur)
                    if main:
                        atb, atk = ps.next()
                        for hd in range(4):
                            mm(atb[:, hd * 128:(hd + 1) * 128], kp[:, hd, :], qp[:, hd, :], True, True,
                               [kpk, qpk], [atk], sig=(hd == 3))
                        AT, ATk = ATr.next()
                        tt("vector", AT[:].rearrange("p h n -> p (h n)"), atb[:, :], tri4, ALU.mult, [atk], [ATk])
                    kvb, kvk = ps.next()
                    for hd in range(4):
                        mm(kvb[0:64, hd * 128:(hd + 1) * 128], kpp[0:64, hd * 64:(hd + 1) * 64],
                           vtok[0:64, hd * 128:(hd + 1) * 128], True, True, [kppk, vtk], [kvk], sig=(hd == 3))
                    S.op("vector", lambda e, S1f=S1f, S0f=S0f, d_=decb(0): e.tensor_tensor(S1f[:], S0f[:], d_, ALU.mult),
                         [k0 + "f", deck], [k1 + "f"])
                    tt("vector", S1f[:].rearrange("p h n -> p (h n)"), S1f[:].rearrange("p h n -> p (h n)"), kvb[0:64, :],
                       ALU.add, [k1 + "f", kvk], [k1 + "f"])
                    cp("scalar", S1b[:], S1f[:], [k1 + "f"], [k1 + "b"])
                    kvb2, kvk2 = ps.next()
                    for hd in range(4):
                        mm(kvb2[0:64, hd * 128:(hd + 1) * 128], kpp[64:128, hd * 64:(hd + 1) * 64],
                           vtok[64:128, hd * 128:(hd + 1) * 128], True, True, [kppk, vtk], [kvk2], sig=(hd == 3))
                    if main:
                        ob, obk = ps.next()
                        for hd in range(4):
                            o_ap = ob[:, hd * 128:(hd + 1) * 128]
                            mm(o_ap, AT[:, hd, :], vtok[:, hd * 128:(hd + 1) * 128], True, False, [ATk, vtk], [obk], sig=False)
                            mm(o_ap, Qc0[:, hd, :], S0b[:, hd, :], False, False, ["Qc0", k0 + "b"], [obk], sig=False)
                            mm(o_ap, Qc1[:, hd, :], S1b[:, hd, :], False, True, ["Qc1", k1 + "b"], [obk], sig=(hd == 3))
                    S.op("vector", lambda e, S1f=S1f, S0f=S0f, d_=decb(1): e.tensor_tensor(S0f[:], S1f[:], d_, ALU.mult),
                         [k1 + "f", deck], [k0 + "f"])
                    tt("vector", S0f[:].rearrange("p h n -> p (h n)"), S0f[:].rearrange("p h n -> p (h n)"), kvb2[0:64, :],
                       ALU.add, [k0 + "f", kvk2], [k0 + "f"])
                    cp("scalar", S0b[:], S0f[:], [k0 + "f"], [k0 + "b"])
                    if pre:
                        return

                    mix, mixk = mixr.next()
                    sq, sqk = osq.next()
                    sm4, sm4k = smr.next()
                    for hd in range(4):
                        act(sq[:, hd * 128:(hd + 1) * 128], ob[:, hd * 128:(hd + 1) * 128], AF.Square, [obk], [sqk, sm4k],
                            accum=sm4[:, hd:hd + 1])
                    rstd_from_ss(sm4, sm4k, 0, 4, 128.0, cnt=4)
                    for hd in range(4):
                        stt(mix[:, 512 + hd * 128:512 + (hd + 1) * 128], ob[:, hd * 128:(hd + 1) * 128], sm4[:, 4 + hd:5 + hd],
                            sg_t[:, hd * 128:(hd + 1) * 128], ALU.mult, ALU.mult, [obk, sm4k, sgk], [mixk])

                    sc, sck = scr.next()
                    for pair in range(4):
                        bank, bk = ps.next()
                        for j in range(2):
                            h = pair * 2 + j
                            for c in range(2):
                                mm(bank[:, j * 256 + c * 128:j * 256 + (c + 1) * 128], qa[:, h, :],
                                   kTs[:, (t + c) % 3, h // 4, :], True, True, [qak, "kT%d" % ((t + c) % 3)], [bk],
                                   sig=(j == 1 and c == 1))
                        tt("vector", sc[:, pair * 2:pair * 2 + 2, :].rearrange("p h n -> p (h n)"), bank[:, :],
                           maskF if t == 0 else mask2, ALU.add, [bk], [sck])
                    sm2, sm2k = smr.next()
                    S.op("vector", lambda e: e.tensor_reduce(sm2[:, 0:8], sc[:], AX.X, ALU.max), [sck], [sm2k])
                    tt("vector", sm2[:, 0:8], sm2[:, 0:8], sinks, ALU.max, [sm2k], [sm2k])
                    ts("vector", sm2[:, 0:8], sm2[:, 0:8], -1.0, None, ALU.mult, None, [sm2k], [sm2k])
                    sm3, sm3k = smr.next()
                    pb, pbk = pbr.next()
                    for h in range(8):
                        act(pb[:, h, :], sc[:, h, :], AF.Exp, [sck, sm2k], [pbk, sm3k], bias=sm2[:, h:h + 1],
                            accum=sm3[:, h:h + 1])
                    tt("vector", sm2[:, 8:16], sinks, sm2[:, 0:8], ALU.add, [sm2k], [sm2k])
                    act(sm2[:, 8:16], sm2[:, 8:16], AF.Exp, [sm2k], [sm2k])
                    tt("vector", sm3[:, 0:8], sm3[:, 0:8], sm2[:, 8:16], ALU.add, [sm2k, sm3k], [sm3k])
                    S.op("vector", lambda e: e.reciprocal(sm3[:, 8:16], sm3[:, 0:8]), [sm3k], [sm3k])
                    pT, pTk = pTr.next()
                    for q4 in range(4):
                        bank, bk = ps.next()
                        for j in range(2):
                            h = q4 * 2 + j
                            for c in range(2):
                                mm(bank[:, (j * 2 + c) * 128:(j * 2 + c + 1) * 128], pb[:, h, c * 128:(c + 1) * 128],
                                   identb[:], True, True, [pbk], [bk], sig=(j == 1 and c == 1))
                        cp("scalar" if q4 % 2 == 0 else "vector",
                           pT[:, q4 * 4:q4 * 4 + 4, :].rearrange("p a n -> p (a n)"), bank[:, :], [bk], [pTk])
                    ab, abk = ps.next()
                    for h in range(8):
                        kv = h // 4
                        mm(ab[:, h * 64:(h + 1) * 64], pT[:, h * 2, :], vs[:, t % 3, kv * 64:(kv + 1) * 64], True, False,
                           [pTk, "v%d" % (t % 3)], [abk], sig=False)
                        mm(ab[:, h * 64:(h + 1) * 64], pT[:, h * 2 + 1, :], vs[:, (t + 1) % 3, kv * 64:(kv + 1) * 64], False, True,
                           [pTk, "v%d" % ((t + 1) % 3)], [abk], sig=(h == 7))
                    for h in range(8):
                        ts("vector", mix[:, h * 64:(h + 1) * 64], ab[:, h * 64:(h + 1) * 64],
                           sm3[:, 8 + h:9 + h], None, ALU.mult, None, [abk, sm3k], [mixk])

                    mixT, mixTk = mixTr.next()
                    transpose8(mix, mixk, mixT, mixTk, "scalar")
                    ybanks = []
                    for n in range(2):
                        bank, bk = ps.next()
                        for kc in range(8):
                            mm(bank[:, :], mixT[:, kc, :], w_out[:, kc, n * 512:(n + 1) * 512], kc == 0, kc == 7,
                               [mixTk], [bk], sig=(kc == 7))
                        ybanks.append((bank, bk))
                    sm5, sm5k = smr.next()
                    tm, tk = tmpr.next()
                    for n in range(2):
                        act(tm[:, n * 512:(n + 1) * 512], ybanks[n][0][:, :], AF.Square, [ybanks[n][1]], [tk, sm5k],
                            accum=sm5[:, n:n + 1])
                    tt("vector", sm5[:, 2:3], sm5[:, 0:1], sm5[:, 1:2], ALU.add, [sm5k], [sm5k])
                    rstd_from_ss(sm5, sm5k, 2, 3, float(D))
                    for n in range(2):
                        stt(tm[:, n * 512:(n + 1) * 512], ybanks[n][0][:, :], sm5[:, 3:4], mods[:, GP1, n * 512:(n + 1) * 512],
                            ALU.mult, ALU.mult, [ybanks[n][1], sm5k], [tk])
                    x1, x1k = x1r.next()
                    tt("gpsimd", x1[:, :], tm[:, :], xt[:, :], ALU.add, [tk, xk], [x1k])
                    dma("sync", x1_v[t], x1[:, :], [x1k], ["x1d%d" % t])

                    h2, h2k = h2r.next()
                    norm_mod(x1, x1k, GM2, SH2, h2[:, :], h2k, sm5, sm5k, 4)
                    hf, hfk = h2Tf.next()
                    for half in range(2):
                        bank, bk = ps.next()
                        for j in range(4):
                            kc = half * 4 + j
                            mm(bank[:, j * 128:(j + 1) * 128], h2[:, kc * 128:(kc + 1) * 128], identf, True, True,
                               [h2k], [bk], sig=(j == 3))
                        cp("vector" if half == 0 else "scalar", hf[:, half * 4:half * 4 + 4, :].rearrange("p k n -> p (k n)"),
                           bank[:, :], [bk], [hfk])
                    hbf, hbfk = h2br.next()
                    cp("gpsimd", hbf[:, :], h2[:, :], [h2k], [hbfk])
                    dma("sync", h2_v[t], hbf[:, :], [hbfk], ["h2d%d" % t])
                    lb, lbk = ps.next()
                    for kc in range(8):
                        mm(lb[:, 0:NEXP], hf[:, kc, :], w_r[:, kc, :], kc == 0, kc == 7, [hfk], [lbk], sig=(kc == 7))
                    lg, lgk = lgr.next()
                    LG, MK, EX, M8 = 0, 1, 2, 3
                    tt("vector", lg[:, LG, :], lb[:, 0:NEXP], brout, ALU.add, [lbk], [lgk])
                    S.op("vector", lambda e: e.max(lg[:, M8, 0:8], lg[:, LG, :]), [lgk], [lgk])
                    ts("vector", lg[:, MK, :], lg[:, LG, :], lg[:, M8, 3:4], None, ALU.is_ge, None, [lgk], [lgk])
                    ts("vector", lg[:, M8, 8:9], lg[:, M8, 0:1], -1.0, None, ALU.mult, None, [lgk], [lgk])
                    act(lg[:, EX, :], lg[:, LG, :], AF.Exp, [lgk], [lgk], bias=lg[:, M8, 8:9])
                    tt("vector", lg[:, EX, :], lg[:, EX, :], lg[:, MK, :], ALU.mult, [lgk], [lgk])
                    S.op("vector", lambda e: e.tensor_reduce(lg[:, M8, 9:10], lg[:, EX, :], AX.X, ALU.add), [lgk], [lgk])
                    S.op("vector", lambda e: e.reciprocal(lg[:, M8, 10:11], lg[:, M8, 9:10]), [lgk], [lgk])
                    ts("vector", G_all[:, t, :], lg[:, EX, :], lg[:, M8, 10:11], None, ALU.mult, None, [lgk], ["G%d" % t])
                    mkb, mkbk = mkbr.next()
                    cp("vector", mkb[:, :], lg[:, MK, :], [lgk], [mkbk])
                    rb, rbk = ps.next()
                    mm(rb[:, 0:NEXP], lstrb[:], mkb[:, :], True, True, [mkbk], [rbk], sig=False)
                    mm(rb[:, NEXP:2 * NEXP], onesb[:], mkb[:, :], True, True, [mkbk], [rbk])
                    stt(Qm_all[:, t, :], rb[:, 0:NEXP], 1.0, Orun[:, :], ALU.add, ALU.add, [rbk, "Orun"], ["Qm%d" % t])
                    tt("vector", Qm_all[:, t, :], Qm_all[:, t, :], lg[:, MK, :], ALU.mult, ["Qm%d" % t, lgk], ["Qm%d" % t])
                    tt("vector", Orun[:, :], Orun[:, :], rb[:, NEXP:2 * NEXP], ALU.add, ["Orun", rbk], ["Orun"])

                def drain(g):
                    for _ in g:
                        pass

                from collections import deque
                pend = deque()
                for (ti, pre_) in [(p, True) for p in range(NP)] + [(t, False) for t in range(NT)]:
                    if (not pre_) and ti == 0 and stage <= 2:
                        break
                    g = tile_body(ti, pre_)
                    try:
                        next(g)
                        pend.append(g)
                    except StopIteration:
                        pass
                    depth = 2 if pre_ else 1
                    while len(pend) > depth:
                        drain(pend.popleft())
                while pend:
                    drain(pend.popleft())
                if stage <= 2:
                    S.enabled = False
                if debug:
                    dma("sync", G_d.ap(), G_all[:].rearrange("p t e -> p (t e)"), ["G%d" % t for t in range(NT)], ["Gd"])
                S.barrier()
                if stage <= 3:
                    S.enabled = False

            h2_v = h2_d.ap().rearrange("(t p) n -> t p n", p=128)
            x1_v = x1_d.ap().rearrange("(t p) n -> t p n", p=128)
            out_v = out_d.ap().rearrange("(t p) n -> t p n", p=128)
            gk_all = sb(P, "gk_all", [128, NT, 4])
            idx4_all = sb(P, "idx4_all", [128, NT, 4], I32)
            flags_i = sb(P, "flags_i", [128, NEXP * JM], I32)
            idxb_i = sb(P, "idxb_i", [128, NEXP * JM], I32)
            with ExitStack() as st:
                flf = sb(st, "flf", [128, NEXP, JM])
                nbt = sb(st, "nbt", [128, NEXP])
                cA = sb(st, "cA", [128, NEXP])
                cB = sb(st, "cB", [128, NEXP])
                pst = sb(st, "pst", [128, NEXP])
                idf = sb(st, "idf", [128, NEXP, JM])
                vr = mkring(st, "vr", 2, [128, 3, NEXP])
                m8r = mkring(st, "m8r", 2, [128, 16])
                h2l = mkring(st, "h2l", 2, [128, D], BF16)
                TH3 = consts[:, C_TH:C_TH + NEXP * 32].rearrange("p (e j) -> p e j", e=NEXP)[:, :, 0:JM]
                S.op("vector", lambda e: e.tensor_tensor(flf[:], Orun[:].unsqueeze(2).to_broadcast([128, NEXP, JM]), TH3,
                                                         ALU.is_gt), ["Orun"], ["flf"])
                S.op("vector", lambda e: e.tensor_reduce(nbt[:], flf[:], AX.X, ALU.add), ["flf"], ["nbt"])
                ts("vector", cA[:], nbt[:], 128.0, None, ALU.mult, None, ["nbt"], ["cA"])
                cp("vector", pst[:], cA[:], ["cA"], ["pst"])
                ca, cb, cak, cbk = cA, cB, "cA", "cB"
                for sft in (1, 2, 4, 8, 16):
                    cp("vector", cb[:, 0:sft], ca[:, 0:sft], [cak], [cbk])
                    tt("vector", cb[:, sft:NEXP], ca[:, sft:NEXP], ca[:, 0:NEXP - sft], ALU.add, [cak], [cbk])
                    ca, cb, cak, cbk = cb, ca, cbk, cak
                tt("vector", pst[:], ca[:], pst[:], ALU.subtract, [cak, "pst"], ["pst"])
                cp("vector", flags_i[:], flf[:].rearrange("p e j -> p (e j)"), ["flf"], ["flags"])
                S.op("vector", lambda e: e.tensor_tensor(idf[:], TH3, pst[:].unsqueeze(2).to_broadcast([128, NEXP, JM]),
                                                         ALU.add), ["pst"], ["idf"])
                ts("vector", idf[:], idf[:], consts[:, C_IOTA:C_IOTA + 1], -65536.0, ALU.add, ALU.add, ["idf"], ["idf"])
                tt("vector", idf[:], idf[:], flf[:], ALU.mult, ["idf", "flf"], ["idf"])
                ts("vector", idf[:], idf[:], 65536.0, None, ALU.add, None, ["idf"], ["idf"])
                cp("vector", idxb_i[:], idf[:].rearrange("p e j -> p (e j)"), ["idf"], ["idxb"])
                for t in range(NT):
                    v, vk = vr.next()
                    ts("vector", v[:, 0, :], Qm_all[:, t, :], 0.0, None, ALU.is_gt, None, [], [vk])
                    tt("vector", v[:, 1, :], Qm_all[:, t, :], pst[:], ALU.add, ["pst"], [vk])
                    tt("vector", v[:, 1, :], v[:, 1, :], v[:, 0, :], ALU.mult, [vk], [vk])
                    m8, m8k = m8r.next()
                    S.op("vector", lambda e, m8=m8, v=v: e.max(m8[:, 0:8], v[:, 1, :]), [vk], [m8k])
                    ts("vector", m8[:, 8:12], m8[:, 0:4], -1.0, None, ALU.add, None, [m8k], [m8k])
                    cp("vector", idx4_all[:, t, :], m8[:, 8:12], [m8k], ["idx4_%d" % t])
                    for k in range(4):
                        ts("vector", v[:, 2, :], v[:, 1, :], m8[:, k:k + 1], None, ALU.is_equal, None, [vk, m8k], [vk])
                        stt(v[:, 0, :], v[:, 2, :], 1.0, G_all[:, t, :], ALU.mult, ALU.mult, [vk], [vk, "gk%d" % t],
                            accum=gk_all[:, t, k:k + 1])
                    h2t, h2tk = h2l.next()
                    dma("sync", h2t[:, :], h2_v[t], [], [h2tk])
                    for k in range(4):
                        S.dma("gpsimd", lambda en, t=t, k=k, h2t=h2t: en.indirect_dma_start(
                            out=xs_d.ap(), out_offset=bass.IndirectOffsetOnAxis(ap=idx4_all[:, t, k:k + 1], axis=0),
                            in_=h2t[:, :], in_offset=None), [h2tk, "idx4_%d" % t], ["xsd"])
                S.barrier()

            with ExitStack() as st:
                w1b = mkring(st, "w1b", 2, [128, 8, 2, D], BF16)
                w2b = mkring(st, "w2b", 2, [128, 8, D], BF16)
                w1s = mkring(st, "w1s", 5, [128, 2, 256])
                w2s = mkring(st, "w2s", 4, [128, 512])
                b1s = mkring(st, "b1s", 1, [1, D])
                b1b = mkring(st, "b1b", 2, [1, 2 * D], BF16)
                xsr = mkring(st, "xsr", 2, [128, D], BF16)
                xsTr = mkring(st, "xsTr", 2, [128, 8, 128], BF16)
                aTr = mkring(st, "aTr", 2, [128, 8, 128], BF16)
                atokr = mkring(st, "atokr", 1, [128, D], BF16)
                xgr = mkring(st, "xgr", 1, [128, 512])
                sgr = mkring(st, "sgr", 1, [128, 512])
                xlr = mkring(st, "xlr", 1, [128, 512])
                ysr = mkring(st, "ysr", 1, [128, D])
                for i_ in range(len(ysr.t)):
                    S.op("gpsimd", lambda e, i_=i_: e.memset(ysr.t[i_][:], 0.0), [], ["ysr%d" % i_])
                w1_v = w1_d.ap().rearrange("e (k p) n -> e p k n", p=128)
                w2_v = w2_d.ap().rearrange("e (k p) n -> e k p n", p=128)

                def gather(e, j):
                    col = e * JM + j
                    xs, xsk = xsr.next()
                    S.dma("gpsimd", lambda en, xs=xs, col=col: en.indirect_dma_start(
                        out=xs[:, :], out_offset=None, in_=xs_d.ap(),
                        in_offset=bass.IndirectOffsetOnAxis(ap=idxb_i[:, col:col + 1], axis=0),
                        bounds_check=breg, oob_is_err=False), [], [xsk])
                    return xs, xsk

                def block(e, j, w1t, w1k, w2t, w2k, bb, bbk, xs, xsk):
                    col = e * JM + j
                    xT, xTk = xsTr.next()
                    transpose8(xs, xsk, xT, xTk, None)
                    aT, aTk = aTr.next()
                    atok, atokk = atokr.next()
                    for fh in range(2):
                        banks = []
                        for two in range(2):
                            bank, bk = ps.next()
                            for kc in range(8):
                                mm(bank[:, :], xT[:, kc, :], w1t[:, kc, two, fh * 512:(fh + 1) * 512], kc == 0, False,
                                   [w1k, xTk], [bk], sig=False)
                            mm(bank[:, :], onesb[0:1, :], bb[0:1, two * D + fh * 512:two * D + (fh + 1) * 512], False, True,
                               [bbk], [bk])
                            banks.append((bank, bk))
                        (bg, bgk), (bl, blk) = banks
                        xg, xgk = xgr.next()
                        ts("vector", xg[:, :], bg[:, :], 7.0, None, ALU.min, None, [bgk], [xgk])
                        sgt, sgk2 = sgr.next()
                        act(sgt[:, :], xg[:, :], AF.Sigmoid, [xgk], [sgk2], scale=1.702)
                        xl, xlk = xlr.next()
                        ts("vector", xl[:, :], bl[:, :], 7.0, -7.0, ALU.min, ALU.max, [blk], [xlk])
                        tt("gpsimd", xg[:, :], xg[:, :], sgt[:, :], ALU.mult, [xgk, sgk2], [xgk])
                        stt(atok[:, fh * 512:(fh + 1) * 512], xl[:, :], 1.0, xg[:, :], ALU.add, ALU.mult, [xlk, xgk], [atokk])
                    transpose8(atok, atokk, aT, aTk, None)
                    ysb, ysk = ysr.next()
                    ybs = [ps.next(), ps.next()]
                    for n in range(2):
                        yb, ybk = ybs[n]
                        for fc in range(8):
                            mm(yb[:, :], aT[:, fc, :], w2t[:, fc, n * 512:(n + 1) * 512], fc == 0, fc == 7,
                               [aTk, w2k], [ybk], sig=(fc == 7))
                    for n in range(2):
                        yb, ybk = ybs[n]
                        cp("scalar" if n == 0 else "vector", ysb[:, n * 512:(n + 1) * 512], yb[:, :], [ybk], [ysk])
                    S.dma("gpsimd", lambda en, ysb=ysb, col=col: en.indirect_dma_start(
                        out=ys_d.ap(), out_offset=bass.IndirectOffsetOnAxis(ap=idxb_i[:, col:col + 1], axis=0),
                        in_=ysb[:, :], in_offset=None, bounds_check=breg, oob_is_err=False), [ysk], ["ysd"])

                def load_weights(e):
                    w1t, w1k = w1b.next()
                    w2t, w2k = w2b.next()
                    bb, bbk = b1b.next()
                    ci = 0
                    for j in range(8):
                        for kh in range(4):
                            ws, wsk = w1s.next()
                            dma("sync", ws[:], w1_v[e][:, kh * 2:(kh + 1) * 2, j * 256:(j + 1) * 256], [], [wsk])
                            cp("scalar" if ci % 2 == 0 else "vector", w1t[:, kh * 2:(kh + 1) * 2, :, j * 128:(j + 1) * 128],
                               ws[:].rearrange("p k (m two) -> p k two m", two=2), [wsk], [w1k])
                            ci += 1
                        for nh in range(2):
                            s2, s2k = w2s.next()
                            dma("sync", s2[:], w2_v[e][j][:, nh * 512:(nh + 1) * 512], [], [s2k])
                            cp("scalar" if ci % 2 == 0 else "vector", w2t[:, j, nh * 512:(nh + 1) * 512], s2[:], [s2k], [w2k])
                            ci += 1
                    for bh in range(2):
                        bs, bsk = b1s.next()
                        dma("sync", bs[:], b1r_d.ap()[e:e + 1, bh * D:(bh + 1) * D], [], [bsk])
                        cp("vector", bb[:, bh * D:(bh + 1) * D], bs[:], [bsk], [bbk])
                    return w1t, w1k, w2t, w2k, bb, bbk

                wnext = load_weights(0)
                for e in range(NE):
                    w1t, w1k, w2t, w2k, bb, bbk = wnext
                    if e + 1 < NE:
                        wnext = load_weights(e + 1)
                    S.chain_begin()
                    nxt = gather(e, 0)
                    for j in range(JM):
                        S.level_push(flags_i[0:1, e * JM + j:e * JM + j + 1])
                        cur = nxt
                        if j + 1 < JM:
                            nxt = gather(e, j + 1)
                        block(e, j, w1t, w1k, w2t, w2k, bb, bbk, cur[0], cur[1])
                    S.chain_end()
                S.barrier()

            with ExitStack() as st:
                b2_sb = sb(st, "b2_sb", [NEXP, D])
                dma("sync", b2_sb[:], b2_d.ap(), [], ["b2"])
                yr = mkring(st, "yr", 4, [128, D])
                accr = mkring(st, "accr", 2, [128, D])
                GTr = mkring(st, "GTr", 2, [NEXP, 128])
                x1l = mkring(st, "x1l", 2, [128, D])
                outr = mkring(st, "outr", 2, [128, D])
                junk2 = mkring(st, "junk2", 1, [128, D], BF16)
                smq = mkring(st, "smq", 2, [128, 8])
                for t in range(NT):
                    ys4 = []
                    for k in range(4):
                        y_, yk_ = yr.next()
                        S.dma("gpsimd", lambda en, y_=y_, t=t, k=k: en.indirect_dma_start(
                            out=y_[:, :], out_offset=None, in_=ys_d.ap(),
                            in_offset=bass.IndirectOffsetOnAxis(ap=idx4_all[:, t, k:k + 1], axis=0)), [], [yk_])
                        ys4.append((y_, yk_))
                    ac, ack = accr.next()
                    ts("vector", ac[:, :], ys4[0][0][:, :], gk_all[:, t, 0:1], None, ALU.mult, None, [ys4[0][1]], [ack])
                    for k in range(1, 4):
                        stt(ac[:, :], ys4[k][0][:, :], gk_all[:, t, k:k + 1], ac[:, :], ALU.mult, ALU.add,
                            [ys4[k][1], ack], [ack])
                    gb, gbk = ps.next()
                    mm(gb[0:NEXP, 0:128], G_all[:, t, :], identf, True, True, [], [gbk])
                    gt_, gtk = GTr.next()
                    cp("vector", gt_[:, :], gb[0:NEXP, 0:128], [gbk], [gtk])
                    x1t, x1tk = x1l.next()
                    dma("sync", x1t[:, :], x1_v[t], [], [x1tk])
                    sm, smk = smq.next()
                    jk_t, jk = junk2.next()
                    for n in range(2):
                        yb, ybk = ps.next()
                        mm(yb[:, :], gt_[:, :], b2_sb[:, n * 512:(n + 1) * 512], True, True, [gtk, "b2"], [ybk])
                        a_ap = ac[:, n * 512:(n + 1) * 512]
                        tt("vector", a_ap, a_ap, yb[:, :], ALU.add, [ack, ybk], [ack])
                        act(jk_t[:, n * 512:(n + 1) * 512], a_ap, AF.Square, [ack], [jk, smk], accum=sm[:, n:n + 1])
                    tt("vector", sm[:, 2:3], sm[:, 0:1], sm[:, 1:2], ALU.add, [smk], [smk])
                    o_ = sm[:, 3:4]
                    ts("vector", o_, sm[:, 2:3], 1.0 / D, EPS, ALU.mult, ALU.add, [smk], [smk])
                    act(o_, o_, AF.Sqrt, [smk], [smk])
                    S.op("vector", lambda e, o_=o_: e.reciprocal(o_, o_), [smk], [smk])
                    ot, otk = outr.next()
                    stt(ot[:, :], ac[:, :], o_, mods[:, GP2, :], ALU.mult, ALU.mult, [ack, smk], [otk])
                    tt("gpsimd", ot[:, :], ot[:, :], x1t[:, :], ALU.add, [otk, x1tk], [otk])
                    dma("sync", out_v[t], ot[:, :], [otk], ["outd%d" % t])
                S.barrier()
        except _Stop:
            S.barrier()

        with nc.Block() as block:
            S.emit(block, gscr)
    return nc, S


def host_inputs(inp, NT=32, NP=96, segs=None):
    x = np.asarray(inp["x"], np.float32)
    TOK = NT * 128
    f = lambda k: np.ascontiguousarray(np.asarray(inp[k], np.float32)[0])
    w_ada, b_ada = f("w_ada"), f("b_ada")
    gvec = np.concatenate([f("g_pre_mix"), f("g_post_mix"), f("g_pre_ffn"), f("g_post_ffn")])
    gvec_bc = np.ascontiguousarray(np.broadcast_to(gvec[None, :], (128, 4 * D)))
    bada_bc = np.ascontiguousarray(np.broadcast_to(b_ada[None, :], (128, 6 * D)))
    wup_aug = np.concatenate([f("w_gla_gate_up"), f("b_gla_gate")[None, :]], axis=0)
    b1 = f("b_mlp1")
    b1r = np.ascontiguousarray(b1.reshape(NEXP, D, 2).transpose(0, 2, 1).reshape(NEXP, 2 * D))
    qi = np.arange(128)[:, None]
    kj = np.arange(256)[None, :]
    valid = ((kj < 128) & (kj > qi)) | ((kj >= 128) & (kj - 128 <= qi))
    m1 = np.where(valid, 0.0, NEG).astype(np.float32)
    validF = (kj >= 128) & (kj - 128 <= qi)
    mF = np.where(validF, 0.0, NEG).astype(np.float32)
    j = np.arange(128)[:, None]
    i = np.arange(128)[None, :]
    same = (j // 64) == (i // 64)
    tri = (same & (j <= i)).astype(np.float32)
    uincl = tri * (-1.0 / 16.0)
    urev = (same & (j > i)).astype(np.float32) * (-1.0 / 16.0)
    cbase = np.zeros((128, C_TOT), np.float32)
    cbase[:, C_MASK2:C_MASK2 + 512] = np.tile(m1, (1, 2))
    cbase[:, C_TRI4:C_TRI4 + 512] = np.tile(tri, (1, 4))
    cbase[:, C_UINCL:C_UINCL + 128] = uincl
    cbase[:, C_UREV:C_UREV + 128] = urev
    cbase[:, C_IDENT:C_IDENT + 128] = np.eye(128, dtype=np.float32)
    cbase[:, C_GNORM:C_GNORM + 512] = np.tile(f("g_gla_norm")[None, :], (128, 4))
    cbase[:, C_SINK:C_SINK + 8] = f("sinks")[None, :]
    cbase[:, C_BROUT:C_BROUT + NEXP] = f("b_router")[None, :]
    cbase[:, C_ONES:C_ONES + 128] = 1.0
    cbase[:, C_LSTR:C_LSTR + 128] = (j < i).astype(np.float32)
    cbase[:, C_TH:C_TH + NEXP * 32] = np.tile(128.0 * np.arange(32, dtype=np.float32), NEXP)[None, :]
    cbase[:, C_IOTA] = np.arange(128, dtype=np.float32)
    shared = {"w_ada": w_ada, "b_ada_bc": bada_bc, "gvec_bc": gvec_bc, "w_in": f("w_in"), "wup_aug": wup_aug,
              "w_out": f("w_out"), "w_router": f("w_router"), "w_mlp1": f("w_mlp1"), "w_mlp2": f("w_mlp2"),
              "b1r": b1r, "b_mlp2": f("b_mlp2")}
    maps = []
    nseg = SEQ // SEG
    if segs is None:
        segs = [(core // nseg, (core % nseg) * SEG) for core in range(NCORE)]
    for (b, s0) in segs:
        cm = cbase.copy()
        cm[:, C_MASKF:C_MASKF + 512] = np.tile(mF if s0 == 0 else m1, (1, 2))
        xpre = np.zeros((max(NP, 1) * 128, D), np.float32)
        p0 = s0 - NP * 128
        for p in range(NP):
            a = p0 + p * 128
            if a >= 0:
                xpre[p * 128:(p + 1) * 128] = x[b, a:a + 128]
                cm[:, C_PFLAG + p] = 1.0
        mp = dict(shared)
        mp["x"] = np.ascontiguousarray(x[b, s0:s0 + TOK])
        mp["xpre"] = xpre
        mp["cT"] = np.ascontiguousarray(np.asarray(inp["c"], np.float32)[b].reshape(8, 128).T)
        mp["consts"] = cm
        maps.append(mp)
    return maps


_CACHE = {}


def kernel(**inputs):
    if "nc" not in _CACHE:
        _CACHE["nc"] = build_nc()[0]
    nc = _CACHE["nc"]
    maps = host_inputs(inputs)
    res = run_bass_kernel_spmd(nc, maps, core_ids=list(range(NCORE)))
    out = np.empty((2, SEQ, D), np.float32)
    nseg = SEQ // SEG
    for core in range(NCORE):
        b, seg = core // nseg, core % nseg
        out[b, seg * SEG:(seg + 1) * SEG] = np.asarray(res.results[core]["out"], np.float32)
    return out
```

```python
import os
import numpy as np
from contextlib import ExitStack
import concourse.bass as bass
import concourse.mybir as mybir
from concourse.bass_utils import run_bass_kernel_spmd

F32 = mybir.dt.float32
BF16 = mybir.dt.bfloat16
I32 = mybir.dt.int32
ALU = mybir.AluOpType
AF = mybir.ActivationFunctionType
AX = mybir.AxisListType

D = 1024
SEQ = 16384
NCORE = 8
SEG = 4096
INW = 2320
NEXP = 32
EPS = 1e-6
NEG = -30000.0

ENGS = ("sync", "scalar", "vector", "gpsimd", "tensor")
CENG = ("scalar", "vector", "gpsimd", "tensor")
DENG = ("sync", "gpsimd", "scalar")
NDSEM = 8

C_MASK2 = 0
C_MASKF = 512
C_TRI4 = 1024
C_UINCL = 1536
C_UREV = 1664
C_IDENT = 1792
C_GNORM = 1920
C_SINK = 2432
C_BROUT = 2440
C_PFLAG = 2472
C_ONES = 2568
C_LSTR = 2696
C_TH = 2824
C_IOTA = 3848
C_TOT = 3856


class Sched:
    def __init__(self, nc, stack):
        self.nc = nc
        self.q = {e: [] for e in ENGS}
        self.esem = {e: stack.enter_context(nc.semaphore("es_" + e)) for e in CENG}
        self.ecnt = {e: 0 for e in CENG}
        self.dsem = {e: [stack.enter_context(nc.semaphore("ds_%s%d" % (e, i))) for i in range(NDSEM)]
                     for e in DENG}
        self.dcnt = {e: 0 for e in DENG}
        self.waited = {e: {} for e in ENGS}
        self.lastw = {}
        self.readers = {}
        self.nins = 0
        self.enabled = True
        self.cur_guard = None
        self.gid = 0
        self.chain_flags = {}
        self.chain_base = {}
        self.allwaits = {}

    def _deps(self, reads, writes):
        toks = []
        for k in reads:
            if k in self.lastw:
                toks.append(self.lastw[k])
        for k in writes:
            if k in self.lastw:
                toks.append(self.lastw[k])
            toks.extend(self.readers.get(k, ()))
        return toks

    def _need(self, eng, toks):
        out = {}
        for (sid, sem, val, owner) in toks:
            if owner == eng and eng == "tensor":
                continue
            if self.waited[eng].get(sid, 0) >= val:
                continue
            if sid not in out or out[sid][1] < val:
                out[sid] = (sem, val)
        for sid, (sem, val) in out.items():
            self.waited[eng][sid] = val
        if self.cur_guard is not None and self.cur_guard[1] > 0:
            key = self.cur_guard
            d = self.allwaits.setdefault(key, {})
            for sid, (sem, val) in out.items():
                if sid not in d or d[sid][1] < val:
                    d[sid] = (sem, val)
        return list(out.values())

    def _commit(self, tok, reads, writes):
        for k in writes:
            self.lastw[k] = tok
            self.readers[k] = []
        for k in reads:
            if k not in writes:
                self.readers.setdefault(k, []).append(tok)

    def op(self, eng, fn, reads=(), writes=(), sig=True):
        if not self.enabled:
            return
        px = [k for k in reads if k.startswith("ps")]
        if px:
            reads = [k for k in reads if not k.startswith("ps")]
            writes = list(writes) + px
        waits = self._need(eng, self._deps(reads, writes))
        if sig:
            self.ecnt[eng] += 1
            val = self.ecnt[eng]
        else:
            val = self.ecnt[eng] + 1
        tok = ("e_" + eng, self.esem[eng], val, eng)
        self.q[eng].append((waits, fn, (self.esem[eng], 1) if sig else None, self.cur_guard))
        self._commit(tok, reads, writes)
        self.nins += 1 + len(waits)

    def dma(self, eng, fn, reads=(), writes=()):
        if not self.enabled:
            return
        i = self.dcnt[eng]
        self.dcnt[eng] += 1
        sem = self.dsem[eng][i % NDSEM]
        sid = "d_%s%d" % (eng, i % NDSEM)
        val = 16 * (i // NDSEM + 1)
        toks = self._deps(reads, writes)
        if val > 16:
            toks.append((sid, sem, val - 16, "dma"))
        waits = self._need(eng, toks)
        self.q[eng].append((waits, fn, (sem, 16), self.cur_guard))
        self._commit((sid, sem, val, "dma"), reads, writes)
        self.nins += 1 + len(waits)

    def barrier(self):
        if not self.enabled:
            return
        toks = []
        for e in CENG:
            if self.ecnt[e] > 0:
                toks.append(("e_" + e, self.esem[e], self.ecnt[e], "x"))
        for e in DENG:
            n = self.dcnt[e]
            for j in range(min(n, NDSEM)):
                cnt = (n - 1 - j) // NDSEM + 1
                toks.append(("d_%s%d" % (e, j), self.dsem[e][j], 16 * cnt, "dma"))
        for e in ENGS:
            waits = self._need(e, toks)
            if waits:
                self.q[e].append((waits, None, None, None))
        self.lastw = {}
        self.readers = {}

    def chain_begin(self):
        self.gid += 1
        self.cur_guard = (self.gid, 0)
        self.chain_flags[self.gid] = [None]
        self._wsnap = {e: dict(self.waited[e]) for e in ENGS}

    def _counters(self):
        c = {}
        for e in CENG:
            c["e_" + e] = self.ecnt[e]
        for e in DENG:
            n = self.dcnt[e]
            for j in range(min(n, NDSEM)):
                c["d_%s%d" % (e, j)] = 16 * ((n - 1 - j) // NDSEM + 1)
        return c

    def level_push(self, flag_ap):
        cid, d = self.cur_guard
        self.chain_flags[cid].append(flag_ap)
        self.chain_base.setdefault(cid, [None]).append(self._counters())
        self.cur_guard = (cid, d + 1)

    def chain_end(self):
        self.cur_guard = None
        self.waited = self._wsnap

    def emit(self, block, scratch):
        for e in ENGS:
            def body(engine, e=e):
                tot = {}
                entries = self.q[e]
                state = {"reg": None}

                def emit_entry(ent):
                    waits, fn, inc, _ = ent
                    for sem, val in waits:
                        engine.wait_ge(sem, val)
                    if fn is not None:
                        ins = fn(engine)
                        if inc is not None:
                            ins.then_inc(inc[0], inc[1])
                            tot[id(inc[0])] = tot.get(id(inc[0]), 0) + inc[1]

                def depth_of(ent):
                    return 0 if ent[3] is None else ent[3][1]

                def emit_level(i, end, cid, depth):
                    while i < end:
                        d = depth_of(entries[i])
                        if d == depth:
                            emit_entry(entries[i])
                            i += 1
                            continue
                        flag = self.chain_flags[cid][depth + 1]
                        if state["reg"] is None:
                            state["reg"] = engine.alloc_register("gflag_" + e)
                        reg = state["reg"]
                        adds = {}
                        for ent in entries[i:end]:
                            if ent[2] is not None and ent[1] is not None:
                                k = id(ent[2][0])
                                adds[k] = (ent[2][0], adds.get(k, (None, 0))[1] + ent[2][1])
                        before = dict(tot)
                        engine.reg_load(reg, flag)
                        with engine.If_ne(reg, 0):
                            emit_level(i, end, cid, depth + 1)
                        if adds:
                            with engine.Else():
                                base = self.chain_base[cid][depth + 1]
                                maxd = len(self.chain_flags[cid]) - 1
                                ext = {}
                                for dd in range(depth + 1, maxd + 1):
                                    for sid, (sem, val) in self.allwaits.get((cid, dd), {}).items():
                                        v = min(val, base.get(sid, 0))
                                        if v > 0 and (sid not in ext or ext[sid][1] < v):
                                            ext[sid] = (sem, v)
                                for sid, (sem, v) in ext.items():
                                    engine.wait_ge(sem, v)
                                for k, (sem, n) in adds.items():
                                    b = before.get(k, 0)
                                    if b > 0:
                                        engine.wait_ge(sem, b)
                                    if e == "gpsimd" and any(sem is d_ for d_ in self.dsem["gpsimd"]):
                                        engine.dma_start(out=scratch[0:1, 8:9], in_=scratch[0:1, 0:1]).then_inc(sem, n)
                                    else:
                                        engine.sem_inc(sem, n)
                        for k, (sem, n) in adds.items():
                            tot[k] = before.get(k, 0) + n
                        i = end

                i = 0
                while i < len(entries):
                    g = entries[i][3]
                    if g is None:
                        emit_entry(entries[i])
                        i += 1
                        continue
                    cid = g[0]
                    end = i
                    while end < len(entries) and entries[end][3] is not None and entries[end][3][0] == cid:
                        end += 1
                    emit_level(i, end, cid, 0)
                    i = end
            getattr(block, e)(body)


class Ring:
    def __init__(self, name, tiles):
        self.t = tiles
        self.name = name
        self.i = 0

    def next(self):
        k = self.i % len(self.t)
        self.i += 1
        return self.t[k], "%s%d" % (self.name, k)


class _Stop(Exception):
    pass


def build_nc(NT=32, NP=96, NE=32, debug=False, stage=9, sub=99):
    nc = bass.Bass("TRN2", target_bir_lowering=False)
    TOK = NT * 128
    TQ = min(8, NT)
    NQ = NT // TQ
    GT = min(4, TQ)
    NG = TQ // GT

    def din(name, shape, dt=F32):
        return nc.dram_tensor(name, list(shape), dt, kind="ExternalInput")

    x_d = din("x", [TOK, D])
    xp_d = din("xpre", [max(NP, 1) * 128, D])
    cT_d = din("cT", [128, 8])
    wada_d = din("w_ada", [D, 6 * D])
    bada_d = din("b_ada_bc", [128, 6 * D])
    gvec_d = din("gvec_bc", [128, 4 * D])
    consts_d = din("consts", [128, C_TOT])
    win_d = din("w_in", [D, INW])
    wup_d = din("wup_aug", [17, 256])
    wout_d = din("w_out", [D, D])
    wr_d = din("w_router", [D, NEXP])
    w1_d = din("w_mlp1", [NEXP, D, 2 * D])
    w2_d = din("w_mlp2", [NEXP, D, D])
    b1r_d = din("b1r", [NEXP, 2 * D])
    b2_d = din("b_mlp2", [NEXP, D])
    out_d = nc.dram_tensor("out", [TOK, D], F32, kind="ExternalOutput")
    x1_d = nc.dram_tensor("x1_d", [TOK, D], F32, kind="ExternalOutput" if debug else "Internal")
    JM = min(32, NT)
    NSLOT = TOK * 4 + NEXP * 128
    NBLK = NSLOT // 128
    h2_d = nc.dram_tensor("h2_d", [TOK, D], BF16)
    xs_d = nc.dram_tensor("xs_d", [NSLOT, D], BF16)
    ys_d = nc.dram_tensor("ys_d", [NSLOT, D], F32)
    G_d = nc.dram_tensor("G_d", [128, NT * NEXP], F32, kind="ExternalOutput") if debug else None

    stack = ExitStack()
    with stack:
        S = Sched(nc, stack)

        def sb(st, name, shape, dt=F32):
            return st.enter_context(nc.sbuf_tensor("s_" + name, list(shape), dt))

        def mkring(st, name, n, shape, dt=F32):
            return Ring(name, [sb(st, "%s_%d" % (name, i), shape, dt) for i in range(n)])

        def mm(out, lhsT, rhs, start, stop, r, w, sig=True):
            S.op("tensor", lambda e: e.matmul(out, lhsT, rhs, start=start, stop=stop), r, w, sig=sig)

        def tr(out, in_, ident, r, w, sig=True):
            S.op("tensor", lambda e: e.transpose(out, in_, ident), r, w, sig=sig)

        def act(out, in_, func, r, w, bias=None, scale=None, accum=None):
            kw = {}
            if bias is not None:
                kw["bias"] = bias
            if scale is not None:
                kw["scale"] = scale
            if accum is not None:
                kw["accum_out"] = accum
            S.op("scalar", lambda e: e.activation(out, in_, func, **kw), r, w)

        def ts(eng, out, in0, s1, s2, op0, op1, r, w, accum=None):
            if accum is not None:
                S.op(eng, lambda e: e.tensor_scalar(out, in0, s1, s2, op0, op1, accum_out=accum), r, w)
            elif op1 is None:
                S.op(eng, lambda e: e.tensor_scalar(out, in0, s1, None, op0), r, w)
            else:
                S.op(eng, lambda e: e.tensor_scalar(out, in0, s1, s2, op0, op1), r, w)

        def tt(eng, out, in0, in1, op, r, w):
            S.op(eng, lambda e: e.tensor_tensor(out, in0, in1, op), r, w)

        def stt(out, in0, scalar, in1, op0, op1, r, w, accum=None):
            if accum is not None:
                S.op("vector", lambda e: e.scalar_tensor_tensor(out, in0, scalar, in1, op0, op1, accum_out=accum), r, w)
            else:
                S.op("vector", lambda e: e.scalar_tensor_tensor(out, in0, scalar, in1, op0, op1), r, w)

        def cp(eng, out, in_, r, w):
            if eng == "scalar":
                S.op(eng, lambda e: e.copy(out, in_), r, w)
            else:
                S.op(eng, lambda e: e.tensor_copy(out, in_), r, w)

        def dma(eng, out, in_, r, w):
            S.dma(eng, lambda e: e.dma_start(out=out, in_=in_), r, w)

        P = stack
        consts = sb(P, "consts", [128, C_TOT])
        identb = sb(P, "identb", [128, 128], BF16)
        mods = sb(P, "mods", [128, 6, D])
        G_all = sb(P, "G_all", [128, NT, NEXP])
        Qm_all = sb(P, "Qm_all", [128, NT, NEXP])
        Orun = sb(P, "Orun", [128, NEXP])
        onesb = sb(P, "onesb", [128, 128], BF16)
        lstrb = sb(P, "lstrb", [128, 128], BF16)
        GM1, SH1, GP1, GM2, SH2, GP2 = range(6)

        ps = Ring("ps", [stack.enter_context(nc.psum_tensor("ps%d" % i, [128, 512], F32)) for i in range(8)])

        identf = consts[:, C_IDENT:C_IDENT + 128]
        ones_f = consts[:, C_ONES:C_ONES + 128]

        dma("sync", consts[:], consts_d.ap(), [], ["consts"])
        cp("vector", identb[:], identf, ["consts"], ["identb"])
        cp("vector", onesb[:], ones_f, ["consts"], ["onesb"])
        cp("vector", lstrb[:], consts[:, C_LSTR:C_LSTR + 128], ["consts"], ["lstrb"])
        S.op("vector", lambda e: e.memset(Orun[:], 0.0), [], ["Orun"])
        gscr = sb(P, "gscr", [1, 16])
        S.op("gpsimd", lambda e: e.memset(gscr[:], 0.0), [], ["gscr"])
        breg = nc.gpsimd.alloc_register("bndreg")
        S.op("gpsimd", lambda e: e.reg_mov(breg, NSLOT - 1), [], [], sig=False)

        try:
            with ExitStack() as st:
                cT = sb(st, "cT", [128, 8])
                sg = sb(st, "sgc", [128, 8])
                rep = sb(st, "rep", [128, 8, 128])
                gvec = sb(st, "gvec", [128, 4 * D])
                bada = sb(st, "bada", [128, 6 * D])
                modbc = sb(st, "modbc", [128, 6 * D])
                wring = mkring(st, "wada", 2, [128, 8, 512])
                zt = sb(st, "zt", [128, D], BF16)
                S.op("gpsimd", lambda e: e.memset(zt[:], 0.0), [], ["zt"])
                xsz_v = xs_d.ap().rearrange("(b p) n -> b p n", p=128)
                ztf = sb(st, "ztf", [128, D])
                S.op("gpsimd", lambda e: e.memset(ztf[:], 0.0), [], ["ztf"])
                ysz_v = ys_d.ap().rearrange("(b p) n -> b p n", p=128)
                for b_ in range(NBLK):
                    dma("scalar", xsz_v[b_], zt[:, :], ["zt"], ["xsz%d" % b_])
                    dma("scalar", ysz_v[b_], ztf[:, :], ["ztf"], ["ysz%d" % b_])
                dma("sync", cT[:], cT_d.ap(), [], ["cT"])
                dma("sync", gvec[:], gvec_d.ap(), [], ["gvec"])
                dma("sync", bada[:], bada_d.ap(), [], ["bada"])
                act(sg[:], cT[:], AF.Sigmoid, ["cT"], ["sgc"])
                tt("vector", sg[:], sg[:], cT[:], ALU.mult, ["sgc", "cT"], ["sgc"])
                for kc in range(8):
                    ts("vector", rep[:, kc, :], ones_f, sg[:, kc:kc + 1], None, ALU.mult, None,
                       ["consts", "sgc"], ["rep%d" % kc])
                wada_v = wada_d.ap().rearrange("(k p) n -> p k n", p=128)
                for ng in range(12):
                    wt, wk = wring.next()
                    dma("sync", wt[:], wada_v[:, :, ng * 512:(ng + 1) * 512], [], [wk])
                    bank, bk = ps.next()
                    for kc in range(8):
                        mm(bank[:, :], rep[:, kc, :], wt[:, kc, :], kc == 0, kc == 7,
                           [wk, "rep%d" % kc], [bk], sig=(kc == 7))
                    tt("vector", modbc[:, ng * 512:(ng + 1) * 512], bank[:, :], bada[:, ng * 512:(ng + 1) * 512],
                       ALU.add, [bk, "bada"], ["modbc"])
                m = lambda i: modbc[:, i * D:(i + 1) * D]
                g = lambda i: gvec[:, i * D:(i + 1) * D]
                stt(mods[:, GM1, :], m(1), 1.0, g(0), ALU.add, ALU.mult, ["modbc", "gvec"], ["mods"])
                cp("vector", mods[:, SH1, :], m(0), ["modbc"], ["mods"])
                tt("vector", mods[:, GP1, :], m(2), g(1), ALU.mult, ["modbc", "gvec"], ["mods"])
                stt(mods[:, GM2, :], m(4), 1.0, g(2), ALU.add, ALU.mult, ["modbc", "gvec"], ["mods"])
                cp("vector", mods[:, SH2, :], m(3), ["modbc"], ["mods"])
                tt("vector", mods[:, GP2, :], m(5), g(3), ALU.mult, ["modbc", "gvec"], ["mods"])
                S.barrier()
                if stage <= 0:
                    S.enabled = False

            with ExitStack() as st:
                w_in = sb(st, "w_in_sb", [128, 8, INW], BF16)
                w_out = sb(st, "w_out_sb", [128, 8, D], BF16)
                w_r = sb(st, "w_r_sb", [128, 8, NEXP])
                wup = sb(st, "wup_sb", [32, 256])
                kTs = sb(st, "kTs", [64, 3, 2, 128], BF16)
                vs = sb(st, "vs", [128, 3, 128], BF16)
                gaT = sb(st, "gaT", [32, 128])
                Sf = [sb(st, "Sf%d" % i, [64, 4, 128]) for i in range(2)]
                Sb = [sb(st, "Sb%d" % i, [64, 4, 128], BF16) for i in range(2)]
                Qc0 = sb(st, "Qc0", [64, 4, 128], BF16)
                Qc1 = sb(st, "Qc1", [64, 4, 128], BF16)

                with ExitStack() as stw:
                    stg = mkring(stw, "stg", 2, [128, 1160])
                    win_v = win_d.ap().rearrange("(k p) n -> k p n", p=128)
                    wout_v = wout_d.ap().rearrange("(k p) n -> k p n", p=128)
                    ceng = ["vector", "gpsimd"]
                    for kc in range(8):
                        for hf_ in range(2):
                            s_, sk = stg.next()
                            dma("sync", s_[:, :], win_v[kc][:, hf_ * 1160:(hf_ + 1) * 1160], [], [sk])
                            cp(ceng[hf_], w_in[:, kc, hf_ * 1160:(hf_ + 1) * 1160], s_[:, :], [sk], ["w_in"])
                    for kc in range(8):
                        s_, sk = stg.next()
                        dma("sync", s_[:, 0:D], wout_v[kc], [], [sk])
                        cp(ceng[kc % 2], w_out[:, kc, :], s_[:, 0:D], [sk], ["w_out"])
                    dma("sync", w_r[:], wr_d.ap().rearrange("(k p) n -> p k n", p=128), [], ["w_r"])
                    dma("sync", wup[0:17, :], wup_d.ap(), [], ["wup"])
                    S.op("vector", lambda e: e.memset(gaT[:], 1.0), [], ["gaT"])
                    for i in range(2):
                        S.op("gpsimd", lambda e, i=i: e.memset(Sf[i][:], 0.0), [], ["Sf%d" % i])
                        S.op("gpsimd", lambda e, i=i: e.memset(Sb[i][:], 0.0), [], ["Sb%d" % i])
                    S.op("gpsimd", lambda e: e.memset(Qc0[:], 0.0), [], ["Qc0"])
                    S.op("gpsimd", lambda e: e.memset(Qc1[:], 0.0), [], ["Qc1"])
                    S.barrier()

                xr = mkring(st, "xr", 2, [128, D])
                tmpr = mkring(st, "tmpr", 1, [128, D])
                hbr = mkring(st, "hbr", 1, [128, D], BF16)
                hTr = mkring(st, "hTr", 3, [128, 8, 128], BF16)
                smr = mkring(st, "smr", 4, [128, 16])
                qTa = mkring(st, "qTa", 2, [64, 8, 128], BF16)
                gqr = mkring(st, "gqr", 2, [64, 4, 128])
                gkr = mkring(st, "gkr", 2, [64, 4, 128])
                ktokr = mkring(st, "ktokr", 3, [128, 256])
                vtokr = mkring(st, "vtokr", 3, [128, 512], BF16)
                sgg = mkring(st, "sgg", 2, [128, 512])
                enr = mkring(st, "enr", 1, [128, 256])
                ltok = mkring(st, "ltok", 1, [128, 256])
                bTr = mkring(st, "bTr", 1, [64, 4, 128])
                nbm = mkring(st, "nbm", 1, [64, 4, 2])
                decr = mkring(st, "decr", 1, [64, 4, 2])
                E1r = mkring(st, "E1r", 1, [64, 4, 128])
                E2r = mkring(st, "E2r", 1, [64, 4, 128])
                E3r = mkring(st, "E3r", 1, [64, 4, 128])
                QpT = mkring(st, "QpT", 1, [64, 4, 128], BF16)
                KpT = mkring(st, "KpT", 1, [64, 4, 128], BF16)
                E4r = mkring(st, "E4r", 1, [128, 256])
                Kpp = mkring(st, "Kpp", 1, [128, 256], BF16)
                ATr = mkring(st, "ATr", 1, [128, 4, 128], BF16)
                scr = mkring(st, "scr", 1, [128, 8, 256])
                pbr = mkring(st, "pbr", 1, [128, 8, 256], BF16)
                pTr = mkring(st, "pTr", 1, [128, 16, 128], BF16)
                mixr = mkring(st, "mixr", 1, [128, D], BF16)
                mixTr = mkring(st, "mixTr", 1, [128, 8, 128], BF16)
                osq = mkring(st, "osq", 1, [128, 512])
                x1r = mkring(st, "x1r", 1, [128, D])
                h2r = mkring(st, "h2r", 1, [128, D])
                h2Tf = mkring(st, "h2Tf", 1, [128, 8, 128])
                h2br = mkring(st, "h2br", 1, [128, D], BF16)
                mkbr = mkring(st, "mkbr", 1, [128, NEXP], BF16)
                lgr = mkring(st, "lgr", 1, [128, 4, NEXP])

                if stage <= 1:
                    S.enabled = False

                mask2 = consts[:, C_MASK2:C_MASK2 + 512]
                maskF = consts[:, C_MASKF:C_MASKF + 512]
                tri4 = consts[:, C_TRI4:C_TRI4 + 512]
                Uincl = consts[:, C_UINCL:C_UINCL + 128]
                Urev = consts[:, C_UREV:C_UREV + 128]
                gnorm = consts[:, C_GNORM:C_GNORM + 512]
                sinks = consts[:, C_SINK:C_SINK + 8]
                brout = consts[:, C_BROUT:C_BROUT + NEXP]

                x_v = x_d.ap().rearrange("(t p) n -> t p n", p=128)
                xp_v = xp_d.ap().rearrange("(t p) n -> t p n", p=128)
                x1_v = x1_d.ap().rearrange("(t p) n -> t p n", p=128)
                out_v = out_d.ap().rearrange("(t p) n -> t p n", p=128)
                h2_v = h2_d.ap().rearrange("(t p) n -> t p n", p=128)

                state = {"cur": 0}

                def rstd_from_ss(sm, smk, col_in, col_out, n, cnt=1):
                    a = sm[:, col_in:col_in + cnt]
                    o = sm[:, col_out:col_out + cnt]
                    ts("vector", o, a, 1.0 / n, EPS, ALU.mult, ALU.add, [smk], [smk])
                    act(o, o, AF.Ln, [smk], [smk])
                    act(o, o, AF.Exp, [smk], [smk], scale=-0.5)

                def norm_mod(x_t, xk, gi, si, out_ap, outk, sm, smk, c0):
                    tm, tk = tmpr.next()
                    act(tm[:, :], x_t[:, :], AF.Square, [xk], [tk, smk], accum=sm[:, c0:c0 + 1])
                    rstd_from_ss(sm, smk, c0, c0 + 1, float(D))
                    stt(tm[:, :], x_t[:, :], sm[:, c0 + 1:c0 + 2], mods[:, gi, :], ALU.mult, ALU.mult, [xk, smk], [tk])
                    tt("gpsimd", out_ap[:, 0:384], tm[:, 0:384], mods[:, si, 0:384], ALU.add, [tk], [outk])
                    tt("vector", out_ap[:, 384:D], tm[:, 384:D], mods[:, si, 384:D], ALU.add, [tk], [outk])

                def transpose8(src, srck, dst, dstk, evac_eng):
                    for half in range(2):
                        bank, bk = ps.next()
                        for j in range(4):
                            kc = half * 4 + j
                            mm(bank[:, j * 128:(j + 1) * 128], src[:, kc * 128:(kc + 1) * 128], identb[:], True, True,
                               [srck], [bk], sig=(j == 3))
                        cp("scalar" if half == 0 else "vector",
                           dst[:, half * 4:half * 4 + 4, :].rearrange("p k n -> p (k n)"), bank[:, :], [bk], [dstk])

                def gla_gates(hT, hTk, slot_rows):
                    bank, bk = ps.next()
                    for kc in range(8):
                        mm(bank[0:16, 0:128], w_in[:, kc, 2304:2320], hT[:, kc, :], kc == 0, kc == 7,
                           [hTk], [bk], sig=(kc == 7))
                    cp("vector", gaT[0:16, :], bank[0:16, 0:128], [bk], ["gaT"])
                    zb, zk = ps.next()
                    mm(zb[:, 0:256], gaT[0:17, :], wup[0:17, :], True, True, ["gaT"], [zk])
                    en, ek = enr.next()
                    act(en[:, :], zb[:, 0:256], AF.Exp, [zk], [ek], scale=-1.0)
                    l, lk = ltok.next()
                    act(l[:, :], en[:, :], AF.Ln, [ek], [lk], bias=1.0)
                    return l, lk

                def tile_body(t, pre):
                    main = not pre
                    slot = t + 1 if main else 0
                    last_pre = pre and (t == NP - 1)
                    xt, xk = xr.next()
                    dma("sync", xt[:, :], (x_v if main else xp_v)[t], [], [xk])
                    sm, smk = smr.next()
                    hb, hbk = hbr.next()
                    norm_mod(xt, xk, GM1, SH1, hb[:, :], hbk, sm, smk, 0)
                    hT, hTk = hTr.next()
                    if sub <= 0:
                        return
                    transpose8(hb, hbk, hT, hTk, "scalar")
                    if debug and main and t == 0:
                        dh = nc.dram_tensor("dbg_h", [128, D], BF16, kind="ExternalOutput")
                        dma("sync", dh.ap(), hb[:, :], [hbk], ["dbg_h"])
                        dhT = nc.dram_tensor("dbg_hT", [128, D], BF16, kind="ExternalOutput")
                        dma("sync", dhT.ap(), hT[:].rearrange("p k n -> p (k n)"), [hTk], ["dbg_hT"])
                    if sub <= 1:
                        return

                    def fm_group(cols_list, bank, bk):
                        for j, c0 in enumerate(cols_list):
                            for kc in range(8):
                                mm(bank[0:64, j * 128:(j + 1) * 128], w_in[:, kc, c0:c0 + 64], hT[:, kc, :],
                                   kc == 0, kc == 7, [hTk], [bk], sig=(kc == 7 and j == len(cols_list) - 1))

                    def tm_group(c0, n, bank, bk, off=0, last=True):
                        for kc in range(8):
                            mm(bank[:, off:off + n], hT[:, kc, :], w_in[:, kc, c0:c0 + n], kc == 0, kc == 7,
                               [hTk], [bk], sig=(kc == 7 and last))

                    if main:
                        qa, qak = qTa.next()
                        for half in range(2):
                            bank, bk = ps.next()
                            fm_group([h * 64 for h in range(half * 4, half * 4 + 4)], bank, bk)
                            S.op("scalar", lambda e, bank=bank, half=half: e.mul(
                                qa[:, half * 4:half * 4 + 4, :].rearrange("p h n -> p (h n)"), bank[0:64, :], 0.125),
                                [bk], [qak])
                        gq, gqk = gqr.next()
                        bank, bk = ps.next()
                        fm_group([768 + h * 64 for h in range(4)], bank, bk)
                        S.op("scalar", lambda e, bank=bank: e.mul(gq[:].rearrange("p h n -> p (h n)"), bank[0:64, :], 0.125),
                             [bk], [gqk])
                        gk, gkk = gkr.next()
                        bank, bk = ps.next()
                        fm_group([1024 + h * 64 for h in range(4)], bank, bk)
                        cp("vector", gk[:].rearrange("p h n -> p (h n)"), bank[0:64, :], [bk], [gkk])
                    KD = os.environ.get("KDBG", "")
                    if KD == "tm":
                        pass
                    elif main or last_pre:
                        bank, bk = ps.next()
                        fm_group([512, 576], bank, bk)
                        cp("vector", kTs[:, slot % 3, :, :],
                           bank[0:64, 0:256].rearrange("p (h n) -> p h n", h=2), [bk], ["kT%d" % (slot % 3)])
                    if KD == "fm":
                        return
                    bank, bk = ps.next()
                    if (main or last_pre) and KD != "tm1b":
                        tm_group(640, 128, bank, bk, off=0, last=False)
                    tm_group(1024, 256, bank, bk, off=128)
                    if (main or last_pre) and KD != "tm1b":
                        cp("vector" if KD == "tm1c" else "scalar", vs[:, slot % 3, :], bank[:, 0:128], [bk], ["v%d" % (slot % 3)])
                    ktok, ktk = ktokr.next()
                    cp("vector", ktok[:, :], bank[:, 128:384], [bk], [ktk])
                    if KD in ("tm1", "tm1b", "tm1c"):
                        return
                    bank, bk = ps.next()
                    tm_group(1280, 512, bank, bk)
                    vtok, vtk = vtokr.next()
                    if pre:
                        ts("vector", vtok[:, :], bank[:, :], consts[:, C_PFLAG + t:C_PFLAG + t + 1], None, ALU.mult, None,
                           [bk], [vtk])
                    else:
                        cp("scalar", vtok[:, :], bank[:, :], [bk], [vtk])
                    if main:
                        bank, bk = ps.next()
                        tm_group(1792, 512, bank, bk)
                        sg_t, sgk = sgg.next()
                        act(sg_t[:, :], bank[:, :], AF.Silu, [bk], [sgk])
                        tt("gpsimd", sg_t[:, :], sg_t[:, :], gnorm, ALU.mult, [sgk], [sgk])

                    if sub <= 2:
                        return
                    yield
                    l, lk = gla_gates(hT, hTk, None)
                    if sub <= 3:
                        return
                    rvb, rvk = ps.next()
                    mm(rvb[:, 0:256], Urev, l[:, :], True, True, [lk], [rvk])
                    E4, E4k = E4r.next()
                    act(E4[:, :], rvb[:, 0:256], AF.Exp, [rvk], [E4k])
                    kpp, kppk = Kpp.next()
                    tt("vector", kpp[:, :], ktok[:, :], E4[:, :], ALU.mult, [ktk, E4k], [kppk])
                    bTb, bTbk = ps.next()
                    if main:
                        for hd in range(4):
                            mm(bTb[0:64, hd * 128:(hd + 1) * 128], l[:, hd * 64:(hd + 1) * 64], Uincl, True, True,
                               [lk], [bTbk], sig=(hd == 3))
                        bT, bTk = bTr.next()
                        cp("vector", bT[:].rearrange("p h n -> p (h n)"), bTb[0:64, :], [bTbk], [bTk])
                        nb, nbk = nbm.next()
                        ts("vector", nb[:], bT[:, :, 31:128:64], -1.0, None, ALU.mult, None, [bTk], [nbk])
                        E1, E1k = E1r.next()
                        for hd in range(4):
                            for c in range(2):
                                act(E1[:, hd, c * 64:(c + 1) * 64], bT[:, hd, c * 64:(c + 1) * 64], AF.Exp,
                                    [bTk, nbk], [E1k], bias=nb[:, hd, c:c + 1])
                        E2, E2k = E2r.next()
                        S.op("vector", lambda e, E2=E2, E1=E1: e.reciprocal(E2[:], E1[:]), [E1k], [E2k])
                        E3, E3k = E3r.next()
                        act(E3[:], bT[:], AF.Exp, [bTk], [E3k])
                        qp, qpk = QpT.next()
                        tt("vector", qp[:], gq[:], E1[:], ALU.mult, [gqk, E1k], [qpk])
                        kp, kpk = KpT.next()
                        tt("gpsimd", kp[:], gk[:], E2[:], ALU.mult, [gkk, E2k], [kpk])
                        tt("gpsimd", Qc0[:, :, 0:64], gq[:, :, 0:64], E3[:, :, 0:64], ALU.mult, [gqk, E3k], ["Qc0"])
                        tt("gpsimd", Qc1[:, :, 64:128], gq[:, :, 64:128], E3[:, :, 64:128], ALU.mult, [gqk, E3k], ["Qc1"])
                        dec = lambda hd, c: E3[:, hd, c * 64 + 63:c * 64 + 64]
                        decb = lambda c, E3=E3: E3[:, :, c * 64 + 63:c * 64 + 64].to_broadcast([64, 4, 128])
                        deck = E3k
                    else:
                        for hd in range(4):
                            mm(bTb[0:64, hd * 2:hd * 2 + 2], l[:, hd * 64:(hd + 1) * 64],
                               consts[:, C_UINCL + 63:C_UINCL + 128:64], True, True, [lk], [bTbk], sig=(hd == 3))
                        dc, dck = decr.next()
                        act(dc[:].rearrange("p h c -> p (h c)"), bTb[0:64, 0:8], AF.Exp, [bTbk], [dck])
                        dec = lambda hd, c: dc[:, hd, c:c + 1]
                        decb = lambda c, dc=dc: dc[:, :, c:c + 1].to_broadcast([64, 4, 128])
                        deck = dck

                    if sub <= 4:
                        return
                    cur = state["cur"]
                    S0f, S0b, S1f, S1b = Sf[cur], Sb[cur], Sf[1 - cur], Sb[1 - cur]
                    k0, k1 = "S%d" % cur, "S%d" % (1 - cur)
                    if main:
                        atb, atk = ps.next()
                        for hd in range(4):
                            mm(atb[:, hd * 128:(hd + 1) * 128], kp[:, hd, :], qp[:, hd, :], True, True,
                               [kpk, qpk], [atk], sig=(hd == 3))
                        AT, ATk = ATr.next()
                        tt("vector", AT[:].rearrange("p h n -> p (h n)"), atb[:, :], tri4, ALU.mult, [atk], [ATk])
                    kvb, kvk = ps.next()
                    for hd in range(4):
                        mm(kvb[0:64, hd * 128:(hd + 1) * 128], kpp[0:64, hd * 64:(hd + 1) * 64],
                           vtok[0:64, hd * 128:(hd + 1) * 128], True, True, [kppk, vtk], [kvk], sig=(hd == 3))
                    S.op("vector", lambda e, S1f=S1f, S0f=S0f, d_=decb(0): e.tensor_tensor(S1f[:], S0f[:], d_, ALU.mult),
                         [k0 + "f", deck], [k1 + "f"])
                    tt("vector", S1f[:].rearrange("p h n -> p (h n)"), S1f[:].rearrange("p h n -> p (h n)"), kvb[0:64, :],
                       ALU.add, [k1 + "f", kvk], [k1 + "f"])
                    cp("scalar", S1b[:], S1f[:], [k1 + "f"], [k1 + "b"])
                    kvb2, kvk2 = ps.next()
                    for hd in range(4):
                        mm(kvb2[0:64, hd * 128:(hd + 1) * 128], kpp[64:128, hd * 64:(hd + 1) * 64],
                           vtok[64:128, hd * 128:(hd + 1) * 128], True, True, [kppk, vtk], [kvk2], sig=(hd == 3))
                    if main:
                        ob, obk = ps.next()
                        for hd in range(4):
                            o_ap = ob[:, hd * 128:(hd + 1) * 128]
                            mm(o_ap, AT[:, hd, :], vtok[:, hd * 128:(hd + 1) * 128], True, False, [ATk, vtk], [obk], sig=False)
                            mm(o_ap, Qc0[:, hd, :], S0b[:, hd, :], False, False, ["Qc0", k0 + "b"], [obk], sig=False)
                            mm(o_ap, Qc1[:, hd, :], S1b[:, hd, :], False, True, ["Qc1", k1 + "b"], [obk], sig=(hd == 3))
                    S.op("vector", lambda e, S1f=S1f, S0f=S0f, d_=decb(1): e.tensor_tensor(S0f[:], S1f[:], d_, ALU.mult),
                         [k1 + "f", deck], [k0 + "f"])
                    tt("vector", S0f[:].rearrange("p h n -> p (h n)"), S0f[:].rearrange("p h n -> p (h n)"), kvb2[0:64, :],
                       ALU.add, [k0 + "f", kvk2], [k0 + "f"])
                    cp("scalar", S0b[:], S0f[:], [k0 + "f"], [k0 + "b"])
                    if pre:
                        return

                    mix, mixk = mixr.next()
                    sq, sqk = osq.next()
                    sm4, sm4k = smr.next()
                    for hd in range(4):
                        act(sq[:, hd * 128:(hd + 1) * 128], ob[:, hd * 128:(hd + 1) * 128], AF.Square, [obk], [sqk, sm4k],
                            accum=sm4[:, hd:hd + 1])
                    rstd_from_ss(sm4, sm4k, 0, 4, 128.0, cnt=4)
                    for hd in range(4):
                        stt(mix[:, 512 + hd * 128:512 + (hd + 1) * 128], ob[:, hd * 128:(hd + 1) * 128], sm4[:, 4 + hd:5 + hd],
                            sg_t[:, hd * 128:(hd + 1) * 128], ALU.mult, ALU.mult, [obk, sm4k, sgk], [mixk])

                    sc, sck = scr.next()
                    for pair in range(4):
                        bank, bk = ps.next()
                        for j in range(2):
                            h = pair * 2 + j
                            for c in range(2):
                                mm(bank[:, j * 256 + c * 128:j * 256 + (c + 1) * 128], qa[:, h, :],
                                   kTs[:, (t + c) % 3, h // 4, :], True, True, [qak, "kT%d" % ((t + c) % 3)], [bk],
                                   sig=(j == 1 and c == 1))
                        tt("vector", sc[:, pair * 2:pair * 2 + 2, :].rearrange("p h n -> p (h n)"), bank[:, :],
                           maskF if t == 0 else mask2, ALU.add, [bk], [sck])
                    sm2, sm2k = smr.next()
                    S.op("vector", lambda e: e.tensor_reduce(sm2[:, 0:8], sc[:], AX.X, ALU.max), [sck], [sm2k])
                    tt("vector", sm2[:, 0:8], sm2[:, 0:8], sinks, ALU.max, [sm2k], [sm2k])
                    ts("vector", sm2[:, 0:8], sm2[:, 0:8], -1.0, None, ALU.mult, None, [sm2k], [sm2k])
                    sm3, sm3k = smr.next()
                    pb, pbk = pbr.next()
                    for h in range(8):
                        act(pb[:, h, :], sc[:, h, :], AF.Exp, [sck, sm2k], [pbk, sm3k], bias=sm2[:, h:h + 1],
                            accum=sm3[:, h:h + 1])
                    tt("vector", sm2[:, 8:16], sinks, sm2[:, 0:8], ALU.add, [sm2k], [sm2k])
                    act(sm2[:, 8:16], sm2[:, 8:16], AF.Exp, [sm2k], [sm2k])
                    tt("vector", sm3[:, 0:8], sm3[:, 0:8], sm2[:, 8:16], ALU.add, [sm2k, sm3k], [sm3k])
                    S.op("vector", lambda e: e.reciprocal(sm3[:, 8:16], sm3[:, 0:8]), [sm3k], [sm3k])
                    pT, pTk = pTr.next()
                    for q4 in range(4):
                        bank, bk = ps.next()
                        for j in range(2):
                            h = q4 * 2 + j
                            for c in range(2):
                                mm(bank[:, (j * 2 + c) * 128:(j * 2 + c + 1) * 128], pb[:, h, c * 128:(c + 1) * 128],
                                   identb[:], True, True, [pbk], [bk], sig=(j == 1 and c == 1))
                        cp("scalar" if q4 % 2 == 0 else "vector",
                           pT[:, q4 * 4:q4 * 4 + 4, :].rearrange("p a n -> p (a n)"), bank[:, :], [bk], [pTk])
                    ab, abk = ps.next()
                    for h in range(8):
                        kv = h // 4
                        mm(ab[:, h * 64:(h + 1) * 64], pT[:, h * 2, :], vs[:, t % 3, kv * 64:(kv + 1) * 64], True, False,
                           [pTk, "v%d" % (t % 3)], [abk], sig=False)
                        mm(ab[:, h * 64:(h + 1) * 64], pT[:, h * 2 + 1, :], vs[:, (t + 1) % 3, kv * 64:(kv + 1) * 64], False, True,
                           [pTk, "v%d" % ((t + 1) % 3)], [abk], sig=(h == 7))
                    for h in range(8):
                        ts("vector", mix[:, h * 64:(h + 1) * 64], ab[:, h * 64:(h + 1) * 64],
                           sm3[:, 8 + h:9 + h], None, ALU.mult, None, [abk, sm3k], [mixk])

                    mixT, mixTk = mixTr.next()
                    transpose8(mix, mixk, mixT, mixTk, "scalar")
                    ybanks = []
                    for n in range(2):
                        bank, bk = ps.next()
                        for kc in range(8):
                            mm(bank[:, :], mixT[:, kc, :], w_out[:, kc, n * 512:(n + 1) * 512], kc == 0, kc == 7,
                               [mixTk], [bk], sig=(kc == 7))
                        ybanks.append((bank, bk))
                    sm5, sm5k = smr.next()
                    tm, tk = tmpr.next()
                    for n in range(2):
                        act(tm[:, n * 512:(n + 1) * 512], ybanks[n][0][:, :], AF.Square, [ybanks[n][1]], [tk, sm5k],
                            accum=sm5[:, n:n + 1])
                    tt("vector", sm5[:, 2:3], sm5[:, 0:1], sm5[:, 1:2], ALU.add, [sm5k], [sm5k])
                    rstd_from_ss(sm5, sm5k, 2, 3, float(D))
                    for n in range(2):
                        stt(tm[:, n * 512:(n + 1) * 512], ybanks[n][0][:, :], sm5[:, 3:4], mods[:, GP1, n * 512:(n + 1) * 512],
                            ALU.mult, ALU.mult, [ybanks[n][1], sm5k], [tk])
                    x1, x1k = x1r.next()
                    tt("gpsimd", x1[:, :], tm[:, :], xt[:, :], ALU.add, [tk, xk], [x1k])
                    dma("sync", x1_v[t], x1[:, :], [x1k], ["x1d%d" % t])

                    h2, h2k = h2r.next()
                    norm_mod(x1, x1k, GM2, SH2, h2[:, :], h2k, sm5, sm5k, 4)
                    hf, hfk = h2Tf.next()
                    for half in range(2):
                        bank, bk = ps.next()
                        for j in range(4):
                            kc = half * 4 + j
                            mm(bank[:, j * 128:(j + 1) * 128], h2[:, kc * 128:(kc + 1) * 128], identf, True, True,
                               [h2k], [bk], sig=(j == 3))
                        cp("vector" if half == 0 else "scalar", hf[:, half * 4:half * 4 + 4, :].rearrange("p k n -> p (k n)"),
                           bank[:, :], [bk], [hfk])
                    hbf, hbfk = h2br.next()
                    cp("gpsimd", hbf[:, :], h2[:, :], [h2k], [hbfk])
                    dma("sync", h2_v[t], hbf[:, :], [hbfk], ["h2d%d" % t])
                    lb, lbk = ps.next()
                    for kc in range(8):
                        mm(lb[:, 0:NEXP], hf[:, kc, :], w_r[:, kc, :], kc == 0, kc == 7, [hfk], [lbk], sig=(kc == 7))
                    lg, lgk = lgr.next()
                    LG, MK, EX, M8 = 0, 1, 2, 3
                    tt("vector", lg[:, LG, :], lb[:, 0:NEXP], brout, ALU.add, [lbk], [lgk])
                    S.op("vector", lambda e: e.max(lg[:, M8, 0:8], lg[:, LG, :]), [lgk], [lgk])
                    ts("vector", lg[:, MK, :], lg[:, LG, :], lg[:, M8, 3:4], None, ALU.is_ge, None, [lgk], [lgk])
                    ts("vector", lg[:, M8, 8:9], lg[:, M8, 0:1], -1.0, None, ALU.mult, None, [lgk], [lgk])
                    act(lg[:, EX, :], lg[:, LG, :], AF.Exp, [lgk], [lgk], bias=lg[:, M8, 8:9])
                    tt("vector", lg[:, EX, :], lg[:, EX, :], lg[:, MK, :], ALU.mult, [lgk], [lgk])
                    S.op("vector", lambda e: e.tensor_reduce(lg[:, M8, 9:10], lg[:, EX, :], AX.X, ALU.add), [lgk], [lgk])
                    S.op("vector", lambda e: e.reciprocal(lg[:, M8, 10:11], lg[:, M8, 9:10]), [lgk], [lgk])
                    ts("vector", G_all[:, t, :], lg[:, EX, :], lg[:, M8, 10:11], None, ALU.mult, None, [lgk], ["G%d" % t])
                    mkb, mkbk = mkbr.next()
                    cp("vector", mkb[:, :], lg[:, MK, :], [lgk], [mkbk])
                    rb, rbk = ps.next()
                    mm(rb[:, 0:NEXP], lstrb[:], mkb[:, :], True, True, [mkbk], [rbk], sig=False)
                    mm(rb[:, NEXP:2 * NEXP], onesb[:], mkb[:, :], True, True, [mkbk], [rbk])
                    stt(Qm_all[:, t, :], rb[:, 0:NEXP], 1.0, Orun[:, :], ALU.add, ALU.add, [rbk, "Orun"], ["Qm%d" % t])
                    tt("vector", Qm_all[:, t, :], Qm_all[:, t, :], lg[:, MK, :], ALU.mult, ["Qm%d" % t, lgk], ["Qm%d" % t])
                    tt("vector", Orun[:, :], Orun[:, :], rb[:, NEXP:2 * NEXP], ALU.add, ["Orun", rbk], ["Orun"])

                def drain(g):
                    for _ in g:
                        pass

                from collections import deque
                pend = deque()
                for (ti, pre_) in [(p, True) for p in range(NP)] + [(t, False) for t in range(NT)]:
                    if (not pre_) and ti == 0 and stage <= 2:
                        break
                    g = tile_body(ti, pre_)
                    try:
                        next(g)
                        pend.append(g)
                    except StopIteration:
                        pass
                    depth = 2 if pre_ else 1
                    while len(pend) > depth:
                        drain(pend.popleft())
                while pend:
                    drain(pend.popleft())
                if stage <= 2:
                    S.enabled = False
                if debug:
                    dma("sync", G_d.ap(), G_all[:].rearrange("p t e -> p (t e)"), ["G%d" % t for t in range(NT)], ["Gd"])
                S.barrier()
                if stage <= 3:
                    S.enabled = False

            h2_v = h2_d.ap().rearrange("(t p) n -> t p n", p=128)
            x1_v = x1_d.ap().rearrange("(t p) n -> t p n", p=128)
            out_v = out_d.ap().rearrange("(t p) n -> t p n", p=128)
            gk_all = sb(P, "gk_all", [128, NT, 4])
            idx4_all = sb(P, "idx4_all", [128, NT, 4], I32)
            flags_i = sb(P, "flags_i", [128, NEXP * JM], I32)
            idxb_i = sb(P, "idxb_i", [128, NEXP * JM], I32)
            with ExitStack() as st:
                flf = sb(st, "flf", [128, NEXP, JM])
                nbt = sb(st, "nbt", [128, NEXP])
                cA = sb(st, "cA", [128, NEXP])
                cB = sb(st, "cB", [128, NEXP])
                pst = sb(st, "pst", [128, NEXP])
                idf = sb(st, "idf", [128, NEXP, JM])
                vr = mkring(st, "vr", 2, [128, 3, NEXP])
                m8r = mkring(st, "m8r", 2, [128, 16])
                h2l = mkring(st, "h2l", 2, [128, D], BF16)
                TH3 = consts[:, C_TH:C_TH + NEXP * 32].rearrange("p (e j) -> p e j", e=NEXP)[:, :, 0:JM]
                S.op("vector", lambda e: e.tensor_tensor(flf[:], Orun[:].unsqueeze(2).to_broadcast([128, NEXP, JM]), TH3,
                                                         ALU.is_gt), ["Orun"], ["flf"])
                S.op("vector", lambda e: e.tensor_reduce(nbt[:], flf[:], AX.X, ALU.add), ["flf"], ["nbt"])
                ts("vector", cA[:], nbt[:], 128.0, None, ALU.mult, None, ["nbt"], ["cA"])
                cp("vector", pst[:], cA[:], ["cA"], ["pst"])
                ca, cb, cak, cbk = cA, cB, "cA", "cB"
                for sft in (1, 2, 4, 8, 16):
                    cp("vector", cb[:, 0:sft], ca[:, 0:sft], [cak], [cbk])
                    tt("vector", cb[:, sft:NEXP], ca[:, sft:NEXP], ca[:, 0:NEXP - sft], ALU.add, [cak], [cbk])
                    ca, cb, cak, cbk = cb, ca, cbk, cak
                tt("vector", pst[:], ca[:], pst[:], ALU.subtract, [cak, "pst"], ["pst"])
                cp("vector", flags_i[:], flf[:].rearrange("p e j -> p (e j)"), ["flf"], ["flags"])
                S.op("vector", lambda e: e.tensor_tensor(idf[:], TH3, pst[:].unsqueeze(2).to_broadcast([128, NEXP, JM]),
                                                         ALU.add), ["pst"], ["idf"])
                ts("vector", idf[:], idf[:], consts[:, C_IOTA:C_IOTA + 1], -65536.0, ALU.add, ALU.add, ["idf"], ["idf"])
                tt("vector", idf[:], idf[:], flf[:], ALU.mult, ["idf", "flf"], ["idf"])
                ts("vector", idf[:], idf[:], 65536.0, None, ALU.add, None, ["idf"], ["idf"])
                cp("vector", idxb_i[:], idf[:].rearrange("p e j -> p (e j)"), ["idf"], ["idxb"])
                for t in range(NT):
                    v, vk = vr.next()
                    ts("vector", v[:, 0, :], Qm_all[:, t, :], 0.0, None, ALU.is_gt, None, [], [vk])
                    tt("vector", v[:, 1, :], Qm_all[:, t, :], pst[:], ALU.add, ["pst"], [vk])
                    tt("vector", v[:, 1, :], v[:, 1, :], v[:, 0, :], ALU.mult, [vk], [vk])
                    m8, m8k = m8r.next()
                    S.op("vector", lambda e, m8=m8, v=v: e.max(m8[:, 0:8], v[:, 1, :]), [vk], [m8k])
                    ts("vector", m8[:, 8:12], m8[:, 0:4], -1.0, None, ALU.add, None, [m8k], [m8k])
                    cp("vector", idx4_all[:, t, :], m8[:, 8:12], [m8k], ["idx4_%d" % t])
                    for k in range(4):
                        ts("vector", v[:, 2, :], v[:, 1, :], m8[:, k:k + 1], None, ALU.is_equal, None, [vk, m8k], [vk])
                        stt(v[:, 0, :], v[:, 2, :], 1.0, G_all[:, t, :], ALU.mult, ALU.mult, [vk], [vk, "gk%d" % t],
                            accum=gk_all[:, t, k:k + 1])
                    h2t, h2tk = h2l.next()
                    dma("sync", h2t[:, :], h2_v[t], [], [h2tk])
                    for k in range(4):
                        S.dma("gpsimd", lambda en, t=t, k=k, h2t=h2t: en.indirect_dma_start(
                            out=xs_d.ap(), out_offset=bass.IndirectOffsetOnAxis(ap=idx4_all[:, t, k:k + 1], axis=0),
                            in_=h2t[:, :], in_offset=None), [h2tk, "idx4_%d" % t], ["xsd"])
                S.barrier()

            with ExitStack() as st:
                w1b = mkring(st, "w1b", 2, [128, 8, 2, D], BF16)
                w2b = mkring(st, "w2b", 2, [128, 8, D], BF16)
                w1s = mkring(st, "w1s", 5, [128, 2, 256])
                w2s = mkring(st, "w2s", 4, [128, 512])
                b1s = mkring(st, "b1s", 1, [1, D])
                b1b = mkring(st, "b1b", 2, [1, 2 * D], BF16)
                xsr = mkring(st, "xsr", 2, [128, D], BF16)
                xsTr = mkring(st, "xsTr", 2, [128, 8, 128], BF16)
                aTr = mkring(st, "aTr", 2, [128, 8, 128], BF16)
                atokr = mkring(st, "atokr", 1, [128, D], BF16)
                xgr = mkring(st, "xgr", 1, [128, 512])
                sgr = mkring(st, "sgr", 1, [128, 512])
                xlr = mkring(st, "xlr", 1, [128, 512])
                ysr = mkring(st, "ysr", 1, [128, D])
                for i_ in range(len(ysr.t)):
                    S.op("gpsimd", lambda e, i_=i_: e.memset(ysr.t[i_][:], 0.0), [], ["ysr%d" % i_])
                w1_v = w1_d.ap().rearrange("e (k p) n -> e p k n", p=128)
                w2_v = w2_d.ap().rearrange("e (k p) n -> e k p n", p=128)

                def gather(e, j):
                    col = e * JM + j
                    xs, xsk = xsr.next()
                    S.dma("gpsimd", lambda en, xs=xs, col=col: en.indirect_dma_start(
                        out=xs[:, :], out_offset=None, in_=xs_d.ap(),
                        in_offset=bass.IndirectOffsetOnAxis(ap=idxb_i[:, col:col + 1], axis=0),
                        bounds_check=breg, oob_is_err=False), [], [xsk])
                    return xs, xsk

                def block(e, j, w1t, w1k, w2t, w2k, bb, bbk, xs, xsk):
                    col = e * JM + j
                    xT, xTk = xsTr.next()
                    transpose8(xs, xsk, xT, xTk, None)
                    aT, aTk = aTr.next()
                    atok, atokk = atokr.next()
                    for fh in range(2):
                        banks = []
                        for two in range(2):
                            bank, bk = ps.next()
                            for kc in range(8):
                                mm(bank[:, :], xT[:, kc, :], w1t[:, kc, two, fh * 512:(fh + 1) * 512], kc == 0, False,
                                   [w1k, xTk], [bk], sig=False)
                            mm(bank[:, :], onesb[0:1, :], bb[0:1, two * D + fh * 512:two * D + (fh + 1) * 512], False, True,
                               [bbk], [bk])
                            banks.append((bank, bk))
                        (bg, bgk), (bl, blk) = banks
                        xg, xgk = xgr.next()
                        ts("vector", xg[:, :], bg[:, :], 7.0, None, ALU.min, None, [bgk], [xgk])
                        sgt, sgk2 = sgr.next()
                        act(sgt[:, :], xg[:, :], AF.Sigmoid, [xgk], [sgk2], scale=1.702)
                        xl, xlk = xlr.next()
                        ts("vector", xl[:, :], bl[:, :], 7.0, -7.0, ALU.min, ALU.max, [blk], [xlk])
                        tt("vector", xg[:, :], xg[:, :], sgt[:, :], ALU.mult, [xgk, sgk2], [xgk])
                        stt(atok[:, fh * 512:(fh + 1) * 512], xl[:, :], 1.0, xg[:, :], ALU.add, ALU.mult, [xlk, xgk], [atokk])
                    transpose8(atok, atokk, aT, aTk, None)
                    ysb, ysk = ysr.next()
                    ybs = [ps.next(), ps.next()]
                    for n in range(2):
                        yb, ybk = ybs[n]
                        for fc in range(8):
                            mm(yb[:, :], aT[:, fc, :], w2t[:, fc, n * 512:(n + 1) * 512], fc == 0, fc == 7,
                               [aTk, w2k], [ybk], sig=(fc == 7))
                    for n in range(2):
                        yb, ybk = ybs[n]
                        cp("scalar" if n == 0 else "vector", ysb[:, n * 512:(n + 1) * 512], yb[:, :], [ybk], [ysk])
                    S.dma("gpsimd", lambda en, ysb=ysb, col=col: en.indirect_dma_start(
                        out=ys_d.ap(), out_offset=bass.IndirectOffsetOnAxis(ap=idxb_i[:, col:col + 1], axis=0),
                        in_=ysb[:, :], in_offset=None, bounds_check=breg, oob_is_err=False), [ysk], ["ysd"])

                def load_weights(e):
                    w1t, w1k = w1b.next()
                    w2t, w2k = w2b.next()
                    bb, bbk = b1b.next()
                    ci = 0
                    for j in range(8):
                        for kh in range(4):
                            ws, wsk = w1s.next()
                            dma("sync", ws[:], w1_v[e][:, kh * 2:(kh + 1) * 2, j * 256:(j + 1) * 256], [], [wsk])
                            cp("scalar" if ci % 2 == 0 else "vector", w1t[:, kh * 2:(kh + 1) * 2, :, j * 128:(j + 1) * 128],
                               ws[:].rearrange("p k (m two) -> p k two m", two=2), [wsk], [w1k])
                            ci += 1
                        for nh in range(2):
                            s2, s2k = w2s.next()
                            dma("sync", s2[:], w2_v[e][j][:, nh * 512:(nh + 1) * 512], [], [s2k])
                            cp("scalar" if ci % 2 == 0 else "vector", w2t[:, j, nh * 512:(nh + 1) * 512], s2[:], [s2k], [w2k])
                            ci += 1
                    for bh in range(2):
                        bs, bsk = b1s.next()
                        dma("sync", bs[:], b1r_d.ap()[e:e + 1, bh * D:(bh + 1) * D], [], [bsk])
                        cp("vector", bb[:, bh * D:(bh + 1) * D], bs[:], [bsk], [bbk])
                    return w1t, w1k, w2t, w2k, bb, bbk

                wnext = load_weights(0)
                for e in range(NE):
                    w1t, w1k, w2t, w2k, bb, bbk = wnext
                    if e + 1 < NE:
                        wnext = load_weights(e + 1)
                    S.chain_begin()
                    nxt = gather(e, 0)
                    for j in range(JM):
                        S.level_push(flags_i[0:1, e * JM + j:e * JM + j + 1])
                        cur = nxt
                        if j + 1 < JM:
                            nxt = gather(e, j + 1)
                        block(e, j, w1t, w1k, w2t, w2k, bb, bbk, cur[0], cur[1])
                    S.chain_end()
                S.barrier()

            with ExitStack() as st:
                b2_sb = sb(st, "b2_sb", [NEXP, D])
                dma("sync", b2_sb[:], b2_d.ap(), [], ["b2"])
                yr = mkring(st, "yr", 4, [128, D])
                accr = mkring(st, "accr", 2, [128, D])
                GTr = mkring(st, "GTr", 2, [NEXP, 128])
                x1l = mkring(st, "x1l", 2, [128, D])
                outr = mkring(st, "outr", 2, [128, D])
                junk2 = mkring(st, "junk2", 1, [128, D], BF16)
                smq = mkring(st, "smq", 2, [128, 8])
                for t in range(NT):
                    ys4 = []
                    for k in range(4):
                        y_, yk_ = yr.next()
                        S.dma("gpsimd", lambda en, y_=y_, t=t, k=k: en.indirect_dma_start(
                            out=y_[:, :], out_offset=None, in_=ys_d.ap(),
                            in_offset=bass.IndirectOffsetOnAxis(ap=idx4_all[:, t, k:k + 1], axis=0)), [], [yk_])
                        ys4.append((y_, yk_))
                    ac, ack = accr.next()
                    ts("vector", ac[:, :], ys4[0][0][:, :], gk_all[:, t, 0:1], None, ALU.mult, None, [ys4[0][1]], [ack])
                    for k in range(1, 4):
                        stt(ac[:, :], ys4[k][0][:, :], gk_all[:, t, k:k + 1], ac[:, :], ALU.mult, ALU.add,
                            [ys4[k][1], ack], [ack])
                    gb, gbk = ps.next()
                    mm(gb[0:NEXP, 0:128], G_all[:, t, :], identf, True, True, [], [gbk])
                    gt_, gtk = GTr.next()
                    cp("vector", gt_[:, :], gb[0:NEXP, 0:128], [gbk], [gtk])
                    x1t, x1tk = x1l.next()
                    dma("sync", x1t[:, :], x1_v[t], [], [x1tk])
                    sm, smk = smq.next()
                    jk_t, jk = junk2.next()
                    for n in range(2):
                        yb, ybk = ps.next()
                        mm(yb[:, :], gt_[:, :], b2_sb[:, n * 512:(n + 1) * 512], True, True, [gtk, "b2"], [ybk])
                        a_ap = ac[:, n * 512:(n + 1) * 512]
                        tt("vector", a_ap, a_ap, yb[:, :], ALU.add, [ack, ybk], [ack])
                        act(jk_t[:, n * 512:(n + 1) * 512], a_ap, AF.Square, [ack], [jk, smk], accum=sm[:, n:n + 1])
                    tt("vector", sm[:, 2:3], sm[:, 0:1], sm[:, 1:2], ALU.add, [smk], [smk])
                    o_ = sm[:, 3:4]
                    ts("vector", o_, sm[:, 2:3], 1.0 / D, EPS, ALU.mult, ALU.add, [smk], [smk])
                    act(o_, o_, AF.Sqrt, [smk], [smk])
                    S.op("vector", lambda e, o_=o_: e.reciprocal(o_, o_), [smk], [smk])
                    ot, otk = outr.next()
                    stt(ot[:, :], ac[:, :], o_, mods[:, GP2, :], ALU.mult, ALU.mult, [ack, smk], [otk])
                    tt("gpsimd", ot[:, :], ot[:, :], x1t[:, :], ALU.add, [otk, x1tk], [otk])
                    dma("sync", out_v[t], ot[:, :], [otk], ["outd%d" % t])
                S.barrier()
        except _Stop:
            S.barrier()

        with nc.Block() as block:
            S.emit(block, gscr)
    return nc, S


def host_inputs(inp, NT=32, NP=96, segs=None):
    x = np.asarray(inp["x"], np.float32)
    TOK = NT * 128
    f = lambda k: np.ascontiguousarray(np.asarray(inp[k], np.float32)[0])
    w_ada, b_ada = f("w_ada"), f("b_ada")
    gvec = np.concatenate([f("g_pre_mix"), f("g_post_mix"), f("g_pre_ffn"), f("g_post_ffn")])
    gvec_bc = np.ascontiguousarray(np.broadcast_to(gvec[None, :], (128, 4 * D)))
    bada_bc = np.ascontiguousarray(np.broadcast_to(b_ada[None, :], (128, 6 * D)))
    wup_aug = np.concatenate([f("w_gla_gate_up"), f("b_gla_gate")[None, :]], axis=0)
    b1 = f("b_mlp1")
    b1r = np.ascontiguousarray(b1.reshape(NEXP, D, 2).transpose(0, 2, 1).reshape(NEXP, 2 * D))
    qi = np.arange(128)[:, None]
    kj = np.arange(256)[None, :]
    valid = ((kj < 128) & (kj > qi)) | ((kj >= 128) & (kj - 128 <= qi))
    m1 = np.where(valid, 0.0, NEG).astype(np.float32)
    validF = (kj >= 128) & (kj - 128 <= qi)
    mF = np.where(validF, 0.0, NEG).astype(np.float32)
    j = np.arange(128)[:, None]
    i = np.arange(128)[None, :]
    same = (j // 64) == (i // 64)
    tri = (same & (j <= i)).astype(np.float32)
    uincl = tri * (-1.0 / 16.0)
    urev = (same & (j > i)).astype(np.float32) * (-1.0 / 16.0)
    cbase = np.zeros((128, C_TOT), np.float32)
    cbase[:, C_MASK2:C_MASK2 + 512] = np.tile(m1, (1, 2))
    cbase[:, C_TRI4:C_TRI4 + 512] = np.tile(tri, (1, 4))
    cbase[:, C_UINCL:C_UINCL + 128] = uincl
    cbase[:, C_UREV:C_UREV + 128] = urev
    cbase[:, C_IDENT:C_IDENT + 128] = np.eye(128, dtype=np.float32)
    cbase[:, C_GNORM:C_GNORM + 512] = np.tile(f("g_gla_norm")[None, :], (128, 4))
    cbase[:, C_SINK:C_SINK + 8] = f("sinks")[None, :]
    cbase[:, C_BROUT:C_BROUT + NEXP] = f("b_router")[None, :]
    cbase[:, C_ONES:C_ONES + 128] = 1.0
    cbase[:, C_LSTR:C_LSTR + 128] = (j < i).astype(np.float32)
    cbase[:, C_TH:C_TH + NEXP * 32] = np.tile(128.0 * np.arange(32, dtype=np.float32), NEXP)[None, :]
    cbase[:, C_IOTA] = np.arange(128, dtype=np.float32)
    shared = {"w_ada": w_ada, "b_ada_bc": bada_bc, "gvec_bc": gvec_bc, "w_in": f("w_in"), "wup_aug": wup_aug,
              "w_out": f("w_out"), "w_router": f("w_router"), "w_mlp1": f("w_mlp1"), "w_mlp2": f("w_mlp2"),
              "b1r": b1r, "b_mlp2": f("b_mlp2")}
    maps = []
    nseg = SEQ // SEG
    if segs is None:
        segs = [(core // nseg, (core % nseg) * SEG) for core in range(NCORE)]
    for (b, s0) in segs:
        cm = cbase.copy()
        cm[:, C_MASKF:C_MASKF + 512] = np.tile(mF if s0 == 0 else m1, (1, 2))
        xpre = np.zeros((max(NP, 1) * 128, D), np.float32)
        p0 = s0 - NP * 128
        for p in range(NP):
            a = p0 + p * 128
            if a >= 0:
                xpre[p * 128:(p + 1) * 128] = x[b, a:a + 128]
                cm[:, C_PFLAG + p] = 1.0
        mp = dict(shared)
        mp["x"] = np.ascontiguousarray(x[b, s0:s0 + TOK])
        mp["xpre"] = xpre
        mp["cT"] = np.ascontiguousarray(np.asarray(inp["c"], np.float32)[b].reshape(8, 128).T)
        mp["consts"] = cm
        maps.append(mp)
    return maps


_CACHE = {}


def kernel(**inputs):
    if "nc" not in _CACHE:
        _CACHE["nc"] = build_nc()[0]
    nc = _CACHE["nc"]
    maps = host_inputs(inputs)
    res = run_bass_kernel_spmd(nc, maps, core_ids=list(range(NCORE)))
    out = np.empty((2, SEQ, D), np.float32)
    nseg = SEQ // SEG
    for core in range(NCORE):
        b, seg = core // nseg, core % nseg
        out[b, seg * SEG:(seg + 1) * SEG] = np.asarray(res.results[core]["out"], np.float32)
    return out
```
